# Optimizing a Trainium2 kernel written in Bass

```python
import jax, jax.numpy as jnp
from jax import lax
import numpy as np

D_MODEL = 1024
BATCH = 16
SEQ = 2048
DEPTH = 4

N_MIXERS = 3
HD = 64
H_MIX = 12
H_MEM = 4
MIX_W = H_MIX * HD
MEM_W = H_MEM * HD
N_MEM = 256
D_FF = 2816
ROPE_THETA = 10000.0
EPS = 1e-6
NEG = -1e30
FORCE = 1e9

RET_H = 6
RET_DK = 64
RET_DV = 128
RET_CHUNK = 128

DSA_H = 12
IDX_H = 8
IDX_D = 64
DSA_TOPK_MAX = 256
Q_BLK = 128

NSA_H = 12
NSA_G = 4
CMP_L = 32
CMP_S = 16
SLC_L = 64
SLC_N_MAX = 16
WIN = 512
SLC_Q_BLK = 16

N_A = (DEPTH + 2) // 3
N_B = (DEPTH + 1) // 3
N_C = DEPTH // 3

RET_SIZES = [RET_H * RET_DK, RET_H * RET_DK, RET_H * RET_DV, RET_H * RET_DV, MEM_W]
DSA_SIZES = [DSA_H * HD, HD, HD, IDX_H * IDX_D, IDX_D, IDX_H, MEM_W]
NSA_SIZES = [NSA_H * HD] + [NSA_G * HD] * 6 + [NSA_H * 3, MEM_W]
RET_COLS = sum(RET_SIZES)
DSA_COLS = sum(DSA_SIZES)
NSA_COLS = sum(NSA_SIZES)

kernel_name = 'hybrid_interleaved_retention_dsa_nsa_macaron'


def split_cols(a, sizes):
    return jnp.split(a, np.cumsum(sizes)[:-1].tolist(), axis=-1)


def rms_norm(x, g):
    xf = x.astype(jnp.float32)
    y = xf * lax.rsqrt(jnp.mean(xf * xf, axis=-1, keepdims=True) + EPS)
    return (y * g.astype(jnp.float32)).astype(x.dtype)


def rope(x, pos):
    half = x.shape[-1] // 2
    inv = ROPE_THETA ** (-jnp.arange(half, dtype=jnp.float32) / half)
    ang = pos.astype(jnp.float32)[:, None] * inv[None, :]
    cos = jnp.cos(ang)[:, None, :].astype(x.dtype)
    sin = jnp.sin(ang)[:, None, :].astype(x.dtype)
    x1, x2 = x[..., :half], x[..., half:]
    return jnp.concatenate([x1 * cos - x2 * sin, x2 * cos + x1 * sin], axis=-1)


def swiglu(x, wg, wu, wd):
    return (jax.nn.silu(x @ wg) * (x @ wu)) @ wd


def masked_softmax(logits, valid):
    return jax.nn.softmax(jnp.where(valid, logits.astype(jnp.float32), NEG), axis=-1)


def chunkwise_retention(q, k, v):
    B, L, H, DK = q.shape
    DV = v.shape[-1]
    C = RET_CHUNK
    nc = L // C
    log_g = jnp.log(1.0 - 2.0 ** (-5.0 - jnp.arange(H, dtype=jnp.float32)))
    qc = q.reshape(B, nc, C, H, DK)
    kc = k.reshape(B, nc, C, H, DK)
    vc = v.reshape(B, nc, C, H, DV)
    idx = jnp.arange(C, dtype=jnp.float32)
    diff = idx[:, None] - idx[None, :]
    dmask = jnp.where(diff >= 0, jnp.exp(log_g[:, None, None] * jnp.maximum(diff, 0.0)), 0.0)
    inner = jnp.einsum('bnihd,bnjhd->bnhij', qc, kc) * dmask
    o_inner = jnp.einsum('bnhij,bnjhe->bnihe', inner, vc)
    zeta = jnp.exp(log_g[:, None] * (C - 1.0 - idx)[None, :])
    kv = jnp.einsum('bnjhd,hj,bnjhe->bnhde', kc, zeta, vc)
    chunk_decay = jnp.exp(log_g * C)[:, None, None]

    def step(state, kv_n):
        return state * chunk_decay + kv_n, state

    state0 = jnp.zeros((B, H, DK, DV), kv.dtype)
    _, prev = lax.scan(step, state0, jnp.moveaxis(kv, 1, 0))
    prev = jnp.moveaxis(prev, 0, 1)
    xi = jnp.exp(log_g[:, None] * (idx + 1.0)[None, :])
    o_cross = jnp.einsum('bnihd,bnhde->bnihe', qc, prev) * xi.T[None, None, :, :, None]
    return (o_inner + o_cross).reshape(B, L, H, DV).astype(jnp.float32)


def retention_mixer(h, w_in):
    B, L, _ = h.shape
    pos = jnp.arange(L)
    q, k, v, g, q_mem = split_cols(h @ w_in, RET_SIZES)
    q = rope(q.reshape(B, L, RET_H, RET_DK), pos)
    k = rope(k.reshape(B, L, RET_H, RET_DK), pos) * (RET_DK ** -0.5)
    v = v.reshape(B, L, RET_H, RET_DV)
    o = chunkwise_retention(q, k, v)
    mu = jnp.mean(o, axis=-1, keepdims=True)
    var = jnp.mean(jnp.square(o - mu), axis=-1, keepdims=True)
    o = ((o - mu) * lax.rsqrt(var + EPS)).astype(h.dtype)
    y = jax.nn.silu(g) * o.reshape(B, L, RET_H * RET_DV)
    return y, q_mem


def dsa_mixer(h, w_in, qn, kn):
    B, L, _ = h.shape
    pos = jnp.arange(L)
    q, k, v, iq, ik, iw, q_mem = split_cols(h @ w_in, DSA_SIZES)
    q = rope(rms_norm(q.reshape(B, L, DSA_H, HD), qn), pos)
    k = rope(rms_norm(k.reshape(B, L, 1, HD), kn), pos)[:, :, 0]
    iq = rope(iq.reshape(B, L, IDX_H, IDX_D), pos)
    ik = rope(ik.reshape(B, L, 1, IDX_D), pos)[:, :, 0]
    topk = min(DSA_TOPK_MAX, L // 4)
    scale = HD ** -0.5
    gather = jax.vmap(lambda t, s: t[s])

    def block(i):
        qs = i * Q_BLK
        qpos = qs + jnp.arange(Q_BLK)
        q_b = lax.dynamic_slice_in_dim(q, qs, Q_BLK, axis=1)
        iq_b = lax.dynamic_slice_in_dim(iq, qs, Q_BLK, axis=1)
        iw_b = lax.dynamic_slice_in_dim(iw, qs, Q_BLK, axis=1)
        rel = jax.nn.relu(jnp.einsum('bqhd,bsd->bqhs', iq_b, ik).astype(jnp.float32))
        score = jnp.einsum('bqh,bqhs->bqs', iw_b.astype(jnp.float32), rel)
        causal = pos[None, :] <= qpos[:, None]
        score = jnp.where(causal[None], score, -jnp.inf)
        _, sel = lax.top_k(score, topk)
        k_sel = gather(k, sel)
        v_sel = gather(v, sel)
        valid = (sel <= qpos[None, :, None])[:, :, None, :]
        lg = jnp.einsum('bqhd,bqkd->bqhk', q_b, k_sel) * scale
        p = masked_softmax(lg, valid)
        return jnp.einsum('bqhk,bqkd->bqhd', p.astype(v_sel.dtype), v_sel)

    o = lax.map(block, jnp.arange(L // Q_BLK))
    o = jnp.moveaxis(o, 0, 1).reshape(B, L, DSA_H * HD)
    return o, q_mem


def nsa_mixer(h, w_in, qn, kn, cmp_pos_k, cmp_pos_v, cmp_wk, cmp_wv):
    B, L, _ = h.shape
    R = NSA_H // NSA_G
    pos = jnp.arange(L)
    scale = HD ** -0.5
    q, kc, vc, ks, vs, kw, vw, gates, q_mem = split_cols(h @ w_in, NSA_SIZES)
    q = rope(rms_norm(q.reshape(B, L, NSA_H, HD), qn), pos).reshape(B, L, NSA_G, R, HD)
    grp = lambda t: t.reshape(B, L, NSA_G, HD)
    kc, vc, vs, vw = grp(kc), grp(vc), grp(vs), grp(vw)
    ks = rope(rms_norm(grp(ks), kn[1]), pos)
    kw = rope(rms_norm(grp(kw), kn[2]), pos)

    n_cmp = (L - CMP_L) // CMP_S + 1
    starts = jnp.arange(n_cmp) * CMP_S
    ends = starts + CMP_L - 1
    blk = starts[:, None] + jnp.arange(CMP_L)[None, :]
    k_blk = kc[:, blk] + cmp_pos_k[None, None, :, None, :]
    v_blk = vc[:, blk] + cmp_pos_v[None, None, :, None, :]
    k_cmp = jnp.einsum('bnlgd,lde->bnge', k_blk, cmp_wk)
    v_cmp = jnp.einsum('bnlgd,lde->bnge', v_blk, cmp_wv)
    k_cmp = rope(rms_norm(k_cmp, kn[0]), ends)
    valid_c = ends[None, :] <= pos[:, None]
    lg = jnp.einsum('bqgrd,bngd->bgrqn', q, k_cmp) * scale
    p_cmp = masked_softmax(lg, valid_c) * jnp.any(valid_c, axis=-1)[:, None]
    o_cmp = jnp.einsum('bgrqn,bngd->bqgrd', p_cmp.astype(v_cmp.dtype), v_cmp)

    n_slc = L // SLC_L
    n_sel = min(SLC_N_MAX, n_slc)
    sstart = jnp.arange(n_slc) * SLC_L
    cover = ((starts[:, None] < sstart[None, :] + SLC_L) &
             (starts[:, None] + CMP_L > sstart[None, :])).astype(jnp.float32)
    imp = jnp.einsum('bgrqn,nj->bgqj', p_cmp, cover)
    jj = jnp.arange(n_slc)[None, :]
    qblk = (pos // SLC_L)[:, None]
    forced = (jj == 0) | (jj == qblk) | (jj == qblk - 1)
    causal_blk = sstart[None, :] <= pos[:, None]
    imp = jnp.where(causal_blk, imp + forced.astype(jnp.float32) * FORCE, NEG)
    _, sel = lax.top_k(imp, n_sel)
    ks_blk = ks.reshape(B, n_slc, SLC_L, NSA_G, HD).transpose(0, 3, 1, 2, 4)
    vs_blk = vs.reshape(B, n_slc, SLC_L, NSA_G, HD).transpose(0, 3, 1, 2, 4)
    gather = jax.vmap(jax.vmap(lambda t, s: t[s]))

    def slc_block(i):
        qs = i * SLC_Q_BLK
        qpos = qs + jnp.arange(SLC_Q_BLK)
        q_c = lax.dynamic_slice_in_dim(q, qs, SLC_Q_BLK, axis=1)
        s_c = lax.dynamic_slice_in_dim(sel, qs, SLC_Q_BLK, axis=2)
        k_g = gather(ks_blk, s_c)
        v_g = gather(vs_blk, s_c)
        kpos = s_c[..., None] * SLC_L + jnp.arange(SLC_L)
        valid = (kpos <= qpos[None, None, :, None, None])[:, :, None]
        lg_s = jnp.einsum('bqgrd,bgqnld->bgrqnl', q_c, k_g) * scale
        lg_s = jnp.where(valid, lg_s.astype(jnp.float32), NEG)
        p = jax.nn.softmax(lg_s.reshape(B, NSA_G, R, SLC_Q_BLK, n_sel * SLC_L), axis=-1).reshape(lg_s.shape)
        return jnp.einsum('bgrqnl,bgqnld->bqgrd', p.astype(v_g.dtype), v_g)

    o_slc = lax.map(slc_block, jnp.arange(L // SLC_Q_BLK))
    o_slc = jnp.moveaxis(o_slc, 0, 1).reshape(B, L, NSA_G, R, HD)

    pad = ((0, 0), (WIN, 0), (0, 0), (0, 0))
    kw_p, vw_p = jnp.pad(kw, pad), jnp.pad(vw, pad)

    def win_block(i):
        qs = i * Q_BLK
        qpos = qs + jnp.arange(Q_BLK)
        kpos = qs - WIN + jnp.arange(WIN + Q_BLK)
        q_b = lax.dynamic_slice_in_dim(q, qs, Q_BLK, axis=1)
        k_b = lax.dynamic_slice_in_dim(kw_p, qs, WIN + Q_BLK, axis=1)
        v_b = lax.dynamic_slice_in_dim(vw_p, qs, WIN + Q_BLK, axis=1)
        d = qpos[:, None] - kpos[None, :]
        valid = (d >= 0) & (d < WIN) & (kpos[None, :] >= 0)
        lg_w = jnp.einsum('bqgrd,bkgd->bgrqk', q_b, k_b) * scale
        p = masked_softmax(lg_w, valid)
        return jnp.einsum('bgrqk,bkgd->bqgrd', p.astype(v_b.dtype), v_b)

    o_win = lax.map(win_block, jnp.arange(L // Q_BLK))
    o_win = jnp.moveaxis(o_win, 0, 1).reshape(B, L, NSA_G, R, HD)

    g = jax.nn.sigmoid(gates.reshape(B, L, NSA_G, R, 3))
    o = g[..., 0:1] * o_cmp + g[..., 1:2] * o_slc + g[..., 2:3] * o_win
    return o.reshape(B, L, NSA_H * HD), q_mem


def memory_attention(q_raw, mem, norm_g, w_kv, qn, kn):
    B, L, _ = q_raw.shape
    M = mem.shape[1]
    k, v = split_cols(rms_norm(mem, norm_g) @ w_kv, [MEM_W, MEM_W])
    k = rms_norm(k.reshape(B, M, H_MEM, HD), kn)
    v = v.reshape(B, M, H_MEM, HD)
    q = rms_norm(q_raw.reshape(B, L, H_MEM, HD), qn)
    lg = jnp.einsum('bqhd,bmhd->bhqm', q, k).astype(jnp.float32) * (HD ** -0.5)
    p = jax.nn.softmax(lg, axis=-1)
    return jnp.einsum('bhqm,bmhd->bqhd', p.astype(v.dtype), v).reshape(B, L, MEM_W)


def setup_inputs(seed: int = 0) -> dict:
    key = jax.random.key(seed)
    ks = jax.random.split(key, 24)
    f32 = jnp.float32

    def nrm(k, shape, fan_in):
        return jax.random.normal(k, shape, f32) * (fan_in ** -0.5)

    def gain(k, shape):
        return 1.0 + 0.05 * jax.random.normal(k, shape, f32)

    return {
        'x': jax.random.normal(ks[0], (BATCH, SEQ, D_MODEL), f32),
        'mem': jax.random.normal(ks[1], (BATCH, N_MEM, D_MODEL), f32),
        'ffn_norm': gain(ks[2], (DEPTH, 2, D_MODEL)),
        'ffn_w_gate': nrm(ks[3], (DEPTH, 2, D_MODEL, D_FF), D_MODEL),
        'ffn_w_up': nrm(ks[4], (DEPTH, 2, D_MODEL, D_FF), D_MODEL),
        'ffn_w_down': nrm(ks[5], (DEPTH, 2, D_FF, D_MODEL), D_FF),
        'mix_norm': gain(ks[6], (DEPTH, D_MODEL)),
        'w_out': nrm(ks[7], (DEPTH, MIX_W + MEM_W, D_MODEL), MIX_W + MEM_W),
        'mem_norm': gain(ks[8], (DEPTH, D_MODEL)),
        'mem_w_kv': nrm(ks[9], (DEPTH, D_MODEL, 2 * MEM_W), D_MODEL),
        'mem_qn': gain(ks[10], (DEPTH, HD)),
        'mem_kn': gain(ks[11], (DEPTH, HD)),
        'ret_w_in': nrm(ks[12], (N_A, D_MODEL, RET_COLS), D_MODEL),
        'dsa_w_in': nrm(ks[13], (N_B, D_MODEL, DSA_COLS), D_MODEL),
        'dsa_qn': gain(ks[14], (N_B, HD)),
        'dsa_kn': gain(ks[15], (N_B, HD)),
        'nsa_w_in': nrm(ks[16], (N_C, D_MODEL, NSA_COLS), D_MODEL),
        'nsa_qn': gain(ks[17], (N_C, HD)),
        'nsa_kn': gain(ks[18], (N_C, 3, HD)),
        'nsa_cmp_pos_k': 0.1 * jax.random.normal(ks[19], (N_C, CMP_L, HD), f32),
        'nsa_cmp_pos_v': 0.1 * jax.random.normal(ks[20], (N_C, CMP_L, HD), f32),
        'nsa_cmp_wk': nrm(ks[21], (N_C, CMP_L, HD, HD), CMP_L * HD),
        'nsa_cmp_wv': nrm(ks[22], (N_C, CMP_L, HD, HD), CMP_L * HD),
    }


def reference(x, mem, ffn_norm, ffn_w_gate, ffn_w_up, ffn_w_down, mix_norm, w_out,
              mem_norm, mem_w_kv, mem_qn, mem_kn, ret_w_in, dsa_w_in, dsa_qn, dsa_kn,
              nsa_w_in, nsa_qn, nsa_kn, nsa_cmp_pos_k, nsa_cmp_pos_v, nsa_cmp_wk, nsa_cmp_wv):
    for i in range(DEPTH):
        h = rms_norm(x, ffn_norm[i, 0])
        x = x + 0.5 * swiglu(h, ffn_w_gate[i, 0], ffn_w_up[i, 0], ffn_w_down[i, 0])
        h = rms_norm(x, mix_norm[i])
        kind, j = i % N_MIXERS, i // N_MIXERS
        if kind == 0:
            y_mix, q_mem = retention_mixer(h, ret_w_in[j])
        elif kind == 1:
            y_mix, q_mem = dsa_mixer(h, dsa_w_in[j], dsa_qn[j], dsa_kn[j])
        else:
            y_mix, q_mem = nsa_mixer(h, nsa_w_in[j], nsa_qn[j], nsa_kn[j], nsa_cmp_pos_k[j],
                                     nsa_cmp_pos_v[j], nsa_cmp_wk[j], nsa_cmp_wv[j])
        y_mem = memory_attention(q_mem, mem, mem_norm[i], mem_w_kv[i], mem_qn[i], mem_kn[i])
        x = x + jnp.concatenate([y_mix, y_mem], axis=-1) @ w_out[i]
        h = rms_norm(x, ffn_norm[i, 1])
        x = x + 0.5 * swiglu(h, ffn_w_gate[i, 1], ffn_w_up[i, 1], ffn_w_down[i, 1])
    return x
```

```python
import os
import numpy as np
from contextlib import ExitStack
import concourse.bass as bass
import concourse.mybir as mybir
from concourse.bass_utils import run_bass_kernel_spmd

F32 = mybir.dt.float32
BF16 = mybir.dt.bfloat16
U8 = mybir.dt.uint8
AF = mybir.ActivationFunctionType
ALU = mybir.AluOpType
AX = mybir.AxisListType

D = 1024
L = 2048
DEPTH = 4
DFF = 2816
NMEM = 256
NCORES = 8
SEQ_PER_CORE = 2
EPS = 1e-6
NT = L // 128

ENGS = ("pe", "act", "dve", "pool", "sp")


class Buf:
    __slots__ = ("name", "w", "r", "excl")

    def __init__(self, name, excl=False):
        self.name = name
        self.excl = excl
        self.w = None
        self.r = {}


def bufs(name, *dims):
    if len(dims) == 1:
        return [Buf(f"{name}{i}") for i in range(dims[0])]
    return [bufs(f"{name}{i}_", *dims[1:]) for i in range(dims[0])]


class Sched:
    def __init__(self):
        self.ops = {e: [] for e in ENGS}
        self.seen = {e: {} for e in ENGS}
        self.dma_count = {}
        self.needed = set()
        self.cap = None

    def _deps(self, eng, reads, writes):
        deps = {}
        for b in reads:
            if b.w is not None:
                deps[b.w[:2]] = max(deps.get(b.w[:2], -1), b.w[2])
            if b.excl:
                for k in b.r.values():
                    if not (k[0] == "eng" and k[1] == eng):
                        deps[k[:2]] = max(deps.get(k[:2], -1), k[2])
        for b in writes:
            if b.w is not None and not (b.w[0] == "eng" and b.w[1] == eng):
                deps[b.w[:2]] = max(deps.get(b.w[:2], -1), b.w[2])
            for k in b.r.values():
                if not (k[0] == "eng" and k[1] == eng):
                    deps[k[:2]] = max(deps.get(k[:2], -1), k[2])
        return self._waits(eng, deps)

    def _waits(self, eng, deps):
        waits = []
        seen = self.seen[eng]
        for k, idx in deps.items():
            if seen.get(k, -1) >= idx:
                continue
            seen[k] = idx
            waits.append((k, idx))
            if k[0] == "eng":
                self.needed.add((k[1], idx))
        return waits

    def capture(self, fn):
        assert self.cap is None
        self.cap = []
        fn()
        rec, self.cap = self.cap, None
        return rec

    def commit_interleaved(self, *lists):
        lists = [l for l in lists if l]
        pos = [0] * len(lists)
        total = sum(len(l) for l in lists)
        for _ in range(total):
            k = min((i for i in range(len(lists)) if pos[i] < len(lists[i])),
                    key=lambda i: (pos[i] + 1) / len(lists[i]))
            kind, args = lists[k][pos[k]]
            pos[k] += 1
            (self.op if kind == "op" else self.dma)(*args)

    def op(self, eng, emit, reads=(), writes=()):
        if self.cap is not None:
            self.cap.append(("op", (eng, emit, tuple(reads), tuple(writes))))
            return None
        waits = self._deps(eng, reads, writes)
        idx = len(self.ops[eng])
        key = ("eng", eng, idx)
        self.ops[eng].append(("op", waits, emit, None))
        for b in reads:
            b.r[("eng", eng)] = key
        for b in writes:
            b.w = key
            b.r = {}
        return key

    def dma(self, queue, sem, emit, reads=(), writes=()):
        if self.cap is not None:
            self.cap.append(("dma", (queue, sem, emit, tuple(reads), tuple(writes))))
            return None
        waits = self._deps(queue, reads, writes)
        c = self.dma_count.get(sem, 0) + 1
        self.dma_count[sem] = c
        key = ("dma", sem, c)
        self.ops[queue].append(("dma", waits, emit, sem))
        for b in reads:
            b.r[("dma", sem)] = key
        for b in writes:
            b.w = key
            b.r = {}
        return key

    def seal(self, sem, bl):
        c = self.dma_count[sem]
        for b in bl:
            if b.w is not None and b.w[0] == "dma" and b.w[1] == sem:
                b.w = ("dma", sem, c)
            k = b.r.get(("dma", sem))
            if k is not None:
                b.r[("dma", sem)] = ("dma", sem, c)

    def barrier(self):
        last = {}
        for e in ENGS:
            idx = None
            for i in range(len(self.ops[e]) - 1, -1, -1):
                if self.ops[e][i][0] == "op":
                    idx = i
                    break
            if idx is not None:
                last[("eng", e)] = idx
        for s, cnt in self.dma_count.items():
            last[("dma", s)] = cnt
        for e in ENGS:
            deps = {k: v for k, v in last.items() if k != ("eng", e)}
            waits = self._waits(e, deps)
            if waits:
                self.ops[e].append(("fin", waits, None, None))

    def finish(self, eng="sp"):
        waits = []
        for sem, c in self.dma_count.items():
            if self.seen[eng].get(("dma", sem), -1) < c:
                waits.append((("dma", sem), c))
        self.ops[eng].append(("fin", waits, None, None))

    def emit(self, nc, stack):
        esem = {e: stack.enter_context(nc.semaphore(f"s_{e}")) for e in ENGS}
        dsem = {s: stack.enter_context(nc.semaphore(f"d_{s}")) for s in self.dma_count}
        val = {}
        for e in ENGS:
            n = 0
            for i in range(len(self.ops[e])):
                if (e, i) in self.needed:
                    n += 1
                    val[(e, i)] = n
        ops = self.ops

        def run(e, engine):
            for i, (kind, waits, emit, sem) in enumerate(ops[e]):
                for k, idx in waits:
                    if k[0] == "eng":
                        engine.wait_ge(esem[k[1]], val[(k[1], idx)])
                    else:
                        engine.wait_ge(dsem[k[1]], 16 * idx)
                if kind == "fin":
                    continue
                ins = emit(engine)
                if kind == "dma":
                    ins.then_inc(dsem[sem], 16)
                elif (e, i) in self.needed:
                    ins.then_inc(esem[e], 1)

        with nc.Block() as block:
            @block.sync
            def _(eng):
                run("sp", eng)

            @block.scalar
            def _(eng):
                run("act", eng)

            @block.vector
            def _(eng):
                run("dve", eng)

            @block.gpsimd
            def _(eng):
                run("pool", eng)

            @block.tensor
            def _(eng):
                run("pe", eng)


class Region:
    def __init__(self, t, off, size):
        self.t = t
        self.base = off
        self.off = off
        self.end = off + size

    def take(self, shape, dtype, parts=None):
        esz = 2 if dtype == BF16 else 4
        n = int(np.prod(shape))
        nb = ((n * esz + 31) // 32) * 32
        assert self.off + nb <= self.end, ("SBUF region overflow", shape, self.off, self.end)
        ap = self.t[:, self.off:self.off + n * esz].bitcast(dtype)
        self.off += nb
        if len(shape) > 1:
            names = " ".join(f"d{i}" for i in range(len(shape)))
            kw = {f"d{i}": int(s) for i, s in enumerate(shape)}
            ap = ap.rearrange(f"p ({names}) -> p {names}", **kw)
        return ap


class Ctx:
    pass


def MM(c, out, lhsT, rhs, start, stop, reads, writes, skip=True):
    c.S.op("pe", lambda e: e.matmul(out, lhsT, rhs, start=start, stop=stop, skip_group_check=skip), reads, writes)


def TR(c, out, in_, reads, writes):
    n = in_.shape[0]
    c.S.op("pe", lambda e: e.transpose(out, in_, c.ident_bf[0:n, 0:n]), list(reads) + [c.constb], writes)


def ACTF(c, out, in_, func, reads, writes, scale=1.0, bias=0.0):
    c.S.op("act", lambda e: e.activation(out=out, in_=in_, func=func, bias=bias, scale=scale), reads, writes)


def TT(c, eng, out, in0, in1, op, reads, writes):
    c.S.op(eng, lambda e: e.tensor_tensor(out=out, in0=in0, in1=in1, op=op), reads, writes)


def TS(c, eng, out, in0, s1, s2, op0, op1, reads, writes):
    if s2 is None:
        c.S.op(eng, lambda e: e.tensor_scalar(out=out, in0=in0, scalar1=s1, scalar2=None, op0=op0), reads, writes)
    else:
        c.S.op(eng, lambda e: e.tensor_scalar(out=out, in0=in0, scalar1=s1, scalar2=s2, op0=op0, op1=op1), reads, writes)


def STT(c, out, in0, scalar, in1, op0, op1, reads, writes):
    c.S.op("dve", lambda e: e.scalar_tensor_tensor(out=out, in0=in0, scalar=scalar, in1=in1, op0=op0, op1=op1),
           reads, writes)


def RED(c, out, in_, op, reads, writes):
    c.S.op("dve", lambda e: e.tensor_reduce(out=out, in_=in_, axis=AX.X, op=op), reads, writes)


def RCP(c, out, in_, reads, writes):
    c.S.op("dve", lambda e: e.reciprocal(out=out, in_=in_), reads, writes)


def CP(c, eng, out, in_, reads, writes):
    if eng == "act":
        c.S.op("act", lambda e: e.copy(out=out, in_=in_), reads, writes)
    else:
        c.S.op(eng, lambda e: e.tensor_copy(out=out, in_=in_), reads, writes)


def MSET(c, eng, ap, val, writes):
    c.S.op(eng, lambda e: e.memset(ap, val), (), writes)


def bank(c, b):
    return c.pst[b // 2][:, (b % 2) * 512:(b % 2) * 512 + 512]


def ps1(c):
    b = c.rot1[c.psp % len(c.rot1)]
    c.psp += 1
    return bank(c, b), [c.psb[b]]


def ps2(c):
    b = c.rot2[c.psp2 % len(c.rot2)]
    c.psp2 += 1
    return c.pst[b // 2][:, :], [c.psb[b], c.psb[b + 1]]


def set_rot(c, rot1, rot2):
    c.rot1, c.rot2 = list(rot1), list(rot2)


CST_IDENT, CST_TRI, CST_COS, CST_SIN, CST_ZK, CST_EPSX, CST_CD = 0, 128, 256, 768, 1280, 1286, 1292
CST_CNEG = 1296
CST_BISC = 1296 + 128
NCST = 1296 + 128 + 32
MASKM = 29952.0
PP_L0 = 64
PP_PL = 464
PP_MIX, PP_MEMN, PP_MQN, PP_MKN, PP_A, PP_B, PP_C, PP_D, PP_POS = 0, 8, 16, 80, 144, 208, 272, 336, 400
NPP = PP_L0 + DEPTH * PP_PL


def host_consts():
    cst = np.zeros((128, NCST), np.float64)
    cst[:, CST_IDENT:CST_IDENT + 128] = np.eye(128)
    j = np.arange(128)[:, None]
    i = np.arange(128)[None, :]
    cst[:, CST_TRI:CST_TRI + 128] = (i >= j)
    cst[:, CST_CNEG:CST_CNEG + 128] = np.where(i <= j, 0.0, -1e30)
    cst[:, CST_BISC:CST_BISC + 32] = 2.0 ** (-np.arange(32))[None, :]
    inv = (np.float32(10000.0) ** (-np.arange(32, dtype=np.float32) / np.float32(32))).astype(np.float32)
    pos = (np.arange(16)[None, :, None] * 128 + np.arange(128)[:, None, None]).astype(np.float32)
    ang = (pos * inv[None, None, :]).astype(np.float32)
    cst[:, CST_COS:CST_COS + 512] = np.cos(ang).reshape(128, 512)
    cst[:, CST_SIN:CST_SIN + 512] = np.sin(ang).reshape(128, 512)
    h = np.arange(6)
    gam = 1.0 - 2.0 ** (-5.0 - h)
    t = np.arange(128)[:, None]
    cst[:, CST_ZK:CST_ZK + 6] = gam[None, :] ** (-(t + 1.0)) / 8.0
    cst[:, CST_EPSX:CST_EPSX + 6] = EPS * gam[None, :] ** (-2.0 * (t + 1.0))
    for pair in range(3):
        cst[0:64, CST_CD + pair] = gam[2 * pair] ** 128
        cst[64:128, CST_CD + pair] = gam[2 * pair + 1] ** 128
    return cst.astype(np.float32)


def host_consts2():
    t = np.arange(128)[:, None]
    s_ = np.arange(128)[None, :]
    masks = np.zeros((128, 768), np.float32)
    masks[:, 0:128] = (s_ <= t)
    masks[:, 128:256] = (s_ > t)
    m = np.arange(256)[None, :] - 128
    masks[:, 256:512] = (16 * m + 31 <= t)
    for k in range(8):
        masks[k, 512 + 128 + k] = 1.0
    czca = np.zeros((128, 128), np.float32)
    mm = np.arange(64)[None, :] - 32
    qb = (t >= 64).astype(np.int64)
    causal = mm <= qb
    forced = (mm == qb) | (mm == qb - 1)
    czca[:, 0:64] = causal
    czca[:, 64:128] = np.where(causal, np.where(forced, 1e9, 0.0), -1e30)
    starts = np.arange(127) * 16
    sst = np.arange(32) * 64
    cover = np.zeros((128, 32), np.float32)
    cover[:127] = ((starts[:, None] < sst[None, :] + 64) & (starts[:, None] + 32 > sst[None, :]))
    inv = (np.float32(10000.0) ** (-np.arange(32, dtype=np.float32) / np.float32(32))).astype(np.float32)
    rope = np.zeros((16, 8, 64), np.float32)
    for ti in range(16):
        for k in range(8):
            n = k if ti == 0 else 8 * ti - 1 + k
            pos = np.float32(16 * n + 31)
            ang = (pos * inv).astype(np.float32)
            rope[ti, k, 0:32] = np.cos(ang)
            rope[ti, k, 32:64] = np.sin(ang)
    return {"c2_masks": masks, "c2_czca": czca, "c2_cover": cover, "c2_rope": rope}


def host_params(inp):
    pp = np.zeros((128, NPP), np.float32)
    f32 = lambda a: np.asarray(a, np.float32)
    pp[:, 0:64] = f32(inp["ffn_norm"]).reshape(DEPTH, 2, 8, 128).transpose(3, 0, 1, 2).reshape(128, 64)
    for l in range(DEPTH):
        b = PP_L0 + l * PP_PL
        pp[:, b + PP_MIX:b + PP_MIX + 8] = f32(inp["mix_norm"][l]).reshape(8, 128).T
        pp[:, b + PP_MEMN:b + PP_MEMN + 8] = f32(inp["mem_norm"][l]).reshape(8, 128).T
        pp[:, b + PP_MQN:b + PP_MQN + 64] = f32(inp["mem_qn"][l])[None, :]
        pp[:, b + PP_MKN:b + PP_MKN + 64] = f32(inp["mem_kn"][l])[None, :]
        kind, j = l % 3, l // 3
        if kind == 1:
            pp[:, b + PP_A:b + PP_A + 64] = f32(inp["dsa_qn"][j])[None, :]
            pp[:, b + PP_B:b + PP_B + 64] = f32(inp["dsa_kn"][j])[None, :]
        elif kind == 2:
            pp[:, b + PP_A:b + PP_A + 64] = f32(inp["nsa_qn"][j])[None, :]
            pp[:, b + PP_B:b + PP_B + 64] = f32(inp["nsa_kn"][j][0])[None, :]
            pp[:, b + PP_C:b + PP_C + 64] = f32(inp["nsa_kn"][j][1])[None, :]
            pp[:, b + PP_D:b + PP_D + 64] = f32(inp["nsa_kn"][j][2])[None, :]
            pp[0:64, b + PP_POS:b + PP_POS + 32] = f32(inp["nsa_cmp_pos_k"][j]).T
            pp[0:64, b + PP_POS + 32:b + PP_POS + 64] = f32(inp["nsa_cmp_pos_v"][j]).T
    return pp


def rmsnorm_block(c, src, src_bufs, ntok, g_ap, sq, sqb, rstd, rstdb, out, out_bufs):
    ACTF(c, sq[:, :, 0:ntok], src, AF.Square, src_bufs, [sqb])
    ps, pb = ps1(c)
    for dc in range(8):
        MM(c, ps[:, 0:ntok], c.ones_bf[:, :], sq[:, dc, 0:ntok], dc == 0, dc == 7, [sqb, c.constb], pb, skip=False)
    ACTF(c, rstd[:, 0:ntok], ps[:, 0:ntok], AF.Sqrt, pb, [rstdb], scale=1.0 / D, bias=float(EPS))
    RCP(c, rstd[:, 0:ntok], rstd[:, 0:ntok], [rstdb], [rstdb])
    for dc in range(8):
        STT(c, out[:, dc, :], src[:, dc, :], g_ap[:, dc:dc + 1], rstd[:, 0:ntok], ALU.mult, ALU.mult,
            [src_bufs[dc], rstdb, c.ppb], [out_bufs[dc]])


NG = 11
NTB = 4
WSLOTS = 3


def ffn(c, wg_d, wu_d, wd_d, g_ap):
    S = c.S
    S.barrier()
    R = Region(c.arena, c.phase_off, c.phase_size)
    sq = R.take((8, 512), BF16)
    sqb = Buf("sq")
    rstd = R.take((512,), F32)
    rstdb = Buf("rstd")
    sg = R.take((512,), BF16)
    sgb = Buf("sg")
    act = [[R.take((512,), BF16) for _ in range(2)] for _ in range(2)]
    actb = bufs("act", 2, 2)
    wg = [R.take((8, 256), BF16) for _ in range(WSLOTS)]
    wu = [R.take((8, 256), BF16) for _ in range(WSLOTS)]
    wd = [R.take((2, D), BF16) for _ in range(WSLOTS)]
    wgb, wub, wdb = bufs("wg", WSLOTS), bufs("wu", WSLOTS), bufs("wd", WSLOTS)
    ps = [c.pst[b // 2][:, (b % 2) * 512:(b % 2) * 512 + 512] for b in range(8)]

    wg_v = wg_d.rearrange("(dc p) f -> p dc f", p=128)
    wu_v = wu_d.rearrange("(dc p) f -> p dc f", p=128)
    wd_v = wd_d.rearrange("(fc p) d -> p fc d", p=128)
    wn = [0]

    def load_w(gi):
        s = wn[0] % WSLOTS
        wn[0] += 1
        fs = slice(gi * 256, (gi + 1) * 256)
        S.dma("pool", f"wg{s}", lambda e: e.dma_start(out=wg[s][:, :, :], in_=wg_v[:, :, fs]), writes=[wgb[s]])
        S.dma("pool", f"wu{s}", lambda e: e.dma_start(out=wu[s][:, :, :], in_=wu_v[:, :, fs]), writes=[wub[s]])
        S.dma("pool", f"wd{s}", lambda e: e.dma_start(out=wd[s][:, :, :], in_=wd_v[:, 2 * gi:2 * gi + 2, :]),
              writes=[wdb[s]])
        return s

    slots = {}
    for gi in range(min(WSLOTS - 1, NG)):
        slots[gi] = load_w(gi)

    for tb in range(NTB):
        ts = slice(tb * 512, (tb + 1) * 512)
        rmsnorm_block(c, c.xT[:, :, ts], [c.xb[dc][tb] for dc in range(8)], 512, g_ap, sq, sqb, rstd, rstdb,
                      c.hT[:, :, ts], [c.hb[dc][tb] for dc in range(8)])

    pend = None
    steps = [(gi, tb) for gi in range(NG) for tb in range(NTB)]
    for it in range(len(steps) + 1):
        cur = steps[it] if it < len(steps) else None
        par = it % 2
        gu = []
        if cur is not None:
            gi, tb = cur
            s = slots[gi]
            ts = slice(tb * 512, (tb + 1) * 512)
            for fcl in range(2):
                for which in range(2):
                    w = wg[s] if which == 0 else wu[s]
                    wb = wgb[s] if which == 0 else wub[s]
                    bank = fcl * 2 + which
                    for dc in range(8):
                        gu.append((ps[bank], w[:, dc, fcl * 128:(fcl + 1) * 128], c.hT[:, dc, ts], dc == 0, dc == 7,
                                   [wb, c.hb[dc][tb]], [c.psb[bank]]))
        dn = []
        if pend is not None:
            ps_, ptb, ppar = pend
            pts = slice(ptb * 512, (ptb + 1) * 512)
            for dc in range(8):
                bank = 4 + dc % 4
                for fcl in range(2):
                    dn.append((ps[bank], wd[ps_][:, fcl, dc * 128:(dc + 1) * 128], act[ppar][fcl][:, :], fcl == 0,
                               fcl == 1, [wdb[ps_], actb[ppar][fcl]], [c.psb[bank]],
                               dc if fcl == 1 else None, bank, pts, ptb))
        gi_ = 0
        di_ = 0
        while gi_ < len(gu) or di_ < len(dn):
            for _ in range(4):
                if gi_ < len(gu):
                    o_, l_, r_, st_, sp_, rd_, wr_ = gu[gi_]
                    MM(c, o_, l_, r_, st_, sp_, rd_, wr_, skip=False)
                    gi_ += 1
                    if gi_ % 16 == 0:
                        fcl = gi_ // 16 - 1
                        ACTF(c, sg[:, :], ps[fcl * 2], AF.Silu, [c.psb[fcl * 2]], [sgb])
                        TT(c, "dve", act[par][fcl][:, :], ps[fcl * 2 + 1], sg[:, :], ALU.mult,
                           [c.psb[fcl * 2 + 1], sgb], [actb[par][fcl]])
            for _ in range(2):
                if di_ < len(dn):
                    o_, l_, r_, st_, sp_, rd_, wr_, dc, bank, pts, ptb = dn[di_]
                    MM(c, o_, l_, r_, st_, sp_, rd_, wr_, skip=False)
                    di_ += 1
                    if dc is not None:
                        STT(c, c.xT[:, dc, pts], ps[bank], 0.5, c.xT[:, dc, pts], ALU.mult, ALU.add,
                            [c.psb[bank], c.xb[dc][ptb]], [c.xb[dc][ptb]])
        pend = (slots[cur[0]], cur[1], par) if cur is not None else None
        if cur is not None and cur[1] == 0 and cur[0] + WSLOTS - 1 < NG:
            slots[cur[0] + WSLOTS - 1] = load_w(cur[0] + WSLOTS - 1)


def head_norm(c, src, src_bufs, H, gain, out, out_bufs, scr, scrb, ss, ssb, eng2="pool"):
    n = src.shape[0]
    s3 = scr[0:n, 0:H * 64].rearrange("p (h d) -> p h d", d=64)
    ACTF(c, s3, src, AF.Square, src_bufs, [scrb])
    RED(c, ss[0:n, 0:H], s3, ALU.add, [scrb], [ssb])
    ACTF(c, ss[0:n, 0:H], ss[0:n, 0:H], AF.Sqrt, [ssb], [ssb], scale=1.0 / 64, bias=float(EPS))
    RCP(c, ss[0:n, 0:H], ss[0:n, 0:H], [ssb], [ssb])
    TT(c, "dve", s3, src, ss[0:n, 0:H].unsqueeze(2).broadcast_to([n, H, 64]), ALU.mult, list(src_bufs) + [ssb], [scrb])
    TT(c, eng2, out, s3, gain[0:n, :].unsqueeze(1).broadcast_to([n, H, 64]), ALU.mult, [scrb, c.ppb], out_bufs)


def mem_prep(c, R, memT_d, wkv_d, pbase, k_memT, v_mem1, kvb):
    S = c.S
    memT = R.take((8, NMEM), F32)
    memb = bufs("memT", 8)
    sqm = R.take((8, NMEM), BF16)
    sqmb = Buf("sqm")
    rstdm = R.take((NMEM,), F32)
    rstdmb = Buf("rstdm")
    memn = R.take((8, NMEM), BF16)
    memnb = bufs("memn", 8)
    wkv = R.take((8, 512), BF16)
    wkvb = Buf("wkv")
    ktok = R.take((256,), BF16)
    ktokb = Buf("ktok")
    scr = R.take((256,), F32)
    scrb = Buf("mscr")
    ss = R.take((8,), F32)
    ssb = Buf("mss")
    mv = memT_d.rearrange("(dc p) m -> p dc m", p=128)
    for dc in range(8):
        S.dma("sp", "memin", lambda e, dc=dc: e.dma_start(out=memT[:, dc, :], in_=mv[:, dc, :]), writes=[memb[dc]])
    S.seal("memin", memb)
    S.dma("pool", "wkv", lambda e: e.dma_start(out=wkv[:, :, :], in_=wkv_d.rearrange("(dc p) f -> p dc f", p=128)),
          writes=[wkvb])
    rmsnorm_block(c, memT, memb, NMEM, c.pp[:, pbase + PP_MEMN:pbase + PP_MEMN + 8], sqm, sqmb, rstdm, rstdmb,
                  memn, memnb)
    MSET(c, "pool", v_mem1[:, :, :, 64:65], 1.0, [kvb])
    for mt in range(2):
        ps, pb = ps1(c)
        for dc in range(8):
            MM(c, ps, memn[:, dc, mt * 128:(mt + 1) * 128], wkv[:, dc, :], dc == 0, dc == 7, [memnb[dc], wkvb], pb,
               skip=False)
        head_norm(c, ps[:, 0:256].rearrange("p (h d) -> p h d", d=64), pb, 4,
                  c.pp[:, pbase + PP_MKN:pbase + PP_MKN + 64], ktok.rearrange("p (h d) -> p h d", d=64), [ktokb],
                  scr, scrb, ss, ssb)
        CP(c, "act", v_mem1[:, mt, :, 0:64], ps[:, 256:512].rearrange("p (h d) -> p h d", d=64), pb, [kvb])
        pt, ptb = ps1(c)
        ptv = pt.bitcast(BF16)
        for cc in range(2):
            TR(c, ptv[:, cc * 128:(cc + 1) * 128], ktok[:, cc * 128:(cc + 1) * 128], [ktokb], ptb)
        CP(c, "dve", k_memT[:, :, mt * 128:(mt + 1) * 128], ptv[:, 0:256].rearrange("p (c m) -> p c m", c=2), ptb, [kvb])


def tile_tail(c, W, qm_ps, qm_pb, tl):
    head_norm(c, qm_ps.rearrange("p (h d) -> p h d", d=64), qm_pb, 4, c.pp[:, W.pbase + PP_MQN:W.pbase + PP_MQN + 64],
              W.qm_tok.rearrange("p (h d) -> p h d", d=64), [W.qm_tokb], W.hscr, W.hscrb, W.hss, W.hssb)
    tile_tail_b(c, W, tl)


def tile_tail_b(c, W, tl):
    pt, ptb = ps1(c)
    ptv = pt.bitcast(BF16)
    for cc in range(2):
        TR(c, ptv[:, cc * 128:(cc + 1) * 128], W.qm_tok[:, cc * 128:(cc + 1) * 128], [W.qm_tokb], ptb)
    CP(c, "act", W.qmT[0][0:64], ptv[0:64, 0:256].rearrange("p (c m) -> p c m", c=2), ptb, [W.qmTb])
    CP(c, "act", W.qmT[1][64:128], ptv[64:128, 0:256].rearrange("p (c m) -> p c m", c=2), ptb, [W.qmTb])
    if c.rot2:
        pm, pmb = ps2(c)
        pms = [pm[:, 0:512], pm[:, 512:1024]]
    for mt in range(2):
        if c.rot2:
            pmt, pmtb = pms[mt], pmb[mt]
        else:
            pmt, pl = ps1(c)
            pmtb = pl[0]
        for h in range(4):
            hp = h % 2
            MM(c, pmt[:, h * 128:(h + 1) * 128], W.k_memT[:, h // 2, mt * 128:(mt + 1) * 128],
               W.qmT[hp][:, h // 2, :], h == 0, True, [W.kvb, W.qmTb], [pmtb])
        ACTF(c, W.PTm[:, mt * 512:(mt + 1) * 512], pmt, AF.Exp, [pmtb], [W.PTmb], scale=0.125)
    po, pob = ps1(c)
    first = True
    for h in range(4):
        for mt in range(2):
            MM(c, po[:, h * 65:(h + 1) * 65], W.PTm[:, (mt * 4 + h) * 128:(mt * 4 + h + 1) * 128], W.v_mem1[:, mt, h, :],
               first, mt == 1 and h == 3, [W.PTmb, W.kvb], pob)
            first = False
    po3 = po[:, 0:260].rearrange("p (h e) -> p h e", e=65)
    RCP(c, W.den4, po3[:, :, 64:65], pob, [W.den4b])
    TT(c, "dve", W.y_tok[:, 768:1024].rearrange("p (h d) -> p h d", d=64), po3[:, :, 0:64],
       W.den4.broadcast_to([128, 4, 64]), ALU.mult, list(pob) + [W.den4b], [W.y_tokb])
    py, pyb = ps1(c)
    pyv = py.bitcast(BF16)
    for cc in range(8):
        TR(c, pyv[:, cc * 128:(cc + 1) * 128], W.y_tok[:, cc * 128:(cc + 1) * 128], [W.y_tokb], pyb)
    CP(c, "act", W.yT[:, :, tl * 128:(tl + 1) * 128], pyv.rearrange("p (c t) -> p c t", c=8), pyb, [W.yTb])


def wout_block(c, W, tb):
    ts = slice(tb * 512, (tb + 1) * 512)
    for dc in range(8):
        pw, pwb = ps1(c)
        for cc in range(8):
            MM(c, pw, W.w_out[:, cc, dc * 128:(dc + 1) * 128], W.yT[:, cc, :], cc == 0, cc == 7, [W.w_outb, W.yTb], pwb,
               skip=False)
        TT(c, "dve", c.xT[:, dc, ts], pw, c.xT[:, dc, ts], ALU.add, list(pwb) + [c.xb[dc][tb]], [c.xb[dc][tb]])


def load_wout(c, W, R, wout_d):
    W.w_out = R.take((8, D), BF16)
    W.w_outb = Buf("w_out")
    wv = wout_d.rearrange("(cc p) d -> p cc d", p=128)
    for cc in range(0, 8, 4):
        c.S.dma("pool", "wout", lambda e, cc=cc: e.dma_start(out=W.w_out[:, cc:cc + 4, :], in_=wv[:, cc:cc + 4, :]),
                writes=[W.w_outb])
    c.S.seal("wout", [W.w_outb])


def tail_bufs(c, W, R, hscr_n=768):
    W.qm_tok = R.take((256,), BF16)
    W.qm_tokb = Buf("qm_tok")
    W.qmT = [R.take((2, 128), BF16) for _ in range(2)]
    W.qmTb = Buf("qmT")
    for a in range(2):
        MSET(c, "pool", W.qmT[a][:, :, :], 0.0, [W.qmTb])
    W.PTm = R.take((1024,), BF16)
    W.PTmb = Buf("PTm")
    W.den4 = R.take((4, 1), F32)
    W.den4b = Buf("den4")
    W.hscr = R.take((hscr_n,), F32)
    W.hscrb = Buf("hscr")
    W.hss = R.take((16,), F32)
    W.hssb = Buf("hss")
    W.y_tok = R.take((1024,), BF16)
    W.y_tokb = Buf("y_tok")
    W.k_memT = R.take((2, NMEM), BF16)
    W.v_mem1 = R.take((2, 4, 65), BF16)
    W.kvb = Buf("memkv")


def mixer_ret(c, l, win_d, wout_d, wkv_d, memT_d):
    S = c.S
    S.barrier()
    set_rot(c, range(8), (0, 2, 4, 6))
    W = Ctx()
    W.pbase = PP_L0 + l * PP_PL
    R1 = Region(c.arena, c.h_off, 32 * 1024)
    R2 = Region(c.arena, c.phase_off, c.phase_size)
    hblk = [R1.take((8, 512), BF16) for _ in range(2)]
    hblkb = bufs("hblk", 2, 8)
    W.yT = R1.take((8, 512), BF16)
    W.yTb = Buf("yT")
    sq = R1.take((8, 512), BF16)
    sqb = Buf("sq")
    w_in = R2.take((8, 2560), BF16)
    w_inb = Buf("w_in")
    load_wout(c, W, R2, wout_d)
    rstd = R2.take((512,), F32)
    rstdb = Buf("rstd")
    tail_bufs(c, W, R2, hscr_n=256)
    tq2 = [R2.take((384,), F32) for _ in range(2)]
    tq = [tq2[0], tq2[1], tq2[0], tq2[1]]
    tqb2 = bufs("tq", 2)
    tqb = [tqb2[0], tqb2[1], tqb2[0], tqb2[1]]
    rk = R2.take((768,), F32)
    rkb = Buf("rk")
    scr = R2.take((768,), F32)
    scrb = Buf("scr")
    state = R2.take((3, 128), F32)
    state_bf = R2.take((3, 128), BF16)
    stb = Buf("state")
    stbb = Buf("state_bf")
    sm = R2.take((8, 6), F32)
    smb = Buf("sm")
    qk_tok = [R2.take((768,), BF16) for _ in range(2)]
    qk_tokb = bufs("qk_tok", 2)
    qT = [[R2.take((3, 128), BF16) for _ in range(2)] for _ in range(2)]
    kT = [R2.take((3, 128), BF16) for _ in range(2)]
    qkTb = bufs("qkT", 2)
    Sm = [R2.take((6, 128), BF16) for _ in range(2)]
    Smb = bufs("Sm", 2)
    v_tok = [R2.take((768,), BF16) for _ in range(2)]
    v_tokb = bufs("v_tok", 2)
    sgt = [R2.take((768,), BF16) for _ in range(2)]
    sgtb = bufs("sgt", 2)
    qm_toks = [W.qm_tok, R2.take((256,), BF16)]
    qm_tokbs = [W.qm_tokb, Buf("qm_tok1")]
    wv = win_d.rearrange("(dc p) f -> p dc f", p=128)
    for dc in range(0, 8, 2):
        S.dma("pool", "win", lambda e, dc=dc: e.dma_start(out=w_in[:, dc:dc + 2, :], in_=wv[:, dc:dc + 2, :]),
              writes=[w_inb])
    S.seal("win", [w_inb])
    Rt = Region(c.arena, c.h_off, 32 * 1024)
    mem_prep(c, Rt, memT_d, wkv_d, W.pbase, W.k_memT, W.v_mem1, W.kvb)
    S.barrier()
    MSET(c, "dve", state[:, :, :], 0.0, [stb])
    MSET(c, "dve", state_bf[:, :, :], 0.0, [stbb])
    for p_ in range(2):
        for a in range(2):
            MSET(c, "pool", qT[p_][a][:, :, :], 0.0, [qkTb[p_]])

    g_ap = c.pp[:, W.pbase + PP_MIX:W.pbase + PP_MIX + 8]
    tri3 = c.tri_f.unsqueeze(1).broadcast_to([128, 6, 128])

    def stage_a(ti):
        set_rot(c, (0, 1, 2, 3), (0, 2))
        tb, tl = ti // 4, ti % 4
        par = ti % 2
        hb, hbb = hblk[tb % 2], hblkb[tb % 2]
        if tl == 0:
            ts = slice(tb * 512, (tb + 1) * 512)
            rmsnorm_block(c, c.xT[:, :, ts], [c.xb[dc][tb] for dc in range(8)], 512, g_ap, sq, sqb, rstd, rstdb, hb, hbb)

        def proj(out_ap, c0, c1, pbufs):
            for dc in range(8):
                MM(c, out_ap, hb[:, dc, tl * 128:(tl + 1) * 128], w_in[:, dc, c0:c1], dc == 0, dc == 7,
                   [hbb[dc], w_inb], pbufs, skip=False)

        P, Pb = ps2(c)
        proj(P[:, 0:384], 0, 384, [Pb[0]])
        proj(P[:, 512:896], 384, 768, [Pb[1]])
        v4 = P.rearrange("p (a b) -> p a b", a=2)[:, :, 0:384].rearrange("p a (h d) -> p a h d", d=64)
        x1, x2 = v4[:, :, :, 0:32], v4[:, :, :, 32:64]
        cosb = c.cos[:, ti, :].unsqueeze(1).unsqueeze(1).broadcast_to([128, 2, 6, 32])
        sinb = c.sin[:, ti, :].unsqueeze(1).unsqueeze(1).broadcast_to([128, 2, 6, 32])
        t4 = [t.rearrange("p (a h d) -> p a h d", a=2, h=6) for t in tq]
        rk4 = rk.rearrange("p (a h d) -> p a h d", a=2, h=6)
        TT(c, "dve", t4[0], x1, cosb, ALU.mult, Pb + [c.constb], [tqb[0]])
        TT(c, "dve", t4[1], x2, sinb, ALU.mult, Pb + [c.constb], [tqb[1]])
        TT(c, "pool", rk4[:, :, :, 0:32], t4[0], t4[1], ALU.subtract, [tqb[0], tqb[1]], [rkb])
        TT(c, "dve", t4[2], x2, cosb, ALU.mult, Pb + [c.constb], [tqb[2]])
        TT(c, "dve", t4[3], x1, sinb, ALU.mult, Pb + [c.constb], [tqb[3]])
        TT(c, "pool", rk4[:, :, :, 32:64], t4[2], t4[3], ALU.add, [tqb[2], tqb[3]], [rkb])
        qkt, qktb = qk_tok[par], qk_tokb[par]
        CP(c, "pool", qkt[:, 0:384], rk[:, 0:384], [rkb], [qktb])
        TT(c, "pool", qkt[:, 384:768].rearrange("p (h d) -> p h d", d=64),
           rk[:, 384:768].rearrange("p (h d) -> p h d", d=64),
           c.zk.unsqueeze(2).broadcast_to([128, 6, 64]), ALU.mult, [rkb, c.constb], [qktb])
        pt, ptb = ps1(c)
        ptv = pt.bitcast(BF16)
        for n in range(6):
            TR(c, ptv[:, n * 128:(n + 1) * 128], qkt[:, n * 128:(n + 1) * 128], [qktb], ptb)
        CP(c, "act", qT[par][0][0:64], ptv[0:64, 0:384].rearrange("p (n t) -> p n t", n=3), ptb, [qkTb[par]])
        CP(c, "act", qT[par][1][64:128], ptv[64:128, 0:384].rearrange("p (n t) -> p n t", n=3), ptb, [qkTb[par]])
        CP(c, "act", kT[par], ptv[:, 384:768].rearrange("p (n t) -> p n t", n=3), ptb, [qkTb[par]])
        P2, P2b = ps2(c)
        for h in range(6):
            hp = h % 2
            MM(c, P2[:, h * 128:(h + 1) * 128], kT[par][:, h // 2, :], qT[par][hp][:, h // 2, :], h % 4 == 0, True,
               [qkTb[par]], [P2b[h // 4]])
        TT(c, "dve", Sm[par], P2[:, 0:768].rearrange("p (h t) -> p h t", h=6), tri3, ALU.mult, P2b + [c.constb], [Smb[par]])
        P3, P3b = ps2(c)
        proj(P3[:, 0:512], 768, 1280, [P3b[0]])
        proj(P3[:, 512:768], 1280, 1536, [P3b[1]])
        CP(c, "act", v_tok[par], P3[:, 0:768], P3b, [v_tokb[par]])
        P6, P6b = ps2(c)
        proj(P6[:, 0:512], 1536, 2048, [P6b[0]])
        proj(P6[:, 512:768], 2048, 2304, [P6b[1]])
        ACTF(c, sgt[par], P6[:, 0:768], AF.Silu, P6b, [sgtb[par]])
        P7, P7b = ps1(c)
        proj(P7[:, 0:256], 2304, 2560, P7b)
        head_norm(c, P7[:, 0:256].rearrange("p (h d) -> p h d", d=64), P7b, 4,
                  c.pp[:, W.pbase + PP_MQN:W.pbase + PP_MQN + 64],
                  qm_toks[par].rearrange("p (h d) -> p h d", d=64), [qm_tokbs[par]], W.hscr, W.hscrb, W.hss, W.hssb)

    def stage_b(ti):
        set_rot(c, (4, 5, 6, 7), (4, 6))
        tb, tl = ti // 4, ti % 4
        par = ti % 2
        P4, P4b = ps2(c)
        for h in range(6):
            hp = h % 2
            MM(c, P4[:, h * 128:(h + 1) * 128], Sm[par][:, h, :], v_tok[par][:, h * 128:(h + 1) * 128], h % 4 == 0,
               ti == 0, [Smb[par], v_tokb[par]], [P4b[h // 4]])
            if ti > 0:
                MM(c, P4[:, h * 128:(h + 1) * 128], qT[par][hp][:, h // 2, :], state_bf[:, h // 2, :], False, True,
                   [qkTb[par], stbb], [P4b[h // 4]])
        if ti < NT - 1:
            P5, P5b = ps2(c)
            for pair in range(3):
                MM(c, P5[:, pair * 256:(pair + 1) * 256], qk_tok[par][:, 384 + pair * 128:384 + (pair + 1) * 128],
                   v_tok[par][:, pair * 256:(pair + 1) * 256], pair % 2 == 0, True, [qk_tokb[par], v_tokb[par]],
                   [P5b[pair // 2]])
            P5v = P5[:, 0:768].rearrange("p (a e) -> p a e", a=3)
            for half in range(2):
                rs = slice(half * 64, (half + 1) * 64)
                TT(c, "dve", state[rs, :, :], P5v[rs, :, half * 128:(half + 1) * 128], state[rs, :, :], ALU.add,
                   P5b + [stb], [stb])
                TT(c, "pool", state[rs, :, :], state[rs, :, :], c.cd[rs, :].unsqueeze(2).broadcast_to([64, 3, 128]),
                   ALU.mult, [stb, c.constb], [stb])
                CP(c, "pool", state_bf[rs, :, :], state[rs, :, :], [stb], [stbb])
        o3 = P4[:, 0:768].rearrange("p (h e) -> p h e", h=6)
        scr3 = scr.rearrange("p (h e) -> p h e", h=6)
        RED(c, sm[:, 0, :], o3, ALU.add, P4b, [smb])
        ACTF(c, scr, P4[:, 0:768], AF.Square, P4b, [scrb])
        RED(c, sm[:, 1, :], scr3, ALU.add, [scrb], [smb])
        TS(c, "dve", sm[:, 2, :], sm[:, 0, :], 1.0 / 128, None, ALU.mult, None, [smb], [smb])
        TT(c, "dve", sm[:, 3, :], sm[:, 2, :], sm[:, 2, :], ALU.mult, [smb], [smb])
        STT(c, sm[:, 4, :], sm[:, 1, :], 1.0 / 128, c.epsx, ALU.mult, ALU.add, [smb, c.constb], [smb])
        TT(c, "dve", sm[:, 4, :], sm[:, 4, :], sm[:, 3, :], ALU.subtract, [smb], [smb])
        ACTF(c, sm[:, 4, :], sm[:, 4, :], AF.Sqrt, [smb], [smb])
        RCP(c, sm[:, 4, :], sm[:, 4, :], [smb], [smb])
        TT(c, "dve", scr3, o3, sm[:, 2, :].unsqueeze(2).broadcast_to([128, 6, 128]), ALU.subtract, P4b + [smb], [scrb])
        TT(c, "pool", scr3, scr3, sm[:, 4, :].unsqueeze(2).broadcast_to([128, 6, 128]), ALU.mult, [scrb, smb], [scrb])
        TT(c, "pool", W.y_tok[:, 0:768], scr, sgt[par], ALU.mult, [scrb, sgtb[par]], [W.y_tokb])
        W.qm_tok, W.qm_tokb = qm_toks[par], qm_tokbs[par]
        tile_tail_b(c, W, tl)
        if tl == 3:
            wout_block(c, W, tb)

    stage_a(0)
    for ti in range(NT):
        la = S.capture(lambda: stage_a(ti + 1)) if ti + 1 < NT else []
        lb = S.capture(lambda: stage_b(ti))
        S.commit_interleaved(la, lb)


def norm_rope(c, B, src, src_bufs, H, gain, ti, out, out_bufs, pos_cos=None, pos_sin=None, pos_bufs=None):
    n = src.shape[0]
    xr3 = B.xr[0:n, 0:H * 64].rearrange("p (h d) -> p h d", d=64)
    cos2 = pos_cos if pos_cos is not None else c.cos[0:n, ti, :]
    sin2 = pos_sin if pos_sin is not None else c.sin[0:n, ti, :]
    cosb = cos2.unsqueeze(1).broadcast_to([n, H, 32])
    sinb = sin2.unsqueeze(1).broadcast_to([n, H, 32])
    t = [q[0:n, 0:H * 32].rearrange("p (h d) -> p h d", d=32) for q in B.tq]
    cb_ = [c.constb] if pos_bufs is None else list(pos_bufs)
    if gain is not None:
        ss = B.ss[0:n, 0:H]
        ACTF(c, xr3, src, AF.Square, src_bufs, [B.xrb])
        RED(c, ss, xr3, ALU.add, [B.xrb], [B.ssb])
        ACTF(c, ss, ss, AF.Sqrt, [B.ssb], [B.ssb], scale=1.0 / 64, bias=float(EPS))
        RCP(c, ss, ss, [B.ssb], [B.ssb])
        TT(c, "dve", xr3, src, gain[0:n, :].unsqueeze(1).broadcast_to([n, H, 64]), ALU.mult,
           list(src_bufs) + [c.ppb], [B.xrb])
        x, xb, engs = xr3, [B.xrb], ("dve", "pool", "dve", "pool")
    else:
        x, xb, engs = src, list(src_bufs), ("dve", "dve", "dve", "dve")
    x1, x2 = x[:, :, 0:32], x[:, :, 32:64]
    TT(c, engs[0], t[0], x1, cosb, ALU.mult, xb + cb_, [B.tqb[0]])
    TT(c, engs[1], t[1], x2, sinb, ALU.mult, xb + cb_, [B.tqb[1]])
    TT(c, engs[2], t[2], x2, cosb, ALU.mult, xb + cb_, [B.tqb[2]])
    TT(c, engs[3], t[3], x1, sinb, ALU.mult, xb + cb_, [B.tqb[3]])
    if gain is not None:
        TT(c, "pool", xr3[:, :, 0:32], t[0], t[1], ALU.subtract, [B.tqb[0], B.tqb[1]], [B.xrb])
        TT(c, "pool", xr3[:, :, 32:64], t[2], t[3], ALU.add, [B.tqb[2], B.tqb[3]], [B.xrb])
        TT(c, "pool", out, xr3, B.ss[0:n, 0:H].unsqueeze(2).broadcast_to([n, H, 64]), ALU.mult, [B.xrb, B.ssb], out_bufs)
    else:
        TT(c, "pool", out[:, :, 0:32], t[0], t[1], ALU.subtract, [B.tqb[0], B.tqb[1]], out_bufs)
        TT(c, "pool", out[:, :, 32:64], t[2], t[3], ALU.add, [B.tqb[2], B.tqb[3]], out_bufs)


def masked_T(c, src_tok, src_bufs, nchunk, dstA, dstB, dst_bufs):
    pt, ptb = ps1(c)
    ptv = pt.bitcast(BF16)
    for n in range(nchunk):
        TR(c, ptv[:, n * 128:(n + 1) * 128], src_tok[:, n * 128:(n + 1) * 128], src_bufs, ptb)
    v = ptv[:, 0:nchunk * 128].rearrange("p (n t) -> p n t", n=nchunk)
    CP(c, "act", dstA[0:64], v[0:64], ptb, dst_bufs)
    CP(c, "act", dstB[64:128], v[64:128], ptb, dst_bufs)


DSA_TOPK = 256
DSA_BIS = 22


def mixer_dsa(c, l, win_d, wout_d, wkv_d, memT_d):
    S = c.S
    S.barrier()
    set_rot(c, (5, 6, 7), (6,))
    W = Ctx()
    W.pbase = PP_L0 + l * PP_PL
    R1 = Region(c.arena, c.h_off, 32 * 1024)
    R2 = Region(c.arena, c.phase_off, c.phase_size)
    hblk = [R1.take((8, 128), BF16) for _ in range(2)]
    hblkb = bufs("hblk", 2, 8)
    sq = R1.take((8, 128), BF16)
    sqb = Buf("sq")
    W.yT = R1.take((8, 512), BF16)
    W.yTb = Buf("yT")
    acc = R1.take((L,), F32)
    accb = Buf("acc")
    rl = [R1.take((512,), F32) for _ in range(2)]
    rlb = bufs("rl", 2)
    PT = [R1.take((1536,), BF16) for _ in range(2)]
    PTb = bufs("PT", 2, 3)
    w_in = R2.take((8, 1736), BF16)
    w_inb = Buf("w_in")
    load_wout(c, W, R2, wout_d)
    rstd = R2.take((128,), F32)
    rstdb = Buf("rstd")
    kdup = R2.take((L,), BF16)
    ikdup = R2.take((L,), BF16)
    v1 = R2.take((NT, 65), BF16)
    cacheb = bufs("kvcache", NT)
    v1b = Buf("v1ones")
    tail_bufs(c, W, R2, hscr_n=256)
    mark = R2.off
    junk = R2.take((L,), BF16)
    junkb = Buf("junk")
    sel = [R2.take((L,), BF16) for _ in range(2)]
    selb = bufs("sel", 2)
    B = Ctx()
    B.xr = R2.take((768,), F32)
    B.xrb = Buf("xr")
    B.tq = [R2.take((384,), F32) for _ in range(4)]
    B.tqb = bufs("tq", 4)
    B.ss = R2.take((16,), F32)
    B.ssb = Buf("ss")
    q_tok = R2.take((768,), BF16)
    q_tokb = Buf("q_tok")
    qAB = [[R2.take((6, 128), BF16) for _ in range(2)] for _ in range(2)]
    qABb = bufs("qAB", 2)
    qm_toks = [W.qm_tok, R2.take((256,), BF16)]
    qm_tokbs = [W.qm_tokb, Buf("qm_tok1")]
    iq_tok = R2.take((512,), BF16)
    iq_tokb = Buf("iq_tok")
    iqAB = [R2.take((4, 128), BF16) for _ in range(2)]
    iqABb = Buf("iqAB")
    k2 = R2.take((2, 128), BF16)
    k2b = Buf("k2")
    iw_t = R2.take((8,), F32)
    iw_tb = Buf("iw")
    bs = R2.take((DSA_BIS + 8,), F32)
    bsb = Buf("bs")
    den12 = R2.take((12, 1), F32)
    den12b = Buf("den12")

    wv = win_d.rearrange("(dc p) f -> p dc f", p=128)
    for dc in range(0, 8, 2):
        S.dma("pool", "win", lambda e, dc=dc: e.dma_start(out=w_in[:, dc:dc + 2, :], in_=wv[:, dc:dc + 2, :]),
              writes=[w_inb])
    S.seal("win", [w_inb])
    Rt = Region(c.arena, c.h_off, 32 * 1024)
    mem_prep(c, Rt, memT_d, wkv_d, W.pbase, W.k_memT, W.v_mem1, W.kvb)
    S.barrier()
    for p_ in range(2):
        for a in range(2):
            MSET(c, "pool", qAB[p_][a][:, :, :], 0.0, [qABb[p_]])
    for a in range(2):
        MSET(c, "pool", iqAB[a][:, :, :], 0.0, [iqABb])
    MSET(c, "pool", v1[:, :, 64:65], 1.0, [v1b])

    g_ap = c.pp[:, W.pbase + PP_MIX:W.pbase + PP_MIX + 8]
    gq = c.pp[:, W.pbase + PP_A:W.pbase + PP_A + 64]
    gk = c.pp[:, W.pbase + PP_B:W.pbase + PP_B + 64]
    mi4 = c.mi_bf.unsqueeze(1).broadcast_to([128, 4, 128])
    SB = [0, 1, 2]
    OB = [3, 4]
    hslot = {}
    for k_, h in enumerate((0, 2, 4, 6)):
        hslot[h] = (0, k_)
    for k_, h in enumerate((8, 10, 9, 11)):
        hslot[h] = (1, k_)
    for k_, h in enumerate((1, 3, 5, 7)):
        hslot[h] = (2, k_)

    def stage_a(ti):
        set_rot(c, (6, 7), (6,))
        tb = ti // 4
        par = ti % 2
        tsl = slice(ti * 128, (ti + 1) * 128)
        hb, hbb = hblk[par], hblkb[par]
        rmsnorm_block(c, c.xT[:, :, tsl], [c.xb[dc][tb] for dc in range(8)], 128, g_ap, sq, sqb, rstd, rstdb, hb, hbb)

        def proj(out_ap, c0, c1, pbufs):
            for dc in range(8):
                MM(c, out_ap, hb[:, dc, :], w_in[:, dc, c0:c1], dc == 0, dc == 7, [hbb[dc], w_inb], pbufs)

        Pq, Pqb = ps2(c)
        proj(Pq[:, 0:512], 0, 512, [Pqb[0]])
        proj(Pq[:, 512:768], 512, 768, [Pqb[1]])
        norm_rope(c, B, Pq[:, 0:768].rearrange("p (h d) -> p h d", d=64), Pqb, 12, gq, ti,
                  q_tok.rearrange("p (h d) -> p h d", d=64), [q_tokb])
        masked_T(c, q_tok, [q_tokb], 6, qAB[par][0], qAB[par][1], [qABb[par]])
        Pi, Pib = ps1(c)
        proj(Pi, 896, 1408, Pib)
        norm_rope(c, B, Pi.rearrange("p (h d) -> p h d", d=64), Pib, 8, None, ti,
                  iq_tok.rearrange("p (h d) -> p h d", d=64), [iq_tokb])
        masked_T(c, iq_tok, [iq_tokb], 4, iqAB[0], iqAB[1], [iqABb])
        Ps, Psb = ps1(c)
        proj(Ps[:, 0:128], 768, 896, Psb)
        proj(Ps[:, 128:192], 1408, 1472, Psb)
        proj(Ps[:, 192:200], 1472, 1480, Psb)
        norm_rope(c, B, Ps[:, 0:64].rearrange("p (h d) -> p h d", d=64), Psb, 1, gk, ti,
                  k2[:, 0, 0:64].rearrange("p (h d) -> p h d", d=64), [k2b])
        norm_rope(c, B, Ps[:, 128:192].rearrange("p (h d) -> p h d", d=64), Psb, 1, None, ti,
                  k2[:, 1, 0:64].rearrange("p (h d) -> p h d", d=64), [k2b])
        CP(c, "pool", k2[:, :, 64:128], k2[:, :, 0:64], [k2b], [k2b])
        CP(c, "act", v1[:, ti, 0:64], Ps[:, 64:128], Psb, [cacheb[ti]])
        CP(c, "act", iw_t, Ps[:, 192:200], Psb, [iw_tb])
        pt, ptb = ps1(c)
        ptv = pt.bitcast(BF16)
        TR(c, ptv[:, 0:128], k2[:, 0, :], [k2b], ptb)
        TR(c, ptv[:, 128:256], k2[:, 1, :], [k2b], ptb)
        CP(c, "act", kdup[:, tsl], ptv[:, 0:128], ptb, [cacheb[ti]])
        CP(c, "act", ikdup[:, tsl], ptv[:, 128:256], ptb, [cacheb[ti]])
        Pm, Pmb = ps1(c)
        proj(Pm[:, 0:256], 1480, 1736, Pmb)
        head_norm(c, Pm[:, 0:256].rearrange("p (h d) -> p h d", d=64), Pmb, 4,
                  c.pp[:, W.pbase + PP_MQN:W.pbase + PP_MQN + 64],
                  qm_toks[par].rearrange("p (h d) -> p h d", d=64), [qm_tokbs[par]], W.hscr, W.hscrb, W.hss, W.hssb)

        Skeys = 128 * (ti + 1)
        nkb = (Skeys + 511) // 512
        n = 0
        for kb in range(nkb):
            wdt = min(512, Skeys - kb * 512)
            ks = slice(kb * 512, kb * 512 + wdt)
            kbufs = [cacheb[j] for j in range(kb * 4, min(ti + 1, kb * 4 + 4))]
            for h in range(8):
                Px, Pxb = ps1(c)
                MM(c, Px[:, 0:wdt], iqAB[h % 2][:, h // 2, :], ikdup[:, ks], True, True, [iqABb] + kbufs, Pxb)
                r_, rb_ = rl[n % 2], rlb[n % 2]
                n += 1
                ACTF(c, r_[:, 0:wdt], Px[:, 0:wdt], AF.Relu, Pxb, [rb_])
                if h == 0:
                    TS(c, "dve", acc[:, ks], r_[:, 0:wdt], iw_t[:, 0:1], None, ALU.mult, None, [rb_, iw_tb], [accb])
                else:
                    STT(c, acc[:, ks], r_[:, 0:wdt], iw_t[:, h:h + 1], acc[:, ks], ALU.mult, ALU.add,
                        [rb_, iw_tb, accb], [accb])
        sl_, slb_ = sel[par], selb[par]
        if Skeys > DSA_TOPK:
            a_ = acc[:, 0:Skeys]
            S.op("dve", lambda e, a_=a_: e.tensor_reduce(out=bs[:, 0:1], in_=a_, axis=AX.X, op=ALU.max,
                                                         apply_absolute_value=True), [accb], [bsb])
            TT(c, "dve", acc[:, tsl], acc[:, tsl], c.cneg, ALU.add, [accb, c.constb], [accb])
            TS(c, "dve", bs[:, 8:8 + DSA_BIS], c.bisc, bs[:, 0:1], None, ALU.mult, None, [bsb, c.constb], [bsb])
            TS(c, "dve", bs[:, 1:2], bs[:, 0:1], 0.0, None, ALU.mult, None, [bsb], [bsb])
            for i in range(DSA_BIS):
                S.op("dve", lambda e, a_=a_, n_=Skeys: e.tensor_scalar(
                    out=junk[:, 0:n_], in0=a_, scalar1=bs[:, 1:2], scalar2=None, op0=ALU.is_ge, op1=ALU.add,
                    accum_out=bs[:, 2:3]), [accb, bsb], [junkb, bsb])
                dn = bs[:, 8 + i + 1:8 + i + 2] if i + 1 < DSA_BIS else bs[:, 8 + i:8 + i + 1]
                STT(c, bs[:, 3:4], bs[:, 2:3], float(DSA_TOPK), dn, ALU.is_ge, ALU.mult, [bsb], [bsb])
                S.op("dve", lambda e, dn=dn: e.scalar_tensor_tensor(out=bs[:, 1:2], in0=bs[:, 1:2], scalar=dn, in1=bs[:, 3:4],
                                                                    op0=ALU.subtract, op1=ALU.add), [bsb], [bsb])
                if i + 1 < DSA_BIS:
                    S.op("dve", lambda e: e.tensor_tensor(out=bs[:, 1:2], in0=bs[:, 1:2], in1=bs[:, 3:4], op=ALU.add),
                         [bsb], [bsb])
            TS(c, "dve", sl_[:, 0:Skeys], a_, bs[:, 1:2], None, ALU.is_ge, None, [accb, bsb], [slb_])
        else:
            TT(c, "dve", acc[:, tsl], acc[:, tsl], c.cneg, ALU.add, [accb, c.constb], [accb])
            TS(c, "dve", sl_[:, 0:Skeys], acc[:, 0:Skeys], -1e29, None, ALU.is_ge, None, [accb], [slb_])

    def stage_b(ti):
        set_rot(c, (5,), ())
        tb, tl = ti // 4, ti % 4
        par = ti % 2
        sl_, slb_ = sel[par], selb[par]
        qa, qb_ = qAB[par][0], qAB[par][1]
        for j in range(ti + 1):
            js = slice(j * 128, (j + 1) * 128)
            pj = j % 2
            p0, p1, p2 = bank(c, SB[0]), bank(c, SB[1]), bank(c, SB[2])
            b0, b1, b2 = [c.psb[SB[0]]], [c.psb[SB[1]]], [c.psb[SB[2]]]
            kb_ = [cacheb[j], qABb[par]]
            MM(c, p0, kdup[:, js], qa[:, 0:4, :], True, False, kb_, b0)
            MM(c, p0, sl_[:, js], mi4, False, True, [slb_, c.constb], b0)
            MM(c, p1[:, 0:256], kdup[:, js], qa[:, 4:6, :], True, False, kb_, b1)
            MM(c, p1[:, 256:512], kdup[:, js], qb_[:, 4:6, :], False, False, kb_, b1)
            MM(c, p1, sl_[:, js], mi4, False, True, [slb_, c.constb], b1)
            MM(c, p2, kdup[:, js], qb_[:, 0:4, :], True, False, kb_, b2)
            MM(c, p2, sl_[:, js], mi4, False, True, [slb_, c.constb], b2)
            for bi, (pp_, bb_) in enumerate(((p0, b0), (p1, b1), (p2, b2))):
                ACTF(c, PT[pj][:, bi * 512:(bi + 1) * 512], pp_, AF.Exp, bb_, [PTb[pj][bi]], scale=0.125,
                     bias=-MASKM / 8)
            for h in range(12):
                bi, sl2 = hslot[h]
                ob = OB[0] if h < 7 else OB[1]
                oc = (h if h < 7 else h - 7) * 65
                MM(c, bank(c, ob)[:, oc:oc + 65], PT[pj][:, (bi * 4 + sl2) * 128:(bi * 4 + sl2 + 1) * 128], v1[:, j, :],
                   j == 0 and h in (0, 7), j == ti and h in (6, 11), [PTb[pj][bi], cacheb[j], v1b], [c.psb[ob]])
        oa = bank(c, OB[0])[:, 0:455].rearrange("p (h e) -> p h e", e=65)
        ob_ = bank(c, OB[1])[:, 0:325].rearrange("p (h e) -> p h e", e=65)
        RCP(c, den12[:, 0:7, :], oa[:, :, 64:65], [c.psb[OB[0]]], [den12b])
        RCP(c, den12[:, 7:12, :], ob_[:, :, 64:65], [c.psb[OB[1]]], [den12b])
        TT(c, "dve", W.y_tok[:, 0:448].rearrange("p (h d) -> p h d", d=64), oa[:, :, 0:64],
           den12[:, 0:7, :].broadcast_to([128, 7, 64]), ALU.mult, [c.psb[OB[0]], den12b], [W.y_tokb])
        TT(c, "dve", W.y_tok[:, 448:768].rearrange("p (h d) -> p h d", d=64), ob_[:, :, 0:64],
           den12[:, 7:12, :].broadcast_to([128, 5, 64]), ALU.mult, [c.psb[OB[1]], den12b], [W.y_tokb])
        W.qm_tok, W.qm_tokb = qm_toks[par], qm_tokbs[par]
        tile_tail_b(c, W, tl)
        if tl == 3:
            wout_block(c, W, tb)

    stage_a(0)
    for ti in range(NT):
        la = S.capture(lambda: stage_a(ti + 1)) if ti + 1 < NT else []
        lb = S.capture(lambda: stage_b(ti))
        S.commit_interleaved(la, lb)


NSA_NCMP = 127


def nsa_hmap(c_, par):
    return 3 * (2 * (c_ // 3) + par) + (c_ % 3)


def mixer_nsa(c, l, win_d, wout_d, wkv_d, memT_d, cw_d):
    S = c.S
    S.barrier()
    set_rot(c, (6, 7), (6,))
    W = Ctx()
    W.pbase = PP_L0 + l * PP_PL
    R1 = Region(c.arena, c.h_off, 32 * 1024)
    R2 = Region(c.arena, c.phase_off, c.phase_size)
    hblk = R1.take((8, 128), BF16)
    hblkb = bufs("hblk", 8)
    sq = R1.take((8, 128), BF16)
    sqb = Buf("sq")
    W.yT = R1.take((8, 512), BF16)
    W.yTb = Buf("yT")
    un0 = R1.off
    B = Ctx()
    B.xr = R1.take((768,), F32)
    B.xrb = Buf("xr")
    B.tq = [R1.take((384,), F32) for _ in range(4)]
    B.tqb = bufs("tq", 4)
    un1 = R1.off
    Ru = Region(c.arena, un0, un1 - un0)
    PT = [Ru.take((1536,), BF16) for _ in range(2)]
    PTb = bufs("PT", 2, 4)
    PTc = Ru.take((1536,), BF16)
    PTcb = bufs("PTc", 4)
    tmp = R1.take((768,), F32)
    tmpb = Buf("tmp")
    yacc = R1.take((768,), F32)
    yaccb = Buf("yacc")
    imp = R1.take((12, 32), F32)
    impb = Buf("imp")
    impm = R1.take((4, 32), F32)
    wk16 = R1.take((4, 32), F32)
    impmb = Buf("impm")
    blk = R1.take((4, 32), BF16)
    blkb = Buf("blk")
    w_in = R2.take((8, 2596), BF16)
    w_inb = Buf("w_in")
    wo1 = R2.take((8, 128), BF16)
    wo = [wo1, wo1]
    wob1 = Buf("wo")
    wob = [wob1, wob1]
    rstd = R2.take((128,), F32)
    rstdb = Buf("rstd")
    ksT = R2.take((2, L), BF16)
    kwT = R2.take((2, 5 * 128), BF16)
    vs1 = R2.take((NT, 4, 65), BF16)
    vw1 = R2.take((5, 4, 65), BF16)
    cacheb = Buf("kvcache")
    tail_bufs(c, W, R2, hscr_n=256)
    mark = R2.off
    B.ss = R2.take((16,), F32)
    B.ssb = Buf("ss")
    q_tok = R2.take((768,), BF16)
    q_tokb = Buf("q_tok")
    qAB = [R2.take((6, 128), BF16) for _ in range(2)]
    qABb = Buf("qAB")
    k_tok = R2.take((2, 256), BF16)
    k_tokb = Buf("k_tok")
    kvcT = [R2.take((4, 144), BF16) for _ in range(2)]
    kvcTb = Buf("kvcT")
    wdup = [R2.take((32, 64), BF16) for _ in range(2)]
    wdupb = Buf("wdup")
    posB = R2.take((2, 32, 8), BF16)
    posBb = Buf("posB")
    k_cmpT = R2.take((2, 128), BF16)
    v_cmpx = R2.take((4, 97), BF16)
    cmpb = Buf("cmpcache")
    cbias = R1.take((512,), F32)
    cbiasb = Buf("cbias")
    cnew_tok = R2.take((512,), BF16)
    cnew_tokb = Buf("cnew_tok")
    gate = R2.take((36,), F32)
    gateb = Buf("gate")
    sm = R2.take((6, 12), F32)
    smb = Buf("sm")
    m8 = R2.take((2, 8), F32)
    m8b = Buf("m8")
    thr = R2.take((4, 1), F32)
    thrb = Buf("thr")
    seltile = R2.take((4, 128), BF16)
    seltileb = Buf("seltile")
    masks_bf = R2.take((768,), BF16)
    tril_bf, far_bf = masks_bf[:, 0:128], masks_bf[:, 128:256]
    cmask_bf, shiftI = masks_bf[:, 256:512], masks_bf[:, 512:768]
    czca = R2.take((128,), F32)
    cse = [R2.take((64,), F32) for _ in range(2)]
    cseb = bufs("cse", 2)
    ncb = Buf("nsaconst")

    wv = win_d.rearrange("(dc p) f -> p dc f", p=128)
    for c_ in range(6):
        for par in range(2):
            h = nsa_hmap(c_, par)
            src = wv[:, :, h * 64:(h + 1) * 64]
            dst = w_in[:, :, c_ * 128 + par * 64:c_ * 128 + (par + 1) * 64]
            S.dma("pool", "win", lambda e, src=src, dst=dst: e.dma_start(out=dst, in_=src), writes=[w_inb])
    for dc in range(0, 8, 2):
        S.dma("pool", "win", lambda e, dc=dc: e.dma_start(out=w_in[:, dc:dc + 2, 768:2596], in_=wv[:, dc:dc + 2, 768:2596]),
              writes=[w_inb])
    S.seal("win", [w_inb])
    for kind in range(2):
        srcw = cw_d[kind].rearrange("l d e -> d l e")
        for half in range(2):
            S.dma("pool", "wcmp", lambda e, kind=kind, half=half, srcw=srcw: e.dma_start(
                out=wdup[kind][half * 64:(half + 1) * 64, :, :], in_=srcw), writes=[wdupb])
    S.seal("wcmp", [wdupb])
    Rt = Region(c.arena, c.h_off, 32 * 1024)
    mem_prep(c, Rt, memT_d, wkv_d, W.pbase, W.k_memT, W.v_mem1, W.kvb)
    S.barrier()
    S.dma("pool", "ncst", lambda e: e.dma_start(out=masks_bf, in_=c.c2["masks"][:, :]), writes=[ncb])
    S.dma("sp", "ncst", lambda e: e.dma_start(out=czca, in_=c.c2["czca"][:, :]), writes=[ncb])
    S.seal("ncst", [ncb])
    for a in range(2):
        MSET(c, "pool", qAB[a][:, :, :], 0.0, [qABb])
        MSET(c, "pool", kvcT[a][:, :, :], 0.0, [kvcTb])
    MSET(c, "pool", k_cmpT[:, :, :], 0.0, [cmpb])
    MSET(c, "pool", v_cmpx[:, :, 0:64], 0.0, [cmpb])
    MSET(c, "pool", v_cmpx[:, :, 64:65], 1.0, [cmpb])
    for g_ in range(4):
        S.dma("pool", "ncov", lambda e, g_=g_: e.dma_start(out=v_cmpx[:, g_, 65:97], in_=c.c2["cover"][:, :]), writes=[cmpb])
    S.seal("ncov", [cmpb])
    MSET(c, "pool", vs1[:, :, :, 64:65], 1.0, [cacheb])
    MSET(c, "pool", vw1[:, :, :, 64:65], 1.0, [cacheb])
    posf = c.pp[:, W.pbase + PP_POS:W.pbase + PP_POS + 64].rearrange("p (k l) -> p k l", k=2)
    CP(c, "dve", posB, posf.unsqueeze(3).broadcast_to([128, 2, 32, 8]), [c.ppb], [posBb])
    Pcb, Pcbb = ps1(c)
    for kind in range(2):
        for l_ in range(32):
            MM(c, Pcb[0:8, kind * 64:(kind + 1) * 64], posB[:, kind, l_, :], wdup[kind][:, l_, :], kind == 0 and l_ == 0,
               l_ == 31, [posBb, wdupb], Pcbb)
    CP(c, "dve", cbias[0:8, :].rearrange("p (k g e) -> p k g e", k=2, g=4),
       Pcb[0:8, 0:128].rearrange("p (k e) -> p k e", k=2).unsqueeze(2).broadcast_to([8, 2, 4, 64]), Pcbb, [cbiasb])

    g_ap = c.pp[:, W.pbase + PP_MIX:W.pbase + PP_MIX + 8]
    gq = c.pp[:, W.pbase + PP_A:W.pbase + PP_A + 64]
    gkc = c.pp[:, W.pbase + PP_B:W.pbase + PP_B + 64]
    gks = c.pp[:, W.pbase + PP_C:W.pbase + PP_C + 64]
    gkw = c.pp[:, W.pbase + PP_D:W.pbase + PP_D + 64]
    mi3 = c.mi_bf.unsqueeze(1).broadcast_to([128, 3, 128])
    SBK = [0, 1, 2, 3]
    OB = [4, 5]
    CB = [4, 5, 6]
    wo_n = [0]

    def head_of(sb, r):
        par, kc_ = sb // 2, sb % 2
        return 3 * (2 * kc_ + par) + r

    def branch(ti, keys, Kc, Vc, kslot, vslot, maskf, coef_col):
        nk = len(keys)
        for n_, j in enumerate(keys):
            par_j = n_ % 2
            ks_ = kslot(j)
            mk = maskf(j)
            for sb in SBK:
                par, kc_ = sb // 2, sb % 2
                g = 2 * kc_ + par
                pb_ = bank(c, sb)
                MM(c, pb_[:, 0:384], Kc[:, kc_, ks_], qAB[par][:, 3 * kc_:3 * kc_ + 3, :], True, mk is None,
                   [cacheb, qABb], [c.psb[sb]])
                if mk is not None:
                    lh, lb = mk[g]
                    MM(c, pb_[:, 0:384], lh, mi3, False, True, lb + [c.constb], [c.psb[sb]])
            for pr in range(2):
                src = c.pst[pr][:, :].rearrange("p (b x) -> p b x", b=2)[:, :, 0:384]
                dst = PT[par_j][:, pr * 768:(pr + 1) * 768].rearrange("p (b x) -> p b x", b=2)
                ACTF(c, dst, src, AF.Exp, [c.psb[2 * pr], c.psb[2 * pr + 1]], [PTb[par_j][2 * pr], PTb[par_j][2 * pr + 1]],
                     scale=0.125, bias=(0.0 if mk is None else -MASKM / 8))
            for sb in SBK:
                for r in range(3):
                    h = head_of(sb, r)
                    g = h // 3
                    ob = OB[0] if h < 7 else OB[1]
                    oc = (h if h < 7 else h - 7) * 65
                    firsts = (0, 7)
                    MM(c, bank(c, ob)[:, oc:oc + 65], PT[par_j][:, sb * 384 + r * 128:sb * 384 + (r + 1) * 128],
                       Vc[:, vslot(j), g, :], n_ == 0 and (sb, r) == first_in_bank[ob], n_ == nk - 1,
                       [PTb[par_j][sb], cacheb], [c.psb[ob]])
        oa = bank(c, OB[0])[:, 0:455].rearrange("p (h e) -> p h e", e=65)
        ob_ = bank(c, OB[1])[:, 0:325].rearrange("p (h e) -> p h e", e=65)
        RCP(c, sm[:, 0, 0:7].unsqueeze(2), oa[:, :, 64:65], [c.psb[OB[0]]], [smb])
        RCP(c, sm[:, 0, 7:12].unsqueeze(2), ob_[:, :, 64:65], [c.psb[OB[1]]], [smb])
        TT(c, "dve", sm[:, 1, :], sm[:, 0, :], gate.rearrange("p (h b) -> p h b", b=3)[:, :, coef_col], ALU.mult,
           [smb, gateb], [smb])
        t3 = tmp.rearrange("p (h d) -> p h d", d=64)
        TT(c, "dve", t3[:, 0:7, :], oa[:, :, 0:64], sm[:, 1, 0:7].unsqueeze(2).broadcast_to([128, 7, 64]), ALU.mult,
           [c.psb[OB[0]], smb], [tmpb])
        TT(c, "dve", t3[:, 7:12, :], ob_[:, :, 0:64], sm[:, 1, 7:12].unsqueeze(2).broadcast_to([128, 5, 64]), ALU.mult,
           [c.psb[OB[1]], smb], [tmpb])
        TT(c, "pool", yacc, yacc, tmp, ALU.add, [yaccb, tmpb], [yaccb])

    first_in_bank = {}
    for sb in SBK:
        for r in range(3):
            h = head_of(sb, r)
            ob = OB[0] if h < 7 else OB[1]
            first_in_bank.setdefault(ob, (sb, r))
    first_in_cbank = {}
    for sb in SBK:
        for r in range(3):
            h = head_of(sb, r)
            first_in_cbank.setdefault(CB[h // 5], (sb, r))

    for ti in range(NT):
        tb, tl = ti // 4, ti % 4
        tsl = slice(ti * 128, (ti + 1) * 128)
        rs5 = ti % 5
        rsl = slice(rs5 * 128, (rs5 + 1) * 128)
        rmsnorm_block(c, c.xT[:, :, tsl], [c.xb[dc][tb] for dc in range(8)], 128, g_ap, sq, sqb, rstd, rstdb, hblk, hblkb)

        def proj(out_ap, c0, c1, pbufs):
            for dc in range(8):
                MM(c, out_ap, hblk[:, dc, :], w_in[:, dc, c0:c1], dc == 0, dc == 7, [hblkb[dc], w_inb], pbufs)

        Pq, Pqb = ps2(c)
        proj(Pq[:, 0:512], 0, 512, [Pqb[0]])
        proj(Pq[:, 512:768], 512, 768, [Pqb[1]])
        norm_rope(c, B, Pq[:, 0:768].rearrange("p (h d) -> p h d", d=64), Pqb, 12, gq, ti,
                  q_tok.rearrange("p (h d) -> p h d", d=64), [q_tokb])
        masked_T(c, q_tok, [q_tokb], 6, qAB[0], qAB[1], [qABb])
        Pk, Pkb = ps2(c)
        proj(Pk[:, 0:256], 1280, 1536, [Pkb[0]])
        proj(Pk[:, 512:768], 1792, 2048, [Pkb[1]])
        norm_rope(c, B, Pk[:, 0:256].rearrange("p (h d) -> p h d", d=64), [Pkb[0]], 4, gks, ti,
                  k_tok[:, 0, :].rearrange("p (h d) -> p h d", d=64), [k_tokb])
        norm_rope(c, B, Pk[:, 512:768].rearrange("p (h d) -> p h d", d=64), [Pkb[1]], 4, gkw, ti,
                  k_tok[:, 1, :].rearrange("p (h d) -> p h d", d=64), [k_tokb])
        pt, ptb = ps1(c)
        ptv = pt.bitcast(BF16)
        for n_ in range(4):
            TR(c, ptv[:, n_ * 128:(n_ + 1) * 128], k_tok[:, n_ // 2, (n_ % 2) * 128:(n_ % 2 + 1) * 128], [k_tokb], ptb)
        CP(c, "act", ksT[:, :, tsl], ptv[:, 0:256].rearrange("p (c t) -> p c t", c=2), ptb, [cacheb])
        CP(c, "act", kwT[:, :, rsl], ptv[:, 256:512].rearrange("p (c t) -> p c t", c=2), ptb, [cacheb])
        Pv, Pvb = ps2(c)
        proj(Pv[:, 0:256], 1536, 1792, [Pvb[0]])
        proj(Pv[:, 512:768], 2048, 2304, [Pvb[1]])
        CP(c, "act", vs1[:, ti, :, 0:64], Pv[:, 0:256].rearrange("p (g d) -> p g d", d=64), [Pvb[0]], [cacheb])
        CP(c, "act", vw1[:, rs5, :, 0:64], Pv[:, 512:768].rearrange("p (g d) -> p g d", d=64), [Pvb[1]], [cacheb])
        Pc, Pcb_ = ps1(c)
        for n_ in range(4):
            for dc in range(8):
                MM(c, Pc[:, n_ * 128:(n_ + 1) * 128], w_in[:, dc, 768 + n_ * 128:768 + (n_ + 1) * 128], hblk[:, dc, :],
                   n_ == 0 and dc == 0, dc == 7, [hblkb[dc], w_inb], Pcb_)
        Pc4 = Pc.rearrange("p (n t) -> p n t", n=4)
        CP(c, "act", kvcT[0][0:64, :, 16:144], Pc4[0:64], Pcb_, [kvcTb])
        CP(c, "act", kvcT[1][64:128, :, 16:144], Pc4[64:128], Pcb_, [kvcTb])
        Pg, Pgb = ps1(c)
        proj(Pg[:, 0:36], 2304, 2340, Pgb)
        proj(Pg[:, 128:384], 2340, 2596, Pgb)
        ACTF(c, gate, Pg[:, 0:36], AF.Exp, Pgb, [gateb], scale=-1.0)
        TS(c, "dve", gate, gate, 1.0, None, ALU.add, None, [gateb], [gateb])
        RCP(c, gate, gate, [gateb], [gateb])
        head_norm(c, Pg[:, 128:384].rearrange("p (h d) -> p h d", d=64), Pgb, 4,
                  c.pp[:, W.pbase + PP_MQN:W.pbase + PP_MQN + 64],
                  W.qm_tok.rearrange("p (h d) -> p h d", d=64), [W.qm_tokb], W.hscr, W.hscrb, W.hss, W.hssb)
        nb = 7 if ti == 0 else 8
        n0 = 0 if ti == 0 else 8 * ti - 1
        off = 16 if ti == 0 else 0
        Pn, Pnb = ps1(c)
        first = True
        for kind in range(2):
            for g in range(4):
                par, ch = g % 2, g // 2
                for l_ in range(32):
                    lhs = kvcT[par][:, kind * 2 + ch, off + l_:off + l_ + 16 * (nb - 1) + 1:16]
                    MM(c, Pn[0:nb, (kind * 4 + g) * 64:(kind * 4 + g + 1) * 64], lhs, wdup[kind][:, l_, :], first,
                       l_ == 31, [kvcTb, wdupb], Pnb)
                    first = False
        for a in range(2):
            hs = slice(a * 64, (a + 1) * 64)
            CP(c, "pool", kvcT[a][hs, :, 0:16], kvcT[a][hs, :, 128:144], [kvcTb], [kvcTb])
        cn = tmp[0:nb, 0:512]
        TT(c, "dve", cn, Pn[0:nb, :], cbias[0:nb, :], ALU.add, Pnb + [cbiasb], [tmpb])
        cs_, csb_ = cse[ti % 2], cseb[ti % 2]
        S.dma("sp", f"cse{ti % 2}", lambda e, cs_=cs_, ti=ti: e.dma_start(out=cs_[0:8, :], in_=c.c2["rope"][ti, :, :]),
              writes=[csb_])
        norm_rope(c, B, cn[:, 0:256].rearrange("p (h d) -> p h d", d=64), [tmpb], 4, gkc, ti,
                  cnew_tok[0:nb, 0:256].rearrange("p (h d) -> p h d", d=64), [cnew_tokb],
                  pos_cos=cs_[0:nb, 0:32], pos_sin=cs_[0:nb, 32:64], pos_bufs=[csb_])
        CP(c, "pool", cnew_tok[0:nb, 256:512], cn[:, 256:512], [tmpb], [cnew_tokb])
        pt2, pt2b = ps1(c)
        pt2v = pt2.bitcast(BF16)
        for ch in range(2):
            TR(c, pt2v[:, ch * 8:ch * 8 + nb], cnew_tok[0:nb, ch * 128:(ch + 1) * 128], [cnew_tokb], pt2b)
        for ch in range(2):
            CP(c, "act", k_cmpT[:, ch, n0:n0 + nb], pt2v[:, ch * 8:ch * 8 + nb], pt2b, [cmpb])
        Psc, Pscb = ps1(c)
        MM(c, Psc[:, 0:256], shiftI[0:nb, 128 - n0:256 - n0], cnew_tok[0:nb, 256:512], True, True, [ncb, cnew_tokb], Pscb)
        TT(c, "dve", v_cmpx[:, :, 0:64], Psc[:, 0:256].rearrange("p (g d) -> p g d", d=64), v_cmpx[:, :, 0:64], ALU.add,
           Pscb + [cmpb], [cmpb])
        S.barrier()
        cm_l = cmask_bf[:, 128 - 8 * ti:128 - 8 * ti + NSA_NCMP]
        for sb in SBK:
            par, kc_ = sb // 2, sb % 2
            pb_ = bank(c, sb)
            MM(c, pb_[0:NSA_NCMP, 0:384], k_cmpT[:, kc_, 0:NSA_NCMP], qAB[par][:, 3 * kc_:3 * kc_ + 3, :], True, False,
               [cmpb, qABb], [c.psb[sb]])
            MM(c, pb_[0:NSA_NCMP, 0:384], cm_l, mi3, False, True, [ncb, c.constb], [c.psb[sb]])
        for pr in range(2):
            src = c.pst[pr][0:NSA_NCMP, :].rearrange("p (b x) -> p b x", b=2)[:, :, 0:384]
            dst = PTc[0:NSA_NCMP, pr * 768:(pr + 1) * 768].rearrange("p (b x) -> p b x", b=2)
            ACTF(c, dst, src, AF.Exp, [c.psb[2 * pr], c.psb[2 * pr + 1]], [PTcb[2 * pr], PTcb[2 * pr + 1]], scale=0.125,
                 bias=-MASKM / 8)
        for sb in SBK:
            for r in range(3):
                h = head_of(sb, r)
                cb_ = CB[h // 5]
                oc = (h % 5) * 97
                MM(c, bank(c, cb_)[:, oc:oc + 97], PTc[0:NSA_NCMP, sb * 384 + r * 128:sb * 384 + (r + 1) * 128],
                   v_cmpx[0:NSA_NCMP, h // 3, :], (sb, r) == first_in_cbank[cb_], True, [PTcb[sb], cmpb], [c.psb[cb_]])
        cviews = [bank(c, CB[0])[:, 0:485].rearrange("p (h e) -> p h e", e=97),
                  bank(c, CB[1])[:, 0:485].rearrange("p (h e) -> p h e", e=97),
                  bank(c, CB[2])[:, 0:194].rearrange("p (h e) -> p h e", e=97)]
        hr = [(0, 5), (5, 10), (10, 12)]
        for k_, (h0, h1) in enumerate(hr):
            TS(c, "dve", sm[:, 0, h0:h1].unsqueeze(2), cviews[k_][:, :, 64:65], 1e-30, None, ALU.max, None,
               [c.psb[CB[k_]]], [smb])
        RCP(c, sm[:, 0, :], sm[:, 0, :], [smb], [smb])
        TT(c, "dve", sm[:, 1, :], sm[:, 0, :], gate.rearrange("p (h b) -> p h b", b=3)[:, :, 0], ALU.mult, [smb, gateb], [smb])
        y3 = yacc.rearrange("p (h d) -> p h d", d=64)
        for k_, (h0, h1) in enumerate(hr):
            TT(c, "dve", y3[:, h0:h1, :], cviews[k_][:, :, 0:64], sm[:, 1, h0:h1].unsqueeze(2).broadcast_to([128, h1 - h0, 64]),
               ALU.mult, [c.psb[CB[k_]], smb], [yaccb])
            TT(c, "dve", imp[:, h0:h1, :], cviews[k_][:, :, 65:97], sm[:, 0, h0:h1].unsqueeze(2).broadcast_to([128, h1 - h0, 32]),
               ALU.mult, [c.psb[CB[k_]], smb], [impb])
        imp4 = imp.rearrange("p (g r) j -> p g r j", r=3)
        TT(c, "dve", impm, imp4[:, :, 0, :], imp4[:, :, 1, :], ALU.add, [impb], [impmb])
        TT(c, "dve", impm, impm, imp4[:, :, 2, :], ALU.add, [impb, impmb], [impmb])
        czs = czca[:, 32 - 2 * ti:64 - 2 * ti].unsqueeze(1).broadcast_to([128, 4, 32])
        cas = czca[:, 64 + 32 - 2 * ti:64 + 64 - 2 * ti].unsqueeze(1).broadcast_to([128, 4, 32])
        TT(c, "dve", impm, impm, czs, ALU.mult, [impmb, ncb], [impmb])
        TT(c, "dve", impm, impm, cas, ALU.add, [impmb, ncb], [impmb])
        MSET(c, "dve", impm[:, :, 0:1], 2e9, [impmb])
        for g in range(4):
            S.op("dve", lambda e, g=g: e.max(out=m8[:, 0, :], in_=impm[:, g, :]), [impmb], [m8b])
            S.op("dve", lambda e, g=g: e.match_replace(out=wk16[:, g, :], in_to_replace=m8[:, 0, :], in_values=impm[:, g, :],
                                                       imm_value=-3e38), [impmb, m8b], [impmb])
            S.op("dve", lambda e, g=g: e.max(out=m8[:, 1, :], in_=wk16[:, g, :]), [impmb], [m8b])
            CP(c, "dve", thr[:, g, :], m8[:, 1, 7:8], [m8b], [thrb])
        TT(c, "dve", blk, impm, thr.broadcast_to([128, 4, 32]), ALU.is_ge, [impmb, thrb], [blkb])
        def slc_mask(j, ti=ti):
            src = blk[:, :, 2 * j:2 * j + 2].unsqueeze(3).broadcast_to([128, 4, 2, 64])
            dst = seltile.rearrange("p g (b s) -> p g b s", b=2)
            if j == ti:
                TT(c, "pool", dst, src, tril_bf.rearrange("p (b s) -> p b s", b=2).unsqueeze(1).broadcast_to([128, 4, 2, 64]),
                   ALU.mult, [blkb, ncb], [seltileb])
            else:
                CP(c, "pool", dst, src, [blkb], [seltileb])
            return {g: (seltile[:, g, :], [seltileb]) for g in range(4)}

        branch(ti, list(range(ti + 1)), ksT, vs1, lambda j: slice(j * 128, (j + 1) * 128), lambda j: j, slc_mask, 1)

        def win_mask(j, ti=ti):
            if j == ti:
                return {g: (tril_bf, [ncb]) for g in range(4)}
            if j == ti - 4:
                return {g: (far_bf, [ncb]) for g in range(4)}
            return None

        branch(ti, list(range(max(0, ti - 4), ti + 1)), kwT, vw1,
               lambda j: slice((j % 5) * 128, (j % 5 + 1) * 128), lambda j: j % 5, win_mask, 2)
        CP(c, "pool", W.y_tok[:, 0:768], yacc, [yaccb], [W.y_tokb])
        tile_tail_b(c, W, tl)
        if tl == 3:
            wout_block_stream(c, W, tb, wout_d, wo, wob, wo_n)
        S.barrier()


def wout_block_stream(c, W, tb, wout_d, wo, wob, wo_n):
    ts = slice(tb * 512, (tb + 1) * 512)
    wv = wout_d.rearrange("(cc p) d -> p cc d", p=128)
    for dc in range(8):
        s = wo_n[0] % 2
        wo_n[0] += 1
        c.S.dma("pool", f"wo{s}", lambda e, s=s, dc=dc: e.dma_start(out=wo[s][:, :, :], in_=wv[:, :, dc * 128:(dc + 1) * 128]),
                writes=[wob[s]])
        pw, pwb = ps1(c)
        for cc in range(8):
            MM(c, pw, wo[s][:, cc, :], W.yT[:, cc, :], cc == 0, cc == 7, [wob[s], W.yTb], pwb, skip=False)
        TT(c, "dve", c.xT[:, dc, ts], pw, c.xT[:, dc, ts], ALU.add, list(pwb) + [c.xb[dc][tb]], [c.xb[dc][tb]])


def build_program(cfg):
    nc = bass.Bass("TRN2", target_bir_lowering=False)
    stack = ExitStack()
    c = Ctx()
    c.nc = nc
    c.S = S = Sched()
    c.debug = set(cfg.get("debug", ()))
    c.dbg_done = set()
    c.seq = 0

    def din(name, shape):
        return nc.dram_tensor(name, list(shape), F32, kind="ExternalInput").ap()

    stages = cfg["stages"]
    xT_d = din("xT", (SEQ_PER_CORE, D, L))
    memT_d = din("memT", (SEQ_PER_CORE, D, NMEM))
    pp_d = din("pp", (128, NPP))
    cst_d = din("cst", (128, NCST))
    wd = {}
    for st in stages:
        if st[0] == "ffn":
            _, l, h = st
            wd[("wg", l, h)] = din(f"wg_{l}_{h}", (D, DFF))
            wd[("wu", l, h)] = din(f"wu_{l}_{h}", (D, DFF))
            wd[("wd", l, h)] = din(f"wd_{l}_{h}", (DFF, D))
        else:
            _, l = st
            kind = l % 3
            ncol = {0: 2560, 1: 1736, 2: 2596}[kind]
            wd[("win", l)] = din(f"win_{l}", (D, ncol))
            wd[("wout", l)] = din(f"wout_{l}", (D, D))
            wd[("wkv", l)] = din(f"wkv_{l}", (D, 512))
            if kind == 2:
                wd[("cw", l)] = din(f"cw_{l}", (2, 32, 64, 64))
                c.c2 = {"masks": din("c2_masks", (128, 768)), "czca": din("c2_czca", (128, 128)),
                        "cover": din("c2_cover", (128, 32)), "rope": din("c2_rope", (16, 8, 64))}
    out_d = nc.dram_tensor("outT", [SEQ_PER_CORE, D, L], F32, kind="ExternalOutput").ap()

    TOTAL = 207 * 1024 + 512
    c.arena = nc.alloc_sbuf_tensor("arena", [128, TOTAL], U8)
    R0 = Region(c.arena, 0, TOTAL)
    c.xT = R0.take((8, L), F32)
    c.xb = bufs("x", 8, NTB)
    c.h_off = R0.off
    c.hT = R0.take((8, L), BF16)
    c.hb = bufs("h", 8, NTB)
    c.cst = R0.take((NCST,), F32)
    c.pp = R0.take((NPP,), F32)
    c.ident_bf = R0.take((128,), BF16)
    c.ones_bf = R0.take((128,), BF16)
    c.mi_bf = R0.take((128,), BF16)
    c.constb = Buf("const")
    c.ppb = Buf("pp")
    c.phase_off = R0.off
    c.phase_size = TOTAL - R0.off
    c.tri_f = c.cst[:, CST_TRI:CST_TRI + 128]
    c.cos = c.cst[:, CST_COS:CST_COS + 512].rearrange("p (a b) -> p a b", a=16)
    c.sin = c.cst[:, CST_SIN:CST_SIN + 512].rearrange("p (a b) -> p a b", a=16)
    c.zk = c.cst[:, CST_ZK:CST_ZK + 6]
    c.epsx = c.cst[:, CST_EPSX:CST_EPSX + 6]
    c.cd = c.cst[:, CST_CD:CST_CD + 3]
    c.cneg = c.cst[:, CST_CNEG:CST_CNEG + 128]
    c.bisc = c.cst[:, CST_BISC:CST_BISC + DSA_BIS]
    c.pst = [nc.alloc_psum_tensor(f"ps{i}", [128, 1024], F32) for i in range(4)]
    c.psb = [Buf(f"ps{i}", excl=True) for i in range(8)]
    c.psp = 0
    c.psp2 = 0
    set_rot(c, range(8), (0, 2, 4, 6))

    S.dma("sp", "misc", lambda e: e.dma_start(out=c.cst[:, :], in_=cst_d[:, :]), writes=[c.constb])
    S.dma("sp", "misc", lambda e: e.dma_start(out=c.pp[:, :], in_=pp_d[:, :]), writes=[c.ppb])
    S.seal("misc", [c.constb, c.ppb])
    S.op("dve", lambda e: e.memset(c.ones_bf[:, :], 1.0), writes=[c.constb])
    S.op("dve", lambda e: e.tensor_copy(out=c.ident_bf[:, :], in_=c.cst[:, CST_IDENT:CST_IDENT + 128]),
         reads=[c.constb], writes=[c.constb])
    S.op("dve", lambda e: e.tensor_scalar(out=c.mi_bf[:, :], in0=c.cst[:, CST_IDENT:CST_IDENT + 128], scalar1=MASKM,
                                          scalar2=None, op0=ALU.mult), reads=[c.constb], writes=[c.constb])

    for s in range(SEQ_PER_CORE):
        c.seq = s
        xv = xT_d[s].rearrange("(dc p) t -> p dc t", p=128)
        for dc in range(8):
            S.dma("sp", "xin", lambda e, dc=dc, xv=xv: e.dma_start(out=c.xT[:, dc, :], in_=xv[:, dc, :]),
                  writes=c.xb[dc])
        S.seal("xin", [b for r in c.xb for b in r])
        for st in stages:
            if st[0] == "ffn":
                _, l, h = st
                ffn(c, wd[("wg", l, h)], wd[("wu", l, h)], wd[("wd", l, h)], c.pp[:, (l * 2 + h) * 8:(l * 2 + h) * 8 + 8])
            else:
                _, l = st
                kind = l % 3
                if kind == 0:
                    mixer_ret(c, l, wd[("win", l)], wd[("wout", l)], wd[("wkv", l)], memT_d[s])
                elif kind == 1:
                    mixer_dsa(c, l, wd[("win", l)], wd[("wout", l)], wd[("wkv", l)], memT_d[s])
                else:
                    mixer_nsa(c, l, wd[("win", l)], wd[("wout", l)], wd[("wkv", l)], memT_d[s], wd[("cw", l)])
        ov = out_d[s].rearrange("(dc p) t -> p dc t", p=128)
        for dc in range(8):
            S.dma("sp", "xout", lambda e, dc=dc, ov=ov: e.dma_start(out=ov[:, dc, :], in_=c.xT[:, dc, :]),
                  reads=c.xb[dc])
        S.seal("xout", [b for r in c.xb for b in r])
    S.finish("sp")
    S.emit(nc, stack)
    stack.close()
    return nc


def dbg_dump(c, name, ap, rbufs, seq):
    if name not in c.debug or seq != 0 or name in c.dbg_done:
        return
    c.dbg_done.add(name)
    n = ap.shape[1]
    d = c.nc.dram_tensor("dbg_" + name, [128, n], F32, kind="ExternalOutput").ap()
    c.S.dma("sp", "dbg", lambda e: e.dma_start(out=d[:, :], in_=ap), reads=rbufs)


def default_stages():
    st = []
    for l in range(DEPTH):
        st += [("ffn", l, 0), ("mix", l), ("ffn", l, 1)]
    return st


def kernel(**inputs):
    cfg = inputs.pop("_cfg", None) or {"stages": default_stages()}
    if os.environ.get("MK_STAGES"):
        cfg = {"stages": [tuple(int(v) if v.isdigit() else v for v in t.split(".")) for t in os.environ["MK_STAGES"].split(",")]}
    f32 = lambda a: np.ascontiguousarray(np.asarray(a, dtype=np.float32))
    x = f32(inputs["x"])
    xT = np.ascontiguousarray(x.transpose(0, 2, 1))
    memT = np.ascontiguousarray(f32(inputs["mem"]).transpose(0, 2, 1))
    nc = build_program(cfg)
    shared = {"pp": host_params(inputs), "cst": host_consts()}
    for st in cfg["stages"]:
        if st[0] == "ffn":
            _, l, h = st
            shared[f"wg_{l}_{h}"] = f32(inputs["ffn_w_gate"][l, h])
            shared[f"wu_{l}_{h}"] = f32(inputs["ffn_w_up"][l, h])
            shared[f"wd_{l}_{h}"] = f32(inputs["ffn_w_down"][l, h])
        else:
            _, l = st
            kind, j = l % 3, l // 3
            name = {0: "ret_w_in", 1: "dsa_w_in", 2: "nsa_w_in"}[kind]
            shared[f"win_{l}"] = f32(inputs[name][j])
            shared[f"wout_{l}"] = f32(inputs["w_out"][l])
            shared[f"wkv_{l}"] = f32(inputs["mem_w_kv"][l])
            if kind == 2:
                shared[f"cw_{l}"] = np.ascontiguousarray(np.stack([f32(inputs["nsa_cmp_wk"][j]), f32(inputs["nsa_cmp_wv"][j])]))
                shared.update(host_consts2())
    in_maps = []
    for i in range(NCORES):
        m = dict(shared)
        m["xT"] = xT[i * SEQ_PER_CORE:(i + 1) * SEQ_PER_CORE]
        m["memT"] = memT[i * SEQ_PER_CORE:(i + 1) * SEQ_PER_CORE]
        in_maps.append(m)
    res = run_bass_kernel_spmd(nc, in_maps, core_ids=list(range(NCORES)))
    outT = np.concatenate([r["outT"] for r in res.results], axis=0)
    if cfg.get("debug"):
        kernel.dbg = {k: v for k, v in res.results[0].items() if k.startswith("dbg_")}
    return np.ascontiguousarray(outT.transpose(0, 2, 1))
```

```python
import os
import numpy as np
from contextlib import ExitStack
import concourse.bass as bass
import concourse.mybir as mybir
from concourse.bass_utils import run_bass_kernel_spmd

F32 = mybir.dt.float32
BF16 = mybir.dt.bfloat16
U8 = mybir.dt.uint8
AF = mybir.ActivationFunctionType
ALU = mybir.AluOpType
AX = mybir.AxisListType

D = 1024
L = 2048
DEPTH = 4
DFF = 2816
NMEM = 256
NCORES = 8
SEQ_PER_CORE = 2
EPS = 1e-6
NT = L // 128

ENGS = ("pe", "act", "dve", "pool", "sp")


class Buf:
    __slots__ = ("name", "w", "r", "excl")

    def __init__(self, name, excl=False):
        self.name = name
        self.excl = excl
        self.w = None
        self.r = {}


def bufs(name, *dims):
    if len(dims) == 1:
        return [Buf(f"{name}{i}") for i in range(dims[0])]
    return [bufs(f"{name}{i}_", *dims[1:]) for i in range(dims[0])]


class Sched:
    def __init__(self):
        self.ops = {e: [] for e in ENGS}
        self.seen = {e: {} for e in ENGS}
        self.dma_count = {}
        self.needed = set()
        self.cap = None

    def _deps(self, eng, reads, writes):
        deps = {}
        for b in reads:
            if b.w is not None:
                deps[b.w[:2]] = max(deps.get(b.w[:2], -1), b.w[2])
            if b.excl:
                for k in b.r.values():
                    if not (k[0] == "eng" and k[1] == eng):
                        deps[k[:2]] = max(deps.get(k[:2], -1), k[2])
        for b in writes:
            if b.w is not None and not (b.w[0] == "eng" and b.w[1] == eng):
                deps[b.w[:2]] = max(deps.get(b.w[:2], -1), b.w[2])
            for k in b.r.values():
                if not (k[0] == "eng" and k[1] == eng):
                    deps[k[:2]] = max(deps.get(k[:2], -1), k[2])
        return self._waits(eng, deps)

    def _waits(self, eng, deps):
        waits = []
        seen = self.seen[eng]
        for k, idx in deps.items():
            if seen.get(k, -1) >= idx:
                continue
            seen[k] = idx
            waits.append((k, idx))
            if k[0] == "eng":
                self.needed.add((k[1], idx))
        return waits

    def capture(self, fn):
        assert self.cap is None
        self.cap = []
        fn()
        rec, self.cap = self.cap, None
        return rec

    def commit_interleaved(self, *lists):
        lists = [l for l in lists if l]
        pos = [0] * len(lists)
        total = sum(len(l) for l in lists)
        for _ in range(total):
            k = min((i for i in range(len(lists)) if pos[i] < len(lists[i])),
                    key=lambda i: (pos[i] + 1) / len(lists[i]))
            kind, args = lists[k][pos[k]]
            pos[k] += 1
            (self.op if kind == "op" else self.dma)(*args)

    def op(self, eng, emit, reads=(), writes=()):
        if self.cap is not None:
            self.cap.append(("op", (eng, emit, tuple(reads), tuple(writes))))
            return None
        waits = self._deps(eng, reads, writes)
        idx = len(self.ops[eng])
        key = ("eng", eng, idx)
        self.ops[eng].append(("op", waits, emit, None))
        for b in reads:
            b.r[("eng", eng)] = key
        for b in writes:
            b.w = key
            b.r = {}
        return key

    def dma(self, queue, sem, emit, reads=(), writes=()):
        if self.cap is not None:
            self.cap.append(("dma", (queue, sem, emit, tuple(reads), tuple(writes))))
            return None
        waits = self._deps(queue, reads, writes)
        c = self.dma_count.get(sem, 0) + 1
        self.dma_count[sem] = c
        key = ("dma", sem, c)
        self.ops[queue].append(("dma", waits, emit, sem))
        for b in reads:
            b.r[("dma", sem)] = key
        for b in writes:
            b.w = key
            b.r = {}
        return key

    def seal(self, sem, bl):
        c = self.dma_count[sem]
        for b in bl:
            if b.w is not None and b.w[0] == "dma" and b.w[1] == sem:
                b.w = ("dma", sem, c)
            k = b.r.get(("dma", sem))
            if k is not None:
                b.r[("dma", sem)] = ("dma", sem, c)

    def barrier(self):
        last = {}
        for e in ENGS:
            idx = None
            for i in range(len(self.ops[e]) - 1, -1, -1):
                if self.ops[e][i][0] == "op":
                    idx = i
                    break
            if idx is not None:
                last[("eng", e)] = idx
        for s, cnt in self.dma_count.items():
            last[("dma", s)] = cnt
        for e in ENGS:
            deps = {k: v for k, v in last.items() if k != ("eng", e)}
            waits = self._waits(e, deps)
            if waits:
                self.ops[e].append(("fin", waits, None, None))

    def finish(self, eng="sp"):
        waits = []
        for sem, c in self.dma_count.items():
            if self.seen[eng].get(("dma", sem), -1) < c:
                waits.append((("dma", sem), c))
        self.ops[eng].append(("fin", waits, None, None))

    def emit(self, nc, stack):
        esem = {e: stack.enter_context(nc.semaphore(f"s_{e}")) for e in ENGS}
        dsem = {s: stack.enter_context(nc.semaphore(f"d_{s}")) for s in self.dma_count}
        val = {}
        for e in ENGS:
            n = 0
            for i in range(len(self.ops[e])):
                if (e, i) in self.needed:
                    n += 1
                    val[(e, i)] = n
        ops = self.ops

        def run(e, engine):
            for i, (kind, waits, emit, sem) in enumerate(ops[e]):
                for k, idx in waits:
                    if k[0] == "eng":
                        engine.wait_ge(esem[k[1]], val[(k[1], idx)])
                    else:
                        engine.wait_ge(dsem[k[1]], 16 * idx)
                if kind == "fin":
                    continue
                ins = emit(engine)
                if kind == "dma":
                    ins.then_inc(dsem[sem], 16)
                elif (e, i) in self.needed:
                    ins.then_inc(esem[e], 1)

        with nc.Block() as block:
            @block.sync
            def _(eng):
                run("sp", eng)

            @block.scalar
            def _(eng):
                run("act", eng)

            @block.vector
            def _(eng):
                run("dve", eng)

            @block.gpsimd
            def _(eng):
                run("pool", eng)

            @block.tensor
            def _(eng):
                run("pe", eng)


class Region:
    def __init__(self, t, off, size):
        self.t = t
        self.base = off
        self.off = off
        self.end = off + size

    def take(self, shape, dtype, parts=None):
        esz = 2 if dtype == BF16 else 4
        n = int(np.prod(shape))
        nb = ((n * esz + 31) // 32) * 32
        assert self.off + nb <= self.end, ("SBUF region overflow", shape, self.off, self.end)
        ap = self.t[:, self.off:self.off + n * esz].bitcast(dtype)
        self.off += nb
        if len(shape) > 1:
            names = " ".join(f"d{i}" for i in range(len(shape)))
            kw = {f"d{i}": int(s) for i, s in enumerate(shape)}
            ap = ap.rearrange(f"p ({names}) -> p {names}", **kw)
        return ap


class Ctx:
    pass


def MM(c, out, lhsT, rhs, start, stop, reads, writes, skip=True):
    c.S.op("pe", lambda e: e.matmul(out, lhsT, rhs, start=start, stop=stop, skip_group_check=skip), reads, writes)


def TR(c, out, in_, reads, writes):
    n = in_.shape[0]
    c.S.op("pe", lambda e: e.transpose(out, in_, c.ident_bf[0:n, 0:n]), list(reads) + [c.constb], writes)


def ACTF(c, out, in_, func, reads, writes, scale=1.0, bias=0.0):
    c.S.op("act", lambda e: e.activation(out=out, in_=in_, func=func, bias=bias, scale=scale), reads, writes)


def TT(c, eng, out, in0, in1, op, reads, writes):
    c.S.op(eng, lambda e: e.tensor_tensor(out=out, in0=in0, in1=in1, op=op), reads, writes)


def TS(c, eng, out, in0, s1, s2, op0, op1, reads, writes):
    if s2 is None:
        c.S.op(eng, lambda e: e.tensor_scalar(out=out, in0=in0, scalar1=s1, scalar2=None, op0=op0), reads, writes)
    else:
        c.S.op(eng, lambda e: e.tensor_scalar(out=out, in0=in0, scalar1=s1, scalar2=s2, op0=op0, op1=op1), reads, writes)


def STT(c, out, in0, scalar, in1, op0, op1, reads, writes):
    c.S.op("dve", lambda e: e.scalar_tensor_tensor(out=out, in0=in0, scalar=scalar, in1=in1, op0=op0, op1=op1),
           reads, writes)


def RED(c, out, in_, op, reads, writes):
    c.S.op("dve", lambda e: e.tensor_reduce(out=out, in_=in_, axis=AX.X, op=op), reads, writes)


def RCP(c, out, in_, reads, writes):
    c.S.op("dve", lambda e: e.reciprocal(out=out, in_=in_), reads, writes)


def CP(c, eng, out, in_, reads, writes):
    if eng == "act":
        c.S.op("act", lambda e: e.copy(out=out, in_=in_), reads, writes)
    else:
        c.S.op(eng, lambda e: e.tensor_copy(out=out, in_=in_), reads, writes)


def MSET(c, eng, ap, val, writes):
    c.S.op(eng, lambda e: e.memset(ap, val), (), writes)


def bank(c, b):
    return c.pst[b // 2][:, (b % 2) * 512:(b % 2) * 512 + 512]


def ps1(c):
    b = c.rot1[c.psp % len(c.rot1)]
    c.psp += 1
    return bank(c, b), [c.psb[b]]


def ps2(c):
    b = c.rot2[c.psp2 % len(c.rot2)]
    c.psp2 += 1
    return c.pst[b // 2][:, :], [c.psb[b], c.psb[b + 1]]


def set_rot(c, rot1, rot2):
    c.rot1, c.rot2 = list(rot1), list(rot2)


CST_IDENT, CST_TRI, CST_COS, CST_SIN, CST_ZK, CST_EPSX, CST_CD = 0, 128, 256, 768, 1280, 1286, 1292
CST_CNEG = 1296
CST_BISC = 1296 + 128
NCST = 1296 + 128 + 32
MASKM = 29952.0
PP_L0 = 64
PP_PL = 464
PP_MIX, PP_MEMN, PP_MQN, PP_MKN, PP_A, PP_B, PP_C, PP_D, PP_POS = 0, 8, 16, 80, 144, 208, 272, 336, 400
NPP = PP_L0 + DEPTH * PP_PL


def host_consts():
    cst = np.zeros((128, NCST), np.float64)
    cst[:, CST_IDENT:CST_IDENT + 128] = np.eye(128)
    j = np.arange(128)[:, None]
    i = np.arange(128)[None, :]
    cst[:, CST_TRI:CST_TRI + 128] = (i >= j)
    cst[:, CST_CNEG:CST_CNEG + 128] = np.where(i <= j, 0.0, -1e30)
    cst[:, CST_BISC:CST_BISC + 32] = 2.0 ** (-np.arange(32))[None, :]
    inv = (np.float32(10000.0) ** (-np.arange(32, dtype=np.float32) / np.float32(32))).astype(np.float32)
    pos = (np.arange(16)[None, :, None] * 128 + np.arange(128)[:, None, None]).astype(np.float32)
    ang = (pos * inv[None, None, :]).astype(np.float32)
    cst[:, CST_COS:CST_COS + 512] = np.cos(ang).reshape(128, 512)
    cst[:, CST_SIN:CST_SIN + 512] = np.sin(ang).reshape(128, 512)
    h = np.arange(6)
    gam = 1.0 - 2.0 ** (-5.0 - h)
    t = np.arange(128)[:, None]
    cst[:, CST_ZK:CST_ZK + 6] = gam[None, :] ** (-(t + 1.0)) / 8.0
    cst[:, CST_EPSX:CST_EPSX + 6] = EPS * gam[None, :] ** (-2.0 * (t + 1.0))
    for pair in range(3):
        cst[0:64, CST_CD + pair] = gam[2 * pair] ** 128
        cst[64:128, CST_CD + pair] = gam[2 * pair + 1] ** 128
    return cst.astype(np.float32)


def host_consts2():
    t = np.arange(128)[:, None]
    s_ = np.arange(128)[None, :]
    masks = np.zeros((128, 768), np.float32)
    masks[:, 0:128] = (s_ <= t)
    masks[:, 128:256] = (s_ > t)
    m = np.arange(256)[None, :] - 128
    masks[:, 256:512] = (16 * m + 31 <= t)
    for k in range(8):
        masks[k, 512 + 128 + k] = 1.0
    czca = np.zeros((128, 128), np.float32)
    mm = np.arange(64)[None, :] - 32
    qb = (t >= 64).astype(np.int64)
    causal = mm <= qb
    forced = (mm == qb) | (mm == qb - 1)
    czca[:, 0:64] = causal
    czca[:, 64:128] = np.where(causal, np.where(forced, 1e9, 0.0), -1e30)
    starts = np.arange(127) * 16
    sst = np.arange(32) * 64
    cover = np.zeros((128, 32), np.float32)
    cover[:127] = ((starts[:, None] < sst[None, :] + 64) & (starts[:, None] + 32 > sst[None, :]))
    inv = (np.float32(10000.0) ** (-np.arange(32, dtype=np.float32) / np.float32(32))).astype(np.float32)
    rope = np.zeros((16, 8, 64), np.float32)
    for ti in range(16):
        for k in range(8):
            n = k if ti == 0 else 8 * ti - 1 + k
            pos = np.float32(16 * n + 31)
            ang = (pos * inv).astype(np.float32)
            rope[ti, k, 0:32] = np.cos(ang)
            rope[ti, k, 32:64] = np.sin(ang)
    return {"c2_masks": masks, "c2_czca": czca, "c2_cover": cover, "c2_rope": rope}


def host_params(inp):
    pp = np.zeros((128, NPP), np.float32)
    f32 = lambda a: np.asarray(a, np.float32)
    pp[:, 0:64] = f32(inp["ffn_norm"]).reshape(DEPTH, 2, 8, 128).transpose(3, 0, 1, 2).reshape(128, 64)
    for l in range(DEPTH):
        b = PP_L0 + l * PP_PL
        pp[:, b + PP_MIX:b + PP_MIX + 8] = f32(inp["mix_norm"][l]).reshape(8, 128).T
        pp[:, b + PP_MEMN:b + PP_MEMN + 8] = f32(inp["mem_norm"][l]).reshape(8, 128).T
        pp[:, b + PP_MQN:b + PP_MQN + 64] = f32(inp["mem_qn"][l])[None, :]
        pp[:, b + PP_MKN:b + PP_MKN + 64] = f32(inp["mem_kn"][l])[None, :]
        kind, j = l % 3, l // 3
        if kind == 1:
            pp[:, b + PP_A:b + PP_A + 64] = f32(inp["dsa_qn"][j])[None, :]
            pp[:, b + PP_B:b + PP_B + 64] = f32(inp["dsa_kn"][j])[None, :]
        elif kind == 2:
            pp[:, b + PP_A:b + PP_A + 64] = f32(inp["nsa_qn"][j])[None, :]
            pp[:, b + PP_B:b + PP_B + 64] = f32(inp["nsa_kn"][j][0])[None, :]
            pp[:, b + PP_C:b + PP_C + 64] = f32(inp["nsa_kn"][j][1])[None, :]
            pp[:, b + PP_D:b + PP_D + 64] = f32(inp["nsa_kn"][j][2])[None, :]
            pp[0:64, b + PP_POS:b + PP_POS + 32] = f32(inp["nsa_cmp_pos_k"][j]).T
            pp[0:64, b + PP_POS + 32:b + PP_POS + 64] = f32(inp["nsa_cmp_pos_v"][j]).T
    return pp


def rmsnorm_block(c, src, src_bufs, ntok, g_ap, sq, sqb, rstd, rstdb, out, out_bufs):
    ACTF(c, sq[:, :, 0:ntok], src, AF.Square, src_bufs, [sqb])
    ps, pb = ps1(c)
    for dc in range(8):
        MM(c, ps[:, 0:ntok], c.ones_bf[:, :], sq[:, dc, 0:ntok], dc == 0, dc == 7, [sqb, c.constb], pb, skip=False)
    ACTF(c, rstd[:, 0:ntok], ps[:, 0:ntok], AF.Sqrt, pb, [rstdb], scale=1.0 / D, bias=float(EPS))
    RCP(c, rstd[:, 0:ntok], rstd[:, 0:ntok], [rstdb], [rstdb])
    for dc in range(8):
        STT(c, out[:, dc, :], src[:, dc, :], g_ap[:, dc:dc + 1], rstd[:, 0:ntok], ALU.mult, ALU.mult,
            [src_bufs[dc], rstdb, c.ppb], [out_bufs[dc]])


NG = 11
NTB = 4
WSLOTS = 3


def ffn(c, wg_d, wu_d, wd_d, g_ap):
    S = c.S
    S.barrier()
    R = Region(c.arena, c.phase_off, c.phase_size)
    sq = R.take((8, 512), BF16)
    sqb = Buf("sq")
    rstd = R.take((512,), F32)
    rstdb = Buf("rstd")
    sg = R.take((512,), BF16)
    sgb = Buf("sg")
    act = [[R.take((512,), BF16) for _ in range(2)] for _ in range(2)]
    actb = bufs("act", 2, 2)
    wg = [R.take((8, 256), BF16) for _ in range(WSLOTS)]
    wu = [R.take((8, 256), BF16) for _ in range(WSLOTS)]
    wd = [R.take((2, D), BF16) for _ in range(WSLOTS)]
    wgb, wub, wdb = bufs("wg", WSLOTS), bufs("wu", WSLOTS), bufs("wd", WSLOTS)
    ps = [c.pst[b // 2][:, (b % 2) * 512:(b % 2) * 512 + 512] for b in range(8)]

    wg_v = wg_d.rearrange("(dc p) f -> p dc f", p=128)
    wu_v = wu_d.rearrange("(dc p) f -> p dc f", p=128)
    wd_v = wd_d.rearrange("(fc p) d -> p fc d", p=128)
    wn = [0]

    def load_w(gi):
        s = wn[0] % WSLOTS
        wn[0] += 1
        fs = slice(gi * 256, (gi + 1) * 256)
        S.dma("pool", f"wg{s}", lambda e: e.dma_start(out=wg[s][:, :, :], in_=wg_v[:, :, fs]), writes=[wgb[s]])
        S.dma("pool", f"wu{s}", lambda e: e.dma_start(out=wu[s][:, :, :], in_=wu_v[:, :, fs]), writes=[wub[s]])
        S.dma("pool", f"wd{s}", lambda e: e.dma_start(out=wd[s][:, :, :], in_=wd_v[:, 2 * gi:2 * gi + 2, :]),
              writes=[wdb[s]])
        return s

    slots = {}
    for gi in range(min(WSLOTS - 1, NG)):
        slots[gi] = load_w(gi)

    for tb in range(NTB):
        ts = slice(tb * 512, (tb + 1) * 512)
        rmsnorm_block(c, c.xT[:, :, ts], [c.xb[dc][tb] for dc in range(8)], 512, g_ap, sq, sqb, rstd, rstdb,
                      c.hT[:, :, ts], [c.hb[dc][tb] for dc in range(8)])

    pend = None
    steps = [(gi, tb) for gi in range(NG) for tb in range(NTB)]
    for it in range(len(steps) + 1):
        cur = steps[it] if it < len(steps) else None
        par = it % 2
        gu = []
        if cur is not None:
            gi, tb = cur
            s = slots[gi]
            ts = slice(tb * 512, (tb + 1) * 512)
            for fcl in range(2):
                for which in range(2):
                    w = wg[s] if which == 0 else wu[s]
                    wb = wgb[s] if which == 0 else wub[s]
                    bank = fcl * 2 + which
                    for dc in range(8):
                        gu.append((ps[bank], w[:, dc, fcl * 128:(fcl + 1) * 128], c.hT[:, dc, ts], dc == 0, dc == 7,
                                   [wb, c.hb[dc][tb]], [c.psb[bank]]))
        dn = []
        if pend is not None:
            ps_, ptb, ppar = pend
            pts = slice(ptb * 512, (ptb + 1) * 512)
            for dc in range(8):
                bank = 4 + dc % 4
                for fcl in range(2):
                    dn.append((ps[bank], wd[ps_][:, fcl, dc * 128:(dc + 1) * 128], act[ppar][fcl][:, :], fcl == 0,
                               fcl == 1, [wdb[ps_], actb[ppar][fcl]], [c.psb[bank]],
                               dc if fcl == 1 else None, bank, pts, ptb))
        gi_ = 0
        di_ = 0
        while gi_ < len(gu) or di_ < len(dn):
            for _ in range(4):
                if gi_ < len(gu):
                    o_, l_, r_, st_, sp_, rd_, wr_ = gu[gi_]
                    MM(c, o_, l_, r_, st_, sp_, rd_, wr_, skip=False)
                    gi_ += 1
                    if gi_ % 16 == 0:
                        fcl = gi_ // 16 - 1
                        ACTF(c, sg[:, :], ps[fcl * 2], AF.Silu, [c.psb[fcl * 2]], [sgb])
                        TT(c, "dve", act[par][fcl][:, :], ps[fcl * 2 + 1], sg[:, :], ALU.mult,
                           [c.psb[fcl * 2 + 1], sgb], [actb[par][fcl]])
            for _ in range(2):
                if di_ < len(dn):
                    o_, l_, r_, st_, sp_, rd_, wr_, dc, bank, pts, ptb = dn[di_]
                    MM(c, o_, l_, r_, st_, sp_, rd_, wr_, skip=False)
                    di_ += 1
                    if dc is not None:
                        STT(c, c.xT[:, dc, pts], ps[bank], 0.5, c.xT[:, dc, pts], ALU.mult, ALU.add,
                            [c.psb[bank], c.xb[dc][ptb]], [c.xb[dc][ptb]])
        pend = (slots[cur[0]], cur[1], par) if cur is not None else None
        if cur is not None and cur[1] == 0 and cur[0] + WSLOTS - 1 < NG:
            slots[cur[0] + WSLOTS - 1] = load_w(cur[0] + WSLOTS - 1)


def head_norm(c, src, src_bufs, H, gain, out, out_bufs, scr, scrb, ss, ssb, eng2="pool"):
    n = src.shape[0]
    s3 = scr[0:n, 0:H * 64].rearrange("p (h d) -> p h d", d=64)
    ACTF(c, s3, src, AF.Square, src_bufs, [scrb])
    RED(c, ss[0:n, 0:H], s3, ALU.add, [scrb], [ssb])
    ACTF(c, ss[0:n, 0:H], ss[0:n, 0:H], AF.Sqrt, [ssb], [ssb], scale=1.0 / 64, bias=float(EPS))
    RCP(c, ss[0:n, 0:H], ss[0:n, 0:H], [ssb], [ssb])
    TT(c, "dve", s3, src, ss[0:n, 0:H].unsqueeze(2).broadcast_to([n, H, 64]), ALU.mult, list(src_bufs) + [ssb], [scrb])
    TT(c, eng2, out, s3, gain[0:n, :].unsqueeze(1).broadcast_to([n, H, 64]), ALU.mult, [scrb, c.ppb], out_bufs)


def mem_prep(c, R, memT_d, wkv_d, pbase, k_memT, v_mem1, kvb):
    S = c.S
    memT = R.take((8, NMEM), F32)
    memb = bufs("memT", 8)
    sqm = R.take((8, NMEM), BF16)
    sqmb = Buf("sqm")
    rstdm = R.take((NMEM,), F32)
    rstdmb = Buf("rstdm")
    memn = R.take((8, NMEM), BF16)
    memnb = bufs("memn", 8)
    wkv = R.take((8, 512), BF16)
    wkvb = Buf("wkv")
    ktok = R.take((256,), BF16)
    ktokb = Buf("ktok")
    scr = R.take((256,), F32)
    scrb = Buf("mscr")
    ss = R.take((8,), F32)
    ssb = Buf("mss")
    mv = memT_d.rearrange("(dc p) m -> p dc m", p=128)
    for dc in range(8):
        S.dma("sp", "memin", lambda e, dc=dc: e.dma_start(out=memT[:, dc, :], in_=mv[:, dc, :]), writes=[memb[dc]])
    S.seal("memin", memb)
    S.dma("pool", "wkv", lambda e: e.dma_start(out=wkv[:, :, :], in_=wkv_d.rearrange("(dc p) f -> p dc f", p=128)),
          writes=[wkvb])
    rmsnorm_block(c, memT, memb, NMEM, c.pp[:, pbase + PP_MEMN:pbase + PP_MEMN + 8], sqm, sqmb, rstdm, rstdmb,
                  memn, memnb)
    MSET(c, "pool", v_mem1[:, :, :, 64:65], 1.0, [kvb])
    for mt in range(2):
        ps, pb = ps1(c)
        for dc in range(8):
            MM(c, ps, memn[:, dc, mt * 128:(mt + 1) * 128], wkv[:, dc, :], dc == 0, dc == 7, [memnb[dc], wkvb], pb,
               skip=False)
        head_norm(c, ps[:, 0:256].rearrange("p (h d) -> p h d", d=64), pb, 4,
                  c.pp[:, pbase + PP_MKN:pbase + PP_MKN + 64], ktok.rearrange("p (h d) -> p h d", d=64), [ktokb],
                  scr, scrb, ss, ssb)
        CP(c, "act", v_mem1[:, mt, :, 0:64], ps[:, 256:512].rearrange("p (h d) -> p h d", d=64), pb, [kvb])
        pt, ptb = ps1(c)
        ptv = pt.bitcast(BF16)
        for cc in range(2):
            TR(c, ptv[:, cc * 128:(cc + 1) * 128], ktok[:, cc * 128:(cc + 1) * 128], [ktokb], ptb)
        CP(c, "dve", k_memT[:, :, mt * 128:(mt + 1) * 128], ptv[:, 0:256].rearrange("p (c m) -> p c m", c=2), ptb, [kvb])


def tile_tail(c, W, qm_ps, qm_pb, tl):
    head_norm(c, qm_ps.rearrange("p (h d) -> p h d", d=64), qm_pb, 4, c.pp[:, W.pbase + PP_MQN:W.pbase + PP_MQN + 64],
              W.qm_tok.rearrange("p (h d) -> p h d", d=64), [W.qm_tokb], W.hscr, W.hscrb, W.hss, W.hssb)
    tile_tail_b(c, W, tl)


def tile_tail_b(c, W, tl):
    pt, ptb = ps1(c)
    ptv = pt.bitcast(BF16)
    for cc in range(2):
        TR(c, ptv[:, cc * 128:(cc + 1) * 128], W.qm_tok[:, cc * 128:(cc + 1) * 128], [W.qm_tokb], ptb)
    CP(c, "act", W.qmT[0][0:64], ptv[0:64, 0:256].rearrange("p (c m) -> p c m", c=2), ptb, [W.qmTb])
    CP(c, "act", W.qmT[1][64:128], ptv[64:128, 0:256].rearrange("p (c m) -> p c m", c=2), ptb, [W.qmTb])
    if c.rot2:
        pm, pmb = ps2(c)
        pms = [pm[:, 0:512], pm[:, 512:1024]]
    for mt in range(2):
        if c.rot2:
            pmt, pmtb = pms[mt], pmb[mt]
        else:
            pmt, pl = ps1(c)
            pmtb = pl[0]
        for h in range(4):
            hp = h % 2
            MM(c, pmt[:, h * 128:(h + 1) * 128], W.k_memT[:, h // 2, mt * 128:(mt + 1) * 128],
               W.qmT[hp][:, h // 2, :], h == 0, True, [W.kvb, W.qmTb], [pmtb])
        ACTF(c, W.PTm[:, mt * 512:(mt + 1) * 512], pmt, AF.Exp, [pmtb], [W.PTmb], scale=0.125)
    po, pob = ps1(c)
    first = True
    for h in range(4):
        for mt in range(2):
            MM(c, po[:, h * 65:(h + 1) * 65], W.PTm[:, (mt * 4 + h) * 128:(mt * 4 + h + 1) * 128], W.v_mem1[:, mt, h, :],
               first, mt == 1 and h == 3, [W.PTmb, W.kvb], pob)
            first = False
    po3 = po[:, 0:260].rearrange("p (h e) -> p h e", e=65)
    RCP(c, W.den4, po3[:, :, 64:65], pob, [W.den4b])
    TT(c, "dve", W.y_tok[:, 768:1024].rearrange("p (h d) -> p h d", d=64), po3[:, :, 0:64],
       W.den4.broadcast_to([128, 4, 64]), ALU.mult, list(pob) + [W.den4b], [W.y_tokb])
    py, pyb = ps1(c)
    pyv = py.bitcast(BF16)
    for cc in range(8):
        TR(c, pyv[:, cc * 128:(cc + 1) * 128], W.y_tok[:, cc * 128:(cc + 1) * 128], [W.y_tokb], pyb)
    CP(c, "act", W.yT[:, :, tl * 128:(tl + 1) * 128], pyv.rearrange("p (c t) -> p c t", c=8), pyb, [W.yTb])


def wout_block(c, W, tb):
    ts = slice(tb * 512, (tb + 1) * 512)
    for dc in range(8):
        pw, pwb = ps1(c)
        for cc in range(8):
            MM(c, pw, W.w_out[:, cc, dc * 128:(dc + 1) * 128], W.yT[:, cc, :], cc == 0, cc == 7, [W.w_outb, W.yTb], pwb,
               skip=False)
        TT(c, "dve", c.xT[:, dc, ts], pw, c.xT[:, dc, ts], ALU.add, list(pwb) + [c.xb[dc][tb]], [c.xb[dc][tb]])


def load_wout(c, W, R, wout_d):
    W.w_out = R.take((8, D), BF16)
    W.w_outb = Buf("w_out")
    wv = wout_d.rearrange("(cc p) d -> p cc d", p=128)
    for cc in range(0, 8, 4):
        c.S.dma("pool", "wout", lambda e, cc=cc: e.dma_start(out=W.w_out[:, cc:cc + 4, :], in_=wv[:, cc:cc + 4, :]),
                writes=[W.w_outb])
    c.S.seal("wout", [W.w_outb])


def tail_bufs(c, W, R, hscr_n=768):
    W.qm_tok = R.take((256,), BF16)
    W.qm_tokb = Buf("qm_tok")
    W.qmT = [R.take((2, 128), BF16) for _ in range(2)]
    W.qmTb = Buf("qmT")
    for a in range(2):
        MSET(c, "pool", W.qmT[a][:, :, :], 0.0, [W.qmTb])
    W.PTm = R.take((1024,), BF16)
    W.PTmb = Buf("PTm")
    W.den4 = R.take((4, 1), F32)
    W.den4b = Buf("den4")
    W.hscr = R.take((hscr_n,), F32)
    W.hscrb = Buf("hscr")
    W.hss = R.take((16,), F32)
    W.hssb = Buf("hss")
    W.y_tok = R.take((1024,), BF16)
    W.y_tokb = Buf("y_tok")
    W.k_memT = R.take((2, NMEM), BF16)
    W.v_mem1 = R.take((2, 4, 65), BF16)
    W.kvb = Buf("memkv")


def mixer_ret(c, l, win_d, wout_d, wkv_d, memT_d):
    S = c.S
    S.barrier()
    set_rot(c, range(8), (0, 2, 4, 6))
    W = Ctx()
    W.pbase = PP_L0 + l * PP_PL
    R1 = Region(c.arena, c.h_off, 32 * 1024)
    R2 = Region(c.arena, c.phase_off, c.phase_size)
    hblk = [R1.take((8, 512), BF16) for _ in range(2)]
    hblkb = bufs("hblk", 2, 8)
    W.yT = R1.take((8, 512), BF16)
    W.yTb = Buf("yT")
    sq = R1.take((8, 512), BF16)
    sqb = Buf("sq")
    w_in = R2.take((8, 2560), BF16)
    w_inb = Buf("w_in")
    load_wout(c, W, R2, wout_d)
    rstd = R2.take((512,), F32)
    rstdb = Buf("rstd")
    tail_bufs(c, W, R2, hscr_n=256)
    tq2 = [R2.take((384,), F32) for _ in range(2)]
    tq = [tq2[0], tq2[1], tq2[0], tq2[1]]
    tqb2 = bufs("tq", 2)
    tqb = [tqb2[0], tqb2[1], tqb2[0], tqb2[1]]
    rk = R2.take((768,), F32)
    rkb = Buf("rk")
    scr = R2.take((768,), F32)
    scrb = Buf("scr")
    state = R2.take((3, 128), F32)
    state_bf = R2.take((3, 128), BF16)
    stb = Buf("state")
    stbb = Buf("state_bf")
    sm = R2.take((8, 6), F32)
    smb = Buf("sm")
    qk_tok = [R2.take((768,), BF16) for _ in range(2)]
    qk_tokb = bufs("qk_tok", 2)
    qT = [[R2.take((3, 128), BF16) for _ in range(2)] for _ in range(2)]
    kT = [R2.take((3, 128), BF16) for _ in range(2)]
    qkTb = bufs("qkT", 2)
    Sm = [R2.take((6, 128), BF16) for _ in range(2)]
    Smb = bufs("Sm", 2)
    v_tok = [R2.take((768,), BF16) for _ in range(2)]
    v_tokb = bufs("v_tok", 2)
    sgt = [R2.take((768,), BF16) for _ in range(2)]
    sgtb = bufs("sgt", 2)
    qm_toks = [W.qm_tok, R2.take((256,), BF16)]
    qm_tokbs = [W.qm_tokb, Buf("qm_tok1")]
    wv = win_d.rearrange("(dc p) f -> p dc f", p=128)
    for dc in range(0, 8, 2):
        S.dma("pool", "win", lambda e, dc=dc: e.dma_start(out=w_in[:, dc:dc + 2, :], in_=wv[:, dc:dc + 2, :]),
              writes=[w_inb])
    S.seal("win", [w_inb])
    Rt = Region(c.arena, c.h_off, 32 * 1024)
    mem_prep(c, Rt, memT_d, wkv_d, W.pbase, W.k_memT, W.v_mem1, W.kvb)
    S.barrier()
    MSET(c, "dve", state[:, :, :], 0.0, [stb])
    MSET(c, "dve", state_bf[:, :, :], 0.0, [stbb])
    for p_ in range(2):
        for a in range(2):
            MSET(c, "pool", qT[p_][a][:, :, :], 0.0, [qkTb[p_]])

    g_ap = c.pp[:, W.pbase + PP_MIX:W.pbase + PP_MIX + 8]
    tri3 = c.tri_f.unsqueeze(1).broadcast_to([128, 6, 128])

    def stage_a(ti):
        set_rot(c, (0, 1, 2, 3), (0, 2))
        tb, tl = ti // 4, ti % 4
        par = ti % 2
        hb, hbb = hblk[tb % 2], hblkb[tb % 2]
        if tl == 0:
            ts = slice(tb * 512, (tb + 1) * 512)
            rmsnorm_block(c, c.xT[:, :, ts], [c.xb[dc][tb] for dc in range(8)], 512, g_ap, sq, sqb, rstd, rstdb, hb, hbb)

        def proj(out_ap, c0, c1, pbufs):
            for dc in range(8):
                MM(c, out_ap, hb[:, dc, tl * 128:(tl + 1) * 128], w_in[:, dc, c0:c1], dc == 0, dc == 7,
                   [hbb[dc], w_inb], pbufs, skip=False)

        P, Pb = ps2(c)
        proj(P[:, 0:384], 0, 384, [Pb[0]])
        proj(P[:, 512:896], 384, 768, [Pb[1]])
        v4 = P.rearrange("p (a b) -> p a b", a=2)[:, :, 0:384].rearrange("p a (h d) -> p a h d", d=64)
        x1, x2 = v4[:, :, :, 0:32], v4[:, :, :, 32:64]
        cosb = c.cos[:, ti, :].unsqueeze(1).unsqueeze(1).broadcast_to([128, 2, 6, 32])
        sinb = c.sin[:, ti, :].unsqueeze(1).unsqueeze(1).broadcast_to([128, 2, 6, 32])
        t4 = [t.rearrange("p (a h d) -> p a h d", a=2, h=6) for t in tq]
        rk4 = rk.rearrange("p (a h d) -> p a h d", a=2, h=6)
        TT(c, "dve", t4[0], x1, cosb, ALU.mult, Pb + [c.constb], [tqb[0]])
        TT(c, "dve", t4[1], x2, sinb, ALU.mult, Pb + [c.constb], [tqb[1]])
        TT(c, "pool", rk4[:, :, :, 0:32], t4[0], t4[1], ALU.subtract, [tqb[0], tqb[1]], [rkb])
        TT(c, "dve", t4[2], x2, cosb, ALU.mult, Pb + [c.constb], [tqb[2]])
        TT(c, "dve", t4[3], x1, sinb, ALU.mult, Pb + [c.constb], [tqb[3]])
        TT(c, "pool", rk4[:, :, :, 32:64], t4[2], t4[3], ALU.add, [tqb[2], tqb[3]], [rkb])
        qkt, qktb = qk_tok[par], qk_tokb[par]
        CP(c, "pool", qkt[:, 0:384], rk[:, 0:384], [rkb], [qktb])
        TT(c, "pool", qkt[:, 384:768].rearrange("p (h d) -> p h d", d=64),
           rk[:, 384:768].rearrange("p (h d) -> p h d", d=64),
           c.zk.unsqueeze(2).broadcast_to([128, 6, 64]), ALU.mult, [rkb, c.constb], [qktb])
        pt, ptb = ps1(c)
        ptv = pt.bitcast(BF16)
        for n in range(6):
            TR(c, ptv[:, n * 128:(n + 1) * 128], qkt[:, n * 128:(n + 1) * 128], [qktb], ptb)
        CP(c, "act", qT[par][0][0:64], ptv[0:64, 0:384].rearrange("p (n t) -> p n t", n=3), ptb, [qkTb[par]])
        CP(c, "act", qT[par][1][64:128], ptv[64:128, 0:384].rearrange("p (n t) -> p n t", n=3), ptb, [qkTb[par]])
        CP(c, "act", kT[par], ptv[:, 384:768].rearrange("p (n t) -> p n t", n=3), ptb, [qkTb[par]])
        P2, P2b = ps2(c)
        for h in range(6):
            hp = h % 2
            MM(c, P2[:, h * 128:(h + 1) * 128], kT[par][:, h // 2, :], qT[par][hp][:, h // 2, :], h % 4 == 0, True,
               [qkTb[par]], [P2b[h // 4]])
        TT(c, "dve", Sm[par], P2[:, 0:768].rearrange("p (h t) -> p h t", h=6), tri3, ALU.mult, P2b + [c.constb], [Smb[par]])
        P3, P3b = ps2(c)
        proj(P3[:, 0:512], 768, 1280, [P3b[0]])
        proj(P3[:, 512:768], 1280, 1536, [P3b[1]])
        CP(c, "act", v_tok[par], P3[:, 0:768], P3b, [v_tokb[par]])
        P6, P6b = ps2(c)
        proj(P6[:, 0:512], 1536, 2048, [P6b[0]])
        proj(P6[:, 512:768], 2048, 2304, [P6b[1]])
        ACTF(c, sgt[par], P6[:, 0:768], AF.Silu, P6b, [sgtb[par]])
        P7, P7b = ps1(c)
        proj(P7[:, 0:256], 2304, 2560, P7b)
        head_norm(c, P7[:, 0:256].rearrange("p (h d) -> p h d", d=64), P7b, 4,
                  c.pp[:, W.pbase + PP_MQN:W.pbase + PP_MQN + 64],
                  qm_toks[par].rearrange("p (h d) -> p h d", d=64), [qm_tokbs[par]], W.hscr, W.hscrb, W.hss, W.hssb)

    def stage_b(ti):
        set_rot(c, (4, 5, 6, 7), (4, 6))
        tb, tl = ti // 4, ti % 4
        par = ti % 2
        P4, P4b = ps2(c)
        for h in range(6):
            hp = h % 2
            MM(c, P4[:, h * 128:(h + 1) * 128], Sm[par][:, h, :], v_tok[par][:, h * 128:(h + 1) * 128], h % 4 == 0,
               ti == 0, [Smb[par], v_tokb[par]], [P4b[h // 4]])
            if ti > 0:
                MM(c, P4[:, h * 128:(h + 1) * 128], qT[par][hp][:, h // 2, :], state_bf[:, h // 2, :], False, True,
                   [qkTb[par], stbb], [P4b[h // 4]])
        if ti < NT - 1:
            P5, P5b = ps2(c)
            for pair in range(3):
                MM(c, P5[:, pair * 256:(pair + 1) * 256], qk_tok[par][:, 384 + pair * 128:384 + (pair + 1) * 128],
                   v_tok[par][:, pair * 256:(pair + 1) * 256], pair % 2 == 0, True, [qk_tokb[par], v_tokb[par]],
                   [P5b[pair // 2]])
            P5v = P5[:, 0:768].rearrange("p (a e) -> p a e", a=3)
            for half in range(2):
                rs = slice(half * 64, (half + 1) * 64)
                TT(c, "dve", state[rs, :, :], P5v[rs, :, half * 128:(half + 1) * 128], state[rs, :, :], ALU.add,
                   P5b + [stb], [stb])
                TT(c, "pool", state[rs, :, :], state[rs, :, :], c.cd[rs, :].unsqueeze(2).broadcast_to([64, 3, 128]),
                   ALU.mult, [stb, c.constb], [stb])
                CP(c, "pool", state_bf[rs, :, :], state[rs, :, :], [stb], [stbb])
        o3 = P4[:, 0:768].rearrange("p (h e) -> p h e", h=6)
        scr3 = scr.rearrange("p (h e) -> p h e", h=6)
        RED(c, sm[:, 0, :], o3, ALU.add, P4b, [smb])
        ACTF(c, scr, P4[:, 0:768], AF.Square, P4b, [scrb])
        RED(c, sm[:, 1, :], scr3, ALU.add, [scrb], [smb])
        TS(c, "dve", sm[:, 2, :], sm[:, 0, :], 1.0 / 128, None, ALU.mult, None, [smb], [smb])
        TT(c, "dve", sm[:, 3, :], sm[:, 2, :], sm[:, 2, :], ALU.mult, [smb], [smb])
        STT(c, sm[:, 4, :], sm[:, 1, :], 1.0 / 128, c.epsx, ALU.mult, ALU.add, [smb, c.constb], [smb])
        TT(c, "dve", sm[:, 4, :], sm[:, 4, :], sm[:, 3, :], ALU.subtract, [smb], [smb])
        ACTF(c, sm[:, 4, :], sm[:, 4, :], AF.Sqrt, [smb], [smb])
        RCP(c, sm[:, 4, :], sm[:, 4, :], [smb], [smb])
        TT(c, "dve", scr3, o3, sm[:, 2, :].unsqueeze(2).broadcast_to([128, 6, 128]), ALU.subtract, P4b + [smb], [scrb])
        TT(c, "pool", scr3, scr3, sm[:, 4, :].unsqueeze(2).broadcast_to([128, 6, 128]), ALU.mult, [scrb, smb], [scrb])
        TT(c, "pool", W.y_tok[:, 0:768], scr, sgt[par], ALU.mult, [scrb, sgtb[par]], [W.y_tokb])
        W.qm_tok, W.qm_tokb = qm_toks[par], qm_tokbs[par]
        tile_tail_b(c, W, tl)
        if tl == 3:
            wout_block(c, W, tb)

    stage_a(0)
    for ti in range(NT):
        la = S.capture(lambda: stage_a(ti + 1)) if ti + 1 < NT else []
        lb = S.capture(lambda: stage_b(ti))
        S.commit_interleaved(la, lb)


def norm_rope(c, B, src, src_bufs, H, gain, ti, out, out_bufs, pos_cos=None, pos_sin=None, pos_bufs=None):
    n = src.shape[0]
    xr3 = B.xr[0:n, 0:H * 64].rearrange("p (h d) -> p h d", d=64)
    cos2 = pos_cos if pos_cos is not None else c.cos[0:n, ti, :]
    sin2 = pos_sin if pos_sin is not None else c.sin[0:n, ti, :]
    cosb = cos2.unsqueeze(1).broadcast_to([n, H, 32])
    sinb = sin2.unsqueeze(1).broadcast_to([n, H, 32])
    t = [q[0:n, 0:H * 32].rearrange("p (h d) -> p h d", d=32) for q in B.tq]
    cb_ = [c.constb] if pos_bufs is None else list(pos_bufs)
    if gain is not None:
        ss = B.ss[0:n, 0:H]
        ACTF(c, xr3, src, AF.Square, src_bufs, [B.xrb])
        RED(c, ss, xr3, ALU.add, [B.xrb], [B.ssb])
        ACTF(c, ss, ss, AF.Sqrt, [B.ssb], [B.ssb], scale=1.0 / 64, bias=float(EPS))
        RCP(c, ss, ss, [B.ssb], [B.ssb])
        TT(c, "dve", xr3, src, gain[0:n, :].unsqueeze(1).broadcast_to([n, H, 64]), ALU.mult,
           list(src_bufs) + [c.ppb], [B.xrb])
        x, xb, engs = xr3, [B.xrb], ("dve", "pool", "dve", "pool")
    else:
        x, xb, engs = src, list(src_bufs), ("dve", "dve", "dve", "dve")
    x1, x2 = x[:, :, 0:32], x[:, :, 32:64]
    TT(c, engs[0], t[0], x1, cosb, ALU.mult, xb + cb_, [B.tqb[0]])
    TT(c, engs[1], t[1], x2, sinb, ALU.mult, xb + cb_, [B.tqb[1]])
    TT(c, engs[2], t[2], x2, cosb, ALU.mult, xb + cb_, [B.tqb[2]])
    TT(c, engs[3], t[3], x1, sinb, ALU.mult, xb + cb_, [B.tqb[3]])
    if gain is not None:
        TT(c, "pool", xr3[:, :, 0:32], t[0], t[1], ALU.subtract, [B.tqb[0], B.tqb[1]], [B.xrb])
        TT(c, "pool", xr3[:, :, 32:64], t[2], t[3], ALU.add, [B.tqb[2], B.tqb[3]], [B.xrb])
        TT(c, "pool", out, xr3, B.ss[0:n, 0:H].unsqueeze(2).broadcast_to([n, H, 64]), ALU.mult, [B.xrb, B.ssb], out_bufs)
    else:
        TT(c, "pool", out[:, :, 0:32], t[0], t[1], ALU.subtract, [B.tqb[0], B.tqb[1]], out_bufs)
        TT(c, "pool", out[:, :, 32:64], t[2], t[3], ALU.add, [B.tqb[2], B.tqb[3]], out_bufs)


def masked_T(c, src_tok, src_bufs, nchunk, dstA, dstB, dst_bufs):
    pt, ptb = ps1(c)
    ptv = pt.bitcast(BF16)
    for n in range(nchunk):
        TR(c, ptv[:, n * 128:(n + 1) * 128], src_tok[:, n * 128:(n + 1) * 128], src_bufs, ptb)
    v = ptv[:, 0:nchunk * 128].rearrange("p (n t) -> p n t", n=nchunk)
    CP(c, "act", dstA[0:64], v[0:64], ptb, dst_bufs)
    CP(c, "act", dstB[64:128], v[64:128], ptb, dst_bufs)


DSA_TOPK = 256
DSA_BIS = 17


def mixer_dsa(c, l, win_d, wout_d, wkv_d, memT_d):
    S = c.S
    S.barrier()
    set_rot(c, (5, 6, 7), (6,))
    W = Ctx()
    W.pbase = PP_L0 + l * PP_PL
    R1 = Region(c.arena, c.h_off, 32 * 1024)
    R2 = Region(c.arena, c.phase_off, c.phase_size)
    hblk = [R1.take((8, 128), BF16) for _ in range(2)]
    hblkb = bufs("hblk", 2, 8)
    sq = R1.take((8, 128), BF16)
    sqb = Buf("sq")
    W.yT = R1.take((8, 512), BF16)
    W.yTb = Buf("yT")
    acc = R1.take((L,), F32)
    accb = Buf("acc")
    rl = [R1.take((512,), F32) for _ in range(2)]
    rlb = bufs("rl", 2)
    PT = [R1.take((1536,), BF16) for _ in range(2)]
    PTb = bufs("PT", 2, 3)
    w_in = R2.take((8, 1736), BF16)
    w_inb = Buf("w_in")
    load_wout(c, W, R2, wout_d)
    rstd = R2.take((128,), F32)
    rstdb = Buf("rstd")
    kdup = R2.take((L,), BF16)
    ikdup = R2.take((L,), BF16)
    v1 = R2.take((NT, 65), BF16)
    cacheb = bufs("kvcache", NT)
    v1b = Buf("v1ones")
    tail_bufs(c, W, R2, hscr_n=256)
    mark = R2.off
    junk = R2.take((L,), BF16)
    junkb = Buf("junk")
    sel = [R2.take((L,), BF16) for _ in range(2)]
    selb = bufs("sel", 2)
    B = Ctx()
    B.xr = R2.take((768,), F32)
    B.xrb = Buf("xr")
    B.tq = [R2.take((384,), F32) for _ in range(4)]
    B.tqb = bufs("tq", 4)
    B.ss = R2.take((16,), F32)
    B.ssb = Buf("ss")
    q_tok = R2.take((768,), BF16)
    q_tokb = Buf("q_tok")
    qAB = [[R2.take((6, 128), BF16) for _ in range(2)] for _ in range(2)]
    qABb = bufs("qAB", 2)
    qm_toks = [W.qm_tok, R2.take((256,), BF16)]
    qm_tokbs = [W.qm_tokb, Buf("qm_tok1")]
    iq_tok = R2.take((512,), BF16)
    iq_tokb = Buf("iq_tok")
    iqAB = [R2.take((4, 128), BF16) for _ in range(2)]
    iqABb = Buf("iqAB")
    k2 = R2.take((2, 128), BF16)
    k2b = Buf("k2")
    iw_t = R2.take((8,), F32)
    iw_tb = Buf("iw")
    bs = R2.take((2 * DSA_BIS + 8,), F32)
    bsb = Buf("bs")
    den12 = R2.take((12, 1), F32)
    den12b = Buf("den12")

    wv = win_d.rearrange("(dc p) f -> p dc f", p=128)
    for dc in range(0, 8, 2):
        S.dma("pool", "win", lambda e, dc=dc: e.dma_start(out=w_in[:, dc:dc + 2, :], in_=wv[:, dc:dc + 2, :]),
              writes=[w_inb])
    S.seal("win", [w_inb])
    Rt = Region(c.arena, c.h_off, 32 * 1024)
    mem_prep(c, Rt, memT_d, wkv_d, W.pbase, W.k_memT, W.v_mem1, W.kvb)
    S.barrier()
    for p_ in range(2):
        for a in range(2):
            MSET(c, "pool", qAB[p_][a][:, :, :], 0.0, [qABb[p_]])
    for a in range(2):
        MSET(c, "pool", iqAB[a][:, :, :], 0.0, [iqABb])
    MSET(c, "pool", v1[:, :, 64:65], 1.0, [v1b])

    g_ap = c.pp[:, W.pbase + PP_MIX:W.pbase + PP_MIX + 8]
    gq = c.pp[:, W.pbase + PP_A:W.pbase + PP_A + 64]
    gk = c.pp[:, W.pbase + PP_B:W.pbase + PP_B + 64]
    mi4 = c.mi_bf.unsqueeze(1).broadcast_to([128, 4, 128])
    SB = [0, 1, 2]
    OB = [3, 4]
    hslot = {}
    for k_, h in enumerate((0, 2, 4, 6)):
        hslot[h] = (0, k_)
    for k_, h in enumerate((8, 10, 9, 11)):
        hslot[h] = (1, k_)
    for k_, h in enumerate((1, 3, 5, 7)):
        hslot[h] = (2, k_)

    def stage_a(ti):
        set_rot(c, (6, 7), (6,))
        tb = ti // 4
        par = ti % 2
        tsl = slice(ti * 128, (ti + 1) * 128)
        hb, hbb = hblk[par], hblkb[par]
        rmsnorm_block(c, c.xT[:, :, tsl], [c.xb[dc][tb] for dc in range(8)], 128, g_ap, sq, sqb, rstd, rstdb, hb, hbb)

        def proj(out_ap, c0, c1, pbufs):
            for dc in range(8):
                MM(c, out_ap, hb[:, dc, :], w_in[:, dc, c0:c1], dc == 0, dc == 7, [hbb[dc], w_inb], pbufs)

        Pq, Pqb = ps2(c)
        proj(Pq[:, 0:512], 0, 512, [Pqb[0]])
        proj(Pq[:, 512:768], 512, 768, [Pqb[1]])
        norm_rope(c, B, Pq[:, 0:768].rearrange("p (h d) -> p h d", d=64), Pqb, 12, gq, ti,
                  q_tok.rearrange("p (h d) -> p h d", d=64), [q_tokb])
        masked_T(c, q_tok, [q_tokb], 6, qAB[par][0], qAB[par][1], [qABb[par]])
        Pi, Pib = ps1(c)
        proj(Pi, 896, 1408, Pib)
        norm_rope(c, B, Pi.rearrange("p (h d) -> p h d", d=64), Pib, 8, None, ti,
                  iq_tok.rearrange("p (h d) -> p h d", d=64), [iq_tokb])
        masked_T(c, iq_tok, [iq_tokb], 4, iqAB[0], iqAB[1], [iqABb])
        Ps, Psb = ps1(c)
        proj(Ps[:, 0:128], 768, 896, Psb)
        proj(Ps[:, 128:192], 1408, 1472, Psb)
        proj(Ps[:, 192:200], 1472, 1480, Psb)
        norm_rope(c, B, Ps[:, 0:64].rearrange("p (h d) -> p h d", d=64), Psb, 1, gk, ti,
                  k2[:, 0, 0:64].rearrange("p (h d) -> p h d", d=64), [k2b])
        norm_rope(c, B, Ps[:, 128:192].rearrange("p (h d) -> p h d", d=64), Psb, 1, None, ti,
                  k2[:, 1, 0:64].rearrange("p (h d) -> p h d", d=64), [k2b])
        CP(c, "pool", k2[:, :, 64:128], k2[:, :, 0:64], [k2b], [k2b])
        CP(c, "act", v1[:, ti, 0:64], Ps[:, 64:128], Psb, [cacheb[ti]])
        CP(c, "act", iw_t, Ps[:, 192:200], Psb, [iw_tb])
        pt, ptb = ps1(c)
        ptv = pt.bitcast(BF16)
        TR(c, ptv[:, 0:128], k2[:, 0, :], [k2b], ptb)
        TR(c, ptv[:, 128:256], k2[:, 1, :], [k2b], ptb)
        CP(c, "act", kdup[:, tsl], ptv[:, 0:128], ptb, [cacheb[ti]])
        CP(c, "act", ikdup[:, tsl], ptv[:, 128:256], ptb, [cacheb[ti]])
        Pm, Pmb = ps1(c)
        proj(Pm[:, 0:256], 1480, 1736, Pmb)
        head_norm(c, Pm[:, 0:256].rearrange("p (h d) -> p h d", d=64), Pmb, 4,
                  c.pp[:, W.pbase + PP_MQN:W.pbase + PP_MQN + 64],
                  qm_toks[par].rearrange("p (h d) -> p h d", d=64), [qm_tokbs[par]], W.hscr, W.hscrb, W.hss, W.hssb)

        Skeys = 128 * (ti + 1)
        nkb = (Skeys + 511) // 512
        n = 0
        for kb in range(nkb):
            wdt = min(512, Skeys - kb * 512)
            ks = slice(kb * 512, kb * 512 + wdt)
            kbufs = [cacheb[j] for j in range(kb * 4, min(ti + 1, kb * 4 + 4))]
            for h in range(8):
                Px, Pxb = ps1(c)
                MM(c, Px[:, 0:wdt], iqAB[h % 2][:, h // 2, :], ikdup[:, ks], True, True, [iqABb] + kbufs, Pxb)
                r_, rb_ = rl[n % 2], rlb[n % 2]
                n += 1
                ACTF(c, r_[:, 0:wdt], Px[:, 0:wdt], AF.Relu, Pxb, [rb_])
                if h == 0:
                    TS(c, "dve", acc[:, ks], r_[:, 0:wdt], iw_t[:, 0:1], None, ALU.mult, None, [rb_, iw_tb], [accb])
                else:
                    STT(c, acc[:, ks], r_[:, 0:wdt], iw_t[:, h:h + 1], acc[:, ks], ALU.mult, ALU.add,
                        [rb_, iw_tb, accb], [accb])
        sl_, slb_ = sel[par], selb[par]
        if Skeys > DSA_TOPK:
            a_ = acc[:, 0:Skeys]
            S.op("dve", lambda e, a_=a_: e.tensor_reduce(out=bs[:, 0:1], in_=a_, axis=AX.X, op=ALU.max,
                                                         apply_absolute_value=True), [accb], [bsb])
            TT(c, "dve", acc[:, tsl], acc[:, tsl], c.cneg, ALU.add, [accb, c.constb], [accb])
            TS(c, "dve", bs[:, 8:8 + DSA_BIS], c.bisc, bs[:, 0:1], None, ALU.mult, None, [bsb, c.constb], [bsb])
            TS(c, "dve", bs[:, 8 + DSA_BIS:8 + 2 * DSA_BIS], bs[:, 8:8 + DSA_BIS], 2.0, None, ALU.mult, None, [bsb], [bsb])
            TS(c, "dve", bs[:, 1:2], bs[:, 0:1], 0.0, None, ALU.mult, None, [bsb], [bsb])
            for i in range(DSA_BIS):
                S.op("dve", lambda e, a_=a_, n_=Skeys: e.tensor_scalar(
                    out=junk[:, 0:n_], in0=a_, scalar1=bs[:, 1:2], scalar2=None, op0=ALU.is_ge, op1=ALU.add,
                    accum_out=bs[:, 2:3]), [accb, bsb], [junkb, bsb])
                last = i + 1 == DSA_BIS
                dn = bs[:, 8 + i:8 + i + 1] if last else bs[:, 8 + i + 1:8 + i + 2]
                d2 = bs[:, 8 + i:8 + i + 1] if last else bs[:, 8 + DSA_BIS + i + 1:8 + DSA_BIS + i + 2]
                STT(c, bs[:, 3:4], bs[:, 2:3], float(DSA_TOPK), d2, ALU.is_ge, ALU.mult, [bsb], [bsb])
                S.op("dve", lambda e, dn=dn: e.scalar_tensor_tensor(out=bs[:, 1:2], in0=bs[:, 1:2], scalar=dn, in1=bs[:, 3:4],
                                                                    op0=ALU.subtract, op1=ALU.add), [bsb], [bsb])
            TS(c, "dve", sl_[:, 0:Skeys], a_, bs[:, 1:2], None, ALU.is_ge, None, [accb, bsb], [slb_])
        else:
            TT(c, "dve", acc[:, tsl], acc[:, tsl], c.cneg, ALU.add, [accb, c.constb], [accb])
            TS(c, "dve", sl_[:, 0:Skeys], acc[:, 0:Skeys], -1e29, None, ALU.is_ge, None, [accb], [slb_])

    def stage_b(ti):
        set_rot(c, (5,), ())
        tb, tl = ti // 4, ti % 4
        par = ti % 2
        sl_, slb_ = sel[par], selb[par]
        qa, qb_ = qAB[par][0], qAB[par][1]
        for j in range(ti + 1):
            js = slice(j * 128, (j + 1) * 128)
            pj = j % 2
            p0, p1, p2 = bank(c, SB[0]), bank(c, SB[1]), bank(c, SB[2])
            b0, b1, b2 = [c.psb[SB[0]]], [c.psb[SB[1]]], [c.psb[SB[2]]]
            kb_ = [cacheb[j], qABb[par]]
            MM(c, p0, kdup[:, js], qa[:, 0:4, :], True, False, kb_, b0)
            MM(c, p0, sl_[:, js], mi4, False, True, [slb_, c.constb], b0)
            MM(c, p1[:, 0:256], kdup[:, js], qa[:, 4:6, :], True, False, kb_, b1)
            MM(c, p1[:, 256:512], kdup[:, js], qb_[:, 4:6, :], False, False, kb_, b1)
            MM(c, p1, sl_[:, js], mi4, False, True, [slb_, c.constb], b1)
            MM(c, p2, kdup[:, js], qb_[:, 0:4, :], True, False, kb_, b2)
            MM(c, p2, sl_[:, js], mi4, False, True, [slb_, c.constb], b2)
            for bi, (pp_, bb_) in enumerate(((p0, b0), (p1, b1), (p2, b2))):
                ACTF(c, PT[pj][:, bi * 512:(bi + 1) * 512], pp_, AF.Exp, bb_, [PTb[pj][bi]], scale=0.125,
                     bias=-MASKM / 8)
            for h in range(12):
                bi, sl2 = hslot[h]
                ob = OB[0] if h < 7 else OB[1]
                oc = (h if h < 7 else h - 7) * 65
                MM(c, bank(c, ob)[:, oc:oc + 65], PT[pj][:, (bi * 4 + sl2) * 128:(bi * 4 + sl2 + 1) * 128], v1[:, j, :],
                   j == 0 and h in (0, 7), j == ti and h in (6, 11), [PTb[pj][bi], cacheb[j], v1b], [c.psb[ob]])
        oa = bank(c, OB[0])[:, 0:455].rearrange("p (h e) -> p h e", e=65)
        ob_ = bank(c, OB[1])[:, 0:325].rearrange("p (h e) -> p h e", e=65)
        RCP(c, den12[:, 0:7, :], oa[:, :, 64:65], [c.psb[OB[0]]], [den12b])
        RCP(c, den12[:, 7:12, :], ob_[:, :, 64:65], [c.psb[OB[1]]], [den12b])
        TT(c, "dve", W.y_tok[:, 0:448].rearrange("p (h d) -> p h d", d=64), oa[:, :, 0:64],
           den12[:, 0:7, :].broadcast_to([128, 7, 64]), ALU.mult, [c.psb[OB[0]], den12b], [W.y_tokb])
        TT(c, "dve", W.y_tok[:, 448:768].rearrange("p (h d) -> p h d", d=64), ob_[:, :, 0:64],
           den12[:, 7:12, :].broadcast_to([128, 5, 64]), ALU.mult, [c.psb[OB[1]], den12b], [W.y_tokb])
        W.qm_tok, W.qm_tokb = qm_toks[par], qm_tokbs[par]
        tile_tail_b(c, W, tl)
        if tl == 3:
            wout_block(c, W, tb)

    stage_a(0)
    for ti in range(NT):
        la = S.capture(lambda: stage_a(ti + 1)) if ti + 1 < NT else []
        lb = S.capture(lambda: stage_b(ti))
        S.commit_interleaved(la, lb)


NSA_NCMP = 127


def nsa_hmap(c_, par):
    return 3 * (2 * (c_ // 3) + par) + (c_ % 3)


def mixer_nsa(c, l, win_d, wout_d, wkv_d, memT_d, cw_d):
    S = c.S
    S.barrier()
    set_rot(c, (6, 7), (6,))
    W = Ctx()
    W.pbase = PP_L0 + l * PP_PL
    R1 = Region(c.arena, c.h_off, 32 * 1024)
    R2 = Region(c.arena, c.phase_off, c.phase_size)
    hblk = R1.take((8, 128), BF16)
    hblkb = bufs("hblk", 8)
    sq = R1.take((8, 128), BF16)
    sqb = Buf("sq")
    W.yT = R1.take((8, 512), BF16)
    W.yTb = Buf("yT")
    un0 = R1.off
    B = Ctx()
    B.xr = R1.take((768,), F32)
    B.xrb = Buf("xr")
    B.tq = [R1.take((384,), F32) for _ in range(4)]
    B.tqb = bufs("tq", 4)
    un1 = R1.off
    Ru = Region(c.arena, un0, un1 - un0)
    PT = [Ru.take((1536,), BF16) for _ in range(2)]
    PTb = bufs("PT", 2, 4)
    PTc = Ru.take((1536,), BF16)
    PTcb = bufs("PTc", 4)
    tmp = R1.take((768,), F32)
    tmpb = Buf("tmp")
    yacc = R1.take((768,), F32)
    yaccb = Buf("yacc")
    imp = R1.take((12, 32), F32)
    impb = Buf("imp")
    impm = R1.take((4, 32), F32)
    wk16 = R1.take((4, 32), F32)
    impmb = Buf("impm")
    blk = R1.take((4, 32), BF16)
    blkb = Buf("blk")
    w_in = R2.take((8, 2596), BF16)
    w_inb = Buf("w_in")
    wo1 = R2.take((8, 128), BF16)
    wo = [wo1, wo1]
    wob1 = Buf("wo")
    wob = [wob1, wob1]
    rstd = R2.take((128,), F32)
    rstdb = Buf("rstd")
    ksT = R2.take((2, L), BF16)
    kwT = R2.take((2, 5 * 128), BF16)
    vs1 = R2.take((NT, 4, 65), BF16)
    vw1 = R2.take((5, 4, 65), BF16)
    cacheb = Buf("kvcache")
    tail_bufs(c, W, R2, hscr_n=256)
    mark = R2.off
    B.ss = R2.take((16,), F32)
    B.ssb = Buf("ss")
    q_tok = R2.take((768,), BF16)
    q_tokb = Buf("q_tok")
    qAB = [R2.take((6, 128), BF16) for _ in range(2)]
    qABb = Buf("qAB")
    k_tok = R2.take((2, 256), BF16)
    k_tokb = Buf("k_tok")
    kvcT = [R2.take((4, 144), BF16) for _ in range(2)]
    kvcTb = Buf("kvcT")
    wdup = [R2.take((32, 64), BF16) for _ in range(2)]
    wdupb = Buf("wdup")
    posB = R2.take((2, 32, 8), BF16)
    posBb = Buf("posB")
    k_cmpT = R2.take((2, 128), BF16)
    v_cmpx = R2.take((4, 97), BF16)
    cmpb = Buf("cmpcache")
    cbias = R1.take((512,), F32)
    cbiasb = Buf("cbias")
    cnew_tok = R2.take((512,), BF16)
    cnew_tokb = Buf("cnew_tok")
    gate = R2.take((36,), F32)
    gateb = Buf("gate")
    sm = R2.take((6, 12), F32)
    smb = Buf("sm")
    m8 = R2.take((2, 8), F32)
    m8b = Buf("m8")
    thr = R2.take((4, 1), F32)
    thrb = Buf("thr")
    seltile = R2.take((4, 128), BF16)
    seltileb = Buf("seltile")
    masks_bf = R2.take((768,), BF16)
    tril_bf, far_bf = masks_bf[:, 0:128], masks_bf[:, 128:256]
    cmask_bf, shiftI = masks_bf[:, 256:512], masks_bf[:, 512:768]
    czca = R2.take((128,), F32)
    cse = [R2.take((64,), F32) for _ in range(2)]
    cseb = bufs("cse", 2)
    ncb = Buf("nsaconst")

    wv = win_d.rearrange("(dc p) f -> p dc f", p=128)
    for c_ in range(6):
        for par in range(2):
            h = nsa_hmap(c_, par)
            src = wv[:, :, h * 64:(h + 1) * 64]
            dst = w_in[:, :, c_ * 128 + par * 64:c_ * 128 + (par + 1) * 64]
            S.dma("pool", "win", lambda e, src=src, dst=dst: e.dma_start(out=dst, in_=src), writes=[w_inb])
    for dc in range(0, 8, 2):
        S.dma("pool", "win", lambda e, dc=dc: e.dma_start(out=w_in[:, dc:dc + 2, 768:2596], in_=wv[:, dc:dc + 2, 768:2596]),
              writes=[w_inb])
    S.seal("win", [w_inb])
    for kind in range(2):
        srcw = cw_d[kind].rearrange("l d e -> d l e")
        for half in range(2):
            S.dma("pool", "wcmp", lambda e, kind=kind, half=half, srcw=srcw: e.dma_start(
                out=wdup[kind][half * 64:(half + 1) * 64, :, :], in_=srcw), writes=[wdupb])
    S.seal("wcmp", [wdupb])
    Rt = Region(c.arena, c.h_off, 32 * 1024)
    mem_prep(c, Rt, memT_d, wkv_d, W.pbase, W.k_memT, W.v_mem1, W.kvb)
    S.barrier()
    S.dma("pool", "ncst", lambda e: e.dma_start(out=masks_bf, in_=c.c2["masks"][:, :]), writes=[ncb])
    S.dma("sp", "ncst", lambda e: e.dma_start(out=czca, in_=c.c2["czca"][:, :]), writes=[ncb])
    S.seal("ncst", [ncb])
    for a in range(2):
        MSET(c, "pool", qAB[a][:, :, :], 0.0, [qABb])
        MSET(c, "pool", kvcT[a][:, :, :], 0.0, [kvcTb])
    MSET(c, "pool", k_cmpT[:, :, :], 0.0, [cmpb])
    MSET(c, "pool", v_cmpx[:, :, 0:64], 0.0, [cmpb])
    MSET(c, "pool", v_cmpx[:, :, 64:65], 1.0, [cmpb])
    for g_ in range(4):
        S.dma("pool", "ncov", lambda e, g_=g_: e.dma_start(out=v_cmpx[:, g_, 65:97], in_=c.c2["cover"][:, :]), writes=[cmpb])
    S.seal("ncov", [cmpb])
    MSET(c, "pool", vs1[:, :, :, 64:65], 1.0, [cacheb])
    MSET(c, "pool", vw1[:, :, :, 64:65], 1.0, [cacheb])
    posf = c.pp[:, W.pbase + PP_POS:W.pbase + PP_POS + 64].rearrange("p (k l) -> p k l", k=2)
    CP(c, "dve", posB, posf.unsqueeze(3).broadcast_to([128, 2, 32, 8]), [c.ppb], [posBb])
    Pcb, Pcbb = ps1(c)
    for kind in range(2):
        for l_ in range(32):
            MM(c, Pcb[0:8, kind * 64:(kind + 1) * 64], posB[:, kind, l_, :], wdup[kind][:, l_, :], kind == 0 and l_ == 0,
               l_ == 31, [posBb, wdupb], Pcbb)
    CP(c, "dve", cbias[0:8, :].rearrange("p (k g e) -> p k g e", k=2, g=4),
       Pcb[0:8, 0:128].rearrange("p (k e) -> p k e", k=2).unsqueeze(2).broadcast_to([8, 2, 4, 64]), Pcbb, [cbiasb])

    g_ap = c.pp[:, W.pbase + PP_MIX:W.pbase + PP_MIX + 8]
    gq = c.pp[:, W.pbase + PP_A:W.pbase + PP_A + 64]
    gkc = c.pp[:, W.pbase + PP_B:W.pbase + PP_B + 64]
    gks = c.pp[:, W.pbase + PP_C:W.pbase + PP_C + 64]
    gkw = c.pp[:, W.pbase + PP_D:W.pbase + PP_D + 64]
    mi3 = c.mi_bf.unsqueeze(1).broadcast_to([128, 3, 128])
    SBK = [0, 1, 2, 3]
    OB = [4, 5]
    CB = [4, 5, 6]
    wo_n = [0]

    def head_of(sb, r):
        par, kc_ = sb // 2, sb % 2
        return 3 * (2 * kc_ + par) + r

    def branch(ti, keys, Kc, Vc, kslot, vslot, maskf, coef_col):
        nk = len(keys)
        for n_, j in enumerate(keys):
            par_j = n_ % 2
            ks_ = kslot(j)
            mk = maskf(j)
            for sb in SBK:
                par, kc_ = sb // 2, sb % 2
                g = 2 * kc_ + par
                pb_ = bank(c, sb)
                MM(c, pb_[:, 0:384], Kc[:, kc_, ks_], qAB[par][:, 3 * kc_:3 * kc_ + 3, :], True, mk is None,
                   [cacheb, qABb], [c.psb[sb]])
                if mk is not None:
                    lh, lb = mk[g]
                    MM(c, pb_[:, 0:384], lh, mi3, False, True, lb + [c.constb], [c.psb[sb]])
            for pr in range(2):
                src = c.pst[pr][:, :].rearrange("p (b x) -> p b x", b=2)[:, :, 0:384]
                dst = PT[par_j][:, pr * 768:(pr + 1) * 768].rearrange("p (b x) -> p b x", b=2)
                ACTF(c, dst, src, AF.Exp, [c.psb[2 * pr], c.psb[2 * pr + 1]], [PTb[par_j][2 * pr], PTb[par_j][2 * pr + 1]],
                     scale=0.125, bias=(0.0 if mk is None else -MASKM / 8))
            for sb in SBK:
                for r in range(3):
                    h = head_of(sb, r)
                    g = h // 3
                    ob = OB[0] if h < 7 else OB[1]
                    oc = (h if h < 7 else h - 7) * 65
                    firsts = (0, 7)
                    MM(c, bank(c, ob)[:, oc:oc + 65], PT[par_j][:, sb * 384 + r * 128:sb * 384 + (r + 1) * 128],
                       Vc[:, vslot(j), g, :], n_ == 0 and (sb, r) == first_in_bank[ob], n_ == nk - 1,
                       [PTb[par_j][sb], cacheb], [c.psb[ob]])
        oa = bank(c, OB[0])[:, 0:455].rearrange("p (h e) -> p h e", e=65)
        ob_ = bank(c, OB[1])[:, 0:325].rearrange("p (h e) -> p h e", e=65)
        RCP(c, sm[:, 0, 0:7].unsqueeze(2), oa[:, :, 64:65], [c.psb[OB[0]]], [smb])
        RCP(c, sm[:, 0, 7:12].unsqueeze(2), ob_[:, :, 64:65], [c.psb[OB[1]]], [smb])
        TT(c, "dve", sm[:, 1, :], sm[:, 0, :], gate.rearrange("p (h b) -> p h b", b=3)[:, :, coef_col], ALU.mult,
           [smb, gateb], [smb])
        t3 = tmp.rearrange("p (h d) -> p h d", d=64)
        TT(c, "dve", t3[:, 0:7, :], oa[:, :, 0:64], sm[:, 1, 0:7].unsqueeze(2).broadcast_to([128, 7, 64]), ALU.mult,
           [c.psb[OB[0]], smb], [tmpb])
        TT(c, "dve", t3[:, 7:12, :], ob_[:, :, 0:64], sm[:, 1, 7:12].unsqueeze(2).broadcast_to([128, 5, 64]), ALU.mult,
           [c.psb[OB[1]], smb], [tmpb])
        TT(c, "pool", yacc, yacc, tmp, ALU.add, [yaccb, tmpb], [yaccb])

    first_in_bank = {}
    for sb in SBK:
        for r in range(3):
            h = head_of(sb, r)
            ob = OB[0] if h < 7 else OB[1]
            first_in_bank.setdefault(ob, (sb, r))
    first_in_cbank = {}
    for sb in SBK:
        for r in range(3):
            h = head_of(sb, r)
            first_in_cbank.setdefault(CB[h // 5], (sb, r))

    for ti in range(NT):
        tb, tl = ti // 4, ti % 4
        tsl = slice(ti * 128, (ti + 1) * 128)
        rs5 = ti % 5
        rsl = slice(rs5 * 128, (rs5 + 1) * 128)
        rmsnorm_block(c, c.xT[:, :, tsl], [c.xb[dc][tb] for dc in range(8)], 128, g_ap, sq, sqb, rstd, rstdb, hblk, hblkb)

        def proj(out_ap, c0, c1, pbufs):
            for dc in range(8):
                MM(c, out_ap, hblk[:, dc, :], w_in[:, dc, c0:c1], dc == 0, dc == 7, [hblkb[dc], w_inb], pbufs)

        Pq, Pqb = ps2(c)
        proj(Pq[:, 0:512], 0, 512, [Pqb[0]])
        proj(Pq[:, 512:768], 512, 768, [Pqb[1]])
        norm_rope(c, B, Pq[:, 0:768].rearrange("p (h d) -> p h d", d=64), Pqb, 12, gq, ti,
                  q_tok.rearrange("p (h d) -> p h d", d=64), [q_tokb])
        masked_T(c, q_tok, [q_tokb], 6, qAB[0], qAB[1], [qABb])
        Pk, Pkb = ps2(c)
        proj(Pk[:, 0:256], 1280, 1536, [Pkb[0]])
        proj(Pk[:, 512:768], 1792, 2048, [Pkb[1]])
        norm_rope(c, B, Pk[:, 0:256].rearrange("p (h d) -> p h d", d=64), [Pkb[0]], 4, gks, ti,
                  k_tok[:, 0, :].rearrange("p (h d) -> p h d", d=64), [k_tokb])
        norm_rope(c, B, Pk[:, 512:768].rearrange("p (h d) -> p h d", d=64), [Pkb[1]], 4, gkw, ti,
                  k_tok[:, 1, :].rearrange("p (h d) -> p h d", d=64), [k_tokb])
        pt, ptb = ps1(c)
        ptv = pt.bitcast(BF16)
        for n_ in range(4):
            TR(c, ptv[:, n_ * 128:(n_ + 1) * 128], k_tok[:, n_ // 2, (n_ % 2) * 128:(n_ % 2 + 1) * 128], [k_tokb], ptb)
        CP(c, "act", ksT[:, :, tsl], ptv[:, 0:256].rearrange("p (c t) -> p c t", c=2), ptb, [cacheb])
        CP(c, "act", kwT[:, :, rsl], ptv[:, 256:512].rearrange("p (c t) -> p c t", c=2), ptb, [cacheb])
        Pv, Pvb = ps2(c)
        proj(Pv[:, 0:256], 1536, 1792, [Pvb[0]])
        proj(Pv[:, 512:768], 2048, 2304, [Pvb[1]])
        CP(c, "act", vs1[:, ti, :, 0:64], Pv[:, 0:256].rearrange("p (g d) -> p g d", d=64), [Pvb[0]], [cacheb])
        CP(c, "act", vw1[:, rs5, :, 0:64], Pv[:, 512:768].rearrange("p (g d) -> p g d", d=64), [Pvb[1]], [cacheb])
        Pc, Pcb_ = ps1(c)
        for n_ in range(4):
            for dc in range(8):
                MM(c, Pc[:, n_ * 128:(n_ + 1) * 128], w_in[:, dc, 768 + n_ * 128:768 + (n_ + 1) * 128], hblk[:, dc, :],
                   n_ == 0 and dc == 0, dc == 7, [hblkb[dc], w_inb], Pcb_)
        Pc4 = Pc.rearrange("p (n t) -> p n t", n=4)
        CP(c, "act", kvcT[0][0:64, :, 16:144], Pc4[0:64], Pcb_, [kvcTb])
        CP(c, "act", kvcT[1][64:128, :, 16:144], Pc4[64:128], Pcb_, [kvcTb])
        Pg, Pgb = ps1(c)
        proj(Pg[:, 0:36], 2304, 2340, Pgb)
        proj(Pg[:, 128:384], 2340, 2596, Pgb)
        ACTF(c, gate, Pg[:, 0:36], AF.Exp, Pgb, [gateb], scale=-1.0)
        TS(c, "dve", gate, gate, 1.0, None, ALU.add, None, [gateb], [gateb])
        RCP(c, gate, gate, [gateb], [gateb])
        head_norm(c, Pg[:, 128:384].rearrange("p (h d) -> p h d", d=64), Pgb, 4,
                  c.pp[:, W.pbase + PP_MQN:W.pbase + PP_MQN + 64],
                  W.qm_tok.rearrange("p (h d) -> p h d", d=64), [W.qm_tokb], W.hscr, W.hscrb, W.hss, W.hssb)
        nb = 7 if ti == 0 else 8
        n0 = 0 if ti == 0 else 8 * ti - 1
        off = 16 if ti == 0 else 0
        Pn, Pnb = ps1(c)
        first = True
        for kind in range(2):
            for g in range(4):
                par, ch = g % 2, g // 2
                for l_ in range(32):
                    lhs = kvcT[par][:, kind * 2 + ch, off + l_:off + l_ + 16 * (nb - 1) + 1:16]
                    MM(c, Pn[0:nb, (kind * 4 + g) * 64:(kind * 4 + g + 1) * 64], lhs, wdup[kind][:, l_, :], first,
                       l_ == 31, [kvcTb, wdupb], Pnb)
                    first = False
        for a in range(2):
            hs = slice(a * 64, (a + 1) * 64)
            CP(c, "pool", kvcT[a][hs, :, 0:16], kvcT[a][hs, :, 128:144], [kvcTb], [kvcTb])
        cn = tmp[0:nb, 0:512]
        TT(c, "dve", cn, Pn[0:nb, :], cbias[0:nb, :], ALU.add, Pnb + [cbiasb], [tmpb])
        cs_, csb_ = cse[ti % 2], cseb[ti % 2]
        S.dma("sp", f"cse{ti % 2}", lambda e, cs_=cs_, ti=ti: e.dma_start(out=cs_[0:8, :], in_=c.c2["rope"][ti, :, :]),
              writes=[csb_])
        norm_rope(c, B, cn[:, 0:256].rearrange("p (h d) -> p h d", d=64), [tmpb], 4, gkc, ti,
                  cnew_tok[0:nb, 0:256].rearrange("p (h d) -> p h d", d=64), [cnew_tokb],
                  pos_cos=cs_[0:nb, 0:32], pos_sin=cs_[0:nb, 32:64], pos_bufs=[csb_])
        CP(c, "pool", cnew_tok[0:nb, 256:512], cn[:, 256:512], [tmpb], [cnew_tokb])
        pt2, pt2b = ps1(c)
        pt2v = pt2.bitcast(BF16)
        for ch in range(2):
            TR(c, pt2v[:, ch * 8:ch * 8 + nb], cnew_tok[0:nb, ch * 128:(ch + 1) * 128], [cnew_tokb], pt2b)
        for ch in range(2):
            CP(c, "act", k_cmpT[:, ch, n0:n0 + nb], pt2v[:, ch * 8:ch * 8 + nb], pt2b, [cmpb])
        Psc, Pscb = ps1(c)
        MM(c, Psc[:, 0:256], shiftI[0:nb, 128 - n0:256 - n0], cnew_tok[0:nb, 256:512], True, True, [ncb, cnew_tokb], Pscb)
        TT(c, "dve", v_cmpx[:, :, 0:64], Psc[:, 0:256].rearrange("p (g d) -> p g d", d=64), v_cmpx[:, :, 0:64], ALU.add,
           Pscb + [cmpb], [cmpb])
        S.barrier()
        cm_l = cmask_bf[:, 128 - 8 * ti:128 - 8 * ti + NSA_NCMP]
        for sb in SBK:
            par, kc_ = sb // 2, sb % 2
            pb_ = bank(c, sb)
            MM(c, pb_[0:NSA_NCMP, 0:384], k_cmpT[:, kc_, 0:NSA_NCMP], qAB[par][:, 3 * kc_:3 * kc_ + 3, :], True, False,
               [cmpb, qABb], [c.psb[sb]])
            MM(c, pb_[0:NSA_NCMP, 0:384], cm_l, mi3, False, True, [ncb, c.constb], [c.psb[sb]])
        for pr in range(2):
            src = c.pst[pr][0:NSA_NCMP, :].rearrange("p (b x) -> p b x", b=2)[:, :, 0:384]
            dst = PTc[0:NSA_NCMP, pr * 768:(pr + 1) * 768].rearrange("p (b x) -> p b x", b=2)
            ACTF(c, dst, src, AF.Exp, [c.psb[2 * pr], c.psb[2 * pr + 1]], [PTcb[2 * pr], PTcb[2 * pr + 1]], scale=0.125,
                 bias=-MASKM / 8)
        for sb in SBK:
            for r in range(3):
                h = head_of(sb, r)
                cb_ = CB[h // 5]
                oc = (h % 5) * 97
                MM(c, bank(c, cb_)[:, oc:oc + 97], PTc[0:NSA_NCMP, sb * 384 + r * 128:sb * 384 + (r + 1) * 128],
                   v_cmpx[0:NSA_NCMP, h // 3, :], (sb, r) == first_in_cbank[cb_], True, [PTcb[sb], cmpb], [c.psb[cb_]])
        cviews = [bank(c, CB[0])[:, 0:485].rearrange("p (h e) -> p h e", e=97),
                  bank(c, CB[1])[:, 0:485].rearrange("p (h e) -> p h e", e=97),
                  bank(c, CB[2])[:, 0:194].rearrange("p (h e) -> p h e", e=97)]
        hr = [(0, 5), (5, 10), (10, 12)]
        for k_, (h0, h1) in enumerate(hr):
            TS(c, "dve", sm[:, 0, h0:h1].unsqueeze(2), cviews[k_][:, :, 64:65], 1e-30, None, ALU.max, None,
               [c.psb[CB[k_]]], [smb])
        RCP(c, sm[:, 0, :], sm[:, 0, :], [smb], [smb])
        TT(c, "dve", sm[:, 1, :], sm[:, 0, :], gate.rearrange("p (h b) -> p h b", b=3)[:, :, 0], ALU.mult, [smb, gateb], [smb])
        y3 = yacc.rearrange("p (h d) -> p h d", d=64)
        for k_, (h0, h1) in enumerate(hr):
            TT(c, "dve", y3[:, h0:h1, :], cviews[k_][:, :, 0:64], sm[:, 1, h0:h1].unsqueeze(2).broadcast_to([128, h1 - h0, 64]),
               ALU.mult, [c.psb[CB[k_]], smb], [yaccb])
            TT(c, "dve", imp[:, h0:h1, :], cviews[k_][:, :, 65:97], sm[:, 0, h0:h1].unsqueeze(2).broadcast_to([128, h1 - h0, 32]),
               ALU.mult, [c.psb[CB[k_]], smb], [impb])
        imp4 = imp.rearrange("p (g r) j -> p g r j", r=3)
        TT(c, "dve", impm, imp4[:, :, 0, :], imp4[:, :, 1, :], ALU.add, [impb], [impmb])
        TT(c, "dve", impm, impm, imp4[:, :, 2, :], ALU.add, [impb, impmb], [impmb])
        czs = czca[:, 32 - 2 * ti:64 - 2 * ti].unsqueeze(1).broadcast_to([128, 4, 32])
        cas = czca[:, 64 + 32 - 2 * ti:64 + 64 - 2 * ti].unsqueeze(1).broadcast_to([128, 4, 32])
        TT(c, "dve", impm, impm, czs, ALU.mult, [impmb, ncb], [impmb])
        TT(c, "dve", impm, impm, cas, ALU.add, [impmb, ncb], [impmb])
        MSET(c, "dve", impm[:, :, 0:1], 2e9, [impmb])
        for g in range(4):
            S.op("dve", lambda e, g=g: e.max(out=m8[:, 0, :], in_=impm[:, g, :]), [impmb], [m8b])
            S.op("dve", lambda e, g=g: e.match_replace(out=wk16[:, g, :], in_to_replace=m8[:, 0, :], in_values=impm[:, g, :],
                                                       imm_value=-3e38), [impmb, m8b], [impmb])
            S.op("dve", lambda e, g=g: e.max(out=m8[:, 1, :], in_=wk16[:, g, :]), [impmb], [m8b])
            CP(c, "dve", thr[:, g, :], m8[:, 1, 7:8], [m8b], [thrb])
        TT(c, "dve", blk, impm, thr.broadcast_to([128, 4, 32]), ALU.is_ge, [impmb, thrb], [blkb])
        def slc_mask(j, ti=ti):
            src = blk[:, :, 2 * j:2 * j + 2].unsqueeze(3).broadcast_to([128, 4, 2, 64])
            dst = seltile.rearrange("p g (b s) -> p g b s", b=2)
            if j == ti:
                TT(c, "pool", dst, src, tril_bf.rearrange("p (b s) -> p b s", b=2).unsqueeze(1).broadcast_to([128, 4, 2, 64]),
                   ALU.mult, [blkb, ncb], [seltileb])
            else:
                CP(c, "pool", dst, src, [blkb], [seltileb])
            return {g: (seltile[:, g, :], [seltileb]) for g in range(4)}

        branch(ti, list(range(ti + 1)), ksT, vs1, lambda j: slice(j * 128, (j + 1) * 128), lambda j: j, slc_mask, 1)

        def win_mask(j, ti=ti):
            if j == ti:
                return {g: (tril_bf, [ncb]) for g in range(4)}
            if j == ti - 4:
                return {g: (far_bf, [ncb]) for g in range(4)}
            return None

        branch(ti, list(range(max(0, ti - 4), ti + 1)), kwT, vw1,
               lambda j: slice((j % 5) * 128, (j % 5 + 1) * 128), lambda j: j % 5, win_mask, 2)
        CP(c, "pool", W.y_tok[:, 0:768], yacc, [yaccb], [W.y_tokb])
        tile_tail_b(c, W, tl)
        if tl == 3:
            wout_block_stream(c, W, tb, wout_d, wo, wob, wo_n)
        S.barrier()


def wout_block_stream(c, W, tb, wout_d, wo, wob, wo_n):
    ts = slice(tb * 512, (tb + 1) * 512)
    wv = wout_d.rearrange("(cc p) d -> p cc d", p=128)
    for dc in range(8):
        s = wo_n[0] % 2
        wo_n[0] += 1
        c.S.dma("pool", f"wo{s}", lambda e, s=s, dc=dc: e.dma_start(out=wo[s][:, :, :], in_=wv[:, :, dc * 128:(dc + 1) * 128]),
                writes=[wob[s]])
        pw, pwb = ps1(c)
        for cc in range(8):
            MM(c, pw, wo[s][:, cc, :], W.yT[:, cc, :], cc == 0, cc == 7, [wob[s], W.yTb], pwb, skip=False)
        TT(c, "dve", c.xT[:, dc, ts], pw, c.xT[:, dc, ts], ALU.add, list(pwb) + [c.xb[dc][tb]], [c.xb[dc][tb]])


def build_program(cfg):
    nc = bass.Bass("TRN2", target_bir_lowering=False)
    stack = ExitStack()
    c = Ctx()
    c.nc = nc
    c.S = S = Sched()
    c.debug = set(cfg.get("debug", ()))
    c.dbg_done = set()
    c.seq = 0

    def din(name, shape):
        return nc.dram_tensor(name, list(shape), F32, kind="ExternalInput").ap()

    stages = cfg["stages"]
    xT_d = din("xT", (SEQ_PER_CORE, D, L))
    memT_d = din("memT", (SEQ_PER_CORE, D, NMEM))
    pp_d = din("pp", (128, NPP))
    cst_d = din("cst", (128, NCST))
    wd = {}
    for st in stages:
        if st[0] == "ffn":
            _, l, h = st
            wd[("wg", l, h)] = din(f"wg_{l}_{h}", (D, DFF))
            wd[("wu", l, h)] = din(f"wu_{l}_{h}", (D, DFF))
            wd[("wd", l, h)] = din(f"wd_{l}_{h}", (DFF, D))
        else:
            _, l = st
            kind = l % 3
            ncol = {0: 2560, 1: 1736, 2: 2596}[kind]
            wd[("win", l)] = din(f"win_{l}", (D, ncol))
            wd[("wout", l)] = din(f"wout_{l}", (D, D))
            wd[("wkv", l)] = din(f"wkv_{l}", (D, 512))
            if kind == 2:
                wd[("cw", l)] = din(f"cw_{l}", (2, 32, 64, 64))
                c.c2 = {"masks": din("c2_masks", (128, 768)), "czca": din("c2_czca", (128, 128)),
                        "cover": din("c2_cover", (128, 32)), "rope": din("c2_rope", (16, 8, 64))}
    out_d = nc.dram_tensor("outT", [SEQ_PER_CORE, D, L], F32, kind="ExternalOutput").ap()

    TOTAL = 207 * 1024 + 512
    c.arena = nc.alloc_sbuf_tensor("arena", [128, TOTAL], U8)
    R0 = Region(c.arena, 0, TOTAL)
    c.xT = R0.take((8, L), F32)
    c.xb = bufs("x", 8, NTB)
    c.h_off = R0.off
    c.hT = R0.take((8, L), BF16)
    c.hb = bufs("h", 8, NTB)
    c.cst = R0.take((NCST,), F32)
    c.pp = R0.take((NPP,), F32)
    c.ident_bf = R0.take((128,), BF16)
    c.ones_bf = R0.take((128,), BF16)
    c.mi_bf = R0.take((128,), BF16)
    c.constb = Buf("const")
    c.ppb = Buf("pp")
    c.phase_off = R0.off
    c.phase_size = TOTAL - R0.off
    c.tri_f = c.cst[:, CST_TRI:CST_TRI + 128]
    c.cos = c.cst[:, CST_COS:CST_COS + 512].rearrange("p (a b) -> p a b", a=16)
    c.sin = c.cst[:, CST_SIN:CST_SIN + 512].rearrange("p (a b) -> p a b", a=16)
    c.zk = c.cst[:, CST_ZK:CST_ZK + 6]
    c.epsx = c.cst[:, CST_EPSX:CST_EPSX + 6]
    c.cd = c.cst[:, CST_CD:CST_CD + 3]
    c.cneg = c.cst[:, CST_CNEG:CST_CNEG + 128]
    c.bisc = c.cst[:, CST_BISC:CST_BISC + DSA_BIS]
    c.pst = [nc.alloc_psum_tensor(f"ps{i}", [128, 1024], F32) for i in range(4)]
    c.psb = [Buf(f"ps{i}", excl=True) for i in range(8)]
    c.psp = 0
    c.psp2 = 0
    set_rot(c, range(8), (0, 2, 4, 6))

    S.dma("sp", "misc", lambda e: e.dma_start(out=c.cst[:, :], in_=cst_d[:, :]), writes=[c.constb])
    S.dma("sp", "misc", lambda e: e.dma_start(out=c.pp[:, :], in_=pp_d[:, :]), writes=[c.ppb])
    S.seal("misc", [c.constb, c.ppb])
    S.op("dve", lambda e: e.memset(c.ones_bf[:, :], 1.0), writes=[c.constb])
    S.op("dve", lambda e: e.tensor_copy(out=c.ident_bf[:, :], in_=c.cst[:, CST_IDENT:CST_IDENT + 128]),
         reads=[c.constb], writes=[c.constb])
    S.op("dve", lambda e: e.tensor_scalar(out=c.mi_bf[:, :], in0=c.cst[:, CST_IDENT:CST_IDENT + 128], scalar1=MASKM,
                                          scalar2=None, op0=ALU.mult), reads=[c.constb], writes=[c.constb])

    for s in range(SEQ_PER_CORE):
        c.seq = s
        xv = xT_d[s].rearrange("(dc p) t -> p dc t", p=128)
        for dc in range(8):
            S.dma("sp", "xin", lambda e, dc=dc, xv=xv: e.dma_start(out=c.xT[:, dc, :], in_=xv[:, dc, :]),
                  writes=c.xb[dc])
        S.seal("xin", [b for r in c.xb for b in r])
        for st in stages:
            if st[0] == "ffn":
                _, l, h = st
                ffn(c, wd[("wg", l, h)], wd[("wu", l, h)], wd[("wd", l, h)], c.pp[:, (l * 2 + h) * 8:(l * 2 + h) * 8 + 8])
            else:
                _, l = st
                kind = l % 3
                if kind == 0:
                    mixer_ret(c, l, wd[("win", l)], wd[("wout", l)], wd[("wkv", l)], memT_d[s])
                elif kind == 1:
                    mixer_dsa(c, l, wd[("win", l)], wd[("wout", l)], wd[("wkv", l)], memT_d[s])
                else:
                    mixer_nsa(c, l, wd[("win", l)], wd[("wout", l)], wd[("wkv", l)], memT_d[s], wd[("cw", l)])
        ov = out_d[s].rearrange("(dc p) t -> p dc t", p=128)
        for dc in range(8):
            S.dma("sp", "xout", lambda e, dc=dc, ov=ov: e.dma_start(out=ov[:, dc, :], in_=c.xT[:, dc, :]),
                  reads=c.xb[dc])
        S.seal("xout", [b for r in c.xb for b in r])
    S.finish("sp")
    S.emit(nc, stack)
    stack.close()
    return nc


def dbg_dump(c, name, ap, rbufs, seq):
    if name not in c.debug or seq != 0 or name in c.dbg_done:
        return
    c.dbg_done.add(name)
    n = ap.shape[1]
    d = c.nc.dram_tensor("dbg_" + name, [128, n], F32, kind="ExternalOutput").ap()
    c.S.dma("sp", "dbg", lambda e: e.dma_start(out=d[:, :], in_=ap), reads=rbufs)


def default_stages():
    st = []
    for l in range(DEPTH):
        st += [("ffn", l, 0), ("mix", l), ("ffn", l, 1)]
    return st


def kernel(**inputs):
    cfg = inputs.pop("_cfg", None) or {"stages": default_stages()}
    if os.environ.get("MK_STAGES"):
        cfg = {"stages": [tuple(int(v) if v.isdigit() else v for v in t.split(".")) for t in os.environ["MK_STAGES"].split(",")]}
    f32 = lambda a: np.ascontiguousarray(np.asarray(a, dtype=np.float32))
    x = f32(inputs["x"])
    xT = np.ascontiguousarray(x.transpose(0, 2, 1))
    memT = np.ascontiguousarray(f32(inputs["mem"]).transpose(0, 2, 1))
    nc = build_program(cfg)
    shared = {"pp": host_params(inputs), "cst": host_consts()}
    for st in cfg["stages"]:
        if st[0] == "ffn":
            _, l, h = st
            shared[f"wg_{l}_{h}"] = f32(inputs["ffn_w_gate"][l, h])
            shared[f"wu_{l}_{h}"] = f32(inputs["ffn_w_up"][l, h])
            shared[f"wd_{l}_{h}"] = f32(inputs["ffn_w_down"][l, h])
        else:
            _, l = st
            kind, j = l % 3, l // 3
            name = {0: "ret_w_in", 1: "dsa_w_in", 2: "nsa_w_in"}[kind]
            shared[f"win_{l}"] = f32(inputs[name][j])
            shared[f"wout_{l}"] = f32(inputs["w_out"][l])
            shared[f"wkv_{l}"] = f32(inputs["mem_w_kv"][l])
            if kind == 2:
                shared[f"cw_{l}"] = np.ascontiguousarray(np.stack([f32(inputs["nsa_cmp_wk"][j]), f32(inputs["nsa_cmp_wv"][j])]))
                shared.update(host_consts2())
    in_maps = []
    for i in range(NCORES):
        m = dict(shared)
        m["xT"] = xT[i * SEQ_PER_CORE:(i + 1) * SEQ_PER_CORE]
        m["memT"] = memT[i * SEQ_PER_CORE:(i + 1) * SEQ_PER_CORE]
        in_maps.append(m)
    res = run_bass_kernel_spmd(nc, in_maps, core_ids=list(range(NCORES)))
    outT = np.concatenate([r["outT"] for r in res.results], axis=0)
    if cfg.get("debug"):
        kernel.dbg = {k: v for k, v in res.results[0].items() if k.startswith("dbg_")}
    return np.ascontiguousarray(outT.transpose(0, 2, 1))
```

```python
import os
import numpy as np
from contextlib import ExitStack
import concourse.bass as bass
import concourse.mybir as mybir
from concourse.bass_utils import run_bass_kernel_spmd

F32 = mybir.dt.float32
BF16 = mybir.dt.bfloat16
U8 = mybir.dt.uint8
AF = mybir.ActivationFunctionType
ALU = mybir.AluOpType
AX = mybir.AxisListType

D = 1024
L = 2048
DEPTH = 4
DFF = 2816
NMEM = 256
NCORES = 8
SEQ_PER_CORE = 2
EPS = 1e-6
NT = L // 128

ENGS = ("pe", "act", "dve", "pool", "sp")


class Buf:
    __slots__ = ("name", "w", "r", "excl")

    def __init__(self, name, excl=False):
        self.name = name
        self.excl = excl
        self.w = None
        self.r = {}


def bufs(name, *dims):
    if len(dims) == 1:
        return [Buf(f"{name}{i}") for i in range(dims[0])]
    return [bufs(f"{name}{i}_", *dims[1:]) for i in range(dims[0])]


class Sched:
    def __init__(self):
        self.ops = {e: [] for e in ENGS}
        self.seen = {e: {} for e in ENGS}
        self.dma_count = {}
        self.needed = set()
        self.cap = None

    def _deps(self, eng, reads, writes):
        deps = {}
        for b in reads:
            if b.w is not None:
                deps[b.w[:2]] = max(deps.get(b.w[:2], -1), b.w[2])
            if b.excl:
                for k in b.r.values():
                    if not (k[0] == "eng" and k[1] == eng):
                        deps[k[:2]] = max(deps.get(k[:2], -1), k[2])
        for b in writes:
            if b.w is not None and not (b.w[0] == "eng" and b.w[1] == eng):
                deps[b.w[:2]] = max(deps.get(b.w[:2], -1), b.w[2])
            for k in b.r.values():
                if not (k[0] == "eng" and k[1] == eng):
                    deps[k[:2]] = max(deps.get(k[:2], -1), k[2])
        return self._waits(eng, deps)

    def _waits(self, eng, deps):
        waits = []
        seen = self.seen[eng]
        for k, idx in deps.items():
            if seen.get(k, -1) >= idx:
                continue
            seen[k] = idx
            waits.append((k, idx))
            if k[0] == "eng":
                self.needed.add((k[1], idx))
        return waits

    def capture(self, fn):
        assert self.cap is None
        self.cap = []
        fn()
        rec, self.cap = self.cap, None
        return rec

    def commit_interleaved(self, *lists):
        lists = [l for l in lists if l]
        pos = [0] * len(lists)
        total = sum(len(l) for l in lists)
        for _ in range(total):
            k = min((i for i in range(len(lists)) if pos[i] < len(lists[i])),
                    key=lambda i: (pos[i] + 1) / len(lists[i]))
            kind, args = lists[k][pos[k]]
            pos[k] += 1
            (self.op if kind == "op" else self.dma)(*args)

    def op(self, eng, emit, reads=(), writes=()):
        if self.cap is not None:
            self.cap.append(("op", (eng, emit, tuple(reads), tuple(writes))))
            return None
        waits = self._deps(eng, reads, writes)
        idx = len(self.ops[eng])
        key = ("eng", eng, idx)
        self.ops[eng].append(("op", waits, emit, None))
        for b in reads:
            b.r[("eng", eng)] = key
        for b in writes:
            b.w = key
            b.r = {}
        return key

    def dma(self, queue, sem, emit, reads=(), writes=()):
        if self.cap is not None:
            self.cap.append(("dma", (queue, sem, emit, tuple(reads), tuple(writes))))
            return None
        waits = self._deps(queue, reads, writes)
        c = self.dma_count.get(sem, 0) + 1
        self.dma_count[sem] = c
        key = ("dma", sem, c)
        self.ops[queue].append(("dma", waits, emit, sem))
        for b in reads:
            b.r[("dma", sem)] = key
        for b in writes:
            b.w = key
            b.r = {}
        return key

    def seal(self, sem, bl):
        c = self.dma_count[sem]
        for b in bl:
            if b.w is not None and b.w[0] == "dma" and b.w[1] == sem:
                b.w = ("dma", sem, c)
            k = b.r.get(("dma", sem))
            if k is not None:
                b.r[("dma", sem)] = ("dma", sem, c)

    def barrier(self):
        last = {}
        for e in ENGS:
            idx = None
            for i in range(len(self.ops[e]) - 1, -1, -1):
                if self.ops[e][i][0] == "op":
                    idx = i
                    break
            if idx is not None:
                last[("eng", e)] = idx
        for s, cnt in self.dma_count.items():
            last[("dma", s)] = cnt
        for e in ENGS:
            deps = {k: v for k, v in last.items() if k != ("eng", e)}
            waits = self._waits(e, deps)
            if waits:
                self.ops[e].append(("fin", waits, None, None))

    def finish(self, eng="sp"):
        waits = []
        for sem, c in self.dma_count.items():
            if self.seen[eng].get(("dma", sem), -1) < c:
                waits.append((("dma", sem), c))
        self.ops[eng].append(("fin", waits, None, None))

    def emit(self, nc, stack):
        esem = {e: stack.enter_context(nc.semaphore(f"s_{e}")) for e in ENGS}
        dsem = {s: stack.enter_context(nc.semaphore(f"d_{s}")) for s in self.dma_count}
        val = {}
        for e in ENGS:
            n = 0
            for i in range(len(self.ops[e])):
                if (e, i) in self.needed:
                    n += 1
                    val[(e, i)] = n
        ops = self.ops

        def run(e, engine):
            for i, (kind, waits, emit, sem) in enumerate(ops[e]):
                for k, idx in waits:
                    if k[0] == "eng":
                        engine.wait_ge(esem[k[1]], val[(k[1], idx)])
                    else:
                        engine.wait_ge(dsem[k[1]], 16 * idx)
                if kind == "fin":
                    continue
                ins = emit(engine)
                if kind == "dma":
                    ins.then_inc(dsem[sem], 16)
                elif (e, i) in self.needed:
                    ins.then_inc(esem[e], 1)

        with nc.Block() as block:
            @block.sync
            def _(eng):
                run("sp", eng)

            @block.scalar
            def _(eng):
                run("act", eng)

            @block.vector
            def _(eng):
                run("dve", eng)

            @block.gpsimd
            def _(eng):
                run("pool", eng)

            @block.tensor
            def _(eng):
                run("pe", eng)


class Region:
    def __init__(self, t, off, size):
        self.t = t
        self.base = off
        self.off = off
        self.end = off + size

    def take(self, shape, dtype, parts=None):
        esz = 2 if dtype == BF16 else 4
        n = int(np.prod(shape))
        nb = ((n * esz + 31) // 32) * 32
        assert self.off + nb <= self.end, ("SBUF region overflow", shape, self.off, self.end)
        ap = self.t[:, self.off:self.off + n * esz].bitcast(dtype)
        self.off += nb
        if len(shape) > 1:
            names = " ".join(f"d{i}" for i in range(len(shape)))
            kw = {f"d{i}": int(s) for i, s in enumerate(shape)}
            ap = ap.rearrange(f"p ({names}) -> p {names}", **kw)
        return ap


class Ctx:
    pass


def MM(c, out, lhsT, rhs, start, stop, reads, writes, skip=True):
    c.S.op("pe", lambda e: e.matmul(out, lhsT, rhs, start=start, stop=stop, skip_group_check=skip), reads, writes)


def TR(c, out, in_, reads, writes):
    n = in_.shape[0]
    c.S.op("pe", lambda e: e.transpose(out, in_, c.ident_bf[0:n, 0:n]), list(reads) + [c.constb], writes)


def ACTF(c, out, in_, func, reads, writes, scale=1.0, bias=0.0):
    c.S.op("act", lambda e: e.activation(out=out, in_=in_, func=func, bias=bias, scale=scale), reads, writes)


def TT(c, eng, out, in0, in1, op, reads, writes):
    c.S.op(eng, lambda e: e.tensor_tensor(out=out, in0=in0, in1=in1, op=op), reads, writes)


def TS(c, eng, out, in0, s1, s2, op0, op1, reads, writes):
    if s2 is None:
        c.S.op(eng, lambda e: e.tensor_scalar(out=out, in0=in0, scalar1=s1, scalar2=None, op0=op0), reads, writes)
    else:
        c.S.op(eng, lambda e: e.tensor_scalar(out=out, in0=in0, scalar1=s1, scalar2=s2, op0=op0, op1=op1), reads, writes)


def STT(c, out, in0, scalar, in1, op0, op1, reads, writes):
    c.S.op("dve", lambda e: e.scalar_tensor_tensor(out=out, in0=in0, scalar=scalar, in1=in1, op0=op0, op1=op1),
           reads, writes)


def RED(c, out, in_, op, reads, writes):
    c.S.op("dve", lambda e: e.tensor_reduce(out=out, in_=in_, axis=AX.X, op=op), reads, writes)


def RCP(c, out, in_, reads, writes):
    c.S.op("dve", lambda e: e.reciprocal(out=out, in_=in_), reads, writes)


def CP(c, eng, out, in_, reads, writes):
    if eng == "act":
        c.S.op("act", lambda e: e.copy(out=out, in_=in_), reads, writes)
    else:
        c.S.op(eng, lambda e: e.tensor_copy(out=out, in_=in_), reads, writes)


def MSET(c, eng, ap, val, writes):
    c.S.op(eng, lambda e: e.memset(ap, val), (), writes)


def bank(c, b):
    return c.pst[b // 2][:, (b % 2) * 512:(b % 2) * 512 + 512]


def ps1(c):
    b = c.rot1[c.psp % len(c.rot1)]
    c.psp += 1
    return bank(c, b), [c.psb[b]]


def ps2(c):
    b = c.rot2[c.psp2 % len(c.rot2)]
    c.psp2 += 1
    return c.pst[b // 2][:, :], [c.psb[b], c.psb[b + 1]]


def set_rot(c, rot1, rot2):
    c.rot1, c.rot2 = list(rot1), list(rot2)


CST_IDENT, CST_TRI, CST_COS, CST_SIN, CST_ZK, CST_EPSX, CST_CD = 0, 128, 256, 768, 1280, 1286, 1292
CST_CNEG = 1296
CST_BISC = 1296 + 128
NCST = 1296 + 128 + 32
MASKM = 29952.0
PP_L0 = 64
PP_PL = 464
PP_MIX, PP_MEMN, PP_MQN, PP_MKN, PP_A, PP_B, PP_C, PP_D, PP_POS = 0, 8, 16, 80, 144, 208, 272, 336, 400
NPP = PP_L0 + DEPTH * PP_PL


def host_consts():
    cst = np.zeros((128, NCST), np.float64)
    cst[:, CST_IDENT:CST_IDENT + 128] = np.eye(128)
    j = np.arange(128)[:, None]
    i = np.arange(128)[None, :]
    cst[:, CST_TRI:CST_TRI + 128] = (i >= j)
    cst[:, CST_CNEG:CST_CNEG + 128] = np.where(i <= j, 0.0, -1e30)
    cst[:, CST_BISC:CST_BISC + 32] = 2.0 ** (-np.arange(32))[None, :]
    inv = (np.float32(10000.0) ** (-np.arange(32, dtype=np.float32) / np.float32(32))).astype(np.float32)
    pos = (np.arange(16)[None, :, None] * 128 + np.arange(128)[:, None, None]).astype(np.float32)
    ang = (pos * inv[None, None, :]).astype(np.float32)
    cst[:, CST_COS:CST_COS + 512] = np.cos(ang).reshape(128, 512)
    cst[:, CST_SIN:CST_SIN + 512] = np.sin(ang).reshape(128, 512)
    h = np.arange(6)
    gam = 1.0 - 2.0 ** (-5.0 - h)
    t = np.arange(128)[:, None]
    cst[:, CST_ZK:CST_ZK + 6] = gam[None, :] ** (-(t + 1.0)) / 8.0
    cst[:, CST_EPSX:CST_EPSX + 6] = EPS * gam[None, :] ** (-2.0 * (t + 1.0))
    for pair in range(3):
        cst[0:64, CST_CD + pair] = gam[2 * pair] ** 128
        cst[64:128, CST_CD + pair] = gam[2 * pair + 1] ** 128
    return cst.astype(np.float32)


def host_consts2():
    t = np.arange(128)[:, None]
    s_ = np.arange(128)[None, :]
    masks = np.zeros((128, 768), np.float32)
    masks[:, 0:128] = (s_ <= t)
    masks[:, 128:256] = (s_ > t)
    m = np.arange(256)[None, :] - 128
    masks[:, 256:512] = (16 * m + 31 <= t)
    for k in range(8):
        masks[k, 512 + 128 + k] = 1.0
    czca = np.zeros((128, 128), np.float32)
    mm = np.arange(64)[None, :] - 32
    qb = (t >= 64).astype(np.int64)
    causal = mm <= qb
    forced = (mm == qb) | (mm == qb - 1)
    czca[:, 0:64] = causal
    czca[:, 64:128] = np.where(causal, np.where(forced, 1e9, 0.0), -1e30)
    starts = np.arange(127) * 16
    sst = np.arange(32) * 64
    cover = np.zeros((128, 32), np.float32)
    cover[:127] = ((starts[:, None] < sst[None, :] + 64) & (starts[:, None] + 32 > sst[None, :]))
    inv = (np.float32(10000.0) ** (-np.arange(32, dtype=np.float32) / np.float32(32))).astype(np.float32)
    rope = np.zeros((16, 8, 64), np.float32)
    for ti in range(16):
        for k in range(8):
            n = k if ti == 0 else 8 * ti - 1 + k
            pos = np.float32(16 * n + 31)
            ang = (pos * inv).astype(np.float32)
            rope[ti, k, 0:32] = np.cos(ang)
            rope[ti, k, 32:64] = np.sin(ang)
    return {"c2_masks": masks, "c2_czca": czca, "c2_cover": cover, "c2_rope": rope}


def host_params(inp):
    pp = np.zeros((128, NPP), np.float32)
    f32 = lambda a: np.asarray(a, np.float32)
    pp[:, 0:64] = f32(inp["ffn_norm"]).reshape(DEPTH, 2, 8, 128).transpose(3, 0, 1, 2).reshape(128, 64)
    for l in range(DEPTH):
        b = PP_L0 + l * PP_PL
        pp[:, b + PP_MIX:b + PP_MIX + 8] = f32(inp["mix_norm"][l]).reshape(8, 128).T
        pp[:, b + PP_MEMN:b + PP_MEMN + 8] = f32(inp["mem_norm"][l]).reshape(8, 128).T
        pp[:, b + PP_MQN:b + PP_MQN + 64] = f32(inp["mem_qn"][l])[None, :]
        pp[:, b + PP_MKN:b + PP_MKN + 64] = f32(inp["mem_kn"][l])[None, :]
        kind, j = l % 3, l // 3
        if kind == 1:
            pp[:, b + PP_A:b + PP_A + 64] = f32(inp["dsa_qn"][j])[None, :]
            pp[:, b + PP_B:b + PP_B + 64] = f32(inp["dsa_kn"][j])[None, :]
        elif kind == 2:
            pp[:, b + PP_A:b + PP_A + 64] = f32(inp["nsa_qn"][j])[None, :]
            pp[:, b + PP_B:b + PP_B + 64] = f32(inp["nsa_kn"][j][0])[None, :]
            pp[:, b + PP_C:b + PP_C + 64] = f32(inp["nsa_kn"][j][1])[None, :]
            pp[:, b + PP_D:b + PP_D + 64] = f32(inp["nsa_kn"][j][2])[None, :]
            pp[0:64, b + PP_POS:b + PP_POS + 32] = f32(inp["nsa_cmp_pos_k"][j]).T
            pp[0:64, b + PP_POS + 32:b + PP_POS + 64] = f32(inp["nsa_cmp_pos_v"][j]).T
    return pp


def rmsnorm_block(c, src, src_bufs, ntok, g_ap, sq, sqb, rstd, rstdb, out, out_bufs):
    ACTF(c, sq[:, :, 0:ntok], src, AF.Square, src_bufs, [sqb])
    ps, pb = ps1(c)
    for dc in range(8):
        MM(c, ps[:, 0:ntok], c.ones_bf[:, :], sq[:, dc, 0:ntok], dc == 0, dc == 7, [sqb, c.constb], pb, skip=False)
    ACTF(c, rstd[:, 0:ntok], ps[:, 0:ntok], AF.Ln, pb, [rstdb], scale=1.0 / D, bias=float(EPS))
    ACTF(c, rstd[:, 0:ntok], rstd[:, 0:ntok], AF.Exp, [rstdb], [rstdb], scale=-0.5)
    for dc in range(8):
        STT(c, out[:, dc, :], src[:, dc, :], g_ap[:, dc:dc + 1], rstd[:, 0:ntok], ALU.mult, ALU.mult,
            [src_bufs[dc], rstdb, c.ppb], [out_bufs[dc]])


NG = 11
NTB = 4
WSLOTS = 3


def ffn(c, wg_d, wu_d, wd_d, g_ap):
    S = c.S
    S.barrier()
    R = Region(c.arena, c.phase_off, c.phase_size)
    sq = R.take((8, 512), BF16)
    sqb = Buf("sq")
    rstd = R.take((512,), F32)
    rstdb = Buf("rstd")
    sg = R.take((512,), BF16)
    sgb = Buf("sg")
    act = [[R.take((512,), BF16) for _ in range(2)] for _ in range(2)]
    actb = bufs("act", 2, 2)
    wg = [R.take((8, 256), BF16) for _ in range(WSLOTS)]
    wu = [R.take((8, 256), BF16) for _ in range(WSLOTS)]
    wd = [R.take((2, D), BF16) for _ in range(WSLOTS)]
    wgb, wub, wdb = bufs("wg", WSLOTS), bufs("wu", WSLOTS), bufs("wd", WSLOTS)
    ps = [c.pst[b // 2][:, (b % 2) * 512:(b % 2) * 512 + 512] for b in range(8)]

    wg_v = wg_d.rearrange("(dc p) f -> p dc f", p=128)
    wu_v = wu_d.rearrange("(dc p) f -> p dc f", p=128)
    wd_v = wd_d.rearrange("(fc p) d -> p fc d", p=128)
    wn = [0]

    def load_w(gi):
        s = wn[0] % WSLOTS
        wn[0] += 1
        fs = slice(gi * 256, (gi + 1) * 256)
        S.dma("pool", f"wg{s}", lambda e: e.dma_start(out=wg[s][:, :, :], in_=wg_v[:, :, fs]), writes=[wgb[s]])
        S.dma("pool", f"wu{s}", lambda e: e.dma_start(out=wu[s][:, :, :], in_=wu_v[:, :, fs]), writes=[wub[s]])
        S.dma("pool", f"wd{s}", lambda e: e.dma_start(out=wd[s][:, :, :], in_=wd_v[:, 2 * gi:2 * gi + 2, :]),
              writes=[wdb[s]])
        return s

    slots = {}
    for gi in range(min(WSLOTS - 1, NG)):
        slots[gi] = load_w(gi)

    def norm_blk(tb):
        ts = slice(tb * 512, (tb + 1) * 512)
        rmsnorm_block(c, c.xT[:, :, ts], [c.xb[dc][tb] for dc in range(8)], 512, g_ap, sq, sqb, rstd, rstdb,
                      c.hT[:, :, ts], [c.hb[dc][tb] for dc in range(8)])

    norm_blk(0)

    pend = None
    steps = [(gi, tb) for gi in range(NG) for tb in range(NTB)]
    for it in range(len(steps) + 1):
        cur = steps[it] if it < len(steps) else None
        par = it % 2
        gu = []
        if cur is not None:
            gi, tb = cur
            s = slots[gi]
            ts = slice(tb * 512, (tb + 1) * 512)
            for fcl in range(2):
                for which in range(2):
                    w = wg[s] if which == 0 else wu[s]
                    wb = wgb[s] if which == 0 else wub[s]
                    bank = fcl * 2 + which
                    for dc in range(8):
                        gu.append((ps[bank], w[:, dc, fcl * 128:(fcl + 1) * 128], c.hT[:, dc, ts], dc == 0, dc == 7,
                                   [wb, c.hb[dc][tb]], [c.psb[bank]]))
        dn = []
        if pend is not None:
            ps_, ptb, ppar = pend
            pts = slice(ptb * 512, (ptb + 1) * 512)
            for dc in range(8):
                bank = 4 + dc % 4
                for fcl in range(2):
                    dn.append((ps[bank], wd[ps_][:, fcl, dc * 128:(dc + 1) * 128], act[ppar][fcl][:, :], fcl == 0,
                               fcl == 1, [wdb[ps_], actb[ppar][fcl]], [c.psb[bank]],
                               dc if fcl == 1 else None, bank, pts, ptb))
        gi_ = 0
        di_ = 0
        while gi_ < len(gu) or di_ < len(dn):
            for _ in range(4):
                if gi_ < len(gu):
                    o_, l_, r_, st_, sp_, rd_, wr_ = gu[gi_]
                    MM(c, o_, l_, r_, st_, sp_, rd_, wr_, skip=False)
                    gi_ += 1
                    if gi_ % 16 == 0:
                        fcl = gi_ // 16 - 1
                        ACTF(c, sg[:, :], ps[fcl * 2], AF.Silu, [c.psb[fcl * 2]], [sgb])
                        TT(c, "dve", act[par][fcl][:, :], ps[fcl * 2 + 1], sg[:, :], ALU.mult,
                           [c.psb[fcl * 2 + 1], sgb], [actb[par][fcl]])
            for _ in range(2):
                if di_ < len(dn):
                    o_, l_, r_, st_, sp_, rd_, wr_, dc, bank, pts, ptb = dn[di_]
                    MM(c, o_, l_, r_, st_, sp_, rd_, wr_, skip=False)
                    di_ += 1
                    if dc is not None:
                        STT(c, c.xT[:, dc, pts], ps[bank], 0.5, c.xT[:, dc, pts], ALU.mult, ALU.add,
                            [c.psb[bank], c.xb[dc][ptb]], [c.xb[dc][ptb]])
        pend = (slots[cur[0]], cur[1], par) if cur is not None else None
        if cur is not None and cur[0] == 0 and cur[1] + 1 < NTB:
            norm_blk(cur[1] + 1)
        if cur is not None and cur[1] == 0 and cur[0] + WSLOTS - 1 < NG:
            slots[cur[0] + WSLOTS - 1] = load_w(cur[0] + WSLOTS - 1)


def head_norm(c, src, src_bufs, H, gain, out, out_bufs, scr, scrb, ss, ssb, eng2="pool"):
    n = src.shape[0]
    s3 = scr[0:n, 0:H * 64].rearrange("p (h d) -> p h d", d=64)
    ACTF(c, s3, src, AF.Square, src_bufs, [scrb])
    RED(c, ss[0:n, 0:H], s3, ALU.add, [scrb], [ssb])
    ACTF(c, ss[0:n, 0:H], ss[0:n, 0:H], AF.Ln, [ssb], [ssb], scale=1.0 / 64, bias=float(EPS))
    ACTF(c, ss[0:n, 0:H], ss[0:n, 0:H], AF.Exp, [ssb], [ssb], scale=-0.5)
    TT(c, "dve", s3, src, ss[0:n, 0:H].unsqueeze(2).broadcast_to([n, H, 64]), ALU.mult, list(src_bufs) + [ssb], [scrb])
    TT(c, eng2, out, s3, gain[0:n, :].unsqueeze(1).broadcast_to([n, H, 64]), ALU.mult, [scrb, c.ppb], out_bufs)


def mem_prep(c, R, memT_d, wkv_d, pbase, k_memT, v_mem1, kvb):
    S = c.S
    memT = R.take((8, NMEM), F32)
    memb = bufs("memT", 8)
    sqm = R.take((8, NMEM), BF16)
    sqmb = Buf("sqm")
    rstdm = R.take((NMEM,), F32)
    rstdmb = Buf("rstdm")
    memn = R.take((8, NMEM), BF16)
    memnb = bufs("memn", 8)
    wkv = R.take((8, 512), BF16)
    wkvb = Buf("wkv")
    ktok = R.take((256,), BF16)
    ktokb = Buf("ktok")
    scr = R.take((256,), F32)
    scrb = Buf("mscr")
    ss = R.take((8,), F32)
    ssb = Buf("mss")
    mv = memT_d.rearrange("(dc p) m -> p dc m", p=128)
    for dc in range(8):
        S.dma("sp", "memin", lambda e, dc=dc: e.dma_start(out=memT[:, dc, :], in_=mv[:, dc, :]), writes=[memb[dc]])
    S.seal("memin", memb)
    S.dma("pool", "wkv", lambda e: e.dma_start(out=wkv[:, :, :], in_=wkv_d.rearrange("(dc p) f -> p dc f", p=128)),
          writes=[wkvb])
    rmsnorm_block(c, memT, memb, NMEM, c.pp[:, pbase + PP_MEMN:pbase + PP_MEMN + 8], sqm, sqmb, rstdm, rstdmb,
                  memn, memnb)
    MSET(c, "pool", v_mem1[:, :, :, 64:65], 1.0, [kvb])
    for mt in range(2):
        ps, pb = ps1(c)
        for dc in range(8):
            MM(c, ps, memn[:, dc, mt * 128:(mt + 1) * 128], wkv[:, dc, :], dc == 0, dc == 7, [memnb[dc], wkvb], pb,
               skip=False)
        head_norm(c, ps[:, 0:256].rearrange("p (h d) -> p h d", d=64), pb, 4,
                  c.pp[:, pbase + PP_MKN:pbase + PP_MKN + 64], ktok.rearrange("p (h d) -> p h d", d=64), [ktokb],
                  scr, scrb, ss, ssb)
        CP(c, "act", v_mem1[:, mt, :, 0:64], ps[:, 256:512].rearrange("p (h d) -> p h d", d=64), pb, [kvb])
        pt, ptb = ps1(c)
        ptv = pt.bitcast(BF16)
        for cc in range(2):
            TR(c, ptv[:, cc * 128:(cc + 1) * 128], ktok[:, cc * 128:(cc + 1) * 128], [ktokb], ptb)
        CP(c, "dve", k_memT[:, :, mt * 128:(mt + 1) * 128], ptv[:, 0:256].rearrange("p (c m) -> p c m", c=2), ptb, [kvb])


def tile_tail(c, W, qm_ps, qm_pb, tl):
    head_norm(c, qm_ps.rearrange("p (h d) -> p h d", d=64), qm_pb, 4, c.pp[:, W.pbase + PP_MQN:W.pbase + PP_MQN + 64],
              W.qm_tok.rearrange("p (h d) -> p h d", d=64), [W.qm_tokb], W.hscr, W.hscrb, W.hss, W.hssb)
    tile_tail_b(c, W, tl)


def tile_tail_b(c, W, tl):
    pt, ptb = ps1(c)
    ptv = pt.bitcast(BF16)
    for cc in range(2):
        TR(c, ptv[:, cc * 128:(cc + 1) * 128], W.qm_tok[:, cc * 128:(cc + 1) * 128], [W.qm_tokb], ptb)
    CP(c, "act", W.qmT[0][0:64], ptv[0:64, 0:256].rearrange("p (c m) -> p c m", c=2), ptb, [W.qmTb])
    CP(c, "act", W.qmT[1][64:128], ptv[64:128, 0:256].rearrange("p (c m) -> p c m", c=2), ptb, [W.qmTb])
    if c.rot2:
        pm, pmb = ps2(c)
        pms = [pm[:, 0:512], pm[:, 512:1024]]
    for mt in range(2):
        if c.rot2:
            pmt, pmtb = pms[mt], pmb[mt]
        else:
            pmt, pl = ps1(c)
            pmtb = pl[0]
        for h in range(4):
            hp = h % 2
            MM(c, pmt[:, h * 128:(h + 1) * 128], W.k_memT[:, h // 2, mt * 128:(mt + 1) * 128],
               W.qmT[hp][:, h // 2, :], h == 0, True, [W.kvb, W.qmTb], [pmtb])
        ACTF(c, W.PTm[:, mt * 512:(mt + 1) * 512], pmt, AF.Exp, [pmtb], [W.PTmb], scale=0.125)
    po, pob = ps1(c)
    first = True
    for h in range(4):
        for mt in range(2):
            MM(c, po[:, h * 65:(h + 1) * 65], W.PTm[:, (mt * 4 + h) * 128:(mt * 4 + h + 1) * 128], W.v_mem1[:, mt, h, :],
               first, mt == 1 and h == 3, [W.PTmb, W.kvb], pob)
            first = False
    po3 = po[:, 0:260].rearrange("p (h e) -> p h e", e=65)
    RCP(c, W.den4, po3[:, :, 64:65], pob, [W.den4b])
    TT(c, "dve", W.y_tok[:, 768:1024].rearrange("p (h d) -> p h d", d=64), po3[:, :, 0:64],
       W.den4.broadcast_to([128, 4, 64]), ALU.mult, list(pob) + [W.den4b], [W.y_tokb])
    py, pyb = ps1(c)
    pyv = py.bitcast(BF16)
    for cc in range(8):
        TR(c, pyv[:, cc * 128:(cc + 1) * 128], W.y_tok[:, cc * 128:(cc + 1) * 128], [W.y_tokb], pyb)
    CP(c, "act", W.yT[:, :, tl * 128:(tl + 1) * 128], pyv.rearrange("p (c t) -> p c t", c=8), pyb, [W.yTb])


def wout_block(c, W, tb):
    ts = slice(tb * 512, (tb + 1) * 512)
    for dc in range(8):
        pw, pwb = ps1(c)
        for cc in range(8):
            MM(c, pw, W.w_out[:, cc, dc * 128:(dc + 1) * 128], W.yT[:, cc, :], cc == 0, cc == 7, [W.w_outb, W.yTb], pwb,
               skip=False)
        TT(c, "dve", c.xT[:, dc, ts], pw, c.xT[:, dc, ts], ALU.add, list(pwb) + [c.xb[dc][tb]], [c.xb[dc][tb]])


def load_wout(c, W, R, wout_d):
    W.w_out = R.take((8, D), BF16)
    W.w_outb = Buf("w_out")
    wv = wout_d.rearrange("(cc p) d -> p cc d", p=128)
    for cc in range(0, 8, 4):
        c.S.dma("pool", "wout", lambda e, cc=cc: e.dma_start(out=W.w_out[:, cc:cc + 4, :], in_=wv[:, cc:cc + 4, :]),
                writes=[W.w_outb])
    c.S.seal("wout", [W.w_outb])


def tail_bufs(c, W, R, hscr_n=768):
    W.qm_tok = R.take((256,), BF16)
    W.qm_tokb = Buf("qm_tok")
    W.qmT = [R.take((2, 128), BF16) for _ in range(2)]
    W.qmTb = Buf("qmT")
    for a in range(2):
        MSET(c, "pool", W.qmT[a][:, :, :], 0.0, [W.qmTb])
    W.PTm = R.take((1024,), BF16)
    W.PTmb = Buf("PTm")
    W.den4 = R.take((4, 1), F32)
    W.den4b = Buf("den4")
    W.hscr = R.take((hscr_n,), F32)
    W.hscrb = Buf("hscr")
    W.hss = R.take((16,), F32)
    W.hssb = Buf("hss")
    W.y_tok = R.take((1024,), BF16)
    W.y_tokb = Buf("y_tok")
    W.k_memT = R.take((2, NMEM), BF16)
    W.v_mem1 = R.take((2, 4, 65), BF16)
    W.kvb = Buf("memkv")


def mixer_ret(c, l, win_d, wout_d, wkv_d, memT_d):
    S = c.S
    S.barrier()
    set_rot(c, range(8), (0, 2, 4, 6))
    W = Ctx()
    W.pbase = PP_L0 + l * PP_PL
    R1 = Region(c.arena, c.h_off, 32 * 1024)
    R2 = Region(c.arena, c.phase_off, c.phase_size)
    hblk = [R1.take((8, 512), BF16) for _ in range(2)]
    hblkb = bufs("hblk", 2, 8)
    W.yT = R1.take((8, 512), BF16)
    W.yTb = Buf("yT")
    sq = R1.take((8, 512), BF16)
    sqb = Buf("sq")
    w_in = R2.take((8, 2560), BF16)
    w_inb = Buf("w_in")
    load_wout(c, W, R2, wout_d)
    rstd = R2.take((512,), F32)
    rstdb = Buf("rstd")
    tail_bufs(c, W, R2, hscr_n=256)
    tq2 = [R2.take((384,), F32) for _ in range(2)]
    tq = [tq2[0], tq2[1], tq2[0], tq2[1]]
    tqb2 = bufs("tq", 2)
    tqb = [tqb2[0], tqb2[1], tqb2[0], tqb2[1]]
    rk = R2.take((768,), F32)
    rkb = Buf("rk")
    scr = R2.take((768,), F32)
    scrb = Buf("scr")
    state = R2.take((3, 128), F32)
    state_bf = R2.take((3, 128), BF16)
    stb = Buf("state")
    stbb = Buf("state_bf")
    sm = R2.take((8, 6), F32)
    smb = Buf("sm")
    qk_tok = [R2.take((768,), BF16) for _ in range(2)]
    qk_tokb = bufs("qk_tok", 2)
    qT = [[R2.take((3, 128), BF16) for _ in range(2)] for _ in range(2)]
    kT = [R2.take((3, 128), BF16) for _ in range(2)]
    qkTb = bufs("qkT", 2)
    Sm = [R2.take((6, 128), BF16) for _ in range(2)]
    Smb = bufs("Sm", 2)
    v_tok = [R2.take((768,), BF16) for _ in range(2)]
    v_tokb = bufs("v_tok", 2)
    sgt = [R2.take((768,), BF16) for _ in range(2)]
    sgtb = bufs("sgt", 2)
    qm_toks = [W.qm_tok, R2.take((256,), BF16)]
    qm_tokbs = [W.qm_tokb, Buf("qm_tok1")]
    wv = win_d.rearrange("(dc p) f -> p dc f", p=128)
    for dc in range(0, 8, 2):
        S.dma("pool", "win", lambda e, dc=dc: e.dma_start(out=w_in[:, dc:dc + 2, :], in_=wv[:, dc:dc + 2, :]),
              writes=[w_inb])
    S.seal("win", [w_inb])
    Rt = Region(c.arena, c.h_off, 32 * 1024)
    mem_prep(c, Rt, memT_d, wkv_d, W.pbase, W.k_memT, W.v_mem1, W.kvb)
    S.barrier()
    MSET(c, "dve", state[:, :, :], 0.0, [stb])
    MSET(c, "dve", state_bf[:, :, :], 0.0, [stbb])
    for p_ in range(2):
        for a in range(2):
            MSET(c, "pool", qT[p_][a][:, :, :], 0.0, [qkTb[p_]])

    g_ap = c.pp[:, W.pbase + PP_MIX:W.pbase + PP_MIX + 8]
    tri3 = c.tri_f.unsqueeze(1).broadcast_to([128, 6, 128])

    def stage_a(ti):
        set_rot(c, (0, 1, 2, 3), (0, 2))
        tb, tl = ti // 4, ti % 4
        par = ti % 2
        hb, hbb = hblk[tb % 2], hblkb[tb % 2]
        if tl == 0:
            ts = slice(tb * 512, (tb + 1) * 512)
            rmsnorm_block(c, c.xT[:, :, ts], [c.xb[dc][tb] for dc in range(8)], 512, g_ap, sq, sqb, rstd, rstdb, hb, hbb)

        def proj(out_ap, c0, c1, pbufs):
            for dc in range(8):
                MM(c, out_ap, hb[:, dc, tl * 128:(tl + 1) * 128], w_in[:, dc, c0:c1], dc == 0, dc == 7,
                   [hbb[dc], w_inb], pbufs, skip=False)

        P, Pb = ps2(c)
        proj(P[:, 0:384], 0, 384, [Pb[0]])
        proj(P[:, 512:896], 384, 768, [Pb[1]])
        v4 = P.rearrange("p (a b) -> p a b", a=2)[:, :, 0:384].rearrange("p a (h d) -> p a h d", d=64)
        x1, x2 = v4[:, :, :, 0:32], v4[:, :, :, 32:64]
        cosb = c.cos[:, ti, :].unsqueeze(1).unsqueeze(1).broadcast_to([128, 2, 6, 32])
        sinb = c.sin[:, ti, :].unsqueeze(1).unsqueeze(1).broadcast_to([128, 2, 6, 32])
        t4 = [t.rearrange("p (a h d) -> p a h d", a=2, h=6) for t in tq]
        rk4 = rk.rearrange("p (a h d) -> p a h d", a=2, h=6)
        TT(c, "dve", t4[0], x1, cosb, ALU.mult, Pb + [c.constb], [tqb[0]])
        TT(c, "dve", t4[1], x2, sinb, ALU.mult, Pb + [c.constb], [tqb[1]])
        TT(c, "pool", rk4[:, :, :, 0:32], t4[0], t4[1], ALU.subtract, [tqb[0], tqb[1]], [rkb])
        TT(c, "dve", t4[2], x2, cosb, ALU.mult, Pb + [c.constb], [tqb[2]])
        TT(c, "dve", t4[3], x1, sinb, ALU.mult, Pb + [c.constb], [tqb[3]])
        TT(c, "pool", rk4[:, :, :, 32:64], t4[2], t4[3], ALU.add, [tqb[2], tqb[3]], [rkb])
        qkt, qktb = qk_tok[par], qk_tokb[par]
        CP(c, "pool", qkt[:, 0:384], rk[:, 0:384], [rkb], [qktb])
        TT(c, "pool", qkt[:, 384:768].rearrange("p (h d) -> p h d", d=64),
           rk[:, 384:768].rearrange("p (h d) -> p h d", d=64),
           c.zk.unsqueeze(2).broadcast_to([128, 6, 64]), ALU.mult, [rkb, c.constb], [qktb])
        pt, ptb = ps1(c)
        ptv = pt.bitcast(BF16)
        for n in range(6):
            TR(c, ptv[:, n * 128:(n + 1) * 128], qkt[:, n * 128:(n + 1) * 128], [qktb], ptb)
        CP(c, "act", qT[par][0][0:64], ptv[0:64, 0:384].rearrange("p (n t) -> p n t", n=3), ptb, [qkTb[par]])
        CP(c, "act", qT[par][1][64:128], ptv[64:128, 0:384].rearrange("p (n t) -> p n t", n=3), ptb, [qkTb[par]])
        CP(c, "act", kT[par], ptv[:, 384:768].rearrange("p (n t) -> p n t", n=3), ptb, [qkTb[par]])
        P2, P2b = ps2(c)
        for h in range(6):
            hp = h % 2
            MM(c, P2[:, h * 128:(h + 1) * 128], kT[par][:, h // 2, :], qT[par][hp][:, h // 2, :], h % 4 == 0, True,
               [qkTb[par]], [P2b[h // 4]])
        TT(c, "dve", Sm[par], P2[:, 0:768].rearrange("p (h t) -> p h t", h=6), tri3, ALU.mult, P2b + [c.constb], [Smb[par]])
        P3, P3b = ps2(c)
        proj(P3[:, 0:512], 768, 1280, [P3b[0]])
        proj(P3[:, 512:768], 1280, 1536, [P3b[1]])
        CP(c, "act", v_tok[par], P3[:, 0:768], P3b, [v_tokb[par]])
        P6, P6b = ps2(c)
        proj(P6[:, 0:512], 1536, 2048, [P6b[0]])
        proj(P6[:, 512:768], 2048, 2304, [P6b[1]])
        ACTF(c, sgt[par], P6[:, 0:768], AF.Silu, P6b, [sgtb[par]])
        P7, P7b = ps1(c)
        proj(P7[:, 0:256], 2304, 2560, P7b)
        head_norm(c, P7[:, 0:256].rearrange("p (h d) -> p h d", d=64), P7b, 4,
                  c.pp[:, W.pbase + PP_MQN:W.pbase + PP_MQN + 64],
                  qm_toks[par].rearrange("p (h d) -> p h d", d=64), [qm_tokbs[par]], W.hscr, W.hscrb, W.hss, W.hssb)

    def stage_b(ti):
        set_rot(c, (4, 5, 6, 7), (4, 6))
        tb, tl = ti // 4, ti % 4
        par = ti % 2
        P4, P4b = ps2(c)
        for h in range(6):
            hp = h % 2
            MM(c, P4[:, h * 128:(h + 1) * 128], Sm[par][:, h, :], v_tok[par][:, h * 128:(h + 1) * 128], h % 4 == 0,
               ti == 0, [Smb[par], v_tokb[par]], [P4b[h // 4]])
            if ti > 0:
                MM(c, P4[:, h * 128:(h + 1) * 128], qT[par][hp][:, h // 2, :], state_bf[:, h // 2, :], False, True,
                   [qkTb[par], stbb], [P4b[h // 4]])
        if ti < NT - 1:
            P5, P5b = ps2(c)
            for pair in range(3):
                MM(c, P5[:, pair * 256:(pair + 1) * 256], qk_tok[par][:, 384 + pair * 128:384 + (pair + 1) * 128],
                   v_tok[par][:, pair * 256:(pair + 1) * 256], pair % 2 == 0, True, [qk_tokb[par], v_tokb[par]],
                   [P5b[pair // 2]])
            P5v = P5[:, 0:768].rearrange("p (a e) -> p a e", a=3)
            for half in range(2):
                rs = slice(half * 64, (half + 1) * 64)
                TT(c, "dve", state[rs, :, :], P5v[rs, :, half * 128:(half + 1) * 128], state[rs, :, :], ALU.add,
                   P5b + [stb], [stb])
                TT(c, "pool", state[rs, :, :], state[rs, :, :], c.cd[rs, :].unsqueeze(2).broadcast_to([64, 3, 128]),
                   ALU.mult, [stb, c.constb], [stb])
                CP(c, "pool", state_bf[rs, :, :], state[rs, :, :], [stb], [stbb])
        o3 = P4[:, 0:768].rearrange("p (h e) -> p h e", h=6)
        scr3 = scr.rearrange("p (h e) -> p h e", h=6)
        RED(c, sm[:, 0, :], o3, ALU.add, P4b, [smb])
        ACTF(c, scr, P4[:, 0:768], AF.Square, P4b, [scrb])
        RED(c, sm[:, 1, :], scr3, ALU.add, [scrb], [smb])
        TS(c, "dve", sm[:, 2, :], sm[:, 0, :], 1.0 / 128, None, ALU.mult, None, [smb], [smb])
        TT(c, "dve", sm[:, 3, :], sm[:, 2, :], sm[:, 2, :], ALU.mult, [smb], [smb])
        STT(c, sm[:, 4, :], sm[:, 1, :], 1.0 / 128, c.epsx, ALU.mult, ALU.add, [smb, c.constb], [smb])
        TT(c, "dve", sm[:, 4, :], sm[:, 4, :], sm[:, 3, :], ALU.subtract, [smb], [smb])
        ACTF(c, sm[:, 4, :], sm[:, 4, :], AF.Ln, [smb], [smb])
        ACTF(c, sm[:, 4, :], sm[:, 4, :], AF.Exp, [smb], [smb], scale=-0.5)
        TT(c, "dve", scr3, o3, sm[:, 2, :].unsqueeze(2).broadcast_to([128, 6, 128]), ALU.subtract, P4b + [smb], [scrb])
        TT(c, "pool", scr3, scr3, sm[:, 4, :].unsqueeze(2).broadcast_to([128, 6, 128]), ALU.mult, [scrb, smb], [scrb])
        TT(c, "pool", W.y_tok[:, 0:768], scr, sgt[par], ALU.mult, [scrb, sgtb[par]], [W.y_tokb])
        W.qm_tok, W.qm_tokb = qm_toks[par], qm_tokbs[par]
        tile_tail_b(c, W, tl)
        if tl == 3:
            wout_block(c, W, tb)

    stage_a(0)
    for ti in range(NT):
        la = S.capture(lambda: stage_a(ti + 1)) if ti + 1 < NT else []
        lb = S.capture(lambda: stage_b(ti))
        S.commit_interleaved(la, lb)


def norm_rope(c, B, src, src_bufs, H, gain, ti, out, out_bufs, pos_cos=None, pos_sin=None, pos_bufs=None):
    n = src.shape[0]
    xr3 = B.xr[0:n, 0:H * 64].rearrange("p (h d) -> p h d", d=64)
    cos2 = pos_cos if pos_cos is not None else c.cos[0:n, ti, :]
    sin2 = pos_sin if pos_sin is not None else c.sin[0:n, ti, :]
    cosb = cos2.unsqueeze(1).broadcast_to([n, H, 32])
    sinb = sin2.unsqueeze(1).broadcast_to([n, H, 32])
    t = [q[0:n, 0:H * 32].rearrange("p (h d) -> p h d", d=32) for q in B.tq]
    cb_ = [c.constb] if pos_bufs is None else list(pos_bufs)
    if gain is not None:
        ss = B.ss[0:n, 0:H]
        ACTF(c, xr3, src, AF.Square, src_bufs, [B.xrb])
        RED(c, ss, xr3, ALU.add, [B.xrb], [B.ssb])
        ACTF(c, ss, ss, AF.Ln, [B.ssb], [B.ssb], scale=1.0 / 64, bias=float(EPS))
        ACTF(c, ss, ss, AF.Exp, [B.ssb], [B.ssb], scale=-0.5)
        TT(c, "dve", xr3, src, gain[0:n, :].unsqueeze(1).broadcast_to([n, H, 64]), ALU.mult,
           list(src_bufs) + [c.ppb], [B.xrb])
        x, xb, engs = xr3, [B.xrb], ("dve", "pool", "dve", "pool")
    else:
        x, xb, engs = src, list(src_bufs), ("dve", "dve", "dve", "dve")
    x1, x2 = x[:, :, 0:32], x[:, :, 32:64]
    TT(c, engs[0], t[0], x1, cosb, ALU.mult, xb + cb_, [B.tqb[0]])
    TT(c, engs[1], t[1], x2, sinb, ALU.mult, xb + cb_, [B.tqb[1]])
    TT(c, engs[2], t[2], x2, cosb, ALU.mult, xb + cb_, [B.tqb[2]])
    TT(c, engs[3], t[3], x1, sinb, ALU.mult, xb + cb_, [B.tqb[3]])
    if gain is not None:
        TT(c, "pool", xr3[:, :, 0:32], t[0], t[1], ALU.subtract, [B.tqb[0], B.tqb[1]], [B.xrb])
        TT(c, "pool", xr3[:, :, 32:64], t[2], t[3], ALU.add, [B.tqb[2], B.tqb[3]], [B.xrb])
        TT(c, "pool", out, xr3, B.ss[0:n, 0:H].unsqueeze(2).broadcast_to([n, H, 64]), ALU.mult, [B.xrb, B.ssb], out_bufs)
    else:
        TT(c, "pool", out[:, :, 0:32], t[0], t[1], ALU.subtract, [B.tqb[0], B.tqb[1]], out_bufs)
        TT(c, "pool", out[:, :, 32:64], t[2], t[3], ALU.add, [B.tqb[2], B.tqb[3]], out_bufs)


def masked_T(c, src_tok, src_bufs, nchunk, dstA, dstB, dst_bufs):
    pt, ptb = ps1(c)
    ptv = pt.bitcast(BF16)
    for n in range(nchunk):
        TR(c, ptv[:, n * 128:(n + 1) * 128], src_tok[:, n * 128:(n + 1) * 128], src_bufs, ptb)
    v = ptv[:, 0:nchunk * 128].rearrange("p (n t) -> p n t", n=nchunk)
    CP(c, "act", dstA[0:64], v[0:64], ptb, dst_bufs)
    CP(c, "act", dstB[64:128], v[64:128], ptb, dst_bufs)


DSA_TOPK = 256
DSA_BIS = 17


def mixer_dsa(c, l, win_d, wout_d, wkv_d, memT_d):
    S = c.S
    S.barrier()
    set_rot(c, (5, 6, 7), (6,))
    W = Ctx()
    W.pbase = PP_L0 + l * PP_PL
    R1 = Region(c.arena, c.h_off, 32 * 1024)
    R2 = Region(c.arena, c.phase_off, c.phase_size)
    hblk = [R1.take((8, 128), BF16) for _ in range(2)]
    hblkb = bufs("hblk", 2, 8)
    sq = R1.take((8, 128), BF16)
    sqb = Buf("sq")
    W.yT = R1.take((8, 512), BF16)
    W.yTb = Buf("yT")
    acc = R1.take((L,), F32)
    accb = Buf("acc")
    rl = [R1.take((512,), F32) for _ in range(2)]
    rlb = bufs("rl", 2)
    PT = [R1.take((1536,), BF16) for _ in range(2)]
    PTb = bufs("PT", 2, 3)
    w_in = R2.take((8, 1736), BF16)
    w_inb = Buf("w_in")
    load_wout(c, W, R2, wout_d)
    rstd = R2.take((128,), F32)
    rstdb = Buf("rstd")
    kdup = R2.take((L,), BF16)
    ikdup = R2.take((L,), BF16)
    v1 = R2.take((NT, 65), BF16)
    cacheb = bufs("kvcache", NT)
    v1b = Buf("v1ones")
    tail_bufs(c, W, R2, hscr_n=256)
    mark = R2.off
    junk = R2.take((L,), BF16)
    junkb = Buf("junk")
    sel = [R2.take((L,), BF16) for _ in range(2)]
    selb = bufs("sel", 2)
    B = Ctx()
    B.xr = R2.take((768,), F32)
    B.xrb = Buf("xr")
    B.tq = [R2.take((384,), F32) for _ in range(4)]
    B.tqb = bufs("tq", 4)
    B.ss = R2.take((16,), F32)
    B.ssb = Buf("ss")
    q_tok = R2.take((768,), BF16)
    q_tokb = Buf("q_tok")
    qAB = [[R2.take((6, 128), BF16) for _ in range(2)] for _ in range(2)]
    qABb = bufs("qAB", 2)
    qm_toks = [W.qm_tok, R2.take((256,), BF16)]
    qm_tokbs = [W.qm_tokb, Buf("qm_tok1")]
    iq_tok = R2.take((512,), BF16)
    iq_tokb = Buf("iq_tok")
    iqAB = [R2.take((4, 128), BF16) for _ in range(2)]
    iqABb = Buf("iqAB")
    k2 = R2.take((2, 128), BF16)
    k2b = Buf("k2")
    iw_t = R2.take((8,), F32)
    iw_tb = Buf("iw")
    bs = R2.take((2 * DSA_BIS + 8,), F32)
    bsb = Buf("bs")
    den12 = R2.take((12, 1), F32)
    den12b = Buf("den12")

    wv = win_d.rearrange("(dc p) f -> p dc f", p=128)
    for dc in range(0, 8, 2):
        S.dma("pool", "win", lambda e, dc=dc: e.dma_start(out=w_in[:, dc:dc + 2, :], in_=wv[:, dc:dc + 2, :]),
              writes=[w_inb])
    S.seal("win", [w_inb])
    Rt = Region(c.arena, c.h_off, 32 * 1024)
    mem_prep(c, Rt, memT_d, wkv_d, W.pbase, W.k_memT, W.v_mem1, W.kvb)
    S.barrier()
    for p_ in range(2):
        for a in range(2):
            MSET(c, "pool", qAB[p_][a][:, :, :], 0.0, [qABb[p_]])
    for a in range(2):
        MSET(c, "pool", iqAB[a][:, :, :], 0.0, [iqABb])
    MSET(c, "pool", v1[:, :, 64:65], 1.0, [v1b])

    g_ap = c.pp[:, W.pbase + PP_MIX:W.pbase + PP_MIX + 8]
    gq = c.pp[:, W.pbase + PP_A:W.pbase + PP_A + 64]
    gk = c.pp[:, W.pbase + PP_B:W.pbase + PP_B + 64]
    mi4 = c.mi_bf.unsqueeze(1).broadcast_to([128, 4, 128])
    SB = [0, 1, 2]
    OB = [3, 4]
    hslot = {}
    for k_, h in enumerate((0, 2, 4, 6)):
        hslot[h] = (0, k_)
    for k_, h in enumerate((8, 10, 9, 11)):
        hslot[h] = (1, k_)
    for k_, h in enumerate((1, 3, 5, 7)):
        hslot[h] = (2, k_)

    def stage_a(ti):
        set_rot(c, (6, 7), (6,))
        tb = ti // 4
        par = ti % 2
        tsl = slice(ti * 128, (ti + 1) * 128)
        hb, hbb = hblk[par], hblkb[par]
        rmsnorm_block(c, c.xT[:, :, tsl], [c.xb[dc][tb] for dc in range(8)], 128, g_ap, sq, sqb, rstd, rstdb, hb, hbb)

        def proj(out_ap, c0, c1, pbufs):
            for dc in range(8):
                MM(c, out_ap, hb[:, dc, :], w_in[:, dc, c0:c1], dc == 0, dc == 7, [hbb[dc], w_inb], pbufs)

        Pq, Pqb = ps2(c)
        proj(Pq[:, 0:512], 0, 512, [Pqb[0]])
        proj(Pq[:, 512:768], 512, 768, [Pqb[1]])
        norm_rope(c, B, Pq[:, 0:768].rearrange("p (h d) -> p h d", d=64), Pqb, 12, gq, ti,
                  q_tok.rearrange("p (h d) -> p h d", d=64), [q_tokb])
        masked_T(c, q_tok, [q_tokb], 6, qAB[par][0], qAB[par][1], [qABb[par]])
        Pi, Pib = ps1(c)
        proj(Pi, 896, 1408, Pib)
        norm_rope(c, B, Pi.rearrange("p (h d) -> p h d", d=64), Pib, 8, None, ti,
                  iq_tok.rearrange("p (h d) -> p h d", d=64), [iq_tokb])
        masked_T(c, iq_tok, [iq_tokb], 4, iqAB[0], iqAB[1], [iqABb])
        Ps, Psb = ps1(c)
        proj(Ps[:, 0:128], 768, 896, Psb)
        proj(Ps[:, 128:192], 1408, 1472, Psb)
        proj(Ps[:, 192:200], 1472, 1480, Psb)
        norm_rope(c, B, Ps[:, 0:64].rearrange("p (h d) -> p h d", d=64), Psb, 1, gk, ti,
                  k2[:, 0, 0:64].rearrange("p (h d) -> p h d", d=64), [k2b])
        norm_rope(c, B, Ps[:, 128:192].rearrange("p (h d) -> p h d", d=64), Psb, 1, None, ti,
                  k2[:, 1, 0:64].rearrange("p (h d) -> p h d", d=64), [k2b])
        CP(c, "pool", k2[:, :, 64:128], k2[:, :, 0:64], [k2b], [k2b])
        CP(c, "act", v1[:, ti, 0:64], Ps[:, 64:128], Psb, [cacheb[ti]])
        CP(c, "act", iw_t, Ps[:, 192:200], Psb, [iw_tb])
        pt, ptb = ps1(c)
        ptv = pt.bitcast(BF16)
        TR(c, ptv[:, 0:128], k2[:, 0, :], [k2b], ptb)
        TR(c, ptv[:, 128:256], k2[:, 1, :], [k2b], ptb)
        CP(c, "act", kdup[:, tsl], ptv[:, 0:128], ptb, [cacheb[ti]])
        CP(c, "act", ikdup[:, tsl], ptv[:, 128:256], ptb, [cacheb[ti]])
        Pm, Pmb = ps1(c)
        proj(Pm[:, 0:256], 1480, 1736, Pmb)
        head_norm(c, Pm[:, 0:256].rearrange("p (h d) -> p h d", d=64), Pmb, 4,
                  c.pp[:, W.pbase + PP_MQN:W.pbase + PP_MQN + 64],
                  qm_toks[par].rearrange("p (h d) -> p h d", d=64), [qm_tokbs[par]], W.hscr, W.hscrb, W.hss, W.hssb)

        Skeys = 128 * (ti + 1)
        nkb = (Skeys + 511) // 512
        n = 0
        for kb in range(nkb):
            wdt = min(512, Skeys - kb * 512)
            ks = slice(kb * 512, kb * 512 + wdt)
            kbufs = [cacheb[j] for j in range(kb * 4, min(ti + 1, kb * 4 + 4))]
            for h in range(8):
                Px, Pxb = ps1(c)
                MM(c, Px[:, 0:wdt], iqAB[h % 2][:, h // 2, :], ikdup[:, ks], True, True, [iqABb] + kbufs, Pxb)
                r_, rb_ = rl[n % 2], rlb[n % 2]
                n += 1
                ACTF(c, r_[:, 0:wdt], Px[:, 0:wdt], AF.Relu, Pxb, [rb_])
                if h == 0:
                    TS(c, "dve", acc[:, ks], r_[:, 0:wdt], iw_t[:, 0:1], None, ALU.mult, None, [rb_, iw_tb], [accb])
                else:
                    STT(c, acc[:, ks], r_[:, 0:wdt], iw_t[:, h:h + 1], acc[:, ks], ALU.mult, ALU.add,
                        [rb_, iw_tb, accb], [accb])
        sl_, slb_ = sel[par], selb[par]
        if Skeys > DSA_TOPK:
            a_ = acc[:, 0:Skeys]
            S.op("dve", lambda e, a_=a_: e.tensor_reduce(out=bs[:, 0:1], in_=a_, axis=AX.X, op=ALU.max,
                                                         apply_absolute_value=True), [accb], [bsb])
            TT(c, "dve", acc[:, tsl], acc[:, tsl], c.cneg, ALU.add, [accb, c.constb], [accb])
            TS(c, "dve", bs[:, 8:8 + DSA_BIS], c.bisc, bs[:, 0:1], None, ALU.mult, None, [bsb, c.constb], [bsb])
            TS(c, "dve", bs[:, 8 + DSA_BIS:8 + 2 * DSA_BIS], bs[:, 8:8 + DSA_BIS], 2.0, None, ALU.mult, None, [bsb], [bsb])
            TS(c, "dve", bs[:, 1:2], bs[:, 0:1], 0.0, None, ALU.mult, None, [bsb], [bsb])
            for i in range(DSA_BIS):
                S.op("dve", lambda e, a_=a_, n_=Skeys: e.tensor_scalar(
                    out=junk[:, 0:n_], in0=a_, scalar1=bs[:, 1:2], scalar2=None, op0=ALU.is_ge, op1=ALU.add,
                    accum_out=bs[:, 2:3]), [accb, bsb], [junkb, bsb])
                last = i + 1 == DSA_BIS
                dn = bs[:, 8 + i:8 + i + 1] if last else bs[:, 8 + i + 1:8 + i + 2]
                d2 = bs[:, 8 + i:8 + i + 1] if last else bs[:, 8 + DSA_BIS + i + 1:8 + DSA_BIS + i + 2]
                STT(c, bs[:, 3:4], bs[:, 2:3], float(DSA_TOPK), d2, ALU.is_ge, ALU.mult, [bsb], [bsb])
                S.op("dve", lambda e, dn=dn: e.scalar_tensor_tensor(out=bs[:, 1:2], in0=bs[:, 1:2], scalar=dn, in1=bs[:, 3:4],
                                                                    op0=ALU.subtract, op1=ALU.add), [bsb], [bsb])
            TS(c, "dve", sl_[:, 0:Skeys], a_, bs[:, 1:2], None, ALU.is_ge, None, [accb, bsb], [slb_])
        else:
            TT(c, "dve", acc[:, tsl], acc[:, tsl], c.cneg, ALU.add, [accb, c.constb], [accb])
            TS(c, "dve", sl_[:, 0:Skeys], acc[:, 0:Skeys], -1e29, None, ALU.is_ge, None, [accb], [slb_])

    def stage_b(ti):
        set_rot(c, (5,), ())
        tb, tl = ti // 4, ti % 4
        par = ti % 2
        sl_, slb_ = sel[par], selb[par]
        qa, qb_ = qAB[par][0], qAB[par][1]
        for j in range(ti + 1):
            js = slice(j * 128, (j + 1) * 128)
            pj = j % 2
            p0, p1, p2 = bank(c, SB[0]), bank(c, SB[1]), bank(c, SB[2])
            b0, b1, b2 = [c.psb[SB[0]]], [c.psb[SB[1]]], [c.psb[SB[2]]]
            kb_ = [cacheb[j], qABb[par]]
            MM(c, p0, kdup[:, js], qa[:, 0:4, :], True, False, kb_, b0)
            MM(c, p0, sl_[:, js], mi4, False, True, [slb_, c.constb], b0)
            MM(c, p1[:, 0:256], kdup[:, js], qa[:, 4:6, :], True, False, kb_, b1)
            MM(c, p1[:, 256:512], kdup[:, js], qb_[:, 4:6, :], False, False, kb_, b1)
            MM(c, p1, sl_[:, js], mi4, False, True, [slb_, c.constb], b1)
            MM(c, p2, kdup[:, js], qb_[:, 0:4, :], True, False, kb_, b2)
            MM(c, p2, sl_[:, js], mi4, False, True, [slb_, c.constb], b2)
            for bi, (pp_, bb_) in enumerate(((p0, b0), (p1, b1), (p2, b2))):
                ACTF(c, PT[pj][:, bi * 512:(bi + 1) * 512], pp_, AF.Exp, bb_, [PTb[pj][bi]], scale=0.125,
                     bias=-MASKM / 8)
            for h in range(12):
                bi, sl2 = hslot[h]
                ob = OB[0] if h < 7 else OB[1]
                oc = (h if h < 7 else h - 7) * 65
                MM(c, bank(c, ob)[:, oc:oc + 65], PT[pj][:, (bi * 4 + sl2) * 128:(bi * 4 + sl2 + 1) * 128], v1[:, j, :],
                   j == 0 and h in (0, 7), j == ti and h in (6, 11), [PTb[pj][bi], cacheb[j], v1b], [c.psb[ob]])
        oa = bank(c, OB[0])[:, 0:455].rearrange("p (h e) -> p h e", e=65)
        ob_ = bank(c, OB[1])[:, 0:325].rearrange("p (h e) -> p h e", e=65)
        RCP(c, den12[:, 0:7, :], oa[:, :, 64:65], [c.psb[OB[0]]], [den12b])
        RCP(c, den12[:, 7:12, :], ob_[:, :, 64:65], [c.psb[OB[1]]], [den12b])
        TT(c, "dve", W.y_tok[:, 0:448].rearrange("p (h d) -> p h d", d=64), oa[:, :, 0:64],
           den12[:, 0:7, :].broadcast_to([128, 7, 64]), ALU.mult, [c.psb[OB[0]], den12b], [W.y_tokb])
        TT(c, "dve", W.y_tok[:, 448:768].rearrange("p (h d) -> p h d", d=64), ob_[:, :, 0:64],
           den12[:, 7:12, :].broadcast_to([128, 5, 64]), ALU.mult, [c.psb[OB[1]], den12b], [W.y_tokb])
        W.qm_tok, W.qm_tokb = qm_toks[par], qm_tokbs[par]
        tile_tail_b(c, W, tl)
        if tl == 3:
            wout_block(c, W, tb)

    stage_a(0)
    for ti in range(NT):
        la = S.capture(lambda: stage_a(ti + 1)) if ti + 1 < NT else []
        lb = S.capture(lambda: stage_b(ti))
        S.commit_interleaved(la, lb)


NSA_NCMP = 127


def nsa_hmap(c_, par):
    return 3 * (2 * (c_ // 3) + par) + (c_ % 3)


def mixer_nsa(c, l, win_d, wout_d, wkv_d, memT_d, cw_d):
    S = c.S
    S.barrier()
    set_rot(c, (6, 7), (6,))
    W = Ctx()
    W.pbase = PP_L0 + l * PP_PL
    R1 = Region(c.arena, c.h_off, 32 * 1024)
    R2 = Region(c.arena, c.phase_off, c.phase_size)
    hblk = R1.take((8, 128), BF16)
    hblkb = bufs("hblk", 8)
    sq = R1.take((8, 128), BF16)
    sqb = Buf("sq")
    W.yT = R1.take((8, 512), BF16)
    W.yTb = Buf("yT")
    un0 = R1.off
    B = Ctx()
    B.xr = R1.take((768,), F32)
    B.xrb = Buf("xr")
    B.tq = [R1.take((384,), F32) for _ in range(4)]
    B.tqb = bufs("tq", 4)
    un1 = R1.off
    Ru = Region(c.arena, un0, un1 - un0)
    PT = [Ru.take((1536,), BF16) for _ in range(2)]
    PTb = bufs("PT", 2, 4)
    PTc = Ru.take((1536,), BF16)
    PTcb = bufs("PTc", 4)
    tmp = R1.take((768,), F32)
    tmpb = Buf("tmp")
    yacc = R1.take((768,), F32)
    yaccb = Buf("yacc")
    imp = R1.take((12, 32), F32)
    impb = Buf("imp")
    impm = R1.take((4, 32), F32)
    wk16 = R1.take((4, 32), F32)
    impmb = Buf("impm")
    blk = R1.take((4, 32), BF16)
    blkb = Buf("blk")
    w_in = R2.take((8, 2596), BF16)
    w_inb = Buf("w_in")
    wo1 = R2.take((8, 128), BF16)
    wo = [wo1, wo1]
    wob1 = Buf("wo")
    wob = [wob1, wob1]
    rstd = R2.take((128,), F32)
    rstdb = Buf("rstd")
    ksT = R2.take((2, L), BF16)
    kwT = R2.take((2, 5 * 128), BF16)
    vs1 = R2.take((NT, 4, 65), BF16)
    vw1 = R2.take((5, 4, 65), BF16)
    cacheb = Buf("kvcache")
    tail_bufs(c, W, R2, hscr_n=256)
    mark = R2.off
    B.ss = R2.take((16,), F32)
    B.ssb = Buf("ss")
    q_tok = R2.take((768,), BF16)
    q_tokb = Buf("q_tok")
    qAB = [R2.take((6, 128), BF16) for _ in range(2)]
    qABb = Buf("qAB")
    k_tok = R2.take((2, 256), BF16)
    k_tokb = Buf("k_tok")
    kvcT = [R2.take((4, 144), BF16) for _ in range(2)]
    kvcTb = Buf("kvcT")
    wdup = [R2.take((32, 64), BF16) for _ in range(2)]
    wdupb = Buf("wdup")
    posB = R2.take((2, 32, 8), BF16)
    posBb = Buf("posB")
    k_cmpT = R2.take((2, 128), BF16)
    v_cmpx = R2.take((4, 97), BF16)
    cmpb = Buf("cmpcache")
    cbias = R1.take((512,), F32)
    cbiasb = Buf("cbias")
    cnew_tok = R2.take((512,), BF16)
    cnew_tokb = Buf("cnew_tok")
    gate = R2.take((36,), F32)
    gateb = Buf("gate")
    sm = R2.take((6, 12), F32)
    smb = Buf("sm")
    m8 = R2.take((2, 8), F32)
    m8b = Buf("m8")
    thr = R2.take((4, 1), F32)
    thrb = Buf("thr")
    seltile = R2.take((4, 128), BF16)
    seltileb = Buf("seltile")
    masks_bf = R2.take((768,), BF16)
    tril_bf, far_bf = masks_bf[:, 0:128], masks_bf[:, 128:256]
    cmask_bf, shiftI = masks_bf[:, 256:512], masks_bf[:, 512:768]
    czca = R2.take((128,), F32)
    cse = [R2.take((64,), F32) for _ in range(2)]
    cseb = bufs("cse", 2)
    ncb = Buf("nsaconst")

    wv = win_d.rearrange("(dc p) f -> p dc f", p=128)
    for c_ in range(6):
        for par in range(2):
            h = nsa_hmap(c_, par)
            src = wv[:, :, h * 64:(h + 1) * 64]
            dst = w_in[:, :, c_ * 128 + par * 64:c_ * 128 + (par + 1) * 64]
            S.dma("pool", "win", lambda e, src=src, dst=dst: e.dma_start(out=dst, in_=src), writes=[w_inb])
    for dc in range(0, 8, 2):
        S.dma("pool", "win", lambda e, dc=dc: e.dma_start(out=w_in[:, dc:dc + 2, 768:2596], in_=wv[:, dc:dc + 2, 768:2596]),
              writes=[w_inb])
    S.seal("win", [w_inb])
    for kind in range(2):
        srcw = cw_d[kind].rearrange("l d e -> d l e")
        for half in range(2):
            S.dma("pool", "wcmp", lambda e, kind=kind, half=half, srcw=srcw: e.dma_start(
                out=wdup[kind][half * 64:(half + 1) * 64, :, :], in_=srcw), writes=[wdupb])
    S.seal("wcmp", [wdupb])
    Rt = Region(c.arena, c.h_off, 32 * 1024)
    mem_prep(c, Rt, memT_d, wkv_d, W.pbase, W.k_memT, W.v_mem1, W.kvb)
    S.barrier()
    S.dma("pool", "ncst", lambda e: e.dma_start(out=masks_bf, in_=c.c2["masks"][:, :]), writes=[ncb])
    S.dma("sp", "ncst", lambda e: e.dma_start(out=czca, in_=c.c2["czca"][:, :]), writes=[ncb])
    S.seal("ncst", [ncb])
    for a in range(2):
        MSET(c, "pool", qAB[a][:, :, :], 0.0, [qABb])
        MSET(c, "pool", kvcT[a][:, :, :], 0.0, [kvcTb])
    MSET(c, "pool", k_cmpT[:, :, :], 0.0, [cmpb])
    MSET(c, "pool", v_cmpx[:, :, 0:64], 0.0, [cmpb])
    MSET(c, "pool", v_cmpx[:, :, 64:65], 1.0, [cmpb])
    for g_ in range(4):
        S.dma("pool", "ncov", lambda e, g_=g_: e.dma_start(out=v_cmpx[:, g_, 65:97], in_=c.c2["cover"][:, :]), writes=[cmpb])
    S.seal("ncov", [cmpb])
    MSET(c, "pool", vs1[:, :, :, 64:65], 1.0, [cacheb])
    MSET(c, "pool", vw1[:, :, :, 64:65], 1.0, [cacheb])
    posf = c.pp[:, W.pbase + PP_POS:W.pbase + PP_POS + 64].rearrange("p (k l) -> p k l", k=2)
    CP(c, "dve", posB, posf.unsqueeze(3).broadcast_to([128, 2, 32, 8]), [c.ppb], [posBb])
    Pcb, Pcbb = ps1(c)
    for kind in range(2):
        for l_ in range(32):
            MM(c, Pcb[0:8, kind * 64:(kind + 1) * 64], posB[:, kind, l_, :], wdup[kind][:, l_, :], kind == 0 and l_ == 0,
               l_ == 31, [posBb, wdupb], Pcbb)
    CP(c, "dve", cbias[0:8, :].rearrange("p (k g e) -> p k g e", k=2, g=4),
       Pcb[0:8, 0:128].rearrange("p (k e) -> p k e", k=2).unsqueeze(2).broadcast_to([8, 2, 4, 64]), Pcbb, [cbiasb])

    g_ap = c.pp[:, W.pbase + PP_MIX:W.pbase + PP_MIX + 8]
    gq = c.pp[:, W.pbase + PP_A:W.pbase + PP_A + 64]
    gkc = c.pp[:, W.pbase + PP_B:W.pbase + PP_B + 64]
    gks = c.pp[:, W.pbase + PP_C:W.pbase + PP_C + 64]
    gkw = c.pp[:, W.pbase + PP_D:W.pbase + PP_D + 64]
    mi3 = c.mi_bf.unsqueeze(1).broadcast_to([128, 3, 128])
    SBK = [0, 1, 2, 3]
    OB = [4, 5]
    CB = [4, 5, 6]
    wo_n = [0]

    def head_of(sb, r):
        par, kc_ = sb // 2, sb % 2
        return 3 * (2 * kc_ + par) + r

    def branch(ti, keys, Kc, Vc, kslot, vslot, maskf, coef_col):
        nk = len(keys)
        for n_, j in enumerate(keys):
            par_j = n_ % 2
            ks_ = kslot(j)
            mk = maskf(j)
            for sb in SBK:
                par, kc_ = sb // 2, sb % 2
                g = 2 * kc_ + par
                pb_ = bank(c, sb)
                MM(c, pb_[:, 0:384], Kc[:, kc_, ks_], qAB[par][:, 3 * kc_:3 * kc_ + 3, :], True, mk is None,
                   [cacheb, qABb], [c.psb[sb]])
                if mk is not None:
                    lh, lb = mk[g]
                    MM(c, pb_[:, 0:384], lh, mi3, False, True, lb + [c.constb], [c.psb[sb]])
            for pr in range(2):
                src = c.pst[pr][:, :].rearrange("p (b x) -> p b x", b=2)[:, :, 0:384]
                dst = PT[par_j][:, pr * 768:(pr + 1) * 768].rearrange("p (b x) -> p b x", b=2)
                ACTF(c, dst, src, AF.Exp, [c.psb[2 * pr], c.psb[2 * pr + 1]], [PTb[par_j][2 * pr], PTb[par_j][2 * pr + 1]],
                     scale=0.125, bias=(0.0 if mk is None else -MASKM / 8))
            for sb in SBK:
                for r in range(3):
                    h = head_of(sb, r)
                    g = h // 3
                    ob = OB[0] if h < 7 else OB[1]
                    oc = (h if h < 7 else h - 7) * 65
                    firsts = (0, 7)
                    MM(c, bank(c, ob)[:, oc:oc + 65], PT[par_j][:, sb * 384 + r * 128:sb * 384 + (r + 1) * 128],
                       Vc[:, vslot(j), g, :], n_ == 0 and (sb, r) == first_in_bank[ob], n_ == nk - 1,
                       [PTb[par_j][sb], cacheb], [c.psb[ob]])
        oa = bank(c, OB[0])[:, 0:455].rearrange("p (h e) -> p h e", e=65)
        ob_ = bank(c, OB[1])[:, 0:325].rearrange("p (h e) -> p h e", e=65)
        RCP(c, sm[:, 0, 0:7].unsqueeze(2), oa[:, :, 64:65], [c.psb[OB[0]]], [smb])
        RCP(c, sm[:, 0, 7:12].unsqueeze(2), ob_[:, :, 64:65], [c.psb[OB[1]]], [smb])
        TT(c, "dve", sm[:, 1, :], sm[:, 0, :], gate.rearrange("p (h b) -> p h b", b=3)[:, :, coef_col], ALU.mult,
           [smb, gateb], [smb])
        t3 = tmp.rearrange("p (h d) -> p h d", d=64)
        TT(c, "dve", t3[:, 0:7, :], oa[:, :, 0:64], sm[:, 1, 0:7].unsqueeze(2).broadcast_to([128, 7, 64]), ALU.mult,
           [c.psb[OB[0]], smb], [tmpb])
        TT(c, "dve", t3[:, 7:12, :], ob_[:, :, 0:64], sm[:, 1, 7:12].unsqueeze(2).broadcast_to([128, 5, 64]), ALU.mult,
           [c.psb[OB[1]], smb], [tmpb])
        TT(c, "pool", yacc, yacc, tmp, ALU.add, [yaccb, tmpb], [yaccb])

    first_in_bank = {}
    for sb in SBK:
        for r in range(3):
            h = head_of(sb, r)
            ob = OB[0] if h < 7 else OB[1]
            first_in_bank.setdefault(ob, (sb, r))
    first_in_cbank = {}
    for sb in SBK:
        for r in range(3):
            h = head_of(sb, r)
            first_in_cbank.setdefault(CB[h // 5], (sb, r))

    for ti in range(NT):
        tb, tl = ti // 4, ti % 4
        tsl = slice(ti * 128, (ti + 1) * 128)
        rs5 = ti % 5
        rsl = slice(rs5 * 128, (rs5 + 1) * 128)
        rmsnorm_block(c, c.xT[:, :, tsl], [c.xb[dc][tb] for dc in range(8)], 128, g_ap, sq, sqb, rstd, rstdb, hblk, hblkb)

        def proj(out_ap, c0, c1, pbufs):
            for dc in range(8):
                MM(c, out_ap, hblk[:, dc, :], w_in[:, dc, c0:c1], dc == 0, dc == 7, [hblkb[dc], w_inb], pbufs)

        Pq, Pqb = ps2(c)
        proj(Pq[:, 0:512], 0, 512, [Pqb[0]])
        proj(Pq[:, 512:768], 512, 768, [Pqb[1]])
        norm_rope(c, B, Pq[:, 0:768].rearrange("p (h d) -> p h d", d=64), Pqb, 12, gq, ti,
                  q_tok.rearrange("p (h d) -> p h d", d=64), [q_tokb])
        masked_T(c, q_tok, [q_tokb], 6, qAB[0], qAB[1], [qABb])
        Pk, Pkb = ps2(c)
        proj(Pk[:, 0:256], 1280, 1536, [Pkb[0]])
        proj(Pk[:, 512:768], 1792, 2048, [Pkb[1]])
        norm_rope(c, B, Pk[:, 0:256].rearrange("p (h d) -> p h d", d=64), [Pkb[0]], 4, gks, ti,
                  k_tok[:, 0, :].rearrange("p (h d) -> p h d", d=64), [k_tokb])
        norm_rope(c, B, Pk[:, 512:768].rearrange("p (h d) -> p h d", d=64), [Pkb[1]], 4, gkw, ti,
                  k_tok[:, 1, :].rearrange("p (h d) -> p h d", d=64), [k_tokb])
        pt, ptb = ps1(c)
        ptv = pt.bitcast(BF16)
        for n_ in range(4):
            TR(c, ptv[:, n_ * 128:(n_ + 1) * 128], k_tok[:, n_ // 2, (n_ % 2) * 128:(n_ % 2 + 1) * 128], [k_tokb], ptb)
        CP(c, "act", ksT[:, :, tsl], ptv[:, 0:256].rearrange("p (c t) -> p c t", c=2), ptb, [cacheb])
        CP(c, "act", kwT[:, :, rsl], ptv[:, 256:512].rearrange("p (c t) -> p c t", c=2), ptb, [cacheb])
        Pv, Pvb = ps2(c)
        proj(Pv[:, 0:256], 1536, 1792, [Pvb[0]])
        proj(Pv[:, 512:768], 2048, 2304, [Pvb[1]])
        CP(c, "act", vs1[:, ti, :, 0:64], Pv[:, 0:256].rearrange("p (g d) -> p g d", d=64), [Pvb[0]], [cacheb])
        CP(c, "act", vw1[:, rs5, :, 0:64], Pv[:, 512:768].rearrange("p (g d) -> p g d", d=64), [Pvb[1]], [cacheb])
        Pc, Pcb_ = ps1(c)
        for n_ in range(4):
            for dc in range(8):
                MM(c, Pc[:, n_ * 128:(n_ + 1) * 128], w_in[:, dc, 768 + n_ * 128:768 + (n_ + 1) * 128], hblk[:, dc, :],
                   n_ == 0 and dc == 0, dc == 7, [hblkb[dc], w_inb], Pcb_)
        Pc4 = Pc.rearrange("p (n t) -> p n t", n=4)
        CP(c, "act", kvcT[0][0:64, :, 16:144], Pc4[0:64], Pcb_, [kvcTb])
        CP(c, "act", kvcT[1][64:128, :, 16:144], Pc4[64:128], Pcb_, [kvcTb])
        Pg, Pgb = ps1(c)
        proj(Pg[:, 0:36], 2304, 2340, Pgb)
        proj(Pg[:, 128:384], 2340, 2596, Pgb)
        ACTF(c, gate, Pg[:, 0:36], AF.Exp, Pgb, [gateb], scale=-1.0)
        TS(c, "dve", gate, gate, 1.0, None, ALU.add, None, [gateb], [gateb])
        RCP(c, gate, gate, [gateb], [gateb])
        head_norm(c, Pg[:, 128:384].rearrange("p (h d) -> p h d", d=64), Pgb, 4,
                  c.pp[:, W.pbase + PP_MQN:W.pbase + PP_MQN + 64],
                  W.qm_tok.rearrange("p (h d) -> p h d", d=64), [W.qm_tokb], W.hscr, W.hscrb, W.hss, W.hssb)
        nb = 7 if ti == 0 else 8
        n0 = 0 if ti == 0 else 8 * ti - 1
        off = 16 if ti == 0 else 0
        Pn, Pnb = ps1(c)
        first = True
        for kind in range(2):
            for g in range(4):
                par, ch = g % 2, g // 2
                for l_ in range(32):
                    lhs = kvcT[par][:, kind * 2 + ch, off + l_:off + l_ + 16 * (nb - 1) + 1:16]
                    MM(c, Pn[0:nb, (kind * 4 + g) * 64:(kind * 4 + g + 1) * 64], lhs, wdup[kind][:, l_, :], first,
                       l_ == 31, [kvcTb, wdupb], Pnb)
                    first = False
        for a in range(2):
            hs = slice(a * 64, (a + 1) * 64)
            CP(c, "pool", kvcT[a][hs, :, 0:16], kvcT[a][hs, :, 128:144], [kvcTb], [kvcTb])
        cn = tmp[0:nb, 0:512]
        TT(c, "dve", cn, Pn[0:nb, :], cbias[0:nb, :], ALU.add, Pnb + [cbiasb], [tmpb])
        cs_, csb_ = cse[ti % 2], cseb[ti % 2]
        S.dma("sp", f"cse{ti % 2}", lambda e, cs_=cs_, ti=ti: e.dma_start(out=cs_[0:8, :], in_=c.c2["rope"][ti, :, :]),
              writes=[csb_])
        norm_rope(c, B, cn[:, 0:256].rearrange("p (h d) -> p h d", d=64), [tmpb], 4, gkc, ti,
                  cnew_tok[0:nb, 0:256].rearrange("p (h d) -> p h d", d=64), [cnew_tokb],
                  pos_cos=cs_[0:nb, 0:32], pos_sin=cs_[0:nb, 32:64], pos_bufs=[csb_])
        CP(c, "pool", cnew_tok[0:nb, 256:512], cn[:, 256:512], [tmpb], [cnew_tokb])
        pt2, pt2b = ps1(c)
        pt2v = pt2.bitcast(BF16)
        for ch in range(2):
            TR(c, pt2v[:, ch * 8:ch * 8 + nb], cnew_tok[0:nb, ch * 128:(ch + 1) * 128], [cnew_tokb], pt2b)
        for ch in range(2):
            CP(c, "act", k_cmpT[:, ch, n0:n0 + nb], pt2v[:, ch * 8:ch * 8 + nb], pt2b, [cmpb])
        Psc, Pscb = ps1(c)
        MM(c, Psc[:, 0:256], shiftI[0:nb, 128 - n0:256 - n0], cnew_tok[0:nb, 256:512], True, True, [ncb, cnew_tokb], Pscb)
        TT(c, "dve", v_cmpx[:, :, 0:64], Psc[:, 0:256].rearrange("p (g d) -> p g d", d=64), v_cmpx[:, :, 0:64], ALU.add,
           Pscb + [cmpb], [cmpb])
        S.barrier()
        cm_l = cmask_bf[:, 128 - 8 * ti:128 - 8 * ti + NSA_NCMP]
        for sb in SBK:
            par, kc_ = sb // 2, sb % 2
            pb_ = bank(c, sb)
            MM(c, pb_[0:NSA_NCMP, 0:384], k_cmpT[:, kc_, 0:NSA_NCMP], qAB[par][:, 3 * kc_:3 * kc_ + 3, :], True, False,
               [cmpb, qABb], [c.psb[sb]])
            MM(c, pb_[0:NSA_NCMP, 0:384], cm_l, mi3, False, True, [ncb, c.constb], [c.psb[sb]])
        for pr in range(2):
            src = c.pst[pr][0:NSA_NCMP, :].rearrange("p (b x) -> p b x", b=2)[:, :, 0:384]
            dst = PTc[0:NSA_NCMP, pr * 768:(pr + 1) * 768].rearrange("p (b x) -> p b x", b=2)
            ACTF(c, dst, src, AF.Exp, [c.psb[2 * pr], c.psb[2 * pr + 1]], [PTcb[2 * pr], PTcb[2 * pr + 1]], scale=0.125,
                 bias=-MASKM / 8)
        for sb in SBK:
            for r in range(3):
                h = head_of(sb, r)
                cb_ = CB[h // 5]
                oc = (h % 5) * 97
                MM(c, bank(c, cb_)[:, oc:oc + 97], PTc[0:NSA_NCMP, sb * 384 + r * 128:sb * 384 + (r + 1) * 128],
                   v_cmpx[0:NSA_NCMP, h // 3, :], (sb, r) == first_in_cbank[cb_], True, [PTcb[sb], cmpb], [c.psb[cb_]])
        cviews = [bank(c, CB[0])[:, 0:485].rearrange("p (h e) -> p h e", e=97),
                  bank(c, CB[1])[:, 0:485].rearrange("p (h e) -> p h e", e=97),
                  bank(c, CB[2])[:, 0:194].rearrange("p (h e) -> p h e", e=97)]
        hr = [(0, 5), (5, 10), (10, 12)]
        for k_, (h0, h1) in enumerate(hr):
            TS(c, "dve", sm[:, 0, h0:h1].unsqueeze(2), cviews[k_][:, :, 64:65], 1e-30, None, ALU.max, None,
               [c.psb[CB[k_]]], [smb])
        RCP(c, sm[:, 0, :], sm[:, 0, :], [smb], [smb])
        TT(c, "dve", sm[:, 1, :], sm[:, 0, :], gate.rearrange("p (h b) -> p h b", b=3)[:, :, 0], ALU.mult, [smb, gateb], [smb])
        y3 = yacc.rearrange("p (h d) -> p h d", d=64)
        for k_, (h0, h1) in enumerate(hr):
            TT(c, "dve", y3[:, h0:h1, :], cviews[k_][:, :, 0:64], sm[:, 1, h0:h1].unsqueeze(2).broadcast_to([128, h1 - h0, 64]),
               ALU.mult, [c.psb[CB[k_]], smb], [yaccb])
            TT(c, "dve", imp[:, h0:h1, :], cviews[k_][:, :, 65:97], sm[:, 0, h0:h1].unsqueeze(2).broadcast_to([128, h1 - h0, 32]),
               ALU.mult, [c.psb[CB[k_]], smb], [impb])
        imp4 = imp.rearrange("p (g r) j -> p g r j", r=3)
        TT(c, "dve", impm, imp4[:, :, 0, :], imp4[:, :, 1, :], ALU.add, [impb], [impmb])
        TT(c, "dve", impm, impm, imp4[:, :, 2, :], ALU.add, [impb, impmb], [impmb])
        czs = czca[:, 32 - 2 * ti:64 - 2 * ti].unsqueeze(1).broadcast_to([128, 4, 32])
        cas = czca[:, 64 + 32 - 2 * ti:64 + 64 - 2 * ti].unsqueeze(1).broadcast_to([128, 4, 32])
        TT(c, "dve", impm, impm, czs, ALU.mult, [impmb, ncb], [impmb])
        TT(c, "dve", impm, impm, cas, ALU.add, [impmb, ncb], [impmb])
        MSET(c, "dve", impm[:, :, 0:1], 2e9, [impmb])
        for g in range(4):
            S.op("dve", lambda e, g=g: e.max(out=m8[:, 0, :], in_=impm[:, g, :]), [impmb], [m8b])
            S.op("dve", lambda e, g=g: e.match_replace(out=wk16[:, g, :], in_to_replace=m8[:, 0, :], in_values=impm[:, g, :],
                                                       imm_value=-3e38), [impmb, m8b], [impmb])
            S.op("dve", lambda e, g=g: e.max(out=m8[:, 1, :], in_=wk16[:, g, :]), [impmb], [m8b])
            CP(c, "dve", thr[:, g, :], m8[:, 1, 7:8], [m8b], [thrb])
        TT(c, "dve", blk, impm, thr.broadcast_to([128, 4, 32]), ALU.is_ge, [impmb, thrb], [blkb])
        def slc_mask(j, ti=ti):
            src = blk[:, :, 2 * j:2 * j + 2].unsqueeze(3).broadcast_to([128, 4, 2, 64])
            dst = seltile.rearrange("p g (b s) -> p g b s", b=2)
            if j == ti:
                TT(c, "pool", dst, src, tril_bf.rearrange("p (b s) -> p b s", b=2).unsqueeze(1).broadcast_to([128, 4, 2, 64]),
                   ALU.mult, [blkb, ncb], [seltileb])
            else:
                CP(c, "pool", dst, src, [blkb], [seltileb])
            return {g: (seltile[:, g, :], [seltileb]) for g in range(4)}

        branch(ti, list(range(ti + 1)), ksT, vs1, lambda j: slice(j * 128, (j + 1) * 128), lambda j: j, slc_mask, 1)

        def win_mask(j, ti=ti):
            if j == ti:
                return {g: (tril_bf, [ncb]) for g in range(4)}
            if j == ti - 4:
                return {g: (far_bf, [ncb]) for g in range(4)}
            return None

        branch(ti, list(range(max(0, ti - 4), ti + 1)), kwT, vw1,
               lambda j: slice((j % 5) * 128, (j % 5 + 1) * 128), lambda j: j % 5, win_mask, 2)
        CP(c, "pool", W.y_tok[:, 0:768], yacc, [yaccb], [W.y_tokb])
        tile_tail_b(c, W, tl)
        if tl == 3:
            wout_block_stream(c, W, tb, wout_d, wo, wob, wo_n)
        S.barrier()


def wout_block_stream(c, W, tb, wout_d, wo, wob, wo_n):
    ts = slice(tb * 512, (tb + 1) * 512)
    wv = wout_d.rearrange("(cc p) d -> p cc d", p=128)
    for dc in range(8):
        s = wo_n[0] % 2
        wo_n[0] += 1
        c.S.dma("pool", f"wo{s}", lambda e, s=s, dc=dc: e.dma_start(out=wo[s][:, :, :], in_=wv[:, :, dc * 128:(dc + 1) * 128]),
                writes=[wob[s]])
        pw, pwb = ps1(c)
        for cc in range(8):
            MM(c, pw, wo[s][:, cc, :], W.yT[:, cc, :], cc == 0, cc == 7, [wob[s], W.yTb], pwb, skip=False)
        TT(c, "dve", c.xT[:, dc, ts], pw, c.xT[:, dc, ts], ALU.add, list(pwb) + [c.xb[dc][tb]], [c.xb[dc][tb]])


def build_program(cfg):
    nc = bass.Bass("TRN2", target_bir_lowering=False)
    stack = ExitStack()
    c = Ctx()
    c.nc = nc
    c.S = S = Sched()
    c.debug = set(cfg.get("debug", ()))
    c.dbg_done = set()
    c.seq = 0

    def din(name, shape):
        return nc.dram_tensor(name, list(shape), F32, kind="ExternalInput").ap()

    stages = cfg["stages"]
    xT_d = din("xT", (SEQ_PER_CORE, D, L))
    memT_d = din("memT", (SEQ_PER_CORE, D, NMEM))
    pp_d = din("pp", (128, NPP))
    cst_d = din("cst", (128, NCST))
    wd = {}
    for st in stages:
        if st[0] == "ffn":
            _, l, h = st
            wd[("wg", l, h)] = din(f"wg_{l}_{h}", (D, DFF))
            wd[("wu", l, h)] = din(f"wu_{l}_{h}", (D, DFF))
            wd[("wd", l, h)] = din(f"wd_{l}_{h}", (DFF, D))
        else:
            _, l = st
            kind = l % 3
            ncol = {0: 2560, 1: 1736, 2: 2596}[kind]
            wd[("win", l)] = din(f"win_{l}", (D, ncol))
            wd[("wout", l)] = din(f"wout_{l}", (D, D))
            wd[("wkv", l)] = din(f"wkv_{l}", (D, 512))
            if kind == 2:
                wd[("cw", l)] = din(f"cw_{l}", (2, 32, 64, 64))
                c.c2 = {"masks": din("c2_masks", (128, 768)), "czca": din("c2_czca", (128, 128)),
                        "cover": din("c2_cover", (128, 32)), "rope": din("c2_rope", (16, 8, 64))}
    out_d = nc.dram_tensor("outT", [SEQ_PER_CORE, D, L], F32, kind="ExternalOutput").ap()

    TOTAL = 207 * 1024 + 512
    c.arena = nc.alloc_sbuf_tensor("arena", [128, TOTAL], U8)
    R0 = Region(c.arena, 0, TOTAL)
    c.xT = R0.take((8, L), F32)
    c.xb = bufs("x", 8, NTB)
    c.h_off = R0.off
    c.hT = R0.take((8, L), BF16)
    c.hb = bufs("h", 8, NTB)
    c.cst = R0.take((NCST,), F32)
    c.pp = R0.take((NPP,), F32)
    c.ident_bf = R0.take((128,), BF16)
    c.ones_bf = R0.take((128,), BF16)
    c.mi_bf = R0.take((128,), BF16)
    c.constb = Buf("const")
    c.ppb = Buf("pp")
    c.phase_off = R0.off
    c.phase_size = TOTAL - R0.off
    c.tri_f = c.cst[:, CST_TRI:CST_TRI + 128]
    c.cos = c.cst[:, CST_COS:CST_COS + 512].rearrange("p (a b) -> p a b", a=16)
    c.sin = c.cst[:, CST_SIN:CST_SIN + 512].rearrange("p (a b) -> p a b", a=16)
    c.zk = c.cst[:, CST_ZK:CST_ZK + 6]
    c.epsx = c.cst[:, CST_EPSX:CST_EPSX + 6]
    c.cd = c.cst[:, CST_CD:CST_CD + 3]
    c.cneg = c.cst[:, CST_CNEG:CST_CNEG + 128]
    c.bisc = c.cst[:, CST_BISC:CST_BISC + DSA_BIS]
    c.pst = [nc.alloc_psum_tensor(f"ps{i}", [128, 1024], F32) for i in range(4)]
    c.psb = [Buf(f"ps{i}", excl=True) for i in range(8)]
    c.psp = 0
    c.psp2 = 0
    set_rot(c, range(8), (0, 2, 4, 6))

    S.dma("sp", "misc", lambda e: e.dma_start(out=c.cst[:, :], in_=cst_d[:, :]), writes=[c.constb])
    S.dma("sp", "misc", lambda e: e.dma_start(out=c.pp[:, :], in_=pp_d[:, :]), writes=[c.ppb])
    S.seal("misc", [c.constb, c.ppb])
    S.op("dve", lambda e: e.memset(c.ones_bf[:, :], 1.0), writes=[c.constb])
    S.op("dve", lambda e: e.tensor_copy(out=c.ident_bf[:, :], in_=c.cst[:, CST_IDENT:CST_IDENT + 128]),
         reads=[c.constb], writes=[c.constb])
    S.op("dve", lambda e: e.tensor_scalar(out=c.mi_bf[:, :], in0=c.cst[:, CST_IDENT:CST_IDENT + 128], scalar1=MASKM,
                                          scalar2=None, op0=ALU.mult), reads=[c.constb], writes=[c.constb])

    for s in range(SEQ_PER_CORE):
        c.seq = s
        xv = xT_d[s].rearrange("(dc p) t -> p dc t", p=128)
        for dc in range(8):
            S.dma("sp", "xin", lambda e, dc=dc, xv=xv: e.dma_start(out=c.xT[:, dc, :], in_=xv[:, dc, :]),
                  writes=c.xb[dc])
        S.seal("xin", [b for r in c.xb for b in r])
        for st in stages:
            if st[0] == "ffn":
                _, l, h = st
                ffn(c, wd[("wg", l, h)], wd[("wu", l, h)], wd[("wd", l, h)], c.pp[:, (l * 2 + h) * 8:(l * 2 + h) * 8 + 8])
            else:
                _, l = st
                kind = l % 3
                if kind == 0:
                    mixer_ret(c, l, wd[("win", l)], wd[("wout", l)], wd[("wkv", l)], memT_d[s])
                elif kind == 1:
                    mixer_dsa(c, l, wd[("win", l)], wd[("wout", l)], wd[("wkv", l)], memT_d[s])
                else:
                    mixer_nsa(c, l, wd[("win", l)], wd[("wout", l)], wd[("wkv", l)], memT_d[s], wd[("cw", l)])
        ov = out_d[s].rearrange("(dc p) t -> p dc t", p=128)
        for dc in range(8):
            S.dma("sp", "xout", lambda e, dc=dc, ov=ov: e.dma_start(out=ov[:, dc, :], in_=c.xT[:, dc, :]),
                  reads=c.xb[dc])
        S.seal("xout", [b for r in c.xb for b in r])
    S.finish("sp")
    S.emit(nc, stack)
    stack.close()
    return nc


def dbg_dump(c, name, ap, rbufs, seq):
    if name not in c.debug or seq != 0 or name in c.dbg_done:
        return
    c.dbg_done.add(name)
    n = ap.shape[1]
    d = c.nc.dram_tensor("dbg_" + name, [128, n], F32, kind="ExternalOutput").ap()
    c.S.dma("sp", "dbg", lambda e: e.dma_start(out=d[:, :], in_=ap), reads=rbufs)


def default_stages():
    st = []
    for l in range(DEPTH):
        st += [("ffn", l, 0), ("mix", l), ("ffn", l, 1)]
    return st


def kernel(**inputs):
    cfg = inputs.pop("_cfg", None) or {"stages": default_stages()}
    if os.environ.get("MK_STAGES"):
        cfg = {"stages": [tuple(int(v) if v.isdigit() else v for v in t.split(".")) for t in os.environ["MK_STAGES"].split(",")]}
    f32 = lambda a: np.ascontiguousarray(np.asarray(a, dtype=np.float32))
    x = f32(inputs["x"])
    xT = np.ascontiguousarray(x.transpose(0, 2, 1))
    memT = np.ascontiguousarray(f32(inputs["mem"]).transpose(0, 2, 1))
    nc = build_program(cfg)
    shared = {"pp": host_params(inputs), "cst": host_consts()}
    for st in cfg["stages"]:
        if st[0] == "ffn":
            _, l, h = st
            shared[f"wg_{l}_{h}"] = f32(inputs["ffn_w_gate"][l, h])
            shared[f"wu_{l}_{h}"] = f32(inputs["ffn_w_up"][l, h])
            shared[f"wd_{l}_{h}"] = f32(inputs["ffn_w_down"][l, h])
        else:
            _, l = st
            kind, j = l % 3, l // 3
            name = {0: "ret_w_in", 1: "dsa_w_in", 2: "nsa_w_in"}[kind]
            shared[f"win_{l}"] = f32(inputs[name][j])
            shared[f"wout_{l}"] = f32(inputs["w_out"][l])
            shared[f"wkv_{l}"] = f32(inputs["mem_w_kv"][l])
            if kind == 2:
                shared[f"cw_{l}"] = np.ascontiguousarray(np.stack([f32(inputs["nsa_cmp_wk"][j]), f32(inputs["nsa_cmp_wv"][j])]))
                shared.update(host_consts2())
    in_maps = []
    for i in range(NCORES):
        m = dict(shared)
        m["xT"] = xT[i * SEQ_PER_CORE:(i + 1) * SEQ_PER_CORE]
        m["memT"] = memT[i * SEQ_PER_CORE:(i + 1) * SEQ_PER_CORE]
        in_maps.append(m)
    res = run_bass_kernel_spmd(nc, in_maps, core_ids=list(range(NCORES)))
    outT = np.concatenate([r["outT"] for r in res.results], axis=0)
    if cfg.get("debug"):
        kernel.dbg = {k: v for k, v in res.results[0].items() if k.startswith("dbg_")}
    return np.ascontiguousarray(outT.transpose(0, 2, 1))
```

```python
import os
import numpy as np
from contextlib import ExitStack
import concourse.bass as bass
import concourse.mybir as mybir
from concourse.bass_utils import run_bass_kernel_spmd

F32 = mybir.dt.float32
BF16 = mybir.dt.bfloat16
U8 = mybir.dt.uint8
AF = mybir.ActivationFunctionType
ALU = mybir.AluOpType
AX = mybir.AxisListType

D = 1024
L = 2048
DEPTH = 4
DFF = 2816
NMEM = 256
NCORES = 8
SEQ_PER_CORE = 2
EPS = 1e-6
NT = L // 128

ENGS = ("pe", "act", "dve", "pool", "sp")


class Buf:
    __slots__ = ("name", "w", "r", "excl")

    def __init__(self, name, excl=False):
        self.name = name
        self.excl = excl
        self.w = None
        self.r = {}


def bufs(name, *dims):
    if len(dims) == 1:
        return [Buf(f"{name}{i}") for i in range(dims[0])]
    return [bufs(f"{name}{i}_", *dims[1:]) for i in range(dims[0])]


class Sched:
    def __init__(self):
        self.ops = {e: [] for e in ENGS}
        self.seen = {e: {} for e in ENGS}
        self.dma_count = {}
        self.needed = set()
        self.cap = None

    def _deps(self, eng, reads, writes):
        deps = {}
        for b in reads:
            if b.w is not None:
                deps[b.w[:2]] = max(deps.get(b.w[:2], -1), b.w[2])
            if b.excl:
                for k in b.r.values():
                    if not (k[0] == "eng" and k[1] == eng):
                        deps[k[:2]] = max(deps.get(k[:2], -1), k[2])
        for b in writes:
            if b.w is not None and not (b.w[0] == "eng" and b.w[1] == eng):
                deps[b.w[:2]] = max(deps.get(b.w[:2], -1), b.w[2])
            for k in b.r.values():
                if not (k[0] == "eng" and k[1] == eng):
                    deps[k[:2]] = max(deps.get(k[:2], -1), k[2])
        return self._waits(eng, deps)

    def _waits(self, eng, deps):
        waits = []
        seen = self.seen[eng]
        for k, idx in deps.items():
            if seen.get(k, -1) >= idx:
                continue
            seen[k] = idx
            waits.append((k, idx))
            if k[0] == "eng":
                self.needed.add((k[1], idx))
        return waits

    def capture(self, fn):
        assert self.cap is None
        self.cap = []
        fn()
        rec, self.cap = self.cap, None
        return rec

    def commit_interleaved(self, *lists):
        lists = [l for l in lists if l]
        pos = [0] * len(lists)
        total = sum(len(l) for l in lists)
        for _ in range(total):
            k = min((i for i in range(len(lists)) if pos[i] < len(lists[i])),
                    key=lambda i: (pos[i] + 1) / len(lists[i]))
            kind, args = lists[k][pos[k]]
            pos[k] += 1
            (self.op if kind == "op" else self.dma)(*args)

    def op(self, eng, emit, reads=(), writes=()):
        if self.cap is not None:
            self.cap.append(("op", (eng, emit, tuple(reads), tuple(writes))))
            return None
        waits = self._deps(eng, reads, writes)
        idx = len(self.ops[eng])
        key = ("eng", eng, idx)
        self.ops[eng].append(("op", waits, emit, None))
        for b in reads:
            b.r[("eng", eng)] = key
        for b in writes:
            b.w = key
            b.r = {}
        return key

    def dma(self, queue, sem, emit, reads=(), writes=()):
        if self.cap is not None:
            self.cap.append(("dma", (queue, sem, emit, tuple(reads), tuple(writes))))
            return None
        waits = self._deps(queue, reads, writes)
        c = self.dma_count.get(sem, 0) + 1
        self.dma_count[sem] = c
        key = ("dma", sem, c)
        self.ops[queue].append(("dma", waits, emit, sem))
        for b in reads:
            b.r[("dma", sem)] = key
        for b in writes:
            b.w = key
            b.r = {}
        return key

    def seal(self, sem, bl):
        c = self.dma_count[sem]
        for b in bl:
            if b.w is not None and b.w[0] == "dma" and b.w[1] == sem:
                b.w = ("dma", sem, c)
            k = b.r.get(("dma", sem))
            if k is not None:
                b.r[("dma", sem)] = ("dma", sem, c)

    def barrier(self):
        last = {}
        for e in ENGS:
            idx = None
            for i in range(len(self.ops[e]) - 1, -1, -1):
                if self.ops[e][i][0] == "op":
                    idx = i
                    break
            if idx is not None:
                last[("eng", e)] = idx
        for s, cnt in self.dma_count.items():
            last[("dma", s)] = cnt
        for e in ENGS:
            deps = {k: v for k, v in last.items() if k != ("eng", e)}
            waits = self._waits(e, deps)
            if waits:
                self.ops[e].append(("fin", waits, None, None))

    def finish(self, eng="sp"):
        waits = []
        for sem, c in self.dma_count.items():
            if self.seen[eng].get(("dma", sem), -1) < c:
                waits.append((("dma", sem), c))
        self.ops[eng].append(("fin", waits, None, None))

    def emit(self, nc, stack):
        esem = {e: stack.enter_context(nc.semaphore(f"s_{e}")) for e in ENGS}
        dsem = {s: stack.enter_context(nc.semaphore(f"d_{s}")) for s in self.dma_count}
        val = {}
        for e in ENGS:
            n = 0
            for i in range(len(self.ops[e])):
                if (e, i) in self.needed:
                    n += 1
                    val[(e, i)] = n
        ops = self.ops

        def run(e, engine):
            for i, (kind, waits, emit, sem) in enumerate(ops[e]):
                for k, idx in waits:
                    if k[0] == "eng":
                        engine.wait_ge(esem[k[1]], val[(k[1], idx)])
                    else:
                        engine.wait_ge(dsem[k[1]], 16 * idx)
                if kind == "fin":
                    continue
                ins = emit(engine)
                if kind == "dma":
                    ins.then_inc(dsem[sem], 16)
                elif (e, i) in self.needed:
                    ins.then_inc(esem[e], 1)

        with nc.Block() as block:
            @block.sync
            def _(eng):
                run("sp", eng)

            @block.scalar
            def _(eng):
                run("act", eng)

            @block.vector
            def _(eng):
                run("dve", eng)

            @block.gpsimd
            def _(eng):
                run("pool", eng)

            @block.tensor
            def _(eng):
                run("pe", eng)


class Region:
    def __init__(self, t, off, size):
        self.t = t
        self.base = off
        self.off = off
        self.end = off + size

    def take(self, shape, dtype, parts=None):
        esz = 2 if dtype == BF16 else 4
        n = int(np.prod(shape))
        nb = ((n * esz + 31) // 32) * 32
        assert self.off + nb <= self.end, ("SBUF region overflow", shape, self.off, self.end)
        ap = self.t[:, self.off:self.off + n * esz].bitcast(dtype)
        self.off += nb
        if len(shape) > 1:
            names = " ".join(f"d{i}" for i in range(len(shape)))
            kw = {f"d{i}": int(s) for i, s in enumerate(shape)}
            ap = ap.rearrange(f"p ({names}) -> p {names}", **kw)
        return ap


class Ctx:
    pass


def MM(c, out, lhsT, rhs, start, stop, reads, writes, skip=True):
    c.S.op("pe", lambda e: e.matmul(out, lhsT, rhs, start=start, stop=stop, skip_group_check=skip), reads, writes)


def TR(c, out, in_, reads, writes):
    n = in_.shape[0]
    c.S.op("pe", lambda e: e.transpose(out, in_, c.ident_bf[0:n, 0:n]), list(reads) + [c.constb], writes)


def ACTF(c, out, in_, func, reads, writes, scale=1.0, bias=0.0):
    c.S.op("act", lambda e: e.activation(out=out, in_=in_, func=func, bias=bias, scale=scale), reads, writes)


def TT(c, eng, out, in0, in1, op, reads, writes):
    c.S.op(eng, lambda e: e.tensor_tensor(out=out, in0=in0, in1=in1, op=op), reads, writes)


def TS(c, eng, out, in0, s1, s2, op0, op1, reads, writes):
    if s2 is None:
        c.S.op(eng, lambda e: e.tensor_scalar(out=out, in0=in0, scalar1=s1, scalar2=None, op0=op0), reads, writes)
    else:
        c.S.op(eng, lambda e: e.tensor_scalar(out=out, in0=in0, scalar1=s1, scalar2=s2, op0=op0, op1=op1), reads, writes)


def STT(c, out, in0, scalar, in1, op0, op1, reads, writes):
    c.S.op("dve", lambda e: e.scalar_tensor_tensor(out=out, in0=in0, scalar=scalar, in1=in1, op0=op0, op1=op1),
           reads, writes)


def RED(c, out, in_, op, reads, writes):
    c.S.op("dve", lambda e: e.tensor_reduce(out=out, in_=in_, axis=AX.X, op=op), reads, writes)


def RCP(c, out, in_, reads, writes):
    c.S.op("dve", lambda e: e.reciprocal(out=out, in_=in_), reads, writes)


def CP(c, eng, out, in_, reads, writes):
    if eng == "act":
        c.S.op("act", lambda e: e.copy(out=out, in_=in_), reads, writes)
    else:
        c.S.op(eng, lambda e: e.tensor_copy(out=out, in_=in_), reads, writes)


def MSET(c, eng, ap, val, writes):
    c.S.op(eng, lambda e: e.memset(ap, val), (), writes)


def bank(c, b):
    return c.pst[b // 2][:, (b % 2) * 512:(b % 2) * 512 + 512]


def ps1(c):
    b = c.rot1[c.psp % len(c.rot1)]
    c.psp += 1
    return bank(c, b), [c.psb[b]]


def ps2(c):
    b = c.rot2[c.psp2 % len(c.rot2)]
    c.psp2 += 1
    return c.pst[b // 2][:, :], [c.psb[b], c.psb[b + 1]]


def set_rot(c, rot1, rot2):
    c.rot1, c.rot2 = list(rot1), list(rot2)


CST_IDENT, CST_TRI, CST_COS, CST_SIN, CST_ZK, CST_EPSX, CST_CD = 0, 128, 256, 768, 1280, 1286, 1292
CST_CNEG = 1296
CST_BISC = 1296 + 128
NCST = 1296 + 128 + 32
MASKM = 29952.0
PP_L0 = 64
PP_PL = 464
PP_MIX, PP_MEMN, PP_MQN, PP_MKN, PP_A, PP_B, PP_C, PP_D, PP_POS = 0, 8, 16, 80, 144, 208, 272, 336, 400
NPP = PP_L0 + DEPTH * PP_PL


def host_consts():
    cst = np.zeros((128, NCST), np.float64)
    cst[:, CST_IDENT:CST_IDENT + 128] = np.eye(128)
    j = np.arange(128)[:, None]
    i = np.arange(128)[None, :]
    cst[:, CST_TRI:CST_TRI + 128] = (i >= j)
    cst[:, CST_CNEG:CST_CNEG + 128] = np.where(i <= j, 0.0, -1e30)
    cst[:, CST_BISC:CST_BISC + 32] = 2.0 ** (-np.arange(32))[None, :]
    inv = (np.float32(10000.0) ** (-np.arange(32, dtype=np.float32) / np.float32(32))).astype(np.float32)
    pos = (np.arange(16)[None, :, None] * 128 + np.arange(128)[:, None, None]).astype(np.float32)
    ang = (pos * inv[None, None, :]).astype(np.float32)
    cst[:, CST_COS:CST_COS + 512] = np.cos(ang).reshape(128, 512)
    cst[:, CST_SIN:CST_SIN + 512] = np.sin(ang).reshape(128, 512)
    h = np.arange(6)
    gam = 1.0 - 2.0 ** (-5.0 - h)
    t = np.arange(128)[:, None]
    cst[:, CST_ZK:CST_ZK + 6] = gam[None, :] ** (-(t + 1.0)) / 8.0
    cst[:, CST_EPSX:CST_EPSX + 6] = EPS * gam[None, :] ** (-2.0 * (t + 1.0))
    for pair in range(3):
        cst[0:64, CST_CD + pair] = gam[2 * pair] ** 128
        cst[64:128, CST_CD + pair] = gam[2 * pair + 1] ** 128
    return cst.astype(np.float32)


def host_consts2():
    t = np.arange(128)[:, None]
    s_ = np.arange(128)[None, :]
    masks = np.zeros((128, 768), np.float32)
    masks[:, 0:128] = (s_ <= t)
    masks[:, 128:256] = (s_ > t)
    m = np.arange(256)[None, :] - 128
    masks[:, 256:512] = (16 * m + 31 <= t)
    for k in range(8):
        masks[k, 512 + 128 + k] = 1.0
    czca = np.zeros((128, 128), np.float32)
    mm = np.arange(64)[None, :] - 32
    qb = (t >= 64).astype(np.int64)
    causal = mm <= qb
    forced = (mm == qb) | (mm == qb - 1)
    czca[:, 0:64] = causal
    czca[:, 64:128] = np.where(causal, np.where(forced, 1e9, 0.0), -1e30)
    starts = np.arange(127) * 16
    sst = np.arange(32) * 64
    cover = np.zeros((128, 32), np.float32)
    cover[:127] = ((starts[:, None] < sst[None, :] + 64) & (starts[:, None] + 32 > sst[None, :]))
    inv = (np.float32(10000.0) ** (-np.arange(32, dtype=np.float32) / np.float32(32))).astype(np.float32)
    rope = np.zeros((16, 8, 64), np.float32)
    for ti in range(16):
        for k in range(8):
            n = k if ti == 0 else 8 * ti - 1 + k
            pos = np.float32(16 * n + 31)
            ang = (pos * inv).astype(np.float32)
            rope[ti, k, 0:32] = np.cos(ang)
            rope[ti, k, 32:64] = np.sin(ang)
    return {"c2_masks": masks, "c2_czca": czca, "c2_cover": cover, "c2_rope": rope}


def host_params(inp):
    pp = np.zeros((128, NPP), np.float32)
    f32 = lambda a: np.asarray(a, np.float32)
    pp[:, 0:64] = f32(inp["ffn_norm"]).reshape(DEPTH, 2, 8, 128).transpose(3, 0, 1, 2).reshape(128, 64)
    for l in range(DEPTH):
        b = PP_L0 + l * PP_PL
        pp[:, b + PP_MIX:b + PP_MIX + 8] = f32(inp["mix_norm"][l]).reshape(8, 128).T
        pp[:, b + PP_MEMN:b + PP_MEMN + 8] = f32(inp["mem_norm"][l]).reshape(8, 128).T
        pp[:, b + PP_MQN:b + PP_MQN + 64] = f32(inp["mem_qn"][l])[None, :]
        pp[:, b + PP_MKN:b + PP_MKN + 64] = f32(inp["mem_kn"][l])[None, :]
        kind, j = l % 3, l // 3
        if kind == 1:
            pp[:, b + PP_A:b + PP_A + 64] = f32(inp["dsa_qn"][j])[None, :]
            pp[:, b + PP_B:b + PP_B + 64] = f32(inp["dsa_kn"][j])[None, :]
        elif kind == 2:
            pp[:, b + PP_A:b + PP_A + 64] = f32(inp["nsa_qn"][j])[None, :]
            pp[:, b + PP_B:b + PP_B + 64] = f32(inp["nsa_kn"][j][0])[None, :]
            pp[:, b + PP_C:b + PP_C + 64] = f32(inp["nsa_kn"][j][1])[None, :]
            pp[:, b + PP_D:b + PP_D + 64] = f32(inp["nsa_kn"][j][2])[None, :]
            pp[0:64, b + PP_POS:b + PP_POS + 32] = f32(inp["nsa_cmp_pos_k"][j]).T
            pp[0:64, b + PP_POS + 32:b + PP_POS + 64] = f32(inp["nsa_cmp_pos_v"][j]).T
    return pp


def rmsnorm_block(c, src, src_bufs, ntok, g_ap, sq, sqb, rstd, rstdb, out, out_bufs):
    ACTF(c, sq[:, :, 0:ntok], src, AF.Square, src_bufs, [sqb])
    ps, pb = ps1(c)
    for dc in range(8):
        MM(c, ps[:, 0:ntok], c.ones_bf[:, :], sq[:, dc, 0:ntok], dc == 0, dc == 7, [sqb, c.constb], pb, skip=False)
    ACTF(c, rstd[:, 0:ntok], ps[:, 0:ntok], AF.Ln, pb, [rstdb], scale=1.0 / D, bias=float(EPS))
    ACTF(c, rstd[:, 0:ntok], rstd[:, 0:ntok], AF.Exp, [rstdb], [rstdb], scale=-0.5)
    for dc in range(8):
        STT(c, out[:, dc, :], src[:, dc, :], g_ap[:, dc:dc + 1], rstd[:, 0:ntok], ALU.mult, ALU.mult,
            [src_bufs[dc], rstdb, c.ppb], [out_bufs[dc]])


NG = 11
NTB = 4
WSLOTS = 3


def ffn(c, wg_d, wu_d, wd_d, g_ap):
    S = c.S
    S.barrier()
    R = Region(c.arena, c.phase_off, c.phase_size)
    sq = R.take((8, 512), BF16)
    sqb = Buf("sq")
    rstd = R.take((512,), F32)
    rstdb = Buf("rstd")
    sg = R.take((512,), BF16)
    sgb = Buf("sg")
    act = [[R.take((512,), BF16) for _ in range(2)] for _ in range(2)]
    actb = bufs("act", 2, 2)
    wg = [R.take((8, 256), BF16) for _ in range(WSLOTS)]
    wu = [R.take((8, 256), BF16) for _ in range(WSLOTS)]
    wd = [R.take((2, D), BF16) for _ in range(WSLOTS)]
    wgb, wub, wdb = bufs("wg", WSLOTS), bufs("wu", WSLOTS), bufs("wd", WSLOTS)
    ps = [c.pst[b // 2][:, (b % 2) * 512:(b % 2) * 512 + 512] for b in range(8)]

    wg_v = wg_d.rearrange("(dc p) f -> p dc f", p=128)
    wu_v = wu_d.rearrange("(dc p) f -> p dc f", p=128)
    wd_v = wd_d.rearrange("(fc p) d -> p fc d", p=128)
    wn = [0]

    def load_w(gi):
        s = wn[0] % WSLOTS
        wn[0] += 1
        fs = slice(gi * 256, (gi + 1) * 256)
        S.dma("pool", f"wg{s}", lambda e: e.dma_start(out=wg[s][:, :, :], in_=wg_v[:, :, fs]), writes=[wgb[s]])
        S.dma("pool", f"wu{s}", lambda e: e.dma_start(out=wu[s][:, :, :], in_=wu_v[:, :, fs]), writes=[wub[s]])
        S.dma("pool", f"wd{s}", lambda e: e.dma_start(out=wd[s][:, :, :], in_=wd_v[:, 2 * gi:2 * gi + 2, :]),
              writes=[wdb[s]])
        return s

    slots = {}
    for gi in range(min(WSLOTS - 1, NG)):
        slots[gi] = load_w(gi)

    def norm_blk(tb):
        ts = slice(tb * 512, (tb + 1) * 512)
        rmsnorm_block(c, c.xT[:, :, ts], [c.xb[dc][tb] for dc in range(8)], 512, g_ap, sq, sqb, rstd, rstdb,
                      c.hT[:, :, ts], [c.hb[dc][tb] for dc in range(8)])

    norm_blk(0)

    pend = None
    steps = [(gi, tb) for gi in range(NG) for tb in range(NTB)]
    for it in range(len(steps) + 1):
        cur = steps[it] if it < len(steps) else None
        par = it % 2
        gu = []
        if cur is not None:
            gi, tb = cur
            s = slots[gi]
            ts = slice(tb * 512, (tb + 1) * 512)
            for fcl in range(2):
                for which in range(2):
                    w = wg[s] if which == 0 else wu[s]
                    wb = wgb[s] if which == 0 else wub[s]
                    bank = fcl * 2 + which
                    for dc in range(8):
                        gu.append((ps[bank], w[:, dc, fcl * 128:(fcl + 1) * 128], c.hT[:, dc, ts], dc == 0, dc == 7,
                                   [wb, c.hb[dc][tb]], [c.psb[bank]]))
        dn = []
        if pend is not None:
            ps_, ptb, ppar = pend
            pts = slice(ptb * 512, (ptb + 1) * 512)
            for dc in range(8):
                bank = 4 + dc % 4
                for fcl in range(2):
                    dn.append((ps[bank], wd[ps_][:, fcl, dc * 128:(dc + 1) * 128], act[ppar][fcl][:, :], fcl == 0,
                               fcl == 1, [wdb[ps_], actb[ppar][fcl]], [c.psb[bank]],
                               dc if fcl == 1 else None, bank, pts, ptb))
        gi_ = 0
        di_ = 0
        while gi_ < len(gu) or di_ < len(dn):
            for _ in range(4):
                if gi_ < len(gu):
                    o_, l_, r_, st_, sp_, rd_, wr_ = gu[gi_]
                    MM(c, o_, l_, r_, st_, sp_, rd_, wr_, skip=False)
                    gi_ += 1
                    if gi_ % 16 == 0:
                        fcl = gi_ // 16 - 1
                        ACTF(c, sg[:, :], ps[fcl * 2], AF.Silu, [c.psb[fcl * 2]], [sgb])
                        TT(c, "dve", act[par][fcl][:, :], ps[fcl * 2 + 1], sg[:, :], ALU.mult,
                           [c.psb[fcl * 2 + 1], sgb], [actb[par][fcl]])
            for _ in range(2):
                if di_ < len(dn):
                    o_, l_, r_, st_, sp_, rd_, wr_, dc, bank, pts, ptb = dn[di_]
                    MM(c, o_, l_, r_, st_, sp_, rd_, wr_, skip=False)
                    di_ += 1
                    if dc is not None:
                        STT(c, c.xT[:, dc, pts], ps[bank], 0.5, c.xT[:, dc, pts], ALU.mult, ALU.add,
                            [c.psb[bank], c.xb[dc][ptb]], [c.xb[dc][ptb]])
        pend = (slots[cur[0]], cur[1], par) if cur is not None else None
        if cur is not None and cur[0] == 0 and cur[1] + 1 < NTB:
            norm_blk(cur[1] + 1)
        if cur is not None and cur[1] == 0 and cur[0] + WSLOTS - 1 < NG:
            slots[cur[0] + WSLOTS - 1] = load_w(cur[0] + WSLOTS - 1)


def head_norm(c, src, src_bufs, H, gain, out, out_bufs, scr, scrb, ss, ssb, eng2="pool"):
    n = src.shape[0]
    s3 = scr[0:n, 0:H * 64].rearrange("p (h d) -> p h d", d=64)
    ACTF(c, s3, src, AF.Square, src_bufs, [scrb])
    RED(c, ss[0:n, 0:H], s3, ALU.add, [scrb], [ssb])
    ACTF(c, ss[0:n, 0:H], ss[0:n, 0:H], AF.Ln, [ssb], [ssb], scale=1.0 / 64, bias=float(EPS))
    ACTF(c, ss[0:n, 0:H], ss[0:n, 0:H], AF.Exp, [ssb], [ssb], scale=-0.5)
    TT(c, "dve", s3, src, ss[0:n, 0:H].unsqueeze(2).broadcast_to([n, H, 64]), ALU.mult, list(src_bufs) + [ssb], [scrb])
    TT(c, eng2, out, s3, gain[0:n, :].unsqueeze(1).broadcast_to([n, H, 64]), ALU.mult, [scrb, c.ppb], out_bufs)


def mem_prep(c, R, memT_d, wkv_d, pbase, k_memT, v_mem1, kvb):
    S = c.S
    memT = R.take((8, NMEM), F32)
    memb = bufs("memT", 8)
    sqm = R.take((8, NMEM), BF16)
    sqmb = Buf("sqm")
    rstdm = R.take((NMEM,), F32)
    rstdmb = Buf("rstdm")
    memn = R.take((8, NMEM), BF16)
    memnb = bufs("memn", 8)
    wkv = R.take((8, 512), BF16)
    wkvb = Buf("wkv")
    ktok = R.take((256,), BF16)
    ktokb = Buf("ktok")
    scr = R.take((256,), F32)
    scrb = Buf("mscr")
    ss = R.take((8,), F32)
    ssb = Buf("mss")
    mv = memT_d.rearrange("(dc p) m -> p dc m", p=128)
    for dc in range(8):
        S.dma("sp", "memin", lambda e, dc=dc: e.dma_start(out=memT[:, dc, :], in_=mv[:, dc, :]), writes=[memb[dc]])
    S.seal("memin", memb)
    S.dma("pool", "wkv", lambda e: e.dma_start(out=wkv[:, :, :], in_=wkv_d.rearrange("(dc p) f -> p dc f", p=128)),
          writes=[wkvb])
    rmsnorm_block(c, memT, memb, NMEM, c.pp[:, pbase + PP_MEMN:pbase + PP_MEMN + 8], sqm, sqmb, rstdm, rstdmb,
                  memn, memnb)
    MSET(c, "pool", v_mem1[:, :, :, 64:65], 1.0, [kvb])
    for mt in range(2):
        ps, pb = ps1(c)
        for dc in range(8):
            MM(c, ps, memn[:, dc, mt * 128:(mt + 1) * 128], wkv[:, dc, :], dc == 0, dc == 7, [memnb[dc], wkvb], pb,
               skip=False)
        head_norm(c, ps[:, 0:256].rearrange("p (h d) -> p h d", d=64), pb, 4,
                  c.pp[:, pbase + PP_MKN:pbase + PP_MKN + 64], ktok.rearrange("p (h d) -> p h d", d=64), [ktokb],
                  scr, scrb, ss, ssb)
        CP(c, "act", v_mem1[:, mt, :, 0:64], ps[:, 256:512].rearrange("p (h d) -> p h d", d=64), pb, [kvb])
        pt, ptb = ps1(c)
        ptv = pt.bitcast(BF16)
        for cc in range(2):
            TR(c, ptv[:, cc * 128:(cc + 1) * 128], ktok[:, cc * 128:(cc + 1) * 128], [ktokb], ptb)
        CP(c, "dve", k_memT[:, :, mt * 128:(mt + 1) * 128], ptv[:, 0:256].rearrange("p (c m) -> p c m", c=2), ptb, [kvb])


def tile_tail(c, W, qm_ps, qm_pb, tl):
    head_norm(c, qm_ps.rearrange("p (h d) -> p h d", d=64), qm_pb, 4, c.pp[:, W.pbase + PP_MQN:W.pbase + PP_MQN + 64],
              W.qm_tok.rearrange("p (h d) -> p h d", d=64), [W.qm_tokb], W.hscr, W.hscrb, W.hss, W.hssb)
    tile_tail_b(c, W, tl)


def tile_tail_b(c, W, tl):
    pt, ptb = ps1(c)
    ptv = pt.bitcast(BF16)
    for cc in range(2):
        TR(c, ptv[:, cc * 128:(cc + 1) * 128], W.qm_tok[:, cc * 128:(cc + 1) * 128], [W.qm_tokb], ptb)
    CP(c, "act", W.qmT[0][0:64], ptv[0:64, 0:256].rearrange("p (c m) -> p c m", c=2), ptb, [W.qmTb])
    CP(c, "act", W.qmT[1][64:128], ptv[64:128, 0:256].rearrange("p (c m) -> p c m", c=2), ptb, [W.qmTb])
    if c.rot2:
        pm, pmb = ps2(c)
        pms = [pm[:, 0:512], pm[:, 512:1024]]
    for mt in range(2):
        if c.rot2:
            pmt, pmtb = pms[mt], pmb[mt]
        else:
            pmt, pl = ps1(c)
            pmtb = pl[0]
        for h in range(4):
            hp = h % 2
            MM(c, pmt[:, h * 128:(h + 1) * 128], W.k_memT[:, h // 2, mt * 128:(mt + 1) * 128],
               W.qmT[hp][:, h // 2, :], h == 0, True, [W.kvb, W.qmTb], [pmtb])
        ACTF(c, W.PTm[:, mt * 512:(mt + 1) * 512], pmt, AF.Exp, [pmtb], [W.PTmb], scale=0.125)
    po, pob = ps1(c)
    first = True
    for h in range(4):
        for mt in range(2):
            MM(c, po[:, h * 65:(h + 1) * 65], W.PTm[:, (mt * 4 + h) * 128:(mt * 4 + h + 1) * 128], W.v_mem1[:, mt, h, :],
               first, mt == 1 and h == 3, [W.PTmb, W.kvb], pob)
            first = False
    po3 = po[:, 0:260].rearrange("p (h e) -> p h e", e=65)
    RCP(c, W.den4, po3[:, :, 64:65], pob, [W.den4b])
    TT(c, "dve", W.y_tok[:, 768:1024].rearrange("p (h d) -> p h d", d=64), po3[:, :, 0:64],
       W.den4.broadcast_to([128, 4, 64]), ALU.mult, list(pob) + [W.den4b], [W.y_tokb])
    py, pyb = ps1(c)
    pyv = py.bitcast(BF16)
    for cc in range(8):
        TR(c, pyv[:, cc * 128:(cc + 1) * 128], W.y_tok[:, cc * 128:(cc + 1) * 128], [W.y_tokb], pyb)
    CP(c, "act", W.yT[:, :, tl * 128:(tl + 1) * 128], pyv.rearrange("p (c t) -> p c t", c=8), pyb, [W.yTb])


def wout_block(c, W, tb):
    ts = slice(tb * 512, (tb + 1) * 512)
    for dc in range(8):
        pw, pwb = ps1(c)
        for cc in range(8):
            MM(c, pw, W.w_out[:, cc, dc * 128:(dc + 1) * 128], W.yT[:, cc, :], cc == 0, cc == 7, [W.w_outb, W.yTb], pwb,
               skip=False)
        TT(c, "dve", c.xT[:, dc, ts], pw, c.xT[:, dc, ts], ALU.add, list(pwb) + [c.xb[dc][tb]], [c.xb[dc][tb]])


def load_wout(c, W, R, wout_d):
    W.w_out = R.take((8, D), BF16)
    W.w_outb = Buf("w_out")
    wv = wout_d.rearrange("(cc p) d -> p cc d", p=128)
    for cc in range(0, 8, 4):
        c.S.dma("pool", "wout", lambda e, cc=cc: e.dma_start(out=W.w_out[:, cc:cc + 4, :], in_=wv[:, cc:cc + 4, :]),
                writes=[W.w_outb])
    c.S.seal("wout", [W.w_outb])


def tail_bufs(c, W, R, hscr_n=768):
    W.qm_tok = R.take((256,), BF16)
    W.qm_tokb = Buf("qm_tok")
    W.qmT = [R.take((2, 128), BF16) for _ in range(2)]
    W.qmTb = Buf("qmT")
    for a in range(2):
        MSET(c, "pool", W.qmT[a][:, :, :], 0.0, [W.qmTb])
    W.PTm = R.take((1024,), BF16)
    W.PTmb = Buf("PTm")
    W.den4 = R.take((4, 1), F32)
    W.den4b = Buf("den4")
    W.hscr = R.take((hscr_n,), F32)
    W.hscrb = Buf("hscr")
    W.hss = R.take((16,), F32)
    W.hssb = Buf("hss")
    W.y_tok = R.take((1024,), BF16)
    W.y_tokb = Buf("y_tok")
    W.k_memT = R.take((2, NMEM), BF16)
    W.v_mem1 = R.take((2, 4, 65), BF16)
    W.kvb = Buf("memkv")


def mixer_ret(c, l, win_d, wout_d, wkv_d, memT_d):
    S = c.S
    S.barrier()
    set_rot(c, range(8), (0, 2, 4, 6))
    W = Ctx()
    W.pbase = PP_L0 + l * PP_PL
    R1 = Region(c.arena, c.h_off, 32 * 1024)
    R2 = Region(c.arena, c.phase_off, c.phase_size)
    hblk = [R1.take((8, 512), BF16) for _ in range(2)]
    hblkb = bufs("hblk", 2, 8)
    W.yT = R1.take((8, 512), BF16)
    W.yTb = Buf("yT")
    sq = R1.take((8, 512), BF16)
    sqb = Buf("sq")
    w_in = R2.take((8, 2560), BF16)
    w_inb = Buf("w_in")
    load_wout(c, W, R2, wout_d)
    rstd = R2.take((512,), F32)
    rstdb = Buf("rstd")
    tail_bufs(c, W, R2, hscr_n=256)
    tq2 = [R2.take((384,), F32) for _ in range(2)]
    tq = [tq2[0], tq2[1], tq2[0], tq2[1]]
    tqb2 = bufs("tq", 2)
    tqb = [tqb2[0], tqb2[1], tqb2[0], tqb2[1]]
    rk = R2.take((768,), F32)
    rkb = Buf("rk")
    scr = R2.take((768,), F32)
    scrb = Buf("scr")
    state = R2.take((3, 128), F32)
    state_bf = R2.take((3, 128), BF16)
    stb = Buf("state")
    stbb = Buf("state_bf")
    sm = R2.take((8, 6), F32)
    smb = Buf("sm")
    qk_tok = [R2.take((768,), BF16) for _ in range(2)]
    qk_tokb = bufs("qk_tok", 2)
    qT = [[R2.take((3, 128), BF16) for _ in range(2)] for _ in range(2)]
    kT = [R2.take((3, 128), BF16) for _ in range(2)]
    qkTb = bufs("qkT", 2)
    Sm = [R2.take((6, 128), BF16) for _ in range(2)]
    Smb = bufs("Sm", 2)
    v_tok = [R2.take((768,), BF16) for _ in range(2)]
    v_tokb = bufs("v_tok", 2)
    sgt = [R2.take((768,), BF16) for _ in range(2)]
    sgtb = bufs("sgt", 2)
    qm_toks = [W.qm_tok, R2.take((256,), BF16)]
    qm_tokbs = [W.qm_tokb, Buf("qm_tok1")]
    wv = win_d.rearrange("(dc p) f -> p dc f", p=128)
    for dc in range(0, 8, 2):
        S.dma("pool", "win", lambda e, dc=dc: e.dma_start(out=w_in[:, dc:dc + 2, :], in_=wv[:, dc:dc + 2, :]),
              writes=[w_inb])
    S.seal("win", [w_inb])
    Rt = Region(c.arena, c.h_off, 32 * 1024)
    mem_prep(c, Rt, memT_d, wkv_d, W.pbase, W.k_memT, W.v_mem1, W.kvb)
    S.barrier()
    MSET(c, "dve", state[:, :, :], 0.0, [stb])
    MSET(c, "dve", state_bf[:, :, :], 0.0, [stbb])
    for p_ in range(2):
        for a in range(2):
            MSET(c, "pool", qT[p_][a][:, :, :], 0.0, [qkTb[p_]])

    g_ap = c.pp[:, W.pbase + PP_MIX:W.pbase + PP_MIX + 8]
    tri3 = c.tri_f.unsqueeze(1).broadcast_to([128, 6, 128])

    def stage_a(ti):
        set_rot(c, (0, 1, 2, 3), (0, 2))
        tb, tl = ti // 4, ti % 4
        par = ti % 2
        hb, hbb = hblk[tb % 2], hblkb[tb % 2]
        if tl == 0:
            ts = slice(tb * 512, (tb + 1) * 512)
            rmsnorm_block(c, c.xT[:, :, ts], [c.xb[dc][tb] for dc in range(8)], 512, g_ap, sq, sqb, rstd, rstdb, hb, hbb)

        def proj(out_ap, c0, c1, pbufs):
            for dc in range(8):
                MM(c, out_ap, hb[:, dc, tl * 128:(tl + 1) * 128], w_in[:, dc, c0:c1], dc == 0, dc == 7,
                   [hbb[dc], w_inb], pbufs, skip=False)

        P, Pb = ps2(c)
        proj(P[:, 0:384], 0, 384, [Pb[0]])
        proj(P[:, 512:896], 384, 768, [Pb[1]])
        v4 = P.rearrange("p (a b) -> p a b", a=2)[:, :, 0:384].rearrange("p a (h d) -> p a h d", d=64)
        x1, x2 = v4[:, :, :, 0:32], v4[:, :, :, 32:64]
        cosb = c.cos[:, ti, :].unsqueeze(1).unsqueeze(1).broadcast_to([128, 2, 6, 32])
        sinb = c.sin[:, ti, :].unsqueeze(1).unsqueeze(1).broadcast_to([128, 2, 6, 32])
        t4 = [t.rearrange("p (a h d) -> p a h d", a=2, h=6) for t in tq]
        rk4 = rk.rearrange("p (a h d) -> p a h d", a=2, h=6)
        TT(c, "dve", t4[0], x1, cosb, ALU.mult, Pb + [c.constb], [tqb[0]])
        TT(c, "dve", t4[1], x2, sinb, ALU.mult, Pb + [c.constb], [tqb[1]])
        TT(c, "pool", rk4[:, :, :, 0:32], t4[0], t4[1], ALU.subtract, [tqb[0], tqb[1]], [rkb])
        TT(c, "dve", t4[2], x2, cosb, ALU.mult, Pb + [c.constb], [tqb[2]])
        TT(c, "dve", t4[3], x1, sinb, ALU.mult, Pb + [c.constb], [tqb[3]])
        TT(c, "pool", rk4[:, :, :, 32:64], t4[2], t4[3], ALU.add, [tqb[2], tqb[3]], [rkb])
        qkt, qktb = qk_tok[par], qk_tokb[par]
        CP(c, "pool", qkt[:, 0:384], rk[:, 0:384], [rkb], [qktb])
        TT(c, "pool", qkt[:, 384:768].rearrange("p (h d) -> p h d", d=64),
           rk[:, 384:768].rearrange("p (h d) -> p h d", d=64),
           c.zk.unsqueeze(2).broadcast_to([128, 6, 64]), ALU.mult, [rkb, c.constb], [qktb])
        pt, ptb = ps1(c)
        ptv = pt.bitcast(BF16)
        for n in range(6):
            TR(c, ptv[:, n * 128:(n + 1) * 128], qkt[:, n * 128:(n + 1) * 128], [qktb], ptb)
        CP(c, "act", qT[par][0][0:64], ptv[0:64, 0:384].rearrange("p (n t) -> p n t", n=3), ptb, [qkTb[par]])
        CP(c, "act", qT[par][1][64:128], ptv[64:128, 0:384].rearrange("p (n t) -> p n t", n=3), ptb, [qkTb[par]])
        CP(c, "act", kT[par], ptv[:, 384:768].rearrange("p (n t) -> p n t", n=3), ptb, [qkTb[par]])
        P2, P2b = ps2(c)
        for h in range(6):
            hp = h % 2
            MM(c, P2[:, h * 128:(h + 1) * 128], kT[par][:, h // 2, :], qT[par][hp][:, h // 2, :], h % 4 == 0, True,
               [qkTb[par]], [P2b[h // 4]])
        TT(c, "dve", Sm[par], P2[:, 0:768].rearrange("p (h t) -> p h t", h=6), tri3, ALU.mult, P2b + [c.constb], [Smb[par]])
        P3, P3b = ps2(c)
        proj(P3[:, 0:512], 768, 1280, [P3b[0]])
        proj(P3[:, 512:768], 1280, 1536, [P3b[1]])
        CP(c, "act", v_tok[par], P3[:, 0:768], P3b, [v_tokb[par]])
        P6, P6b = ps2(c)
        proj(P6[:, 0:512], 1536, 2048, [P6b[0]])
        proj(P6[:, 512:768], 2048, 2304, [P6b[1]])
        ACTF(c, sgt[par], P6[:, 0:768], AF.Silu, P6b, [sgtb[par]])
        P7, P7b = ps1(c)
        proj(P7[:, 0:256], 2304, 2560, P7b)
        head_norm(c, P7[:, 0:256].rearrange("p (h d) -> p h d", d=64), P7b, 4,
                  c.pp[:, W.pbase + PP_MQN:W.pbase + PP_MQN + 64],
                  qm_toks[par].rearrange("p (h d) -> p h d", d=64), [qm_tokbs[par]], W.hscr, W.hscrb, W.hss, W.hssb)

    def stage_b(ti):
        set_rot(c, (4, 5, 6, 7), (4, 6))
        tb, tl = ti // 4, ti % 4
        par = ti % 2
        P4, P4b = ps2(c)
        for h in range(6):
            hp = h % 2
            MM(c, P4[:, h * 128:(h + 1) * 128], Sm[par][:, h, :], v_tok[par][:, h * 128:(h + 1) * 128], h % 4 == 0,
               ti == 0, [Smb[par], v_tokb[par]], [P4b[h // 4]])
            if ti > 0:
                MM(c, P4[:, h * 128:(h + 1) * 128], qT[par][hp][:, h // 2, :], state_bf[:, h // 2, :], False, True,
                   [qkTb[par], stbb], [P4b[h // 4]])
        if ti < NT - 1:
            P5, P5b = ps2(c)
            for pair in range(3):
                MM(c, P5[:, pair * 256:(pair + 1) * 256], qk_tok[par][:, 384 + pair * 128:384 + (pair + 1) * 128],
                   v_tok[par][:, pair * 256:(pair + 1) * 256], pair % 2 == 0, True, [qk_tokb[par], v_tokb[par]],
                   [P5b[pair // 2]])
            P5v = P5[:, 0:768].rearrange("p (a e) -> p a e", a=3)
            for half in range(2):
                rs = slice(half * 64, (half + 1) * 64)
                TT(c, "dve", state[rs, :, :], P5v[rs, :, half * 128:(half + 1) * 128], state[rs, :, :], ALU.add,
                   P5b + [stb], [stb])
                TT(c, "pool", state[rs, :, :], state[rs, :, :], c.cd[rs, :].unsqueeze(2).broadcast_to([64, 3, 128]),
                   ALU.mult, [stb, c.constb], [stb])
                CP(c, "pool", state_bf[rs, :, :], state[rs, :, :], [stb], [stbb])
        o3 = P4[:, 0:768].rearrange("p (h e) -> p h e", h=6)
        scr3 = scr.rearrange("p (h e) -> p h e", h=6)
        RED(c, sm[:, 0, :], o3, ALU.add, P4b, [smb])
        ACTF(c, scr, P4[:, 0:768], AF.Square, P4b, [scrb])
        RED(c, sm[:, 1, :], scr3, ALU.add, [scrb], [smb])
        TS(c, "dve", sm[:, 2, :], sm[:, 0, :], 1.0 / 128, None, ALU.mult, None, [smb], [smb])
        TT(c, "dve", sm[:, 3, :], sm[:, 2, :], sm[:, 2, :], ALU.mult, [smb], [smb])
        STT(c, sm[:, 4, :], sm[:, 1, :], 1.0 / 128, c.epsx, ALU.mult, ALU.add, [smb, c.constb], [smb])
        TT(c, "dve", sm[:, 4, :], sm[:, 4, :], sm[:, 3, :], ALU.subtract, [smb], [smb])
        ACTF(c, sm[:, 4, :], sm[:, 4, :], AF.Ln, [smb], [smb])
        ACTF(c, sm[:, 4, :], sm[:, 4, :], AF.Exp, [smb], [smb], scale=-0.5)
        TT(c, "dve", scr3, o3, sm[:, 2, :].unsqueeze(2).broadcast_to([128, 6, 128]), ALU.subtract, P4b + [smb], [scrb])
        TT(c, "pool", scr3, scr3, sm[:, 4, :].unsqueeze(2).broadcast_to([128, 6, 128]), ALU.mult, [scrb, smb], [scrb])
        TT(c, "pool", W.y_tok[:, 0:768], scr, sgt[par], ALU.mult, [scrb, sgtb[par]], [W.y_tokb])
        W.qm_tok, W.qm_tokb = qm_toks[par], qm_tokbs[par]
        tile_tail_b(c, W, tl)
        if tl == 3:
            wout_block(c, W, tb)

    stage_a(0)
    for ti in range(NT):
        la = S.capture(lambda: stage_a(ti + 1)) if ti + 1 < NT else []
        lb = S.capture(lambda: stage_b(ti))
        S.commit_interleaved(la, lb)


def norm_rope(c, B, src, src_bufs, H, gain, ti, out, out_bufs, pos_cos=None, pos_sin=None, pos_bufs=None):
    n = src.shape[0]
    xr3 = B.xr[0:n, 0:H * 64].rearrange("p (h d) -> p h d", d=64)
    cos2 = pos_cos if pos_cos is not None else c.cos[0:n, ti, :]
    sin2 = pos_sin if pos_sin is not None else c.sin[0:n, ti, :]
    cosb = cos2.unsqueeze(1).broadcast_to([n, H, 32])
    sinb = sin2.unsqueeze(1).broadcast_to([n, H, 32])
    t = [q[0:n, 0:H * 32].rearrange("p (h d) -> p h d", d=32) for q in B.tq]
    cb_ = [c.constb] if pos_bufs is None else list(pos_bufs)
    if gain is not None:
        ss = B.ss[0:n, 0:H]
        ACTF(c, xr3, src, AF.Square, src_bufs, [B.xrb])
        RED(c, ss, xr3, ALU.add, [B.xrb], [B.ssb])
        ACTF(c, ss, ss, AF.Ln, [B.ssb], [B.ssb], scale=1.0 / 64, bias=float(EPS))
        ACTF(c, ss, ss, AF.Exp, [B.ssb], [B.ssb], scale=-0.5)
        TT(c, "dve", xr3, src, gain[0:n, :].unsqueeze(1).broadcast_to([n, H, 64]), ALU.mult,
           list(src_bufs) + [c.ppb], [B.xrb])
        x, xb, engs = xr3, [B.xrb], ("dve", "pool", "dve", "pool")
    else:
        x, xb, engs = src, list(src_bufs), ("dve", "dve", "dve", "dve")
    x1, x2 = x[:, :, 0:32], x[:, :, 32:64]
    TT(c, engs[0], t[0], x1, cosb, ALU.mult, xb + cb_, [B.tqb[0]])
    TT(c, engs[1], t[1], x2, sinb, ALU.mult, xb + cb_, [B.tqb[1]])
    TT(c, engs[2], t[2], x2, cosb, ALU.mult, xb + cb_, [B.tqb[2]])
    TT(c, engs[3], t[3], x1, sinb, ALU.mult, xb + cb_, [B.tqb[3]])
    if gain is not None:
        TT(c, "pool", xr3[:, :, 0:32], t[0], t[1], ALU.subtract, [B.tqb[0], B.tqb[1]], [B.xrb])
        TT(c, "pool", xr3[:, :, 32:64], t[2], t[3], ALU.add, [B.tqb[2], B.tqb[3]], [B.xrb])
        TT(c, "pool", out, xr3, B.ss[0:n, 0:H].unsqueeze(2).broadcast_to([n, H, 64]), ALU.mult, [B.xrb, B.ssb], out_bufs)
    else:
        TT(c, "pool", out[:, :, 0:32], t[0], t[1], ALU.subtract, [B.tqb[0], B.tqb[1]], out_bufs)
        TT(c, "pool", out[:, :, 32:64], t[2], t[3], ALU.add, [B.tqb[2], B.tqb[3]], out_bufs)


def masked_T(c, src_tok, src_bufs, nchunk, dstA, dstB, dst_bufs):
    pt, ptb = ps1(c)
    ptv = pt.bitcast(BF16)
    for n in range(nchunk):
        TR(c, ptv[:, n * 128:(n + 1) * 128], src_tok[:, n * 128:(n + 1) * 128], src_bufs, ptb)
    v = ptv[:, 0:nchunk * 128].rearrange("p (n t) -> p n t", n=nchunk)
    CP(c, "act", dstA[0:64], v[0:64], ptb, dst_bufs)
    CP(c, "act", dstB[64:128], v[64:128], ptb, dst_bufs)


DSA_TOPK = 256
DSA_BIS = 17


def mixer_dsa(c, l, win_d, wout_d, wkv_d, memT_d):
    S = c.S
    S.barrier()
    set_rot(c, (5, 6, 7), (6,))
    W = Ctx()
    W.pbase = PP_L0 + l * PP_PL
    R1 = Region(c.arena, c.h_off, 32 * 1024)
    R2 = Region(c.arena, c.phase_off, c.phase_size)
    hblk = [R1.take((8, 128), BF16) for _ in range(2)]
    hblkb = bufs("hblk", 2, 8)
    sq = R1.take((8, 128), BF16)
    sqb = Buf("sq")
    W.yT = R1.take((8, 512), BF16)
    W.yTb = Buf("yT")
    acc = R1.take((L,), F32)
    accb = Buf("acc")
    rl = [R1.take((512,), F32) for _ in range(2)]
    rlb = bufs("rl", 2)
    PT = [R1.take((1536,), BF16) for _ in range(2)]
    PTb = bufs("PT", 2, 3)
    w_in = R2.take((8, 1736), BF16)
    w_inb = Buf("w_in")
    load_wout(c, W, R2, wout_d)
    rstd = R2.take((128,), F32)
    rstdb = Buf("rstd")
    kdup = R2.take((L,), BF16)
    ikdup = R2.take((L,), BF16)
    v1 = R2.take((NT, 65), BF16)
    cacheb = bufs("kvcache", NT)
    v1b = Buf("v1ones")
    tail_bufs(c, W, R2, hscr_n=256)
    mark = R2.off
    junk = R2.take((L,), BF16)
    junkb = Buf("junk")
    sel = [R2.take((L,), BF16) for _ in range(2)]
    selb = bufs("sel", 2)
    B = Ctx()
    B.xr = R2.take((768,), F32)
    B.xrb = Buf("xr")
    B.tq = [R2.take((384,), F32) for _ in range(4)]
    B.tqb = bufs("tq", 4)
    B.ss = R2.take((16,), F32)
    B.ssb = Buf("ss")
    q_tok = R2.take((768,), BF16)
    q_tokb = Buf("q_tok")
    qAB = [[R2.take((6, 128), BF16) for _ in range(2)] for _ in range(2)]
    qABb = bufs("qAB", 2)
    qm_toks = [W.qm_tok, R2.take((256,), BF16)]
    qm_tokbs = [W.qm_tokb, Buf("qm_tok1")]
    iq_tok = R2.take((512,), BF16)
    iq_tokb = Buf("iq_tok")
    iqAB = [R2.take((4, 128), BF16) for _ in range(2)]
    iqABb = Buf("iqAB")
    k2 = R2.take((2, 128), BF16)
    k2b = Buf("k2")
    iw_t = R2.take((8,), F32)
    iw_tb = Buf("iw")
    bs = R2.take((2 * DSA_BIS + 8,), F32)
    bsb = Buf("bs")
    den12 = R2.take((12, 1), F32)
    den12b = Buf("den12")

    wv = win_d.rearrange("(dc p) f -> p dc f", p=128)
    for dc in range(0, 8, 2):
        S.dma("pool", "win", lambda e, dc=dc: e.dma_start(out=w_in[:, dc:dc + 2, :], in_=wv[:, dc:dc + 2, :]),
              writes=[w_inb])
    S.seal("win", [w_inb])
    Rt = Region(c.arena, c.h_off, 32 * 1024)
    mem_prep(c, Rt, memT_d, wkv_d, W.pbase, W.k_memT, W.v_mem1, W.kvb)
    S.barrier()
    for p_ in range(2):
        for a in range(2):
            MSET(c, "pool", qAB[p_][a][:, :, :], 0.0, [qABb[p_]])
    for a in range(2):
        MSET(c, "pool", iqAB[a][:, :, :], 0.0, [iqABb])
    MSET(c, "pool", v1[:, :, 64:65], 1.0, [v1b])

    g_ap = c.pp[:, W.pbase + PP_MIX:W.pbase + PP_MIX + 8]
    gq = c.pp[:, W.pbase + PP_A:W.pbase + PP_A + 64]
    gk = c.pp[:, W.pbase + PP_B:W.pbase + PP_B + 64]
    mi4 = c.mi_bf.unsqueeze(1).broadcast_to([128, 4, 128])
    SB = [0, 1, 2]
    OB = [3, 4]
    hslot = {}
    for k_, h in enumerate((0, 2, 4, 6)):
        hslot[h] = (0, k_)
    for k_, h in enumerate((8, 10, 9, 11)):
        hslot[h] = (1, k_)
    for k_, h in enumerate((1, 3, 5, 7)):
        hslot[h] = (2, k_)

    def stage_a(ti):
        set_rot(c, (6, 7), (6,))
        tb = ti // 4
        par = ti % 2
        tsl = slice(ti * 128, (ti + 1) * 128)
        hb, hbb = hblk[par], hblkb[par]
        rmsnorm_block(c, c.xT[:, :, tsl], [c.xb[dc][tb] for dc in range(8)], 128, g_ap, sq, sqb, rstd, rstdb, hb, hbb)

        def proj(out_ap, c0, c1, pbufs):
            for dc in range(8):
                MM(c, out_ap, hb[:, dc, :], w_in[:, dc, c0:c1], dc == 0, dc == 7, [hbb[dc], w_inb], pbufs)

        Pq, Pqb = ps2(c)
        proj(Pq[:, 0:512], 0, 512, [Pqb[0]])
        proj(Pq[:, 512:768], 512, 768, [Pqb[1]])
        norm_rope(c, B, Pq[:, 0:768].rearrange("p (h d) -> p h d", d=64), Pqb, 12, gq, ti,
                  q_tok.rearrange("p (h d) -> p h d", d=64), [q_tokb])
        masked_T(c, q_tok, [q_tokb], 6, qAB[par][0], qAB[par][1], [qABb[par]])
        Pi, Pib = ps1(c)
        proj(Pi, 896, 1408, Pib)
        norm_rope(c, B, Pi.rearrange("p (h d) -> p h d", d=64), Pib, 8, None, ti,
                  iq_tok.rearrange("p (h d) -> p h d", d=64), [iq_tokb])
        masked_T(c, iq_tok, [iq_tokb], 4, iqAB[0], iqAB[1], [iqABb])
        Ps, Psb = ps1(c)
        proj(Ps[:, 0:128], 768, 896, Psb)
        proj(Ps[:, 128:192], 1408, 1472, Psb)
        proj(Ps[:, 192:200], 1472, 1480, Psb)
        norm_rope(c, B, Ps[:, 0:64].rearrange("p (h d) -> p h d", d=64), Psb, 1, gk, ti,
                  k2[:, 0, 0:64].rearrange("p (h d) -> p h d", d=64), [k2b])
        norm_rope(c, B, Ps[:, 128:192].rearrange("p (h d) -> p h d", d=64), Psb, 1, None, ti,
                  k2[:, 1, 0:64].rearrange("p (h d) -> p h d", d=64), [k2b])
        CP(c, "pool", k2[:, :, 64:128], k2[:, :, 0:64], [k2b], [k2b])
        CP(c, "act", v1[:, ti, 0:64], Ps[:, 64:128], Psb, [cacheb[ti]])
        CP(c, "act", iw_t, Ps[:, 192:200], Psb, [iw_tb])
        pt, ptb = ps1(c)
        ptv = pt.bitcast(BF16)
        TR(c, ptv[:, 0:128], k2[:, 0, :], [k2b], ptb)
        TR(c, ptv[:, 128:256], k2[:, 1, :], [k2b], ptb)
        CP(c, "act", kdup[:, tsl], ptv[:, 0:128], ptb, [cacheb[ti]])
        CP(c, "act", ikdup[:, tsl], ptv[:, 128:256], ptb, [cacheb[ti]])
        Pm, Pmb = ps1(c)
        proj(Pm[:, 0:256], 1480, 1736, Pmb)
        head_norm(c, Pm[:, 0:256].rearrange("p (h d) -> p h d", d=64), Pmb, 4,
                  c.pp[:, W.pbase + PP_MQN:W.pbase + PP_MQN + 64],
                  qm_toks[par].rearrange("p (h d) -> p h d", d=64), [qm_tokbs[par]], W.hscr, W.hscrb, W.hss, W.hssb)

        Skeys = 128 * (ti + 1)
        nkb = (Skeys + 511) // 512
        n = 0
        for kb in range(nkb):
            wdt = min(512, Skeys - kb * 512)
            ks = slice(kb * 512, kb * 512 + wdt)
            kbufs = [cacheb[j] for j in range(kb * 4, min(ti + 1, kb * 4 + 4))]
            for h in range(8):
                Px, Pxb = ps1(c)
                MM(c, Px[:, 0:wdt], iqAB[h % 2][:, h // 2, :], ikdup[:, ks], True, True, [iqABb] + kbufs, Pxb)
                r_, rb_ = rl[n % 2], rlb[n % 2]
                n += 1
                ACTF(c, r_[:, 0:wdt], Px[:, 0:wdt], AF.Relu, Pxb, [rb_])
                if h == 0:
                    TS(c, "dve", acc[:, ks], r_[:, 0:wdt], iw_t[:, 0:1], None, ALU.mult, None, [rb_, iw_tb], [accb])
                else:
                    STT(c, acc[:, ks], r_[:, 0:wdt], iw_t[:, h:h + 1], acc[:, ks], ALU.mult, ALU.add,
                        [rb_, iw_tb, accb], [accb])
        sl_, slb_ = sel[par], selb[par]
        if Skeys > DSA_TOPK:
            a_ = acc[:, 0:Skeys]
            S.op("dve", lambda e, a_=a_: e.tensor_reduce(out=bs[:, 0:1], in_=a_, axis=AX.X, op=ALU.max,
                                                         apply_absolute_value=True), [accb], [bsb])
            TT(c, "dve", acc[:, tsl], acc[:, tsl], c.cneg, ALU.add, [accb, c.constb], [accb])
            TS(c, "dve", bs[:, 8:8 + DSA_BIS], c.bisc, bs[:, 0:1], None, ALU.mult, None, [bsb, c.constb], [bsb])
            TS(c, "dve", bs[:, 8 + DSA_BIS:8 + 2 * DSA_BIS], bs[:, 8:8 + DSA_BIS], 2.0, None, ALU.mult, None, [bsb], [bsb])
            TS(c, "dve", bs[:, 1:2], bs[:, 0:1], 0.0, None, ALU.mult, None, [bsb], [bsb])
            for i in range(DSA_BIS):
                S.op("dve", lambda e, a_=a_, n_=Skeys: e.tensor_scalar(
                    out=junk[:, 0:n_], in0=a_, scalar1=bs[:, 1:2], scalar2=None, op0=ALU.is_ge, op1=ALU.add,
                    accum_out=bs[:, 2:3]), [accb, bsb], [junkb, bsb])
                last = i + 1 == DSA_BIS
                dn = bs[:, 8 + i:8 + i + 1] if last else bs[:, 8 + i + 1:8 + i + 2]
                d2 = bs[:, 8 + i:8 + i + 1] if last else bs[:, 8 + DSA_BIS + i + 1:8 + DSA_BIS + i + 2]
                STT(c, bs[:, 3:4], bs[:, 2:3], float(DSA_TOPK), d2, ALU.is_ge, ALU.mult, [bsb], [bsb])
                S.op("dve", lambda e, dn=dn: e.scalar_tensor_tensor(out=bs[:, 1:2], in0=bs[:, 1:2], scalar=dn, in1=bs[:, 3:4],
                                                                    op0=ALU.subtract, op1=ALU.add), [bsb], [bsb])
            TS(c, "dve", sl_[:, 0:Skeys], a_, bs[:, 1:2], None, ALU.is_ge, None, [accb, bsb], [slb_])
        else:
            TT(c, "dve", acc[:, tsl], acc[:, tsl], c.cneg, ALU.add, [accb, c.constb], [accb])
            TS(c, "dve", sl_[:, 0:Skeys], acc[:, 0:Skeys], -1e29, None, ALU.is_ge, None, [accb], [slb_])

    def stage_b(ti):
        set_rot(c, (5,), ())
        tb, tl = ti // 4, ti % 4
        par = ti % 2
        sl_, slb_ = sel[par], selb[par]
        qa, qb_ = qAB[par][0], qAB[par][1]
        for j in range(ti + 1):
            js = slice(j * 128, (j + 1) * 128)
            pj = j % 2
            p0, p1, p2 = bank(c, SB[0]), bank(c, SB[1]), bank(c, SB[2])
            b0, b1, b2 = [c.psb[SB[0]]], [c.psb[SB[1]]], [c.psb[SB[2]]]
            kb_ = [cacheb[j], qABb[par]]
            MM(c, p0, kdup[:, js], qa[:, 0:4, :], True, False, kb_, b0)
            MM(c, p0, sl_[:, js], mi4, False, True, [slb_, c.constb], b0)
            MM(c, p1[:, 0:256], kdup[:, js], qa[:, 4:6, :], True, False, kb_, b1)
            MM(c, p1[:, 256:512], kdup[:, js], qb_[:, 4:6, :], False, False, kb_, b1)
            MM(c, p1, sl_[:, js], mi4, False, True, [slb_, c.constb], b1)
            MM(c, p2, kdup[:, js], qb_[:, 0:4, :], True, False, kb_, b2)
            MM(c, p2, sl_[:, js], mi4, False, True, [slb_, c.constb], b2)
            for bi, (pp_, bb_) in enumerate(((p0, b0), (p1, b1), (p2, b2))):
                ACTF(c, PT[pj][:, bi * 512:(bi + 1) * 512], pp_, AF.Exp, bb_, [PTb[pj][bi]], scale=0.125,
                     bias=-MASKM / 8)
            for h in range(12):
                bi, sl2 = hslot[h]
                ob = OB[0] if h < 7 else OB[1]
                oc = (h if h < 7 else h - 7) * 65
                MM(c, bank(c, ob)[:, oc:oc + 65], PT[pj][:, (bi * 4 + sl2) * 128:(bi * 4 + sl2 + 1) * 128], v1[:, j, :],
                   j == 0 and h in (0, 7), j == ti and h in (6, 11), [PTb[pj][bi], cacheb[j], v1b], [c.psb[ob]])
        oa = bank(c, OB[0])[:, 0:455].rearrange("p (h e) -> p h e", e=65)
        ob_ = bank(c, OB[1])[:, 0:325].rearrange("p (h e) -> p h e", e=65)
        RCP(c, den12[:, 0:7, :], oa[:, :, 64:65], [c.psb[OB[0]]], [den12b])
        RCP(c, den12[:, 7:12, :], ob_[:, :, 64:65], [c.psb[OB[1]]], [den12b])
        TT(c, "dve", W.y_tok[:, 0:448].rearrange("p (h d) -> p h d", d=64), oa[:, :, 0:64],
           den12[:, 0:7, :].broadcast_to([128, 7, 64]), ALU.mult, [c.psb[OB[0]], den12b], [W.y_tokb])
        TT(c, "dve", W.y_tok[:, 448:768].rearrange("p (h d) -> p h d", d=64), ob_[:, :, 0:64],
           den12[:, 7:12, :].broadcast_to([128, 5, 64]), ALU.mult, [c.psb[OB[1]], den12b], [W.y_tokb])
        W.qm_tok, W.qm_tokb = qm_toks[par], qm_tokbs[par]
        tile_tail_b(c, W, tl)
        if tl == 3:
            wout_block(c, W, tb)

    stage_a(0)
    for ti in range(NT):
        la = S.capture(lambda: stage_a(ti + 1)) if ti + 1 < NT else []
        lb = S.capture(lambda: stage_b(ti))
        S.commit_interleaved(la, lb)


NSA_NCMP = 127


def nsa_hmap(c_, par):
    return 3 * (2 * (c_ // 3) + par) + (c_ % 3)


def mixer_nsa(c, l, win_d, wout_d, wkv_d, memT_d, cw_d):
    S = c.S
    S.barrier()
    set_rot(c, (6, 7), (6,))
    W = Ctx()
    W.pbase = PP_L0 + l * PP_PL
    R1 = Region(c.arena, c.h_off, 32 * 1024)
    R2 = Region(c.arena, c.phase_off, c.phase_size)
    hblk = R1.take((8, 128), BF16)
    hblkb = bufs("hblk", 8)
    sq = R1.take((8, 128), BF16)
    sqb = Buf("sq")
    W.yT = R1.take((8, 512), BF16)
    W.yTb = Buf("yT")
    un0 = R1.off
    B = Ctx()
    B.xr = R1.take((768,), F32)
    B.xrb = Buf("xr")
    B.tq = [R1.take((384,), F32) for _ in range(4)]
    B.tqb = bufs("tq", 4)
    un1 = R1.off
    Ru = Region(c.arena, un0, un1 - un0)
    PT = [Ru.take((1536,), BF16) for _ in range(2)]
    PTb = bufs("PT", 2, 4)
    PTc = Ru.take((1536,), BF16)
    PTcb = bufs("PTc", 4)
    tmp = R1.take((768,), F32)
    tmpb = Buf("tmp")
    yacc = R1.take((768,), F32)
    yaccb = Buf("yacc")
    imp = R1.take((12, 32), F32)
    impb = Buf("imp")
    impm = R1.take((4, 32), F32)
    wk16 = R1.take((4, 32), F32)
    impmb = Buf("impm")
    blk = R1.take((4, 32), BF16)
    blkb = Buf("blk")
    w_in = R2.take((8, 2596), BF16)
    w_inb = Buf("w_in")
    wo1 = R2.take((8, 128), BF16)
    wo = [wo1, wo1]
    wob1 = Buf("wo")
    wob = [wob1, wob1]
    rstd = R2.take((128,), F32)
    rstdb = Buf("rstd")
    ksT = R2.take((2, L), BF16)
    kwT = R2.take((2, 5 * 128), BF16)
    vs1 = R2.take((NT, 4, 65), BF16)
    vw1 = R2.take((5, 4, 65), BF16)
    cacheb = Buf("kvcache")
    tail_bufs(c, W, R2, hscr_n=256)
    mark = R2.off
    B.ss = R2.take((16,), F32)
    B.ssb = Buf("ss")
    q_tok = R2.take((768,), BF16)
    q_tokb = Buf("q_tok")
    qAB = [R2.take((6, 128), BF16) for _ in range(2)]
    qABb = Buf("qAB")
    k_tok = R2.take((2, 256), BF16)
    k_tokb = Buf("k_tok")
    kvcT = [R2.take((4, 144), BF16) for _ in range(2)]
    kvcTb = Buf("kvcT")
    wdup = [R2.take((32, 64), BF16) for _ in range(2)]
    wdupb = Buf("wdup")
    posB = R2.take((2, 32, 8), BF16)
    posBb = Buf("posB")
    k_cmpT = R2.take((2, 128), BF16)
    v_cmpx = R2.take((4, 97), BF16)
    cmpb = Buf("cmpcache")
    cbias = R1.take((512,), F32)
    cbiasb = Buf("cbias")
    cnew_tok = R2.take((512,), BF16)
    cnew_tokb = Buf("cnew_tok")
    gate = R2.take((36,), F32)
    gateb = Buf("gate")
    sm = R2.take((6, 12), F32)
    smb = Buf("sm")
    m8 = R2.take((2, 8), F32)
    m8b = Buf("m8")
    thr = R2.take((4, 1), F32)
    thrb = Buf("thr")
    seltile = R2.take((4, 128), BF16)
    seltileb = Buf("seltile")
    masks_bf = R2.take((768,), BF16)
    tril_bf, far_bf = masks_bf[:, 0:128], masks_bf[:, 128:256]
    cmask_bf, shiftI = masks_bf[:, 256:512], masks_bf[:, 512:768]
    czca = R2.take((128,), F32)
    cse = [R2.take((64,), F32) for _ in range(2)]
    cseb = bufs("cse", 2)
    ncb = Buf("nsaconst")

    wv = win_d.rearrange("(dc p) f -> p dc f", p=128)
    for c_ in range(6):
        for par in range(2):
            h = nsa_hmap(c_, par)
            src = wv[:, :, h * 64:(h + 1) * 64]
            dst = w_in[:, :, c_ * 128 + par * 64:c_ * 128 + (par + 1) * 64]
            S.dma("pool", "win", lambda e, src=src, dst=dst: e.dma_start(out=dst, in_=src), writes=[w_inb])
    for dc in range(0, 8, 2):
        S.dma("pool", "win", lambda e, dc=dc: e.dma_start(out=w_in[:, dc:dc + 2, 768:2596], in_=wv[:, dc:dc + 2, 768:2596]),
              writes=[w_inb])
    S.seal("win", [w_inb])
    for kind in range(2):
        srcw = cw_d[kind].rearrange("l d e -> d l e")
        for half in range(2):
            S.dma("pool", "wcmp", lambda e, kind=kind, half=half, srcw=srcw: e.dma_start(
                out=wdup[kind][half * 64:(half + 1) * 64, :, :], in_=srcw), writes=[wdupb])
    S.seal("wcmp", [wdupb])
    Rt = Region(c.arena, c.h_off, 32 * 1024)
    mem_prep(c, Rt, memT_d, wkv_d, W.pbase, W.k_memT, W.v_mem1, W.kvb)
    S.barrier()
    S.dma("pool", "ncst", lambda e: e.dma_start(out=masks_bf, in_=c.c2["masks"][:, :]), writes=[ncb])
    S.dma("sp", "ncst", lambda e: e.dma_start(out=czca, in_=c.c2["czca"][:, :]), writes=[ncb])
    S.seal("ncst", [ncb])
    for a in range(2):
        MSET(c, "pool", qAB[a][:, :, :], 0.0, [qABb])
        MSET(c, "pool", kvcT[a][:, :, :], 0.0, [kvcTb])
    MSET(c, "pool", k_cmpT[:, :, :], 0.0, [cmpb])
    MSET(c, "pool", v_cmpx[:, :, 0:64], 0.0, [cmpb])
    MSET(c, "pool", v_cmpx[:, :, 64:65], 1.0, [cmpb])
    for g_ in range(4):
        S.dma("pool", "ncov", lambda e, g_=g_: e.dma_start(out=v_cmpx[:, g_, 65:97], in_=c.c2["cover"][:, :]), writes=[cmpb])
    S.seal("ncov", [cmpb])
    MSET(c, "pool", vs1[:, :, :, 64:65], 1.0, [cacheb])
    MSET(c, "pool", vw1[:, :, :, 64:65], 1.0, [cacheb])
    posf = c.pp[:, W.pbase + PP_POS:W.pbase + PP_POS + 64].rearrange("p (k l) -> p k l", k=2)
    CP(c, "dve", posB, posf.unsqueeze(3).broadcast_to([128, 2, 32, 8]), [c.ppb], [posBb])
    Pcb, Pcbb = ps1(c)
    for kind in range(2):
        for l_ in range(32):
            MM(c, Pcb[0:8, kind * 64:(kind + 1) * 64], posB[:, kind, l_, :], wdup[kind][:, l_, :], kind == 0 and l_ == 0,
               l_ == 31, [posBb, wdupb], Pcbb)
    CP(c, "dve", cbias[0:8, :].rearrange("p (k g e) -> p k g e", k=2, g=4),
       Pcb[0:8, 0:128].rearrange("p (k e) -> p k e", k=2).unsqueeze(2).broadcast_to([8, 2, 4, 64]), Pcbb, [cbiasb])

    g_ap = c.pp[:, W.pbase + PP_MIX:W.pbase + PP_MIX + 8]
    gq = c.pp[:, W.pbase + PP_A:W.pbase + PP_A + 64]
    gkc = c.pp[:, W.pbase + PP_B:W.pbase + PP_B + 64]
    gks = c.pp[:, W.pbase + PP_C:W.pbase + PP_C + 64]
    gkw = c.pp[:, W.pbase + PP_D:W.pbase + PP_D + 64]
    mi3 = c.mi_bf.unsqueeze(1).broadcast_to([128, 3, 128])
    SBK = [0, 1, 2, 3]
    OB = [4, 5]
    CB = [4, 5, 6]
    wo_n = [0]

    def head_of(sb, r):
        par, kc_ = sb // 2, sb % 2
        return 3 * (2 * kc_ + par) + r

    def branch(ti, keys, Kc, Vc, kslot, vslot, maskf, coef_col):
        nk = len(keys)
        for n_, j in enumerate(keys):
            par_j = n_ % 2
            ks_ = kslot(j)
            mk = maskf(j)
            for sb in SBK:
                par, kc_ = sb // 2, sb % 2
                g = 2 * kc_ + par
                pb_ = bank(c, sb)
                MM(c, pb_[:, 0:384], Kc[:, kc_, ks_], qAB[par][:, 3 * kc_:3 * kc_ + 3, :], True, mk is None,
                   [cacheb, qABb], [c.psb[sb]])
                if mk is not None:
                    lh, lb = mk[g]
                    MM(c, pb_[:, 0:384], lh, mi3, False, True, lb + [c.constb], [c.psb[sb]])
            for pr in range(2):
                src = c.pst[pr][:, :].rearrange("p (b x) -> p b x", b=2)[:, :, 0:384]
                dst = PT[par_j][:, pr * 768:(pr + 1) * 768].rearrange("p (b x) -> p b x", b=2)
                ACTF(c, dst, src, AF.Exp, [c.psb[2 * pr], c.psb[2 * pr + 1]], [PTb[par_j][2 * pr], PTb[par_j][2 * pr + 1]],
                     scale=0.125, bias=(0.0 if mk is None else -MASKM / 8))
            for sb in SBK:
                for r in range(3):
                    h = head_of(sb, r)
                    g = h // 3
                    ob = OB[0] if h < 7 else OB[1]
                    oc = (h if h < 7 else h - 7) * 65
                    firsts = (0, 7)
                    MM(c, bank(c, ob)[:, oc:oc + 65], PT[par_j][:, sb * 384 + r * 128:sb * 384 + (r + 1) * 128],
                       Vc[:, vslot(j), g, :], n_ == 0 and (sb, r) == first_in_bank[ob], n_ == nk - 1,
                       [PTb[par_j][sb], cacheb], [c.psb[ob]])
        oa = bank(c, OB[0])[:, 0:455].rearrange("p (h e) -> p h e", e=65)
        ob_ = bank(c, OB[1])[:, 0:325].rearrange("p (h e) -> p h e", e=65)
        RCP(c, sm[:, 0, 0:7].unsqueeze(2), oa[:, :, 64:65], [c.psb[OB[0]]], [smb])
        RCP(c, sm[:, 0, 7:12].unsqueeze(2), ob_[:, :, 64:65], [c.psb[OB[1]]], [smb])
        TT(c, "dve", sm[:, 1, :], sm[:, 0, :], gate.rearrange("p (h b) -> p h b", b=3)[:, :, coef_col], ALU.mult,
           [smb, gateb], [smb])
        t3 = tmp.rearrange("p (h d) -> p h d", d=64)
        TT(c, "dve", t3[:, 0:7, :], oa[:, :, 0:64], sm[:, 1, 0:7].unsqueeze(2).broadcast_to([128, 7, 64]), ALU.mult,
           [c.psb[OB[0]], smb], [tmpb])
        TT(c, "dve", t3[:, 7:12, :], ob_[:, :, 0:64], sm[:, 1, 7:12].unsqueeze(2).broadcast_to([128, 5, 64]), ALU.mult,
           [c.psb[OB[1]], smb], [tmpb])
        TT(c, "pool", yacc, yacc, tmp, ALU.add, [yaccb, tmpb], [yaccb])

    first_in_bank = {}
    for sb in SBK:
        for r in range(3):
            h = head_of(sb, r)
            ob = OB[0] if h < 7 else OB[1]
            first_in_bank.setdefault(ob, (sb, r))
    first_in_cbank = {}
    for sb in SBK:
        for r in range(3):
            h = head_of(sb, r)
            first_in_cbank.setdefault(CB[h // 5], (sb, r))

    for ti in range(NT):
        tb, tl = ti // 4, ti % 4
        tsl = slice(ti * 128, (ti + 1) * 128)
        rs5 = ti % 5
        rsl = slice(rs5 * 128, (rs5 + 1) * 128)
        rmsnorm_block(c, c.xT[:, :, tsl], [c.xb[dc][tb] for dc in range(8)], 128, g_ap, sq, sqb, rstd, rstdb, hblk, hblkb)

        def proj(out_ap, c0, c1, pbufs):
            for dc in range(8):
                MM(c, out_ap, hblk[:, dc, :], w_in[:, dc, c0:c1], dc == 0, dc == 7, [hblkb[dc], w_inb], pbufs)

        Pq, Pqb = ps2(c)
        proj(Pq[:, 0:512], 0, 512, [Pqb[0]])
        proj(Pq[:, 512:768], 512, 768, [Pqb[1]])
        norm_rope(c, B, Pq[:, 0:768].rearrange("p (h d) -> p h d", d=64), Pqb, 12, gq, ti,
                  q_tok.rearrange("p (h d) -> p h d", d=64), [q_tokb])
        masked_T(c, q_tok, [q_tokb], 6, qAB[0], qAB[1], [qABb])
        Pk, Pkb = ps2(c)
        proj(Pk[:, 0:256], 1280, 1536, [Pkb[0]])
        proj(Pk[:, 512:768], 1792, 2048, [Pkb[1]])
        norm_rope(c, B, Pk[:, 0:256].rearrange("p (h d) -> p h d", d=64), [Pkb[0]], 4, gks, ti,
                  k_tok[:, 0, :].rearrange("p (h d) -> p h d", d=64), [k_tokb])
        norm_rope(c, B, Pk[:, 512:768].rearrange("p (h d) -> p h d", d=64), [Pkb[1]], 4, gkw, ti,
                  k_tok[:, 1, :].rearrange("p (h d) -> p h d", d=64), [k_tokb])
        pt, ptb = ps1(c)
        ptv = pt.bitcast(BF16)
        for n_ in range(4):
            TR(c, ptv[:, n_ * 128:(n_ + 1) * 128], k_tok[:, n_ // 2, (n_ % 2) * 128:(n_ % 2 + 1) * 128], [k_tokb], ptb)
        CP(c, "act", ksT[:, :, tsl], ptv[:, 0:256].rearrange("p (c t) -> p c t", c=2), ptb, [cacheb])
        CP(c, "act", kwT[:, :, rsl], ptv[:, 256:512].rearrange("p (c t) -> p c t", c=2), ptb, [cacheb])
        Pv, Pvb = ps2(c)
        proj(Pv[:, 0:256], 1536, 1792, [Pvb[0]])
        proj(Pv[:, 512:768], 2048, 2304, [Pvb[1]])
        CP(c, "act", vs1[:, ti, :, 0:64], Pv[:, 0:256].rearrange("p (g d) -> p g d", d=64), [Pvb[0]], [cacheb])
        CP(c, "act", vw1[:, rs5, :, 0:64], Pv[:, 512:768].rearrange("p (g d) -> p g d", d=64), [Pvb[1]], [cacheb])
        Pc, Pcb_ = ps1(c)
        for n_ in range(4):
            for dc in range(8):
                MM(c, Pc[:, n_ * 128:(n_ + 1) * 128], w_in[:, dc, 768 + n_ * 128:768 + (n_ + 1) * 128], hblk[:, dc, :],
                   n_ == 0 and dc == 0, dc == 7, [hblkb[dc], w_inb], Pcb_)
        Pc4 = Pc.rearrange("p (n t) -> p n t", n=4)
        CP(c, "act", kvcT[0][0:64, :, 16:144], Pc4[0:64], Pcb_, [kvcTb])
        CP(c, "act", kvcT[1][64:128, :, 16:144], Pc4[64:128], Pcb_, [kvcTb])
        Pg, Pgb = ps1(c)
        proj(Pg[:, 0:36], 2304, 2340, Pgb)
        proj(Pg[:, 128:384], 2340, 2596, Pgb)
        ACTF(c, gate, Pg[:, 0:36], AF.Exp, Pgb, [gateb], scale=-1.0)
        TS(c, "dve", gate, gate, 1.0, None, ALU.add, None, [gateb], [gateb])
        RCP(c, gate, gate, [gateb], [gateb])
        head_norm(c, Pg[:, 128:384].rearrange("p (h d) -> p h d", d=64), Pgb, 4,
                  c.pp[:, W.pbase + PP_MQN:W.pbase + PP_MQN + 64],
                  W.qm_tok.rearrange("p (h d) -> p h d", d=64), [W.qm_tokb], W.hscr, W.hscrb, W.hss, W.hssb)
        nb = 7 if ti == 0 else 8
        n0 = 0 if ti == 0 else 8 * ti - 1
        off = 16 if ti == 0 else 0
        Pn, Pnb = ps1(c)
        first = True
        for kind in range(2):
            for g in range(4):
                par, ch = g % 2, g // 2
                for l_ in range(32):
                    lhs = kvcT[par][:, kind * 2 + ch, off + l_:off + l_ + 16 * (nb - 1) + 1:16]
                    MM(c, Pn[0:nb, (kind * 4 + g) * 64:(kind * 4 + g + 1) * 64], lhs, wdup[kind][:, l_, :], first,
                       l_ == 31, [kvcTb, wdupb], Pnb)
                    first = False
        for a in range(2):
            hs = slice(a * 64, (a + 1) * 64)
            CP(c, "pool", kvcT[a][hs, :, 0:16], kvcT[a][hs, :, 128:144], [kvcTb], [kvcTb])
        cn = tmp[0:nb, 0:512]
        TT(c, "dve", cn, Pn[0:nb, :], cbias[0:nb, :], ALU.add, Pnb + [cbiasb], [tmpb])
        cs_, csb_ = cse[ti % 2], cseb[ti % 2]
        S.dma("sp", f"cse{ti % 2}", lambda e, cs_=cs_, ti=ti: e.dma_start(out=cs_[0:8, :], in_=c.c2["rope"][ti, :, :]),
              writes=[csb_])
        norm_rope(c, B, cn[:, 0:256].rearrange("p (h d) -> p h d", d=64), [tmpb], 4, gkc, ti,
                  cnew_tok[0:nb, 0:256].rearrange("p (h d) -> p h d", d=64), [cnew_tokb],
                  pos_cos=cs_[0:nb, 0:32], pos_sin=cs_[0:nb, 32:64], pos_bufs=[csb_])
        CP(c, "pool", cnew_tok[0:nb, 256:512], cn[:, 256:512], [tmpb], [cnew_tokb])
        pt2, pt2b = ps1(c)
        pt2v = pt2.bitcast(BF16)
        for ch in range(2):
            TR(c, pt2v[:, ch * 8:ch * 8 + nb], cnew_tok[0:nb, ch * 128:(ch + 1) * 128], [cnew_tokb], pt2b)
        for ch in range(2):
            CP(c, "act", k_cmpT[:, ch, n0:n0 + nb], pt2v[:, ch * 8:ch * 8 + nb], pt2b, [cmpb])
        Psc, Pscb = ps1(c)
        MM(c, Psc[:, 0:256], shiftI[0:nb, 128 - n0:256 - n0], cnew_tok[0:nb, 256:512], True, True, [ncb, cnew_tokb], Pscb)
        TT(c, "dve", v_cmpx[:, :, 0:64], Psc[:, 0:256].rearrange("p (g d) -> p g d", d=64), v_cmpx[:, :, 0:64], ALU.add,
           Pscb + [cmpb], [cmpb])
        S.barrier()
        cm_l = cmask_bf[:, 128 - 8 * ti:128 - 8 * ti + NSA_NCMP]
        for sb in SBK:
            par, kc_ = sb // 2, sb % 2
            pb_ = bank(c, sb)
            MM(c, pb_[0:NSA_NCMP, 0:384], k_cmpT[:, kc_, 0:NSA_NCMP], qAB[par][:, 3 * kc_:3 * kc_ + 3, :], True, False,
               [cmpb, qABb], [c.psb[sb]])
            MM(c, pb_[0:NSA_NCMP, 0:384], cm_l, mi3, False, True, [ncb, c.constb], [c.psb[sb]])
        for pr in range(2):
            src = c.pst[pr][0:NSA_NCMP, :].rearrange("p (b x) -> p b x", b=2)[:, :, 0:384]
            dst = PTc[0:NSA_NCMP, pr * 768:(pr + 1) * 768].rearrange("p (b x) -> p b x", b=2)
            ACTF(c, dst, src, AF.Exp, [c.psb[2 * pr], c.psb[2 * pr + 1]], [PTcb[2 * pr], PTcb[2 * pr + 1]], scale=0.125,
                 bias=-MASKM / 8)
        for sb in SBK:
            for r in range(3):
                h = head_of(sb, r)
                cb_ = CB[h // 5]
                oc = (h % 5) * 97
                MM(c, bank(c, cb_)[:, oc:oc + 97], PTc[0:NSA_NCMP, sb * 384 + r * 128:sb * 384 + (r + 1) * 128],
                   v_cmpx[0:NSA_NCMP, h // 3, :], (sb, r) == first_in_cbank[cb_], True, [PTcb[sb], cmpb], [c.psb[cb_]])
        cviews = [bank(c, CB[0])[:, 0:485].rearrange("p (h e) -> p h e", e=97),
                  bank(c, CB[1])[:, 0:485].rearrange("p (h e) -> p h e", e=97),
                  bank(c, CB[2])[:, 0:194].rearrange("p (h e) -> p h e", e=97)]
        hr = [(0, 5), (5, 10), (10, 12)]
        for k_, (h0, h1) in enumerate(hr):
            TS(c, "dve", sm[:, 0, h0:h1].unsqueeze(2), cviews[k_][:, :, 64:65], 1e-30, None, ALU.max, None,
               [c.psb[CB[k_]]], [smb])
        RCP(c, sm[:, 0, :], sm[:, 0, :], [smb], [smb])
        TT(c, "dve", sm[:, 1, :], sm[:, 0, :], gate.rearrange("p (h b) -> p h b", b=3)[:, :, 0], ALU.mult, [smb, gateb], [smb])
        y3 = yacc.rearrange("p (h d) -> p h d", d=64)
        for k_, (h0, h1) in enumerate(hr):
            TT(c, "dve", y3[:, h0:h1, :], cviews[k_][:, :, 0:64], sm[:, 1, h0:h1].unsqueeze(2).broadcast_to([128, h1 - h0, 64]),
               ALU.mult, [c.psb[CB[k_]], smb], [yaccb])
            TT(c, "dve", imp[:, h0:h1, :], cviews[k_][:, :, 65:97], sm[:, 0, h0:h1].unsqueeze(2).broadcast_to([128, h1 - h0, 32]),
               ALU.mult, [c.psb[CB[k_]], smb], [impb])
        imp4 = imp.rearrange("p (g r) j -> p g r j", r=3)
        TT(c, "dve", impm, imp4[:, :, 0, :], imp4[:, :, 1, :], ALU.add, [impb], [impmb])
        TT(c, "dve", impm, impm, imp4[:, :, 2, :], ALU.add, [impb, impmb], [impmb])
        czs = czca[:, 32 - 2 * ti:64 - 2 * ti].unsqueeze(1).broadcast_to([128, 4, 32])
        cas = czca[:, 64 + 32 - 2 * ti:64 + 64 - 2 * ti].unsqueeze(1).broadcast_to([128, 4, 32])
        TT(c, "dve", impm, impm, czs, ALU.mult, [impmb, ncb], [impmb])
        TT(c, "dve", impm, impm, cas, ALU.add, [impmb, ncb], [impmb])
        MSET(c, "dve", impm[:, :, 0:1], 2e9, [impmb])
        for g in range(4):
            S.op("dve", lambda e, g=g: e.max(out=m8[:, 0, :], in_=impm[:, g, :]), [impmb], [m8b])
            S.op("dve", lambda e, g=g: e.match_replace(out=wk16[:, g, :], in_to_replace=m8[:, 0, :], in_values=impm[:, g, :],
                                                       imm_value=-3e38), [impmb, m8b], [impmb])
            S.op("dve", lambda e, g=g: e.max(out=m8[:, 1, :], in_=wk16[:, g, :]), [impmb], [m8b])
            CP(c, "dve", thr[:, g, :], m8[:, 1, 7:8], [m8b], [thrb])
        TT(c, "dve", blk, impm, thr.broadcast_to([128, 4, 32]), ALU.is_ge, [impmb, thrb], [blkb])
        def slc_mask(j, ti=ti):
            src = blk[:, :, 2 * j:2 * j + 2].unsqueeze(3).broadcast_to([128, 4, 2, 64])
            dst = seltile.rearrange("p g (b s) -> p g b s", b=2)
            if j == ti:
                TT(c, "pool", dst, src, tril_bf.rearrange("p (b s) -> p b s", b=2).unsqueeze(1).broadcast_to([128, 4, 2, 64]),
                   ALU.mult, [blkb, ncb], [seltileb])
            else:
                CP(c, "pool", dst, src, [blkb], [seltileb])
            return {g: (seltile[:, g, :], [seltileb]) for g in range(4)}

        branch(ti, list(range(ti + 1)), ksT, vs1, lambda j: slice(j * 128, (j + 1) * 128), lambda j: j, slc_mask, 1)

        def win_mask(j, ti=ti):
            if j == ti:
                return {g: (tril_bf, [ncb]) for g in range(4)}
            if j == ti - 4:
                return {g: (far_bf, [ncb]) for g in range(4)}
            return None

        branch(ti, list(range(max(0, ti - 4), ti + 1)), kwT, vw1,
               lambda j: slice((j % 5) * 128, (j % 5 + 1) * 128), lambda j: j % 5, win_mask, 2)
        CP(c, "pool", W.y_tok[:, 0:768], yacc, [yaccb], [W.y_tokb])
        tile_tail_b(c, W, tl)
        if tl == 3:
            wout_block_stream(c, W, tb, wout_d, wo, wob, wo_n)


def wout_block_stream(c, W, tb, wout_d, wo, wob, wo_n):
    ts = slice(tb * 512, (tb + 1) * 512)
    wv = wout_d.rearrange("(cc p) d -> p cc d", p=128)
    for dc in range(8):
        s = wo_n[0] % 2
        wo_n[0] += 1
        c.S.dma("pool", f"wo{s}", lambda e, s=s, dc=dc: e.dma_start(out=wo[s][:, :, :], in_=wv[:, :, dc * 128:(dc + 1) * 128]),
                writes=[wob[s]])
        pw, pwb = ps1(c)
        for cc in range(8):
            MM(c, pw, wo[s][:, cc, :], W.yT[:, cc, :], cc == 0, cc == 7, [wob[s], W.yTb], pwb, skip=False)
        TT(c, "dve", c.xT[:, dc, ts], pw, c.xT[:, dc, ts], ALU.add, list(pwb) + [c.xb[dc][tb]], [c.xb[dc][tb]])


def build_program(cfg):
    nc = bass.Bass("TRN2", target_bir_lowering=False)
    stack = ExitStack()
    c = Ctx()
    c.nc = nc
    c.S = S = Sched()
    c.debug = set(cfg.get("debug", ()))
    c.dbg_done = set()
    c.seq = 0

    def din(name, shape):
        return nc.dram_tensor(name, list(shape), F32, kind="ExternalInput").ap()

    stages = cfg["stages"]
    xT_d = din("xT", (SEQ_PER_CORE, D, L))
    memT_d = din("memT", (SEQ_PER_CORE, D, NMEM))
    pp_d = din("pp", (128, NPP))
    cst_d = din("cst", (128, NCST))
    wd = {}
    for st in stages:
        if st[0] == "ffn":
            _, l, h = st
            wd[("wg", l, h)] = din(f"wg_{l}_{h}", (D, DFF))
            wd[("wu", l, h)] = din(f"wu_{l}_{h}", (D, DFF))
            wd[("wd", l, h)] = din(f"wd_{l}_{h}", (DFF, D))
        else:
            _, l = st
            kind = l % 3
            ncol = {0: 2560, 1: 1736, 2: 2596}[kind]
            wd[("win", l)] = din(f"win_{l}", (D, ncol))
            wd[("wout", l)] = din(f"wout_{l}", (D, D))
            wd[("wkv", l)] = din(f"wkv_{l}", (D, 512))
            if kind == 2:
                wd[("cw", l)] = din(f"cw_{l}", (2, 32, 64, 64))
                c.c2 = {"masks": din("c2_masks", (128, 768)), "czca": din("c2_czca", (128, 128)),
                        "cover": din("c2_cover", (128, 32)), "rope": din("c2_rope", (16, 8, 64))}
    out_d = nc.dram_tensor("outT", [SEQ_PER_CORE, D, L], F32, kind="ExternalOutput").ap()

    TOTAL = 207 * 1024 + 512
    c.arena = nc.alloc_sbuf_tensor("arena", [128, TOTAL], U8)
    R0 = Region(c.arena, 0, TOTAL)
    c.xT = R0.take((8, L), F32)
    c.xb = bufs("x", 8, NTB)
    c.h_off = R0.off
    c.hT = R0.take((8, L), BF16)
    c.hb = bufs("h", 8, NTB)
    c.cst = R0.take((NCST,), F32)
    c.pp = R0.take((NPP,), F32)
    c.ident_bf = R0.take((128,), BF16)
    c.ones_bf = R0.take((128,), BF16)
    c.mi_bf = R0.take((128,), BF16)
    c.constb = Buf("const")
    c.ppb = Buf("pp")
    c.phase_off = R0.off
    c.phase_size = TOTAL - R0.off
    c.tri_f = c.cst[:, CST_TRI:CST_TRI + 128]
    c.cos = c.cst[:, CST_COS:CST_COS + 512].rearrange("p (a b) -> p a b", a=16)
    c.sin = c.cst[:, CST_SIN:CST_SIN + 512].rearrange("p (a b) -> p a b", a=16)
    c.zk = c.cst[:, CST_ZK:CST_ZK + 6]
    c.epsx = c.cst[:, CST_EPSX:CST_EPSX + 6]
    c.cd = c.cst[:, CST_CD:CST_CD + 3]
    c.cneg = c.cst[:, CST_CNEG:CST_CNEG + 128]
    c.bisc = c.cst[:, CST_BISC:CST_BISC + DSA_BIS]
    c.pst = [nc.alloc_psum_tensor(f"ps{i}", [128, 1024], F32) for i in range(4)]
    c.psb = [Buf(f"ps{i}", excl=True) for i in range(8)]
    c.psp = 0
    c.psp2 = 0
    set_rot(c, range(8), (0, 2, 4, 6))

    S.dma("sp", "misc", lambda e: e.dma_start(out=c.cst[:, :], in_=cst_d[:, :]), writes=[c.constb])
    S.dma("sp", "misc", lambda e: e.dma_start(out=c.pp[:, :], in_=pp_d[:, :]), writes=[c.ppb])
    S.seal("misc", [c.constb, c.ppb])
    S.op("dve", lambda e: e.memset(c.ones_bf[:, :], 1.0), writes=[c.constb])
    S.op("dve", lambda e: e.tensor_copy(out=c.ident_bf[:, :], in_=c.cst[:, CST_IDENT:CST_IDENT + 128]),
         reads=[c.constb], writes=[c.constb])
    S.op("dve", lambda e: e.tensor_scalar(out=c.mi_bf[:, :], in0=c.cst[:, CST_IDENT:CST_IDENT + 128], scalar1=MASKM,
                                          scalar2=None, op0=ALU.mult), reads=[c.constb], writes=[c.constb])

    for s in range(SEQ_PER_CORE):
        c.seq = s
        xv = xT_d[s].rearrange("(dc p) t -> p dc t", p=128)
        for dc in range(8):
            S.dma("sp", "xin", lambda e, dc=dc, xv=xv: e.dma_start(out=c.xT[:, dc, :], in_=xv[:, dc, :]),
                  writes=c.xb[dc])
        S.seal("xin", [b for r in c.xb for b in r])
        for st in stages:
            if st[0] == "ffn":
                _, l, h = st
                ffn(c, wd[("wg", l, h)], wd[("wu", l, h)], wd[("wd", l, h)], c.pp[:, (l * 2 + h) * 8:(l * 2 + h) * 8 + 8])
            else:
                _, l = st
                kind = l % 3
                if kind == 0:
                    mixer_ret(c, l, wd[("win", l)], wd[("wout", l)], wd[("wkv", l)], memT_d[s])
                elif kind == 1:
                    mixer_dsa(c, l, wd[("win", l)], wd[("wout", l)], wd[("wkv", l)], memT_d[s])
                else:
                    mixer_nsa(c, l, wd[("win", l)], wd[("wout", l)], wd[("wkv", l)], memT_d[s], wd[("cw", l)])
        ov = out_d[s].rearrange("(dc p) t -> p dc t", p=128)
        for dc in range(8):
            S.dma("sp", "xout", lambda e, dc=dc, ov=ov: e.dma_start(out=ov[:, dc, :], in_=c.xT[:, dc, :]),
                  reads=c.xb[dc])
        S.seal("xout", [b for r in c.xb for b in r])
    S.finish("sp")
    S.emit(nc, stack)
    stack.close()
    return nc


def dbg_dump(c, name, ap, rbufs, seq):
    if name not in c.debug or seq != 0 or name in c.dbg_done:
        return
    c.dbg_done.add(name)
    n = ap.shape[1]
    d = c.nc.dram_tensor("dbg_" + name, [128, n], F32, kind="ExternalOutput").ap()
    c.S.dma("sp", "dbg", lambda e: e.dma_start(out=d[:, :], in_=ap), reads=rbufs)


def default_stages():
    st = []
    for l in range(DEPTH):
        st += [("ffn", l, 0), ("mix", l), ("ffn", l, 1)]
    return st


def kernel(**inputs):
    cfg = inputs.pop("_cfg", None) or {"stages": default_stages()}
    if os.environ.get("MK_STAGES"):
        cfg = {"stages": [tuple(int(v) if v.isdigit() else v for v in t.split(".")) for t in os.environ["MK_STAGES"].split(",")]}
    f32 = lambda a: np.ascontiguousarray(np.asarray(a, dtype=np.float32))
    x = f32(inputs["x"])
    xT = np.ascontiguousarray(x.transpose(0, 2, 1))
    memT = np.ascontiguousarray(f32(inputs["mem"]).transpose(0, 2, 1))
    nc = build_program(cfg)
    shared = {"pp": host_params(inputs), "cst": host_consts()}
    for st in cfg["stages"]:
        if st[0] == "ffn":
            _, l, h = st
            shared[f"wg_{l}_{h}"] = f32(inputs["ffn_w_gate"][l, h])
            shared[f"wu_{l}_{h}"] = f32(inputs["ffn_w_up"][l, h])
            shared[f"wd_{l}_{h}"] = f32(inputs["ffn_w_down"][l, h])
        else:
            _, l = st
            kind, j = l % 3, l // 3
            name = {0: "ret_w_in", 1: "dsa_w_in", 2: "nsa_w_in"}[kind]
            shared[f"win_{l}"] = f32(inputs[name][j])
            shared[f"wout_{l}"] = f32(inputs["w_out"][l])
            shared[f"wkv_{l}"] = f32(inputs["mem_w_kv"][l])
            if kind == 2:
                shared[f"cw_{l}"] = np.ascontiguousarray(np.stack([f32(inputs["nsa_cmp_wk"][j]), f32(inputs["nsa_cmp_wv"][j])]))
                shared.update(host_consts2())
    in_maps = []
    for i in range(NCORES):
        m = dict(shared)
        m["xT"] = xT[i * SEQ_PER_CORE:(i + 1) * SEQ_PER_CORE]
        m["memT"] = memT[i * SEQ_PER_CORE:(i + 1) * SEQ_PER_CORE]
        in_maps.append(m)
    res = run_bass_kernel_spmd(nc, in_maps, core_ids=list(range(NCORES)))
    outT = np.concatenate([r["outT"] for r in res.results], axis=0)
    if cfg.get("debug"):
        kernel.dbg = {k: v for k, v in res.results[0].items() if k.startswith("dbg_")}
    return np.ascontiguousarray(outT.transpose(0, 2, 1))
```
